# Optimizing a Trainium2 kernel written in Bass

```python
import math
import jax, jax.numpy as jnp
from jax import lax
import numpy as np

D_MODEL = 1024
BATCH = 8
SEQ = 2048
DEPTH = 1

HEAD_DIM = 64
MIX_WIDTH = D_MODEL
DIFF_WIDTH = MIX_WIDTH // 2
SB_WIDTH = MIX_WIDTH - DIFF_WIDTH
N_DIFF_HEADS = DIFF_WIDTH // (2 * HEAD_DIM)
DIFF_V_DIM = 2 * HEAD_DIM
N_SB_HEADS = SB_WIDTH // HEAD_DIM
QBLOCK = 128
NUM_BUCKETS = 32
MAX_DISTANCE = 128
N_EXPERTS = 32
TOP_K = 4
D_EXPERT = D_MODEL
SWIGLU_LIMIT = 7.0
SWIGLU_ALPHA = 1.702
PLE_DIM = 256
NORM_EPS = 1e-6
SPLIT_SIZES = (N_DIFF_HEADS * HEAD_DIM, N_DIFF_HEADS * HEAD_DIM, N_DIFF_HEADS * HEAD_DIM,
               N_DIFF_HEADS * HEAD_DIM, N_DIFF_HEADS * DIFF_V_DIM,
               N_SB_HEADS * HEAD_DIM, N_SB_HEADS * HEAD_DIM, N_SB_HEADS * HEAD_DIM)
IN_WIDTH = sum(SPLIT_SIZES)

kernel_name = "hybrid_diffattn_stickbreak_moe_ple"


def rms_norm(x, g):
    x32 = x.astype(jnp.float32)
    y = x32 * lax.rsqrt(jnp.mean(x32 * x32, axis=-1, keepdims=True) + NORM_EPS)
    return (y * g.astype(jnp.float32)).astype(x.dtype)


def t5_bucket(rel):
    n = jnp.maximum(rel, 0)
    max_exact = NUM_BUCKETS // 2
    nf = jnp.maximum(n, 1).astype(jnp.float32)
    large = max_exact + (jnp.log(nf / max_exact) / math.log(MAX_DISTANCE / max_exact)
                         * (NUM_BUCKETS - max_exact)).astype(jnp.int32)
    large = jnp.minimum(large, NUM_BUCKETS - 1)
    return jnp.where(n < max_exact, n, large)


def to_query_blocks(t):
    b, s, h, d = t.shape
    return t.reshape(b, s // QBLOCK, QBLOCK, h, d).transpose(1, 0, 3, 2, 4)


def from_query_blocks(t):
    nb, b, h, blk, d = t.shape
    return t.transpose(1, 0, 3, 2, 4).reshape(b, nb * blk, h * d)


def diff_attention(q1, q2, k1, k2, v, lam, subln_g, lambda_init, rel_bias):
    seq = q1.shape[1]
    scale = HEAD_DIM ** -0.5
    k1h, k2h, vh = [t.transpose(0, 2, 1, 3) for t in (k1, k2, v)]
    kpos = jnp.arange(seq)
    bias_table = rel_bias.astype(jnp.float32)

    def block(args):
        q1b, q2b, qpos = args
        rel = qpos[:, None] - kpos[None, :]
        causal = rel >= 0
        bias = bias_table[t5_bucket(rel)].transpose(2, 0, 1)

        def softmax_map(qb, kh):
            s = jnp.einsum('bhqd,bhkd->bhqk', qb, kh).astype(jnp.float32) * scale + bias
            s = jnp.where(causal, s, -jnp.inf)
            return jax.nn.softmax(s, axis=-1)

        a = softmax_map(q1b, k1h) - lam * softmax_map(q2b, k2h)
        o = jnp.einsum('bhqk,bhkd->bhqd', a.astype(vh.dtype), vh)
        return rms_norm(o, subln_g) * (1.0 - lambda_init)

    nb = seq // QBLOCK
    qpos = jnp.arange(seq).reshape(nb, QBLOCK)
    out = lax.map(block, (to_query_blocks(q1), to_query_blocks(q2), qpos))
    return from_query_blocks(out)


def stick_breaking_attention(q, k, v):
    seq = q.shape[1]
    scale = HEAD_DIM ** -0.5
    kh, vh = [t.transpose(0, 2, 1, 3) for t in (k, v)]
    kpos = jnp.arange(seq)

    def block(args):
        qb, qpos = args
        z = jnp.einsum('bhqd,bhkd->bhqk', qb, kh).astype(jnp.float32) * scale
        strict = kpos[None, :] < qpos[:, None]
        log_keep = jnp.where(strict, jax.nn.log_sigmoid(-z), 0.0)
        later = lax.cumsum(log_keep, axis=3, reverse=True) - log_keep
        w = jnp.where(strict, jnp.exp(jax.nn.log_sigmoid(z) + later), 0.0)
        return jnp.einsum('bhqk,bhkd->bhqd', w.astype(vh.dtype), vh)

    nb = seq // QBLOCK
    qpos = jnp.arange(seq).reshape(nb, QBLOCK)
    out = lax.map(block, (to_query_blocks(q), qpos))
    return from_query_blocks(out)


def moe_ffn(xn, router_w, router_b, w_gate_up, b_gate_up, w_down, b_down):
    b, s, d = xn.shape
    t = xn.reshape(b * s, d)
    logits = (t @ router_w + router_b).astype(jnp.float32)
    top_v, top_i = lax.top_k(logits, TOP_K)
    gates = jax.nn.softmax(top_v, axis=-1)
    combine = jnp.einsum('nk,nke->ne', gates, jax.nn.one_hot(top_i, N_EXPERTS, dtype=jnp.float32))
    y = jnp.zeros((b * s, d), jnp.float32)
    for e in range(N_EXPERTS):
        gu = t @ w_gate_up[e] + b_gate_up[e]
        glu = jnp.minimum(gu[:, ::2], SWIGLU_LIMIT)
        lin = jnp.clip(gu[:, 1::2], -SWIGLU_LIMIT, SWIGLU_LIMIT)
        act = glu * jax.nn.sigmoid(SWIGLU_ALPHA * glu) * (lin + 1.0)
        y = y + combine[:, e:e + 1] * (act @ w_down[e] + b_down[e])
    return y.astype(xn.dtype).reshape(b, s, d)


def setup_inputs(seed: int = 0) -> dict:
    key = jax.random.key(seed)
    ks = jax.random.split(key, 22)
    f32 = jnp.float32
    nrm = lambda k, shape: jax.random.normal(k, shape, f32)
    return {
        "x": nrm(ks[0], (BATCH, SEQ, D_MODEL)),
        "p": nrm(ks[1], (DEPTH, BATCH, SEQ, PLE_DIM)),
        "w_in": nrm(ks[2], (DEPTH, D_MODEL, IN_WIDTH)) * D_MODEL ** -0.5,
        "w_out": nrm(ks[3], (DEPTH, MIX_WIDTH, D_MODEL)) * MIX_WIDTH ** -0.5,
        "attn_norm": 1.0 + 0.05 * nrm(ks[4], (DEPTH, D_MODEL)),
        "moe_norm": 1.0 + 0.05 * nrm(ks[5], (DEPTH, D_MODEL)),
        "rel_bias": 0.1 * nrm(ks[6], (NUM_BUCKETS, N_DIFF_HEADS)),
        "lambda_q1": 0.1 * nrm(ks[7], (DEPTH, HEAD_DIM)),
        "lambda_k1": 0.1 * nrm(ks[8], (DEPTH, HEAD_DIM)),
        "lambda_q2": 0.1 * nrm(ks[9], (DEPTH, HEAD_DIM)),
        "lambda_k2": 0.1 * nrm(ks[10], (DEPTH, HEAD_DIM)),
        "subln": 1.0 + 0.05 * nrm(ks[11], (DEPTH, DIFF_V_DIM)),
        "router_w": nrm(ks[12], (DEPTH, D_MODEL, N_EXPERTS)) * D_MODEL ** -0.5,
        "router_b": 0.01 * nrm(ks[13], (DEPTH, N_EXPERTS)),
        "w_gate_up": nrm(ks[14], (DEPTH, N_EXPERTS, D_MODEL, 2 * D_EXPERT)) * D_MODEL ** -0.5,
        "b_gate_up": 0.01 * nrm(ks[15], (DEPTH, N_EXPERTS, 2 * D_EXPERT)),
        "w_down": nrm(ks[16], (DEPTH, N_EXPERTS, D_EXPERT, D_MODEL)) * D_EXPERT ** -0.5,
        "b_down": 0.01 * nrm(ks[17], (DEPTH, N_EXPERTS, D_MODEL)),
        "ple_proj": nrm(ks[18], (DEPTH, PLE_DIM, D_MODEL)) * PLE_DIM ** -0.5,
        "ple_norm": 1.0 + 0.05 * nrm(ks[19], (DEPTH, D_MODEL)),
        "ple_gate": nrm(ks[20], (DEPTH, D_MODEL, D_MODEL)) * D_MODEL ** -0.5,
        "final_norm": 1.0 + 0.05 * nrm(ks[21], (D_MODEL,)),
    }


def reference(x, p, w_in, w_out, attn_norm, moe_norm, rel_bias, lambda_q1, lambda_k1,
              lambda_q2, lambda_k2, subln, router_w, router_b, w_gate_up, b_gate_up,
              w_down, b_down, ple_proj, ple_norm, ple_gate, final_norm):
    split_points = np.cumsum(SPLIT_SIZES)[:-1].tolist()
    h = x
    for i in range(DEPTH):
        hn = rms_norm(h, attn_norm[i])
        proj = hn @ w_in[i]
        b, s, _ = proj.shape
        dq1, dq2, dk1, dk2, dv, sq, sk, sv = jnp.split(proj, split_points, axis=-1)
        hd = lambda t, n: t.reshape(b, s, n, -1)
        lambda_init = 0.8 - 0.6 * math.exp(-0.3 * i)
        lam = (jnp.exp(jnp.sum(lambda_q1[i].astype(jnp.float32) * lambda_k1[i].astype(jnp.float32)))
               - jnp.exp(jnp.sum(lambda_q2[i].astype(jnp.float32) * lambda_k2[i].astype(jnp.float32)))
               + lambda_init)
        o_diff = diff_attention(hd(dq1, N_DIFF_HEADS), hd(dq2, N_DIFF_HEADS), hd(dk1, N_DIFF_HEADS),
                                hd(dk2, N_DIFF_HEADS), hd(dv, N_DIFF_HEADS), lam, subln[i],
                                lambda_init, rel_bias)
        o_sb = stick_breaking_attention(hd(sq, N_SB_HEADS), hd(sk, N_SB_HEADS), hd(sv, N_SB_HEADS))
        h = h + jnp.concatenate([o_diff, o_sb], axis=-1) @ w_out[i]
        h = h + moe_ffn(rms_norm(h, moe_norm[i]), router_w[i], router_b[i], w_gate_up[i],
                        b_gate_up[i], w_down[i], b_down[i])
        gate = jax.nn.sigmoid(h @ ple_gate[i])
        h = h + gate * rms_norm(p[i] @ ple_proj[i], ple_norm[i])
    return rms_norm(h, final_norm)
```

```python
from contextlib import ExitStack
import math
import numpy as np
import ml_dtypes
import concourse.bass as bass
import concourse.mybir as mybir
from concourse.bass_utils import run_bass_kernel_spmd

F32 = mybir.dt.float32
BF16 = mybir.dt.bfloat16
U32 = mybir.dt.uint32
I32 = mybir.dt.int32
AF = mybir.ActivationFunctionType
ALU = mybir.AluOpType
AX = mybir.AxisListType

S = 2048
D = 1024
NT = 16
NE = 32
CAP = 2048
RC = 512
NROUND = 1
EPS = 1e-6
ENG = {"pe": "tensor", "act": "scalar", "dve": "vector", "pool": "gpsimd", "sp": "sync"}


class Buf:
    __slots__ = ("name", "w", "r", "sem", "cnt")

    def __init__(self, P, name):
        self.name = name
        self.w = []
        self.r = []
        self.sem = None
        self.cnt = 0
        P.bufs.append(self)


class Prog:
    def __init__(self, nc):
        self.nc = nc
        self.recs = {e: [] for e in ENG}
        self.waited = {e: {} for e in ENG}
        self.dma_sems = []
        self.bufs = []

    def _filter(self, eng, deps):
        out = []
        for t in deps:
            if t[0] == "c":
                _, e2, idx = t
                if e2 == eng and eng in ("pe", "sp"):
                    continue
                k = ("c", e2)
                if self.waited[eng].get(k, -1) >= idx:
                    continue
                self.waited[eng][k] = idx
                self.recs[e2][idx]["sig"] = True
                out.append(t)
            else:
                _, b, val = t
                k = ("d", id(b))
                if self.waited[eng].get(k, -1) >= val:
                    continue
                self.waited[eng][k] = val
                out.append(t)
        return out

    def _deps(self, eng, reads, writes):
        deps = []
        for b in reads:
            deps += b.w
        for b in writes:
            deps += b.w
            deps += b.r
        return self._filter(eng, deps)

    def op(self, eng, fn, reads=(), writes=(), accum=False):
        waits = self._deps(eng, reads, writes)
        idx = len(self.recs[eng])
        self.recs[eng].append(dict(waits=waits, fn=fn, sig=False, dma=None))
        tok = ("c", eng, idx)
        for b in reads:
            b.r.append(tok)
        for b in writes:
            if accum:
                b.w.append(tok)
            else:
                b.w = [tok]
                b.r = []
        return tok

    def dma(self, eng, fn, reads=(), writes=(), sembuf=None):
        waits = self._deps(eng, reads, writes)
        sb = sembuf if sembuf is not None else (writes[0] if writes else reads[0])
        if sb.sem is None:
            sb.sem = True
            self.dma_sems.append(sb)
        sb.cnt += 16
        tok = ("d", sb, sb.cnt)
        self.recs[eng].append(dict(waits=waits, fn=fn, sig=False, dma=sb))
        for b in reads:
            b.r.append(tok)
        for b in writes:
            b.w = [tok]
            b.r = []
        return tok

    def barrier(self):
        toks = []
        for e in ENG:
            for i in range(len(self.recs[e]) - 1, -1, -1):
                r = self.recs[e][i]
                if r["fn"] is not None and r["dma"] is None:
                    toks.append(("c", e, i))
                    break
        for b in self.bufs:
            toks += [t for t in b.w + b.r if t[0] == "d"]
            b.w = []
            b.r = []
        for e in ENG:
            w = self._filter(e, [t for t in toks if not (t[0] == "c" and t[1] == e)])
            self.recs[e].append(dict(waits=w, fn=None, sig=False, dma=None))

    def wait_all(self, eng, bufs):
        waits = self._deps(eng, list(bufs), list(bufs))
        self.recs[eng].append(dict(waits=waits, fn=None, sig=False, dma=None))

    def run(self, stack):
        nc = self.nc
        esem = {e: stack.enter_context(nc.semaphore("s_" + e)) for e in ENG}
        for i, b in enumerate(self.dma_sems):
            b.sem = stack.enter_context(nc.semaphore("d%d" % i))
        cnts = {}
        for e in ENG:
            c = 0
            for i, r in enumerate(self.recs[e]):
                if r["sig"]:
                    c += 1
                    cnts[(e, i)] = c
        block = stack.enter_context(nc.Block())

        def body(e):
            def f(engine):
                for r in self.recs[e]:
                    for t in r["waits"]:
                        if t[0] == "c":
                            engine.wait_ge(esem[t[1]], cnts[(t[1], t[2])])
                        else:
                            engine.wait_ge(t[1].sem, t[2])
                    if r["fn"] is None:
                        continue
                    ins = r["fn"](engine)
                    if r["dma"] is not None:
                        ins.then_inc(r["dma"].sem, 16)
                    elif r["sig"]:
                        ins.then_inc(esem[e], 1)
            return f

        block.tensor(body("pe"))
        block.scalar(body("act"))
        block.vector(body("dve"))
        block.gpsimd(body("pool"))
        block.sync(body("sp"))


def build(stop=None):
    nc = bass.Bass("TRN2", target_bir_lowering=False, dynamic_dma_scratch_size=8192)
    dram = lambda n, s, d=F32, k="ExternalInput": nc.dram_tensor(n, s, d, kind=k).ap()
    x_d = dram("x", [S, D])
    p_d = dram("p", [S, 256])
    win_d = dram("w_in", [128, 8, 3072])
    wout_d = dram("w_out", [128, 8, D])
    gains_d = dram("gains", [4, D])
    lamv_d = dram("lamv", [4, 64])
    subln_d = dram("subln", [1, 128])
    bnear_d = dram("bnear", [128, 4 * 2 * 128])
    c31_d = dram("c31", [128, 4])
    rw_d = dram("rw", [128, 8 * 32])
    rb_d = dram("rb", [1, 32])
    wgu_d = dram("wgu", [NE, 8, 128, 8, 256])
    bgl_d = dram("bgl", [128, NE * 8 * 2])
    wd_d = dram("wd", [NE, 8, 128, D])
    bd_d = dram("bd", [NE, D])
    pproj_d = dram("pproj", [128, 2, D])
    pgate_d = dram("pgate", [128, 8, D])
    cb_d = dram("cbf", [128, 5 * 128], BF16)
    cf_d = dram("cf32", [128, 3 * 128])
    out_d = dram("out", [S, D], F32, "ExternalOutput")
    h1_d = dram("h1s", [S, D], F32, "Internal")
    xg_d = dram("xg", [NE * CAP, D], BF16, "Internal")
    yg_d = dram("yg", [NE * CAP, D], F32, "Internal")
    dbg = {}

    st = ExitStack()
    P = Prog(nc)
    B = lambda n: Buf(P, n)
    sbt = lambda n, s, d: st.enter_context(nc.sbuf_tensor("sb_" + n, s, d))
    pall = st.enter_context(nc.psum_tensor("pall", [128, 4096], F32))
    psb = [pall[:, i * 512:(i + 1) * 512] for i in range(8)]

    cb = sbt("cb", [128, 5 * 128], BF16)
    cf = sbt("cf", [128, 3 * 128], F32)
    identb, negtri, negones, ltm, onesb = [cb[:, i * 128:(i + 1) * 128] for i in range(5)]
    identf, mask0, strictm = [cf[:, i * 128:(i + 1) * 128] for i in range(3)]
    gainb = sbt("gainb", [128, D], F32)
    gain2 = sbt("gain2", [128, D], F32)
    ebfix = sbt("ebfix", [128, 4 * 2 * 128], F32)
    c31 = sbt("c31", [128, 4], F32)
    lamt = sbt("lamt", [128, 4 * 64], F32)
    lams = sbt("lams", [128, 8], F32)
    subg = sbt("subg", [128, 128], F32)
    rw = sbt("rw", [128, 8 * 32], F32)
    rbb = sbt("rbb", [128, 32], F32)
    bgl = sbt("bgl", [128, NE * 8 * 2], F32)
    cst = sbt("cst", [128, 8], F32)
    stats = sbt("stats", [128, NT * 8], F32)
    meta_dest = sbt("meta_dest", [128, NT * 4], I32)
    meta_g = sbt("meta_g", [128, NT * 4], F32)
    bConst = B("const")
    bGain = B("gain")
    bGain2 = B("gain2")

    AR = sbt("arena", [128, 152 * 1024], mybir.dt.uint8)
    K = 1024
    OFF_HN, OFF_QK, OFF_AT, OFF_W, OFF_V, OFF_T = 0, 32 * K, 64 * K, 96 * K, 128 * K, 145 * K

    def view(off, shape, dt):
        nb = {F32: 4, BF16: 2, I32: 4, U32: 4}[dt]
        n = 1
        for s_ in shape[1:]:
            n *= s_
        ap = AR[:, off:off + n * nb].bitcast(dt)
        if len(shape) == 3:
            ap = ap.rearrange("p (a b) -> p a b", b=shape[2])
        elif len(shape) == 4:
            ap = ap.rearrange("p (a b c) -> p a b c", b=shape[2], c=shape[3])
        return ap

    P.dma("sp", lambda e: e.dma_start(out=cb[:], in_=cb_d), writes=[bConst])
    bC2 = B("c2"); bC3 = B("c3"); bC4 = B("c4"); bC5 = B("c5"); bC6 = B("c6"); bC7 = B("c7"); bC8 = B("c8"); bC9 = B("c9")
    P.dma("sp", lambda e: e.dma_start(out=cf[:], in_=cf_d), writes=[bC2])
    P.dma("sp", lambda e: e.dma_start(out=ebfix[:], in_=bnear_d), writes=[bC3])
    P.dma("sp", lambda e: e.dma_start(out=c31[:], in_=c31_d), writes=[bC4])
    P.dma("sp", lambda e: e.dma_start(out=lamt[:], in_=lamv_d.rearrange("a b -> (a b)").partition_broadcast(128)), writes=[bC5])
    P.dma("sp", lambda e: e.dma_start(out=subg[:], in_=subln_d.rearrange("a b -> (a b)").partition_broadcast(128)), writes=[bC6])
    P.dma("sp", lambda e: e.dma_start(out=rw[:], in_=rw_d), writes=[bC7])
    P.dma("sp", lambda e: e.dma_start(out=rbb[:], in_=rb_d.rearrange("a b -> (a b)").partition_broadcast(128)), writes=[bC8])
    P.dma("sp", lambda e: e.dma_start(out=bgl[:], in_=bgl_d), writes=[bC9])
    P.dma("sp", lambda e: e.dma_start(out=gainb[:], in_=gains_d[0].partition_broadcast(128)), writes=[bGain])
    bCst = B("cst")
    P.op("dve", lambda e: e.memset(cst[:, 0:1], -0.5), writes=[bCst])
    for h in range(4):
        P.op("dve", lambda e, h=h: e.tensor_scalar(out=ebfix[:, h * 256:(h + 1) * 256], in0=ebfix[:, h * 256:(h + 1) * 256],
                                                   scalar1=c31[:, h:h + 1], scalar2=None, op0=ALU.subtract),
             reads=[bC3, bC4], writes=[bC3])
    P.op("act", lambda e: e.activation(out=ebfix[:], in_=ebfix[:], func=AF.Exp), reads=[bC3], writes=[bC3])
    for h in range(4):
        P.op("dve", lambda e, h=h: e.tensor_tensor(out=ebfix[:, h * 256:h * 256 + 128], in0=ebfix[:, h * 256:h * 256 + 128], in1=mask0, op=ALU.mult),
             reads=[bC3, bC2], writes=[bC3])
    P.op("dve", lambda e: e.tensor_tensor(out=lamt[:, 0:64], in0=lamt[:, 0:64], in1=lamt[:, 64:128], op=ALU.mult), reads=[bC5], writes=[bC5])
    P.op("dve", lambda e: e.tensor_tensor(out=lamt[:, 128:192], in0=lamt[:, 128:192], in1=lamt[:, 192:256], op=ALU.mult), reads=[bC5], writes=[bC5])
    P.op("dve", lambda e: e.tensor_reduce(out=lams[:, 0:1], in_=lamt[:, 0:64], axis=AX.X, op=ALU.add), reads=[bC5], writes=[bC5])
    P.op("dve", lambda e: e.tensor_reduce(out=lams[:, 1:2], in_=lamt[:, 128:192], axis=AX.X, op=ALU.add), reads=[bC5], writes=[bC5])
    P.op("act", lambda e: e.activation(out=lams[:, 2:4], in_=lams[:, 0:2], func=AF.Exp), reads=[bC5], writes=[bC5])
    P.op("dve", lambda e: e.tensor_tensor(out=lams[:, 4:5], in0=lams[:, 3:4], in1=lams[:, 2:3], op=ALU.subtract), reads=[bC5], writes=[bC5])
    P.op("dve", lambda e: e.tensor_scalar(out=lams[:, 4:5], in0=lams[:, 4:5], scalar1=-0.2, scalar2=None, op0=ALU.add), reads=[bC5], writes=[bC5])
    P.op("dve", lambda e: e.tensor_scalar(out=subg[:], in0=subg[:], scalar1=0.8, scalar2=None, op0=ALU.mult), reads=[bC6], writes=[bC6])
    bglv = bgl[:].rearrange("p (a t) -> p a t", t=2)
    P.op("dve", lambda e: e.tensor_scalar(out=bglv[:, :, 1:2], in0=bglv[:, :, 1:2], scalar1=1.0, scalar2=None, op0=ALU.add), reads=[bC9], writes=[bC9])
    neglam = lams[:, 4:5]

    hnT = view(OFF_HN, [128, 8, S], BF16)
    bHn = [B("hn%d" % i) for i in range(NT)]
    xt = [view(OFF_W + i * 4096, [128, D], F32) for i in range(2)]
    xs = [view(OFF_W + 8192 + i * 2048, [128, D], BF16) for i in range(2)]
    junkb = view(OFF_T, [128, D], BF16)
    bXt = [B("xt%d" % i) for i in range(2)]
    bXs = [B("xs%d" % i) for i in range(2)]
    bJ = B("junk")
    bSt = [B("st%d" % i) for i in range(NT)]
    bPs = [B("ps%d" % i) for i in range(8)]

    def rms_stats(i, src, srcbuf, n):
        c0 = i * 8
        P.op("act", lambda e: e.activation(out=junkb[:, 0:n], in_=src, func=AF.Square, accum_out=stats[:, c0:c0 + 1]),
             reads=[srcbuf], writes=[bJ, bSt[i]])
        P.op("dve", lambda e: e.tensor_scalar(out=stats[:, c0 + 1:c0 + 2], in0=stats[:, c0:c0 + 1], scalar1=1.0 / n, scalar2=EPS, op0=ALU.mult, op1=ALU.add),
             reads=[bSt[i]], writes=[bSt[i]])
        P.op("pool", lambda e: e.tensor_tensor(out=stats[:, c0 + 3:c0 + 4], in0=stats[:, c0 + 1:c0 + 2], in1=cst[:, 0:1], op=ALU.pow),
             reads=[bSt[i], bCst], writes=[bSt[i]])
        return stats[:, c0 + 3:c0 + 4]

    for i in range(NT):
        b = i % 2
        P.dma("sp", lambda e, i=i, b=b: e.dma_start(out=xt[b], in_=x_d[i * 128:(i + 1) * 128, :]), writes=[bXt[b]])
        rstd = rms_stats(i, xt[b], bXt[b], D)
        P.op("dve", lambda e, b=b, rstd=rstd: e.scalar_tensor_tensor(out=xs[b], in0=xt[b], scalar=rstd, in1=gainb[:], op0=ALU.mult, op1=ALU.mult),
             reads=[bXt[b], bSt[i], bGain], writes=[bXs[b]])
        pT = psb[b].bitcast(BF16)
        for c in range(8):
            P.op("pe", lambda e, c=c, b=b, pT=pT: e.transpose(pT[:, c * 128:(c + 1) * 128], xs[b][:, c * 128:(c + 1) * 128], identb),
                 reads=[bXs[b], bConst], writes=[bPs[b]])
        P.op("act", lambda e, i=i, pT=pT: e.activation(out=hnT[:, :, i * 128:(i + 1) * 128], in_=pT.rearrange("p (c t) -> p c t", t=128), func=AF.Copy),
             reads=[bPs[b]], writes=[bHn[i]])

    if stop == "A":
        dbg["hnT"] = (hnT, [128, 8, S], BF16)
        return finish(nc, P, st, dbg)

    QK = view(OFF_QK, [128, 8, S], BF16)
    VV = view(OFF_V, [128, NT, 516], BF16)
    wsl = [view(OFF_W + i * 4096, [128, 8, 256], BF16) for i in range(3)]
    bW = [B("wsl%d" % i) for i in range(3)]
    bQK = [B("qk%d" % i) for i in range(8)]
    bV = [B("v%d" % i) for i in range(NT)]
    slab_ctr = [0]
    evac_ctr = [0]

    def project(col0, kind):
        P.barrier()
        if kind == "diff":
            VD4 = VV.rearrange("p t (h c) -> p t h c", c=129)
            P.op("dve", lambda e: e.memset(VD4[:, :, :, 128:129], 1.0), writes=bV)
        for s in range(6):
            wi = slab_ctr[0] % 3
            slab_ctr[0] += 1
            c_lo = col0 + s * 256
            P.dma("pool", lambda e, wi=wi, c_lo=c_lo: e.dma_start(out=wsl[wi], in_=win_d[:, :, c_lo:c_lo + 256]), writes=[bW[wi]])
            if s < 4:
                for gg in range(2):
                    gi = s * 2 + gg
                    for tc in range(4):
                        bk = evac_ctr[0] % 4
                        evac_ctr[0] += 1
                        for c in range(8):
                            P.op("pe", lambda e, wi=wi, gg=gg, tc=tc, c=c, bk=bk: e.matmul(
                                psb[bk], lhsT=wsl[wi][:, c, gg * 128:(gg + 1) * 128], rhs=hnT[:, c, tc * 512:(tc + 1) * 512],
                                start=(c == 0), stop=(c == 7)),
                                reads=[bW[wi]] + bHn[tc * 4:tc * 4 + 4], writes=[bPs[bk]])
                        sc = 0.125 if s < 2 else 1.0
                        if evac_ctr[0] % 2 == 0:
                            P.op("act", lambda e, gi=gi, tc=tc, bk=bk, sc=sc: e.activation(out=QK[:, gi, tc * 512:(tc + 1) * 512], in_=psb[bk], func=AF.Copy, scale=sc),
                                 reads=[bPs[bk]], writes=[bQK[gi]], accum=True)
                        else:
                            P.op("dve", lambda e, gi=gi, tc=tc, bk=bk, sc=sc: e.tensor_scalar(out=QK[:, gi, tc * 512:(tc + 1) * 512], in0=psb[bk], scalar1=sc, scalar2=None, op0=ALU.mult),
                                 reads=[bPs[bk]], writes=[bQK[gi]], accum=True)
            else:
                vs = s - 4
                for i in range(NT):
                    bk = evac_ctr[0] % 4
                    evac_ctr[0] += 1
                    for c in range(8):
                        P.op("pe", lambda e, wi=wi, i=i, c=c, bk=bk: e.matmul(
                            psb[bk][:, 0:256], lhsT=hnT[:, c, i * 128:(i + 1) * 128], rhs=wsl[wi][:, c, :], start=(c == 0), stop=(c == 7)),
                            reads=[bW[wi], bHn[i]], writes=[bPs[bk]])
                    if kind == "diff":
                        dst = VV[:, i, vs * 258:(vs + 1) * 258].rearrange("p (h c) -> p h c", c=129)[:, :, 0:128]
                        src = psb[bk][:, 0:256].rearrange("p (h c) -> p h c", c=128)
                    else:
                        dst = VV[:, i, vs * 256:(vs + 1) * 256]
                        src = psb[bk][:, 0:256]
                    if evac_ctr[0] % 2 == 0:
                        P.op("act", lambda e, dst=dst, src=src: e.activation(out=dst, in_=src, func=AF.Copy), reads=[bPs[bk]], writes=[bV[i]], accum=True)
                    else:
                        P.op("dve", lambda e, dst=dst, src=src: e.tensor_copy(out=dst, in_=src), reads=[bPs[bk]], writes=[bV[i]], accum=True)

    project(0, "diff")
    if stop == "B":
        dbg["QK"] = (QK, [128, 8, S], BF16)
        dbg["VV"] = (VV, [128, NT, 516], BF16)
        return finish(nc, P, st, dbg)

    P.barrier()
    attnT = view(OFF_AT, [128, 8, S], BF16)
    bAT = [B("at%d" % i) for i in range(NT)]
    NSLOT = 32
    Er = [view(OFF_W + i * 1024, [128, 2, 256], BF16) for i in range(NSLOT)]
    bE = [B("E%d" % i) for i in range(NSLOT)]
    def tv(par, k):
        base = OFF_T + par * 1296
        if k == 0:
            return view(base, [128, 130], F32)
        if k == 1:
            return view(base + 520, [128, 130], F32)
        return view(base + 1040, [128, 128], BF16)
    bEp = [B("ep%d" % i) for i in range(4)]
    late = []
    bSS = [B("S%d" % i) for i in range(2)]
    bO = [B("O%d" % m) for m in range(2)]
    bTp = [B("tp%d" % i) for i in range(2)]
    VD4 = VV.rearrange("p t (h c) -> p t h c", c=129)
    ebv = ebfix[:].rearrange("p (h d q) -> p h d q", h=4, d=2)

    units = [(h, c) for h in range(4) for c in range(8)]
    import os
    if stop == 'C1':
        units = units[:1]
    if os.environ.get('KLIM'):
        units = units[:int(os.environ['KLIM'])]
    blk_ctr = [0]
    ep_ctr = [0]

    def av_items(h, c, slots):
        items = []
        for j in range(2):
            for m in range(2):
                kbs = list(range(0, 2 * c + j + 1))
                for kb in kbs:
                    items.append((j, m, kb, kb == 0, kb == kbs[-1]))
        return items

    def emit_av(h, c, slots, it):
        j, m, kb, first, last = it
        ob = psb[4 + m][:, 0:129]
        sl = slots[kb]
        P.op("pe", lambda e: e.matmul(ob, lhsT=Er[sl][:, m, j * 128:(j + 1) * 128], rhs=VD4[:, kb, h, :], start=first, stop=last),
             reads=[bE[sl], bV[kb]], writes=[bO[m]])
        if last:
            par = ep_ctr[0] % 4
            P.op("dve", lambda e: e.tensor_copy(out=tv(par, m)[:, 0:129], in_=ob), reads=[bO[m]], writes=[bEp[par]], accum=(m == 1))
            if m == 1:
                epilogue(h, c, j)

    def epilogue(h, c, j):
        qb = 2 * c + j
        par = ep_ctr[0] % 4
        ep_ctr[0] += 1
        si = qb
        c0 = si * 8
        o1 = tv(par, 0); o2 = tv(par, 1); obf = tv(par, 2)
        ep = [bEp[par]]
        P.op("dve", lambda e: e.reciprocal(out=stats[:, c0:c0 + 1], in_=o1[:, 128:129]), reads=ep, writes=[bSt[si]])
        P.op("dve", lambda e: e.reciprocal(out=stats[:, c0 + 1:c0 + 2], in_=o2[:, 128:129]), reads=ep, writes=[bSt[si]])
        P.op("dve", lambda e: e.tensor_scalar(out=stats[:, c0 + 2:c0 + 3], in0=stats[:, c0 + 1:c0 + 2], scalar1=neglam, scalar2=None, op0=ALU.mult),
             reads=[bSt[si], bC5], writes=[bSt[si]])
        P.op("dve", lambda e: e.tensor_scalar(out=o2[:, 0:128], in0=o2[:, 0:128], scalar1=stats[:, c0 + 2:c0 + 3], scalar2=None, op0=ALU.mult),
             reads=ep + [bSt[si]], writes=ep)
        P.op("dve", lambda e: e.scalar_tensor_tensor(out=o1[:, 0:128], in0=o1[:, 0:128], scalar=stats[:, c0:c0 + 1], in1=o2[:, 0:128], op0=ALU.mult, op1=ALU.add),
             reads=ep + [bSt[si]], writes=ep)
        P.op("dve", lambda e: e.tensor_tensor(out=o2[:, 0:128], in0=o1[:, 0:128], in1=o1[:, 0:128], op=ALU.mult), reads=ep, writes=ep)
        P.op("dve", lambda e: e.tensor_reduce(out=stats[:, c0 + 3:c0 + 4], in_=o2[:, 0:128], axis=AX.X, op=ALU.add), reads=ep, writes=[bSt[si]])
        P.op("dve", lambda e: e.tensor_scalar(out=stats[:, c0 + 4:c0 + 5], in0=stats[:, c0 + 3:c0 + 4], scalar1=1.0 / 128, scalar2=EPS, op0=ALU.mult, op1=ALU.add),
             reads=[bSt[si]], writes=[bSt[si]])
        P.op("pool", lambda e: e.tensor_tensor(out=stats[:, c0 + 5:c0 + 6], in0=stats[:, c0 + 4:c0 + 5], in1=cst[:, 0:1], op=ALU.pow),
             reads=[bSt[si], bCst], writes=[bSt[si]])
        P.op("dve", lambda e: e.scalar_tensor_tensor(out=obf, in0=o1[:, 0:128], scalar=stats[:, c0 + 5:c0 + 6], in1=subg[:], op0=ALU.mult, op1=ALU.mult),
             reads=ep + [bSt[si], bC6], writes=ep)
        tb = psb[6 + par % 2].bitcast(BF16)

        def fin():
            P.op("pe", lambda e: e.transpose(tb[:, 0:128], obf, identb), reads=[bEp[par], bConst], writes=[bTp[par % 2]])
            P.op("dve", lambda e: e.tensor_copy(out=attnT[:, h, qb * 128:(qb + 1) * 128], in_=tb[:, 0:128]), reads=[bTp[par % 2]], writes=[bAT[qb]], accum=True)
        late.append(fin)

    pending = []
    for ui, (h, c) in enumerate(units):
        nb = 2 * c + 2
        slots = {}
        per = 0
        late_now = list(late)
        del late[:]
        for kb in range(nb):
            if kb == 1:
                for f_ in late_now:
                    f_()
            sl = blk_ctr[0] % NSLOT
            blk_ctr[0] += 1
            slots[kb] = sl
            sp_ = kb % 2
            lo = 128 if kb == 2 * c + 1 else 0
            sb3 = pall[:, sp_ * 512:sp_ * 512 + 2048].rearrange("p (m r) -> p m r", m=2)
            for m in range(2):
                P.op("pe", lambda e, m=m, kb=kb, lo=lo, sp_=sp_, h=h, c=c: e.matmul(
                    psb[2 * m + sp_][:, lo:256], lhsT=QK[m * 64:(m + 1) * 64, 4 + h, kb * 128:(kb + 1) * 128],
                    rhs=QK[m * 64:(m + 1) * 64, h, c * 256 + lo:(c + 1) * 256], start=True, stop=True),
                    reads=[bQK[4 + h], bQK[h]], writes=[bSS[sp_]])
            P.op("act", lambda e, sl=sl, lo=lo, sb3=sb3: e.activation(out=Er[sl][:, :, lo:256], in_=sb3[:, :, lo:256], func=AF.Exp),
                 reads=[bSS[sp_]], writes=[bE[sl]])
            for j in range(2):
                d = 2 * c + j - kb
                if 0 <= d <= 1:
                    for m in range(2):
                        P.op("dve", lambda e, sl=sl, m=m, j=j, d=d, h=h: e.tensor_tensor(
                            out=Er[sl][:, m, j * 128:(j + 1) * 128], in0=Er[sl][:, m, j * 128:(j + 1) * 128], in1=ebv[:, h, d, :], op=ALU.mult),
                            reads=[bE[sl], bC3], writes=[bE[sl]])
            for _ in range(per):
                if pending:
                    emit_av(*pending.pop(0))
        pending = [(h, c, slots, it) for it in av_items(h, c, slots)]
        while pending:
            emit_av(*pending.pop(0))
        for f_ in late:
            f_()
        del late[:]
    while pending:
        emit_av(*pending.pop(0))
    for f_ in late:
        f_()

    if stop == "C1":
        dbg["E0"] = (Er[0], [128, 2, 256], BF16)
        dbg["E1"] = (Er[1], [128, 2, 256], BF16)
        dbg["ebfix"] = (ebfix[:], [128, 1024], F32)
        dbg["lams"] = (lams[:], [128, 8], F32)
        dbg["o1s"] = (tv(0, 0), [128, 130], F32)
        dbg["obf"] = (tv(0, 2), [128, 128], BF16)
        dbg["at0"] = (attnT[:, 0, 0:256], [128, 256], BF16)
        dbg["stats"] = (stats[:], [128, 128], F32)
        return finish(nc, P, st, dbg)
    if stop == "C":
        dbg["attnT"] = (attnT, [128, 8, S], BF16)
        return finish(nc, P, st, dbg)

    project(1536, "sb")
    P.barrier()
    Wr = [view(OFF_W + i * 512, [128, 256], BF16) for i in range(4)]
    bWr = [B("Wr%d" % i) for i in range(4)]
    e32 = [view(OFF_W + 2048 + i * 1024, [128, 256], F32) for i in range(2)]
    Lb = [view(OFF_W + 4096 + i * 512, [128, 256], BF16) for i in range(2)]
    Rb = [view(OFF_W + 5120 + i * 512, [128, 256], BF16) for i in range(3)]
    be32 = [B("e32%d" % i) for i in range(2)]
    bL = [B("L%d" % i) for i in range(2)]
    bR = [B("R%d" % i) for i in range(3)]
    bZ = [B("Z%d" % i) for i in range(2)]
    bX = [B("X%d" % i) for i in range(2)]
    bOT = [B("OT%d" % i) for i in range(2)]

    blocks = []
    for hd in range(8):
        for c in range(8):
            for kb in range(2 * c + 1, -1, -1):
                blocks.append((hd, c, kb))
    NB = len(blocks)

    def sb_pe1(i):
        hd, c, kb = blocks[i]
        g, po = hd // 2, (hd % 2) * 64
        lo = 128 if kb == 2 * c + 1 else 0
        pz = i % 2
        P.op("pe", lambda e: e.matmul(psb[pz][:, lo:256], lhsT=QK[po:po + 64, 4 + g, kb * 128:(kb + 1) * 128],
                                      rhs=QK[po:po + 64, g, c * 256 + lo:(c + 1) * 256], start=True, stop=True),
             reads=[bQK[4 + g], bQK[g]], writes=[bZ[pz]])

    def sb_act1(i):
        hd, c, kb = blocks[i]
        lo = 128 if kb == 2 * c + 1 else 0
        pz = i % 2
        first = kb == 2 * c + 1
        P.op("act", lambda e: e.activation(out=e32[pz][:, lo:256], in_=psb[pz][:, lo:256], func=AF.Exp), reads=[bZ[pz]], writes=[be32[pz]])
        if first:
            P.op("dve", lambda e: e.memset(e32[pz][:, 0:128], 0.0), writes=[be32[pz]], accum=True)
        j = kb - 2 * c
        if j >= 0:
            P.op("dve", lambda e: e.tensor_tensor(out=e32[pz][:, j * 128:(j + 1) * 128], in0=e32[pz][:, j * 128:(j + 1) * 128], in1=strictm, op=ALU.mult),
                 reads=[be32[pz], bC2], writes=[be32[pz]])
        P.op("act", lambda e: e.activation(out=Lb[pz][:], in_=e32[pz][:], func=AF.Ln, bias=1.0), reads=[be32[pz]], writes=[bL[pz]])
        if kb > 0:
            if first:
                P.op("dve", lambda e: e.tensor_copy(out=Rb[(i + 1) % 3][:], in_=Lb[pz][:]), reads=[bL[pz]], writes=[bR[(i + 1) % 3]])
            else:
                P.op("dve", lambda e: e.tensor_tensor(out=Rb[(i + 1) % 3][:], in0=Rb[i % 3][:], in1=Lb[pz][:], op=ALU.add),
                     reads=[bL[pz], bR[i % 3]], writes=[bR[(i + 1) % 3]])

    def sb_pe2(i):
        hd, c, kb = blocks[i]
        g, po = hd // 2, (hd % 2) * 64
        lo = 128 if kb == 2 * c + 1 else 0
        pz = i % 2
        first = kb == 2 * c + 1
        xb = psb[2 + pz]
        P.op("pe", lambda e: e.matmul(xb[:, lo:256], lhsT=QK[po:po + 64, 4 + g, kb * 128:(kb + 1) * 128],
                                      rhs=QK[po:po + 64, g, c * 256 + lo:(c + 1) * 256], start=True, stop=False),
             reads=[bQK[4 + g], bQK[g]], writes=[bX[pz]])
        P.op("pe", lambda e: e.matmul(xb[:, lo:256], lhsT=negtri, rhs=Lb[pz][:, lo:256], start=False, stop=first),
             reads=[bL[pz], bConst], writes=[bX[pz]])
        if not first:
            P.op("pe", lambda e: e.matmul(xb[:, lo:256], lhsT=negones, rhs=Rb[i % 3][:, lo:256], start=False, stop=True),
                 reads=[bR[i % 3], bConst], writes=[bX[pz]])

    def sb_act2(i):
        hd, c, kb = blocks[i]
        lo = 128 if kb == 2 * c + 1 else 0
        pz = i % 2
        wi = i % 4
        first = kb == 2 * c + 1
        P.op("act", lambda e: e.activation(out=Wr[wi][:, lo:256], in_=psb[2 + pz][:, lo:256], func=AF.Exp), reads=[bX[pz]], writes=[bWr[wi]])
        if first:
            P.op("dve", lambda e: e.memset(Wr[wi][:, 0:128], 0.0), writes=[bWr[wi]], accum=True)
        j = kb - 2 * c
        if j >= 0:
            P.op("dve", lambda e: e.tensor_tensor(out=Wr[wi][:, j * 128:(j + 1) * 128], in0=Wr[wi][:, j * 128:(j + 1) * 128], in1=strictm, op=ALU.mult),
                 reads=[bWr[wi], bC2], writes=[bWr[wi]])

    unit_ctr = [0]

    def sb_pe3(i):
        hd, c, kb = blocks[i]
        g, po = hd // 2, (hd % 2) * 64
        wi = i % 4
        first = kb == 2 * c + 1
        last = kb == 0
        up = (hd * 8 + c) % 2
        ob = psb[4 + up]
        P.op("pe", lambda e: e.matmul(ob[po:po + 64, 0:256], lhsT=VV[:, kb, hd * 64:(hd + 1) * 64], rhs=Wr[wi][:], start=first, stop=last),
             reads=[bWr[wi], bV[kb]], writes=[bOT[up]])
        if last:
            if (hd * 8 + c) % 2 == 0:
                P.op("act", lambda e: e.activation(out=attnT[po:po + 64, 4 + g, c * 256:(c + 1) * 256], in_=ob[po:po + 64, 0:256], func=AF.Copy),
                     reads=[bOT[up]], writes=[bAT[2 * c], bAT[2 * c + 1]], accum=True)
            else:
                P.op("dve", lambda e: e.tensor_copy(out=attnT[po:po + 64, 4 + g, c * 256:(c + 1) * 256], in_=ob[po:po + 64, 0:256]),
                     reads=[bOT[up]], writes=[bAT[2 * c], bAT[2 * c + 1]], accum=True)

    for s_ in range(NB + 2):
        if s_ < NB:
            sb_pe1(s_)
            sb_act1(s_)
        if 0 <= s_ - 1 < NB:
            sb_pe2(s_ - 1)
            sb_act2(s_ - 1)
        if 0 <= s_ - 2 < NB:
            sb_pe3(s_ - 2)

    if stop == "D":
        dbg["attnT"] = (attnT, [128, 8, S], BF16)
        return finish(nc, P, st, dbg)

    P.barrier()
    KB = 1024
    hres = view(0, [128, NT, D], F32)
    tT = view(96 * KB, [128, 8, S], BF16)
    wob = view(128 * KB, [128, 8, D], BF16)
    xt1 = view(144 * KB, [128, D], F32)
    tb1 = view(148 * KB, [128, D], BF16)
    junk2 = view(150 * KB, [128, D], BF16)
    tmpx = sbt("tmpx", [128, 4 * 512], F32)
    comb = sbt("comb", [128, NT * 32], F32)
    lgs = sbt("lgs", [128, 4 * 32], F32)
    mx8 = sbt("mx8", [128, 16], F32)
    rwb = sbt("rwb", [128, 256], BF16)
    bH = [B("h%d" % i_) for i_ in range(NT)]
    bTT = [B("tT%d" % i_) for i_ in range(NT)]
    bWo = B("wo"); bX1 = B("x1"); bTb = B("tb1"); bJ2 = B("junk2"); bRwb = B("rwb"); bLg = B("lg"); bMx = B("mx"); bComb = B("comb")
    bPE = [B("pe%d" % i_) for i_ in range(8)]
    P.dma("pool", lambda e: e.dma_start(out=wob, in_=wout_d), writes=[bWo])
    P.dma("pool", lambda e: e.dma_start(out=rwb[:], in_=rw_d), writes=[bRwb])
    P.dma("sp", lambda e: e.dma_start(out=gainb[:], in_=gains_d[1].partition_broadcast(128)), writes=[bGain])

    def rms2(i_, src, srcbuf, n):
        c0 = i_ * 8
        P.op("act", lambda e: e.activation(out=junk2[:, 0:n], in_=src, func=AF.Square, accum_out=stats[:, c0:c0 + 1]), reads=[srcbuf], writes=[bJ2, bSt[i_]])
        P.op("dve", lambda e: e.tensor_scalar(out=stats[:, c0 + 1:c0 + 2], in0=stats[:, c0:c0 + 1], scalar1=1.0 / n, scalar2=EPS, op0=ALU.mult, op1=ALU.add), reads=[bSt[i_]], writes=[bSt[i_]])
        P.op("pool", lambda e: e.tensor_tensor(out=stats[:, c0 + 3:c0 + 4], in0=stats[:, c0 + 1:c0 + 2], in1=cst[:, 0:1], op=ALU.pow), reads=[bSt[i_], bCst], writes=[bSt[i_]])
        return stats[:, c0 + 3:c0 + 4]

    def phaseE(i_):
        P.dma("sp", lambda e: e.dma_start(out=xt1, in_=x_d[i_ * 128:(i_ + 1) * 128, :]), writes=[bX1])
        for half in range(2):
            bk = 2 * (i_ % 2) + half
            for c_ in range(8):
                P.op("pe", lambda e, c_=c_, bk=bk, half=half: e.matmul(psb[bk], lhsT=attnT[:, c_, i_ * 128:(i_ + 1) * 128], rhs=wob[:, c_, half * 512:(half + 1) * 512], start=(c_ == 0), stop=(c_ == 7)),
                     reads=[bAT[i_], bWo], writes=[bPE[bk]])
            P.op("dve", lambda e, bk=bk, half=half: e.tensor_tensor(out=hres[:, i_, half * 512:(half + 1) * 512], in0=psb[bk], in1=xt1[:, half * 512:(half + 1) * 512], op=ALU.add),
                 reads=[bPE[bk], bX1], writes=[bH[i_]], accum=(half == 1))
        rstd = rms2(i_, hres[:, i_, :], bH[i_], D)
        P.op("dve", lambda e: e.scalar_tensor_tensor(out=tb1, in0=hres[:, i_, :], scalar=rstd, in1=gainb[:], op0=ALU.mult, op1=ALU.mult), reads=[bH[i_], bSt[i_], bGain], writes=[bTb])
        pT = psb[4 + i_ % 2].bitcast(BF16)
        for c_ in range(8):
            P.op("pe", lambda e, c_=c_: e.transpose(pT[:, c_ * 128:(c_ + 1) * 128], tb1[:, c_ * 128:(c_ + 1) * 128], identb), reads=[bTb, bConst], writes=[bPE[4 + i_ % 2]])
        P.op("act", lambda e: e.activation(out=tT[:, :, i_ * 128:(i_ + 1) * 128], in_=pT.rearrange("p (c t) -> p c t", t=128), func=AF.Copy), reads=[bPE[4 + i_ % 2]], writes=[bTT[i_]])
        for c_ in range(8):
            P.op("pe", lambda e, c_=c_: e.matmul(psb[6][:, 0:32], lhsT=tT[:, c_, i_ * 128:(i_ + 1) * 128], rhs=rwb[:, c_ * 32:(c_ + 1) * 32], start=(c_ == 0), stop=(c_ == 7)),
                 reads=[bTT[i_], bRwb], writes=[bPE[6]])
        lg = lgs[:, 0:32]; exl = lgs[:, 32:64]; msk = lgs[:, 64:96]
        P.op("dve", lambda e: e.tensor_tensor(out=lg, in0=psb[6][:, 0:32], in1=rbb[:], op=ALU.add), reads=[bPE[6], bC8], writes=[bLg])
        P.op("dve", lambda e: e.max(out=mx8[:, 0:8], in_=lg), reads=[bLg], writes=[bMx])
        P.op("dve", lambda e: e.tensor_scalar(out=mx8[:, 8:9], in0=mx8[:, 0:1], scalar1=-1.0, scalar2=None, op0=ALU.mult), reads=[bMx], writes=[bMx])
        P.op("act", lambda e: e.activation(out=exl, in_=lg, func=AF.Exp, bias=mx8[:, 8:9]), reads=[bLg, bMx], writes=[bLg])
        P.op("dve", lambda e: e.tensor_scalar(out=msk, in0=lg, scalar1=mx8[:, 3:4], scalar2=None, op0=ALU.is_ge), reads=[bLg, bMx], writes=[bLg])
        P.op("dve", lambda e: e.tensor_tensor(out=exl, in0=exl, in1=msk, op=ALU.mult), reads=[bLg], writes=[bLg])
        P.op("dve", lambda e: e.tensor_reduce(out=mx8[:, 9:10], in_=exl, axis=AX.X, op=ALU.add), reads=[bLg], writes=[bMx])
        P.op("dve", lambda e: e.reciprocal(out=mx8[:, 10:11], in_=mx8[:, 9:10]), reads=[bMx], writes=[bMx])
        P.op("dve", lambda e: e.tensor_scalar(out=comb[:, i_ * 32:(i_ + 1) * 32], in0=exl, scalar1=mx8[:, 10:11], scalar2=None, op0=ALU.mult), reads=[bLg, bMx], writes=[bComb], accum=True)

    for i_ in range(NT):
        phaseE(i_)

    if stop == "E":
        dbg["h1"] = (hres, [128, NT, D], F32)
        dbg["tT"] = (tT, [128, 8, S], BF16)
        dbg["comb"] = (comb[:], [128, NT * 32], F32)
        return finish(nc, P, st, dbg)

    P.barrier()
    actT = view(64 * KB, [128, 8, S], BF16)
    wdb = view(128 * KB, [128, 8, D], BF16)
    wg = [view(144 * KB + k_ * 4096, [128, 8, 256], BF16) for k_ in range(2)]
    bdt = ebfix
    bWd = B("wd"); bWg = [B("wg%d" % k_) for k_ in range(2)]; bBd = B("bdt")
    bAc = [B("ac%d" % k_) for k_ in range(4)]
    bTg = B("tg"); bTs = B("ts"); bTl = B("tl"); bTy = B("ty")
    tg = tmpx[:, 0:512]; ts = tmpx[:, 512:1024]; tl = tmpx[:, 1024:1536]; ty = tmpx[:, 1536:2048]
    cnt = [0]

    def gu_chunk(e_, j_, k_, tc):
        ba = 2 * (cnt[0] % 2)
        cnt[0] += 1
        for which in range(2):
            for c_ in range(8):
                P.op("pe", lambda e, c_=c_, which=which: e.matmul(psb[ba + which], lhsT=wg[k_][:, c_, which * 128:(which + 1) * 128], rhs=tT[:, c_, tc * 512:(tc + 1) * 512], start=(c_ == 0), stop=(c_ == 7)),
                     reads=[bWg[k_]] + bTT[tc * 4:tc * 4 + 4], writes=[bPE[ba + which]])
        col = (e_ * 8 + j_) * 2
        P.op("dve", lambda e: e.tensor_scalar(out=tg, in0=psb[ba], scalar1=bgl[:, col:col + 1], scalar2=7.0, op0=ALU.add, op1=ALU.min), reads=[bPE[ba], bC9], writes=[bTg])
        P.op("act", lambda e: e.activation(out=ts, in_=tg, func=AF.Sigmoid, scale=1.702), reads=[bTg], writes=[bTs])
        P.op("dve", lambda e: e.tensor_scalar(out=tl, in0=psb[ba + 1], scalar1=bgl[:, col + 1:col + 2], scalar2=8.0, op0=ALU.add, op1=ALU.min), reads=[bPE[ba + 1], bC9], writes=[bTl])
        P.op("pool", lambda e: e.tensor_tensor(out=ts, in0=tg, in1=ts, op=ALU.mult), reads=[bTg, bTs], writes=[bTs])
        P.op("dve", lambda e: e.scalar_tensor_tensor(out=actT[:, j_, tc * 512:(tc + 1) * 512], in0=tl, scalar=-6.0, in1=ts, op0=ALU.max, op1=ALU.mult), reads=[bTl, bTs], writes=[bAc[tc]], accum=True)

    def down_tile(e_, i_, half):
        bk = 4 + cnt[0] % 2
        cnt[0] += 1
        for j_ in range(8):
            P.op("pe", lambda e, j_=j_: e.matmul(psb[bk], lhsT=actT[:, j_, i_ * 128:(i_ + 1) * 128], rhs=wdb[:, j_, half * 512:(half + 1) * 512], start=(j_ == 0), stop=(j_ == 7)),
                 reads=[bAc[i_ // 4], bWd], writes=[bPE[bk]])
        P.op("dve", lambda e: e.tensor_tensor(out=ty, in0=psb[bk], in1=bdt[:, half * 512:(half + 1) * 512], op=ALU.add), reads=[bPE[bk], bBd], writes=[bTy])
        P.op("dve", lambda e: e.scalar_tensor_tensor(out=hres[:, i_, half * 512:(half + 1) * 512], in0=ty, scalar=comb[:, i_ * 32 + e_:i_ * 32 + e_ + 1], in1=hres[:, i_, half * 512:(half + 1) * 512], op0=ALU.mult, op1=ALU.add),
             reads=[bTy, bComb, bH[i_]], writes=[bH[i_]])

    def expert(e_):
        P.dma("sp", lambda e: e.dma_start(out=bdt[:], in_=bd_d[e_].partition_broadcast(128)), writes=[bBd])
        P.dma("pool", lambda e: e.dma_start(out=wdb, in_=wd_d[e_].rearrange("j p n -> p j n")), writes=[bWd])
        for j_ in range(8):
            k_ = (e_ * 8 + j_) % 2
            P.dma("pool", lambda e, j_=j_, k_=k_: e.dma_start(out=wg[k_], in_=wgu_d[e_, j_]), writes=[bWg[k_]])
            for tc in range(4):
                gu_chunk(e_, j_, k_, tc)
        for i_ in range(NT):
            for half in range(2):
                down_tile(e_, i_, half)

    NEX = int(os.environ.get("KNEX", NE))
    for e_ in range(NEX):
        expert(e_)

    if stop == "G":
        dbg["h2"] = (hres, [128, NT, D], F32)
        return finish(nc, P, st, dbg)

    P.barrier()
    pgb = view(128 * KB, [128, 8, D], BF16)
    ppb = view(144 * KB, [128, 2, D], BF16)
    ptile = view(148 * KB, [128, 256], F32)
    pbf = view(149 * KB, [128, 256], BF16)
    pTs = view(149 * KB + 512, [128, 2, 128], BF16)
    pe32 = view(64 * KB, [128, D], F32)
    gate = view(68 * KB, [128, D], F32)
    hbf = view(72 * KB, [128, D], BF16)
    hT = view(74 * KB, [128, 8, 128], BF16)
    otile = [view(76 * KB + k_ * 4096, [128, D], F32) for k_ in range(2)]
    gfin = ebfix
    bPg = B("pg"); bPp = B("pp"); bPt = B("pt"); bPbf = B("pbf"); bPTs = B("pTs"); bPe32 = B("pe32"); bGate = B("gate"); bHbf = B("hbf"); bHT = B("hT")
    bOt = [B("ot%d" % k_) for k_ in range(2)]; bGf = B("gfin"); bOut = B("out")
    P.dma("pool", lambda e: e.dma_start(out=pgb, in_=pgate_d), writes=[bPg])
    P.dma("pool", lambda e: e.dma_start(out=ppb, in_=pproj_d), writes=[bPp])
    P.dma("sp", lambda e: e.dma_start(out=gainb[:], in_=gains_d[2].partition_broadcast(128)), writes=[bGain])
    P.dma("sp", lambda e: e.dma_start(out=gfin[:], in_=gains_d[3].partition_broadcast(128)), writes=[bGf])

    def phaseH(i_):
        k_ = i_ % 2
        P.dma("sp", lambda e: e.dma_start(out=ptile, in_=p_d[i_ * 128:(i_ + 1) * 128, :]), writes=[bPt])
        P.op("dve", lambda e: e.tensor_copy(out=pbf, in_=ptile), reads=[bPt], writes=[bPbf])
        tq = psb[6].bitcast(BF16)
        for c_ in range(2):
            P.op("pe", lambda e, c_=c_: e.transpose(tq[:, c_ * 128:(c_ + 1) * 128], pbf[:, c_ * 128:(c_ + 1) * 128], identb), reads=[bPbf, bConst], writes=[bPE[6]])
        P.op("act", lambda e: e.activation(out=pTs, in_=tq[:, 0:256].rearrange("p (c t) -> p c t", t=128), func=AF.Copy), reads=[bPE[6]], writes=[bPTs])
        for half in range(2):
            for c_ in range(2):
                P.op("pe", lambda e, c_=c_, half=half: e.matmul(psb[half], lhsT=pTs[:, c_, :], rhs=ppb[:, c_, half * 512:(half + 1) * 512], start=(c_ == 0), stop=(c_ == 1)),
                     reads=[bPTs, bPp], writes=[bPE[half]])
            P.op("act", lambda e, half=half: e.activation(out=pe32[:, half * 512:(half + 1) * 512], in_=psb[half], func=AF.Copy), reads=[bPE[half]], writes=[bPe32], accum=(half == 1))
        rp = rms2(i_, pe32, bPe32, D)
        P.op("dve", lambda e: e.scalar_tensor_tensor(out=pe32, in0=pe32, scalar=rp, in1=gainb[:], op0=ALU.mult, op1=ALU.mult), reads=[bPe32, bSt[i_], bGain], writes=[bPe32])
        P.op("dve", lambda e: e.tensor_copy(out=hbf, in_=hres[:, i_, :]), reads=[bH[i_]], writes=[bHbf])
        tq2 = psb[7].bitcast(BF16)
        for c_ in range(8):
            P.op("pe", lambda e, c_=c_: e.transpose(tq2[:, c_ * 128:(c_ + 1) * 128], hbf[:, c_ * 128:(c_ + 1) * 128], identb), reads=[bHbf, bConst], writes=[bPE[7]])
        P.op("act", lambda e: e.activation(out=hT, in_=tq2.rearrange("p (c t) -> p c t", t=128), func=AF.Copy), reads=[bPE[7]], writes=[bHT])
        for half in range(2):
            for c_ in range(8):
                P.op("pe", lambda e, c_=c_, half=half: e.matmul(psb[2 + half], lhsT=hT[:, c_, :], rhs=pgb[:, c_, half * 512:(half + 1) * 512], start=(c_ == 0), stop=(c_ == 7)),
                     reads=[bHT, bPg], writes=[bPE[2 + half]])
            P.op("act", lambda e, half=half: e.activation(out=gate[:, half * 512:(half + 1) * 512], in_=psb[2 + half], func=AF.Sigmoid), reads=[bPE[2 + half]], writes=[bGate], accum=(half == 1))
        P.op("dve", lambda e: e.tensor_tensor(out=gate, in0=gate, in1=pe32, op=ALU.mult), reads=[bGate, bPe32], writes=[bGate])
        P.op("dve", lambda e: e.tensor_tensor(out=hres[:, i_, :], in0=hres[:, i_, :], in1=gate, op=ALU.add), reads=[bH[i_], bGate], writes=[bH[i_]])
        c0 = i_ * 8 + 4
        P.op("act", lambda e: e.activation(out=junk2, in_=hres[:, i_, :], func=AF.Square, accum_out=stats[:, c0:c0 + 1]), reads=[bH[i_]], writes=[bJ2, bSt[i_]])
        P.op("dve", lambda e: e.tensor_scalar(out=stats[:, c0 + 1:c0 + 2], in0=stats[:, c0:c0 + 1], scalar1=1.0 / D, scalar2=EPS, op0=ALU.mult, op1=ALU.add), reads=[bSt[i_]], writes=[bSt[i_]])
        P.op("pool", lambda e: e.tensor_tensor(out=stats[:, c0 + 2:c0 + 3], in0=stats[:, c0 + 1:c0 + 2], in1=cst[:, 0:1], op=ALU.pow), reads=[bSt[i_], bCst], writes=[bSt[i_]])
        P.op("dve", lambda e: e.scalar_tensor_tensor(out=otile[k_], in0=hres[:, i_, :], scalar=stats[:, c0 + 2:c0 + 3], in1=gfin[:], op0=ALU.mult, op1=ALU.mult), reads=[bH[i_], bSt[i_], bGf], writes=[bOt[k_]])
        P.dma("sp", lambda e: e.dma_start(out=out_d[i_ * 128:(i_ + 1) * 128, :], in_=otile[k_]), reads=[bOt[k_]], writes=[bOut], sembuf=bOt[k_])

    for i_ in range(NT):
        phaseH(i_)
    P.wait_all("sp", [bOut] + bOt)
    P.run(st)
    st.close()
    return nc


def finish(nc, P, st, dbg):
    P.barrier()
    outs = []
    for name, (ap, shape, dt) in dbg.items():
        d = nc.dram_tensor("dbg_" + name, shape, dt, kind="ExternalOutput").ap()
        b = Buf(P, "dbg_" + name)
        P.dma("sp", lambda e, d=d, ap=ap: e.dma_start(out=d, in_=ap), writes=[b])
        outs.append(b)
    P.wait_all("sp", outs)
    P.run(st)
    st.close()
    return nc


def _t5_bucket(rel):
    n = np.maximum(rel, 0)
    nf = np.maximum(n, 1).astype(np.float32)
    large = 16 + (np.log(nf / np.float32(16)) / np.float32(math.log(128 / 16)) * np.float32(16)).astype(np.int32)
    large = np.minimum(large, 31)
    return np.where(n < 16, n, large)


def prep_shared(inp):
    f32 = np.float32
    g = lambda k: np.asarray(inp[k], dtype=f32)
    w_in = g("w_in")[0]
    cols = []
    for h in range(4):
        cols += list(range(h * 64, h * 64 + 64)) + list(range(256 + h * 64, 256 + h * 64 + 64))
    for h in range(4):
        cols += list(range(512 + h * 64, 512 + h * 64 + 64)) + list(range(768 + h * 64, 768 + h * 64 + 64))
    cols += list(range(1024, 3072))
    w_in_p = w_in[:, cols]
    chunked = lambda w: np.ascontiguousarray(w.reshape(8, 128, -1).transpose(1, 0, 2))
    sh = {}
    sh["w_in"] = chunked(w_in_p)
    sh["w_out"] = chunked(g("w_out")[0])
    sh["gains"] = np.stack([g("attn_norm")[0], g("moe_norm")[0], g("ple_norm")[0], g("final_norm")], 0)
    sh["lamv"] = np.stack([g("lambda_q1")[0], g("lambda_k1")[0], g("lambda_q2")[0], g("lambda_k2")[0]], 0)
    sh["subln"] = g("subln")
    rb = g("rel_bias")
    k = np.arange(128)[:, None]
    q = np.arange(128)[None, :]
    bn = np.zeros((128, 4, 2, 128), f32)
    for d in range(2):
        bk = _t5_bucket(q + 128 * d - k)
        bn[:, :, d, :] = rb[bk].transpose(0, 2, 1)
    sh["bnear"] = bn.reshape(128, -1)
    sh["c31"] = np.ascontiguousarray(np.broadcast_to(rb[31][None, :], (128, 4)))
    sh["rw"] = chunked(g("router_w")[0]).reshape(128, -1)
    sh["rb"] = g("router_b")
    wgu = g("w_gate_up")[0]
    glu = wgu[:, :, 0::2].reshape(NE, 8, 128, 8, 128)
    lin = wgu[:, :, 1::2].reshape(NE, 8, 128, 8, 128)
    gl = np.concatenate([glu, lin], axis=-1)
    sh["wgu"] = np.ascontiguousarray(gl.transpose(0, 3, 2, 1, 4))
    bgu = g("b_gate_up")[0]
    bg = bgu[:, 0::2].reshape(NE, 8, 128)
    bl = bgu[:, 1::2].reshape(NE, 8, 128)
    sh["bgl"] = np.ascontiguousarray(np.stack([bg, bl], -1).transpose(2, 0, 1, 3)).reshape(128, -1)
    sh["wd"] = np.ascontiguousarray(g("w_down")[0].reshape(NE, 8, 128, D))
    sh["bd"] = g("b_down")[0]
    sh["pproj"] = np.ascontiguousarray(g("ple_proj")[0].reshape(2, 128, D).transpose(1, 0, 2))
    sh["pgate"] = chunked(g("ple_gate")[0])
    ident = np.eye(128, dtype=f32)
    jj = np.arange(128)[:, None]
    kk = np.arange(128)[None, :]
    negtri = -(jj >= kk).astype(f32)
    lt = (jj < kk).astype(f32)
    sh["cbf"] = np.concatenate([ident, negtri, -np.ones((128, 128), f32), lt, np.ones((128, 128), f32)], 1).astype(ml_dtypes.bfloat16)
    sh["cf32"] = np.concatenate([ident, (kk >= jj).astype(f32), (kk > jj).astype(f32)], 1)
    return sh


_CACHE = {}


def kernel(**inputs):
    sh = prep_shared(inputs)
    x = np.asarray(inputs["x"], dtype=np.float32)
    p = np.asarray(inputs["p"], dtype=np.float32)[0]
    if "nc" not in _CACHE:
        _CACHE["nc"] = build()
    nc = _CACHE["nc"]
    in_maps = []
    for c in range(8):
        m = dict(sh)
        m["x"] = np.ascontiguousarray(x[c])
        m["p"] = np.ascontiguousarray(p[c])
        in_maps.append(m)
    res = run_bass_kernel_spmd(nc, in_maps, core_ids=list(range(8)))
    return np.stack([np.asarray(r["out"], dtype=np.float32) for r in res.results], 0)
```

```python
from contextlib import ExitStack
import math
import numpy as np
import ml_dtypes
import concourse.bass as bass
import concourse.mybir as mybir
from concourse.bass_utils import run_bass_kernel_spmd

F32 = mybir.dt.float32
BF16 = mybir.dt.bfloat16
U32 = mybir.dt.uint32
I32 = mybir.dt.int32
AF = mybir.ActivationFunctionType
ALU = mybir.AluOpType
AX = mybir.AxisListType

S = 2048
D = 1024
NT = 16
NE = 32
RC = 384
NCH = 6
CAP = 2048
EPS = 1e-6
ENG = {"pe": "tensor", "act": "scalar", "dve": "vector", "pool": "gpsimd", "sp": "sync"}


class Buf:
    __slots__ = ("name", "w", "r", "sem", "cnt")

    def __init__(self, P, name):
        self.name = name
        self.w = []
        self.r = []
        self.sem = None
        self.cnt = 0
        P.bufs.append(self)


class Prog:
    def __init__(self, nc):
        self.nc = nc
        self.recs = {e: [] for e in ENG}
        self.waited = {e: {} for e in ENG}
        self.dma_sems = []
        self.bufs = []

    def _filter(self, eng, deps):
        out = []
        for t in deps:
            if t[0] == "c":
                _, e2, idx = t
                if e2 == eng and eng in ("pe", "sp"):
                    continue
                k = ("c", e2)
                if self.waited[eng].get(k, -1) >= idx:
                    continue
                self.waited[eng][k] = idx
                self.recs[e2][idx]["sig"] = True
                out.append(t)
            else:
                _, b, val = t
                k = ("d", id(b))
                if self.waited[eng].get(k, -1) >= val:
                    continue
                self.waited[eng][k] = val
                out.append(t)
        return out

    def _deps(self, eng, reads, writes):
        deps = []
        for b in reads:
            deps += b.w
        for b in writes:
            deps += b.w
            deps += b.r
        return self._filter(eng, deps)

    def op(self, eng, fn, reads=(), writes=(), accum=False):
        waits = self._deps(eng, reads, writes)
        idx = len(self.recs[eng])
        self.recs[eng].append(dict(waits=waits, fn=fn, sig=False, dma=None))
        tok = ("c", eng, idx)
        for b in reads:
            b.r.append(tok)
        for b in writes:
            if accum:
                b.w.append(tok)
            else:
                b.w = [tok]
                b.r = []
        return tok

    def dma(self, eng, fn, reads=(), writes=(), sembuf=None):
        waits = self._deps(eng, reads, writes)
        sb = sembuf if sembuf is not None else (writes[0] if writes else reads[0])
        if sb.sem is None:
            sb.sem = True
            self.dma_sems.append(sb)
        sb.cnt += 16
        tok = ("d", sb, sb.cnt)
        self.recs[eng].append(dict(waits=waits, fn=fn, sig=False, dma=sb, dmaval=sb.cnt))
        for b in reads:
            b.r.append(tok)
        for b in writes:
            b.w = [tok]
            b.r = []
        return tok

    def barrier(self):
        toks = []
        for e in ENG:
            for i in range(len(self.recs[e]) - 1, -1, -1):
                r = self.recs[e][i]
                if r["fn"] is not None and r["dma"] is None:
                    toks.append(("c", e, i))
                    break
        for b in self.bufs:
            toks += [t for t in b.w + b.r if t[0] == "d"]
            b.w = []
            b.r = []
        for e in ENG:
            w = self._filter(e, [t for t in toks if not (t[0] == "c" and t[1] == e)])
            self.recs[e].append(dict(waits=w, fn=None, sig=False, dma=None))

    def guard_begin(self, key):
        self._saved_waited = {e: dict(self.waited[e]) for e in ENG}
        for e in ENG:
            self.recs[e].append(dict(waits=[], fn=None, sig=False, dma=None, gb=key))

    def guard_end(self):
        for e in ENG:
            self.recs[e].append(dict(waits=[], fn=None, sig=False, dma=None, ge=True))
        self.waited = self._saved_waited

    def wait_all(self, eng, bufs):
        waits = self._deps(eng, list(bufs), list(bufs))
        self.recs[eng].append(dict(waits=waits, fn=None, sig=False, dma=None))

    def run(self, stack):
        nc = self.nc
        esem = {e: stack.enter_context(nc.semaphore("s_" + e)) for e in ENG}
        for i, b in enumerate(self.dma_sems):
            b.sem = stack.enter_context(nc.semaphore("d%d" % i))
        cnts = {}
        for e in ENG:
            c = 0
            for i, r in enumerate(self.recs[e]):
                if r["sig"]:
                    c += 1
                    cnts[(e, i)] = c
        block = stack.enter_context(nc.Block())

        def body(e):
            def emit(engine, r):
                for t in r["waits"]:
                    if t[0] == "c":
                        engine.wait_ge(esem[t[1]], cnts[(t[1], t[2])])
                    else:
                        engine.wait_ge(t[1].sem, t[2])
                if r["fn"] is None:
                    return
                ins = r["fn"](engine)
                if r["dma"] is not None:
                    ins.then_inc(r["dma"].sem, 16)
                elif r["sig"]:
                    ins.then_inc(esem[e], 1)

            def f(engine):
                recs = self.recs[e]
                reg = rthr = None
                if any("gb" in q for q in recs):
                    reg = stack.enter_context(engine.register("rc_" + e))
                    rthr = stack.enter_context(engine.register("rt_" + e))
                i = 0
                csum = 0
                while i < len(recs):
                    r = recs[i]
                    if "gb" in r:
                        j = i + 1
                        while "ge" not in recs[j]:
                            j += 1
                        inner = recs[i + 1:j]
                        ex_, thr = r["gb"]
                        if any(q["fn"] is not None or q["waits"] for q in inner):
                            engine.reg_load(reg, self.cnt_ap(ex_))
                            engine.reg_mov(rthr, thr)
                            with engine.If_lt(rthr, reg):
                                for q in inner:
                                    emit(engine, q)
                            nsig = sum(1 for q in inner if q["sig"])
                            dmas = [q for q in inner if q["dma"] is not None]
                            if nsig or dmas:
                                with engine.Else():
                                    if nsig:
                                        if csum > 0:
                                            engine.wait_ge(esem[e], csum)
                                        engine.sem_inc(esem[e], nsig)
                                    for q in dmas:
                                        if q["dmaval"] - 16 > 0:
                                            engine.wait_ge(q["dma"].sem, q["dmaval"] - 16)
                                        engine.sem_inc(q["dma"].sem, 16)
                            csum += nsig
                        i = j + 1
                        continue
                    emit(engine, r)
                    if r["sig"]:
                        csum += 1
                    i += 1
            return f

        block.tensor(body("pe"))
        block.scalar(body("act"))
        block.vector(body("dve"))
        block.gpsimd(body("pool"))
        block.sync(body("sp"))


def build(stop=None):
    nc = bass.Bass("TRN2", target_bir_lowering=False, dynamic_dma_scratch_size=8192)
    dram = lambda n, s, d=F32, k="ExternalInput": nc.dram_tensor(n, s, d, kind=k).ap()
    x_d = dram("x", [S, D])
    p_d = dram("p", [S, 256])
    win_d = dram("w_in", [128, 8, 3072])
    wout_d = dram("w_out", [128, 8, D])
    gains_d = dram("gains", [4, D])
    lamv_d = dram("lamv", [4, 64])
    subln_d = dram("subln", [1, 128])
    bnear_d = dram("bnear", [128, 4 * 2 * 128])
    c31_d = dram("c31", [128, 4])
    rw_d = dram("rw", [128, 8 * 32])
    rb_d = dram("rb", [1, 32])
    wgu_d = dram("wgu", [NE, 8, 128, 8, 256])
    bgl_d = dram("bgl", [128, NE * 8 * 2])
    wd_d = dram("wd", [NE, 128, 8, D])
    bd_d = dram("bd", [NE, D])
    pproj_d = dram("pproj", [128, 2, D])
    pgate_d = dram("pgate", [128, 8, D])
    cb_d = dram("cbf", [128, 5 * 128], BF16)
    cf_d = dram("cf32", [128, 3 * 128])
    out_d = dram("out", [S, D], F32, "ExternalOutput")
    h1_d = dram("h1s", [S, D], F32, "Internal")
    xg_d = dram("xg", [NE * CAP, D], BF16, "Internal")
    yg_d = dram("yg", [NE * CAP, D], F32, "Internal")
    dbg = {}

    st = ExitStack()
    P = Prog(nc)
    B = lambda n: Buf(P, n)
    sbt = lambda n, s, d: st.enter_context(nc.sbuf_tensor("sb_" + n, s, d))
    pall = st.enter_context(nc.psum_tensor("pall", [128, 4096], F32))
    psb = [pall[:, i * 512:(i + 1) * 512] for i in range(8)]

    cb = sbt("cb", [128, 5 * 128], BF16)
    cf = sbt("cf", [128, 3 * 128], F32)
    identb, negtri, negones, ltm, onesb = [cb[:, i * 128:(i + 1) * 128] for i in range(5)]
    identf, mask0, strictm = [cf[:, i * 128:(i + 1) * 128] for i in range(3)]
    gainb = sbt("gainb", [128, D], F32)
    gain2 = sbt("gain2", [128, D], F32)
    ebfix = sbt("ebfix", [128, 4 * 2 * 128], F32)
    c31 = sbt("c31", [128, 4], F32)
    lamt = sbt("lamt", [128, 4 * 64], F32)
    lams = sbt("lams", [128, 8], F32)
    subg = sbt("subg", [128, 128], F32)
    rw = sbt("rw", [128, 8 * 32], F32)
    rbb = sbt("rbb", [128, 32], F32)
    bgl = sbt("bgl", [128, NE * 8 * 2], F32)
    cst = sbt("cst", [128, 8], F32)
    stats = sbt("stats", [128, NT * 8], F32)
    meta_dest = sbt("meta_dest", [128, NT * 4], I32)
    meta_g = sbt("meta_g", [128, NT * 4], F32)
    bConst = B("const")
    bGain = B("gain")
    bGain2 = B("gain2")

    AR = sbt("arena", [128, 152 * 1024], mybir.dt.uint8)
    K = 1024
    OFF_HN, OFF_QK, OFF_AT, OFF_W, OFF_V, OFF_T = 0, 32 * K, 64 * K, 96 * K, 128 * K, 145 * K

    def view(off, shape, dt):
        nb = {F32: 4, BF16: 2, I32: 4, U32: 4}[dt]
        n = 1
        for s_ in shape[1:]:
            n *= s_
        ap = AR[:, off:off + n * nb].bitcast(dt)
        if len(shape) == 3:
            ap = ap.rearrange("p (a b) -> p a b", b=shape[2])
        elif len(shape) == 4:
            ap = ap.rearrange("p (a b c) -> p a b c", b=shape[2], c=shape[3])
        return ap

    P.dma("sp", lambda e: e.dma_start(out=cb[:], in_=cb_d), writes=[bConst])
    bC2 = B("c2"); bC3 = B("c3"); bC4 = B("c4"); bC5 = B("c5"); bC6 = B("c6"); bC7 = B("c7"); bC8 = B("c8"); bC9 = B("c9")
    P.dma("sp", lambda e: e.dma_start(out=cf[:], in_=cf_d), writes=[bC2])
    P.dma("sp", lambda e: e.dma_start(out=ebfix[:], in_=bnear_d), writes=[bC3])
    P.dma("sp", lambda e: e.dma_start(out=c31[:], in_=c31_d), writes=[bC4])
    P.dma("sp", lambda e: e.dma_start(out=lamt[:], in_=lamv_d.rearrange("a b -> (a b)").partition_broadcast(128)), writes=[bC5])
    P.dma("sp", lambda e: e.dma_start(out=subg[:], in_=subln_d.rearrange("a b -> (a b)").partition_broadcast(128)), writes=[bC6])
    P.dma("sp", lambda e: e.dma_start(out=rw[:], in_=rw_d), writes=[bC7])
    P.dma("sp", lambda e: e.dma_start(out=rbb[:], in_=rb_d.rearrange("a b -> (a b)").partition_broadcast(128)), writes=[bC8])
    P.dma("sp", lambda e: e.dma_start(out=bgl[:], in_=bgl_d), writes=[bC9])
    P.dma("sp", lambda e: e.dma_start(out=gainb[:], in_=gains_d[0].partition_broadcast(128)), writes=[bGain])
    bCst = B("cst")
    P.op("dve", lambda e: e.memset(cst[:, 0:1], -0.5), writes=[bCst])
    for h in range(4):
        P.op("dve", lambda e, h=h: e.tensor_scalar(out=ebfix[:, h * 256:(h + 1) * 256], in0=ebfix[:, h * 256:(h + 1) * 256],
                                                   scalar1=c31[:, h:h + 1], scalar2=None, op0=ALU.subtract),
             reads=[bC3, bC4], writes=[bC3])
    P.op("act", lambda e: e.activation(out=ebfix[:], in_=ebfix[:], func=AF.Exp), reads=[bC3], writes=[bC3])
    for h in range(4):
        P.op("dve", lambda e, h=h: e.tensor_tensor(out=ebfix[:, h * 256:h * 256 + 128], in0=ebfix[:, h * 256:h * 256 + 128], in1=mask0, op=ALU.mult),
             reads=[bC3, bC2], writes=[bC3])
    P.op("dve", lambda e: e.tensor_tensor(out=lamt[:, 0:64], in0=lamt[:, 0:64], in1=lamt[:, 64:128], op=ALU.mult), reads=[bC5], writes=[bC5])
    P.op("dve", lambda e: e.tensor_tensor(out=lamt[:, 128:192], in0=lamt[:, 128:192], in1=lamt[:, 192:256], op=ALU.mult), reads=[bC5], writes=[bC5])
    P.op("dve", lambda e: e.tensor_reduce(out=lams[:, 0:1], in_=lamt[:, 0:64], axis=AX.X, op=ALU.add), reads=[bC5], writes=[bC5])
    P.op("dve", lambda e: e.tensor_reduce(out=lams[:, 1:2], in_=lamt[:, 128:192], axis=AX.X, op=ALU.add), reads=[bC5], writes=[bC5])
    P.op("act", lambda e: e.activation(out=lams[:, 2:4], in_=lams[:, 0:2], func=AF.Exp), reads=[bC5], writes=[bC5])
    P.op("dve", lambda e: e.tensor_tensor(out=lams[:, 4:5], in0=lams[:, 3:4], in1=lams[:, 2:3], op=ALU.subtract), reads=[bC5], writes=[bC5])
    P.op("dve", lambda e: e.tensor_scalar(out=lams[:, 4:5], in0=lams[:, 4:5], scalar1=-0.2, scalar2=None, op0=ALU.add), reads=[bC5], writes=[bC5])
    P.op("dve", lambda e: e.tensor_scalar(out=subg[:], in0=subg[:], scalar1=0.8, scalar2=None, op0=ALU.mult), reads=[bC6], writes=[bC6])
    bglv = bgl[:].rearrange("p (a t) -> p a t", t=2)
    P.op("dve", lambda e: e.tensor_scalar(out=bglv[:, :, 1:2], in0=bglv[:, :, 1:2], scalar1=1.0, scalar2=None, op0=ALU.add), reads=[bC9], writes=[bC9])
    neglam = lams[:, 4:5]

    hnT = view(OFF_HN, [128, 8, S], BF16)
    bHn = [B("hn%d" % i) for i in range(NT)]
    xt = [view(OFF_W + i * 4096, [128, D], F32) for i in range(2)]
    xs = [view(OFF_W + 8192 + i * 2048, [128, D], BF16) for i in range(2)]
    junkb = view(OFF_T, [128, D], BF16)
    bXt = [B("xt%d" % i) for i in range(2)]
    bXs = [B("xs%d" % i) for i in range(2)]
    bJ = B("junk")
    bSt = [B("st%d" % i) for i in range(NT)]
    bPs = [B("ps%d" % i) for i in range(8)]

    def rms_stats(i, src, srcbuf, n):
        c0 = i * 8
        P.op("act", lambda e: e.activation(out=junkb[:, 0:n], in_=src, func=AF.Square, accum_out=stats[:, c0:c0 + 1]),
             reads=[srcbuf], writes=[bJ, bSt[i]])
        P.op("dve", lambda e: e.tensor_scalar(out=stats[:, c0 + 1:c0 + 2], in0=stats[:, c0:c0 + 1], scalar1=1.0 / n, scalar2=EPS, op0=ALU.mult, op1=ALU.add),
             reads=[bSt[i]], writes=[bSt[i]])
        P.op("pool", lambda e: e.tensor_tensor(out=stats[:, c0 + 3:c0 + 4], in0=stats[:, c0 + 1:c0 + 2], in1=cst[:, 0:1], op=ALU.pow),
             reads=[bSt[i], bCst], writes=[bSt[i]])
        return stats[:, c0 + 3:c0 + 4]

    for i in range(NT):
        b = i % 2
        P.dma("sp", lambda e, i=i, b=b: e.dma_start(out=xt[b], in_=x_d[i * 128:(i + 1) * 128, :]), writes=[bXt[b]])
        rstd = rms_stats(i, xt[b], bXt[b], D)
        P.op("dve", lambda e, b=b, rstd=rstd: e.scalar_tensor_tensor(out=xs[b], in0=xt[b], scalar=rstd, in1=gainb[:], op0=ALU.mult, op1=ALU.mult),
             reads=[bXt[b], bSt[i], bGain], writes=[bXs[b]])
        pT = psb[b].bitcast(BF16)
        for c in range(8):
            P.op("pe", lambda e, c=c, b=b, pT=pT: e.transpose(pT[:, c * 128:(c + 1) * 128], xs[b][:, c * 128:(c + 1) * 128], identb),
                 reads=[bXs[b], bConst], writes=[bPs[b]])
        P.op("act", lambda e, i=i, pT=pT: e.activation(out=hnT[:, :, i * 128:(i + 1) * 128], in_=pT.rearrange("p (c t) -> p c t", t=128), func=AF.Copy),
             reads=[bPs[b]], writes=[bHn[i]])

    if stop == "A":
        dbg["hnT"] = (hnT, [128, 8, S], BF16)
        return finish(nc, P, st, dbg)

    QK = view(OFF_QK, [128, 8, S], BF16)
    VV = view(OFF_V, [128, NT, 516], BF16)
    wsl = [view(OFF_W + i * 4096, [128, 8, 256], BF16) for i in range(3)]
    bW = [B("wsl%d" % i) for i in range(3)]
    bQK = [B("qk%d" % i) for i in range(8)]
    bV = [B("v%d" % i) for i in range(NT)]
    slab_ctr = [0]
    evac_ctr = [0]

    def project(col0, kind):
        P.barrier()
        if kind == "diff":
            VD4 = VV.rearrange("p t (h c) -> p t h c", c=129)
            P.op("dve", lambda e: e.memset(VD4[:, :, :, 128:129], 1.0), writes=bV)
        for s in range(6):
            wi = slab_ctr[0] % 3
            slab_ctr[0] += 1
            c_lo = col0 + s * 256
            P.dma("pool", lambda e, wi=wi, c_lo=c_lo: e.dma_start(out=wsl[wi], in_=win_d[:, :, c_lo:c_lo + 256]), writes=[bW[wi]])
            if s < 4:
                for gg in range(2):
                    gi = s * 2 + gg
                    for tc in range(4):
                        bk = evac_ctr[0] % 4
                        evac_ctr[0] += 1
                        for c in range(8):
                            P.op("pe", lambda e, wi=wi, gg=gg, tc=tc, c=c, bk=bk: e.matmul(
                                psb[bk], lhsT=wsl[wi][:, c, gg * 128:(gg + 1) * 128], rhs=hnT[:, c, tc * 512:(tc + 1) * 512],
                                start=(c == 0), stop=(c == 7)),
                                reads=[bW[wi]] + bHn[tc * 4:tc * 4 + 4], writes=[bPs[bk]])
                        sc = 0.125 if s < 2 else 1.0
                        if evac_ctr[0] % 2 == 0:
                            P.op("act", lambda e, gi=gi, tc=tc, bk=bk, sc=sc: e.activation(out=QK[:, gi, tc * 512:(tc + 1) * 512], in_=psb[bk], func=AF.Copy, scale=sc),
                                 reads=[bPs[bk]], writes=[bQK[gi]], accum=True)
                        else:
                            P.op("dve", lambda e, gi=gi, tc=tc, bk=bk, sc=sc: e.tensor_scalar(out=QK[:, gi, tc * 512:(tc + 1) * 512], in0=psb[bk], scalar1=sc, scalar2=None, op0=ALU.mult),
                                 reads=[bPs[bk]], writes=[bQK[gi]], accum=True)
            else:
                vs = s - 4
                for i in range(NT):
                    bk = evac_ctr[0] % 4
                    evac_ctr[0] += 1
                    for c in range(8):
                        P.op("pe", lambda e, wi=wi, i=i, c=c, bk=bk: e.matmul(
                            psb[bk][:, 0:256], lhsT=hnT[:, c, i * 128:(i + 1) * 128], rhs=wsl[wi][:, c, :], start=(c == 0), stop=(c == 7)),
                            reads=[bW[wi], bHn[i]], writes=[bPs[bk]])
                    if kind == "diff":
                        dst = VV[:, i, vs * 258:(vs + 1) * 258].rearrange("p (h c) -> p h c", c=129)[:, :, 0:128]
                        src = psb[bk][:, 0:256].rearrange("p (h c) -> p h c", c=128)
                    else:
                        dst = VV[:, i, vs * 256:(vs + 1) * 256]
                        src = psb[bk][:, 0:256]
                    if evac_ctr[0] % 2 == 0:
                        P.op("act", lambda e, dst=dst, src=src: e.activation(out=dst, in_=src, func=AF.Copy), reads=[bPs[bk]], writes=[bV[i]], accum=True)
                    else:
                        P.op("dve", lambda e, dst=dst, src=src: e.tensor_copy(out=dst, in_=src), reads=[bPs[bk]], writes=[bV[i]], accum=True)

    project(0, "diff")
    if stop == "B":
        dbg["QK"] = (QK, [128, 8, S], BF16)
        dbg["VV"] = (VV, [128, NT, 516], BF16)
        return finish(nc, P, st, dbg)

    P.barrier()
    attnT = view(OFF_AT, [128, 8, S], BF16)
    bAT = [B("at%d" % i) for i in range(NT)]
    NSLOT = 32
    Er = [view(OFF_W + i * 1024, [128, 2, 256], BF16) for i in range(NSLOT)]
    bE = [B("E%d" % i) for i in range(NSLOT)]
    def tv(par, k):
        base = OFF_T + par * 1296
        if k == 0:
            return view(base, [128, 130], F32)
        if k == 1:
            return view(base + 520, [128, 130], F32)
        return view(base + 1040, [128, 128], BF16)
    bEp = [B("ep%d" % i) for i in range(4)]
    late = []
    bSS = [B("S%d" % i) for i in range(2)]
    bO = [B("O%d" % m) for m in range(2)]
    bTp = [B("tp%d" % i) for i in range(2)]
    VD4 = VV.rearrange("p t (h c) -> p t h c", c=129)
    ebv = ebfix[:].rearrange("p (h d q) -> p h d q", h=4, d=2)

    units = [(h, c) for h in range(4) for c in range(8)]
    import os
    if stop == 'C1':
        units = units[:1]
    if os.environ.get('KLIM'):
        units = units[:int(os.environ['KLIM'])]
    blk_ctr = [0]
    ep_ctr = [0]

    def av_items(h, c, slots):
        items = []
        for j in range(2):
            for m in range(2):
                kbs = list(range(0, 2 * c + j + 1))
                for kb in kbs:
                    items.append((j, m, kb, kb == 0, kb == kbs[-1]))
        return items

    def emit_av(h, c, slots, it):
        j, m, kb, first, last = it
        ob = psb[4 + m][:, 0:129]
        sl = slots[kb]
        P.op("pe", lambda e: e.matmul(ob, lhsT=Er[sl][:, m, j * 128:(j + 1) * 128], rhs=VD4[:, kb, h, :], start=first, stop=last),
             reads=[bE[sl], bV[kb]], writes=[bO[m]])
        if last:
            par = ep_ctr[0] % 4
            P.op("dve", lambda e: e.tensor_copy(out=tv(par, m)[:, 0:129], in_=ob), reads=[bO[m]], writes=[bEp[par]], accum=(m == 1))
            if m == 1:
                epilogue(h, c, j)

    def epilogue(h, c, j):
        qb = 2 * c + j
        par = ep_ctr[0] % 4
        ep_ctr[0] += 1
        si = qb
        c0 = si * 8
        o1 = tv(par, 0); o2 = tv(par, 1); obf = tv(par, 2)
        ep = [bEp[par]]
        P.op("dve", lambda e: e.reciprocal(out=stats[:, c0:c0 + 1], in_=o1[:, 128:129]), reads=ep, writes=[bSt[si]])
        P.op("dve", lambda e: e.reciprocal(out=stats[:, c0 + 1:c0 + 2], in_=o2[:, 128:129]), reads=ep, writes=[bSt[si]])
        P.op("dve", lambda e: e.tensor_scalar(out=stats[:, c0 + 2:c0 + 3], in0=stats[:, c0 + 1:c0 + 2], scalar1=neglam, scalar2=None, op0=ALU.mult),
             reads=[bSt[si], bC5], writes=[bSt[si]])
        P.op("dve", lambda e: e.tensor_scalar(out=o2[:, 0:128], in0=o2[:, 0:128], scalar1=stats[:, c0 + 2:c0 + 3], scalar2=None, op0=ALU.mult),
             reads=ep + [bSt[si]], writes=ep)
        P.op("dve", lambda e: e.scalar_tensor_tensor(out=o1[:, 0:128], in0=o1[:, 0:128], scalar=stats[:, c0:c0 + 1], in1=o2[:, 0:128], op0=ALU.mult, op1=ALU.add),
             reads=ep + [bSt[si]], writes=ep)
        P.op("dve", lambda e: e.tensor_tensor(out=o2[:, 0:128], in0=o1[:, 0:128], in1=o1[:, 0:128], op=ALU.mult), reads=ep, writes=ep)
        P.op("dve", lambda e: e.tensor_reduce(out=stats[:, c0 + 3:c0 + 4], in_=o2[:, 0:128], axis=AX.X, op=ALU.add), reads=ep, writes=[bSt[si]])
        P.op("dve", lambda e: e.tensor_scalar(out=stats[:, c0 + 4:c0 + 5], in0=stats[:, c0 + 3:c0 + 4], scalar1=1.0 / 128, scalar2=EPS, op0=ALU.mult, op1=ALU.add),
             reads=[bSt[si]], writes=[bSt[si]])
        P.op("pool", lambda e: e.tensor_tensor(out=stats[:, c0 + 5:c0 + 6], in0=stats[:, c0 + 4:c0 + 5], in1=cst[:, 0:1], op=ALU.pow),
             reads=[bSt[si], bCst], writes=[bSt[si]])
        P.op("dve", lambda e: e.scalar_tensor_tensor(out=obf, in0=o1[:, 0:128], scalar=stats[:, c0 + 5:c0 + 6], in1=subg[:], op0=ALU.mult, op1=ALU.mult),
             reads=ep + [bSt[si], bC6], writes=ep)
        tb = psb[6 + par % 2].bitcast(BF16)

        def fin():
            P.op("pe", lambda e: e.transpose(tb[:, 0:128], obf, identb), reads=[bEp[par], bConst], writes=[bTp[par % 2]])
            P.op("dve", lambda e: e.tensor_copy(out=attnT[:, h, qb * 128:(qb + 1) * 128], in_=tb[:, 0:128]), reads=[bTp[par % 2]], writes=[bAT[qb]], accum=True)
        late.append(fin)

    pending = []
    for ui, (h, c) in enumerate(units):
        nb = 2 * c + 2
        slots = {}
        per = 0
        late_now = list(late)
        del late[:]
        for kb in range(nb):
            if kb == 1:
                for f_ in late_now:
                    f_()
            sl = blk_ctr[0] % NSLOT
            blk_ctr[0] += 1
            slots[kb] = sl
            sp_ = kb % 2
            lo = 128 if kb == 2 * c + 1 else 0
            sb3 = pall[:, sp_ * 512:sp_ * 512 + 2048].rearrange("p (m r) -> p m r", m=2)
            for m in range(2):
                P.op("pe", lambda e, m=m, kb=kb, lo=lo, sp_=sp_, h=h, c=c: e.matmul(
                    psb[2 * m + sp_][:, lo:256], lhsT=QK[m * 64:(m + 1) * 64, 4 + h, kb * 128:(kb + 1) * 128],
                    rhs=QK[m * 64:(m + 1) * 64, h, c * 256 + lo:(c + 1) * 256], start=True, stop=True),
                    reads=[bQK[4 + h], bQK[h]], writes=[bSS[sp_]])
            P.op("act", lambda e, sl=sl, lo=lo, sb3=sb3: e.activation(out=Er[sl][:, :, lo:256], in_=sb3[:, :, lo:256], func=AF.Exp),
                 reads=[bSS[sp_]], writes=[bE[sl]])
            for j in range(2):
                d = 2 * c + j - kb
                if 0 <= d <= 1:
                    for m in range(2):
                        P.op("dve", lambda e, sl=sl, m=m, j=j, d=d, h=h: e.tensor_tensor(
                            out=Er[sl][:, m, j * 128:(j + 1) * 128], in0=Er[sl][:, m, j * 128:(j + 1) * 128], in1=ebv[:, h, d, :], op=ALU.mult),
                            reads=[bE[sl], bC3], writes=[bE[sl]])
            for _ in range(per):
                if pending:
                    emit_av(*pending.pop(0))
        pending = [(h, c, slots, it) for it in av_items(h, c, slots)]
        while pending:
            emit_av(*pending.pop(0))
        for f_ in late:
            f_()
        del late[:]
    while pending:
        emit_av(*pending.pop(0))
    for f_ in late:
        f_()

    if stop == "C1":
        dbg["E0"] = (Er[0], [128, 2, 256], BF16)
        dbg["E1"] = (Er[1], [128, 2, 256], BF16)
        dbg["ebfix"] = (ebfix[:], [128, 1024], F32)
        dbg["lams"] = (lams[:], [128, 8], F32)
        dbg["o1s"] = (tv(0, 0), [128, 130], F32)
        dbg["obf"] = (tv(0, 2), [128, 128], BF16)
        dbg["at0"] = (attnT[:, 0, 0:256], [128, 256], BF16)
        dbg["stats"] = (stats[:], [128, 128], F32)
        return finish(nc, P, st, dbg)
    if stop == "C":
        dbg["attnT"] = (attnT, [128, 8, S], BF16)
        return finish(nc, P, st, dbg)

    project(1536, "sb")
    P.barrier()
    Wr = [view(OFF_W + i * 512, [128, 256], BF16) for i in range(4)]
    bWr = [B("Wr%d" % i) for i in range(4)]
    e32 = [view(OFF_W + 2048 + i * 1024, [128, 256], F32) for i in range(2)]
    Lb = [view(OFF_W + 4096 + i * 512, [128, 256], BF16) for i in range(2)]
    Rb = [view(OFF_W + 5120 + i * 512, [128, 256], BF16) for i in range(3)]
    be32 = [B("e32%d" % i) for i in range(2)]
    bL = [B("L%d" % i) for i in range(2)]
    bR = [B("R%d" % i) for i in range(3)]
    bZ = [B("Z%d" % i) for i in range(2)]
    bX = [B("X%d" % i) for i in range(2)]
    bOT = [B("OT%d" % i) for i in range(2)]

    blocks = []
    for hd in range(8):
        for c in range(8):
            for kb in range(2 * c + 1, -1, -1):
                blocks.append((hd, c, kb))
    NB = len(blocks)

    def sb_pe1(i):
        hd, c, kb = blocks[i]
        g, po = hd // 2, (hd % 2) * 64
        lo = 128 if kb == 2 * c + 1 else 0
        pz = i % 2
        P.op("pe", lambda e: e.matmul(psb[pz][:, lo:256], lhsT=QK[po:po + 64, 4 + g, kb * 128:(kb + 1) * 128],
                                      rhs=QK[po:po + 64, g, c * 256 + lo:(c + 1) * 256], start=True, stop=True),
             reads=[bQK[4 + g], bQK[g]], writes=[bZ[pz]])

    def sb_act1(i):
        hd, c, kb = blocks[i]
        lo = 128 if kb == 2 * c + 1 else 0
        pz = i % 2
        first = kb == 2 * c + 1
        P.op("act", lambda e: e.activation(out=e32[pz][:, lo:256], in_=psb[pz][:, lo:256], func=AF.Exp), reads=[bZ[pz]], writes=[be32[pz]])
        if first:
            P.op("dve", lambda e: e.memset(e32[pz][:, 0:128], 0.0), writes=[be32[pz]], accum=True)
        j = kb - 2 * c
        if j >= 0:
            P.op("dve", lambda e: e.tensor_tensor(out=e32[pz][:, j * 128:(j + 1) * 128], in0=e32[pz][:, j * 128:(j + 1) * 128], in1=strictm, op=ALU.mult),
                 reads=[be32[pz], bC2], writes=[be32[pz]])
        P.op("act", lambda e: e.activation(out=Lb[pz][:], in_=e32[pz][:], func=AF.Ln, bias=1.0), reads=[be32[pz]], writes=[bL[pz]])
        if kb > 0:
            if first:
                P.op("dve", lambda e: e.tensor_copy(out=Rb[(i + 1) % 3][:], in_=Lb[pz][:]), reads=[bL[pz]], writes=[bR[(i + 1) % 3]])
            else:
                P.op("dve", lambda e: e.tensor_tensor(out=Rb[(i + 1) % 3][:], in0=Rb[i % 3][:], in1=Lb[pz][:], op=ALU.add),
                     reads=[bL[pz], bR[i % 3]], writes=[bR[(i + 1) % 3]])

    def sb_pe2(i):
        hd, c, kb = blocks[i]
        g, po = hd // 2, (hd % 2) * 64
        lo = 128 if kb == 2 * c + 1 else 0
        pz = i % 2
        first = kb == 2 * c + 1
        xb = psb[2 + pz]
        P.op("pe", lambda e: e.matmul(xb[:, lo:256], lhsT=QK[po:po + 64, 4 + g, kb * 128:(kb + 1) * 128],
                                      rhs=QK[po:po + 64, g, c * 256 + lo:(c + 1) * 256], start=True, stop=False),
             reads=[bQK[4 + g], bQK[g]], writes=[bX[pz]])
        P.op("pe", lambda e: e.matmul(xb[:, lo:256], lhsT=negtri, rhs=Lb[pz][:, lo:256], start=False, stop=first),
             reads=[bL[pz], bConst], writes=[bX[pz]])
        if not first:
            P.op("pe", lambda e: e.matmul(xb[:, lo:256], lhsT=negones, rhs=Rb[i % 3][:, lo:256], start=False, stop=True),
                 reads=[bR[i % 3], bConst], writes=[bX[pz]])

    def sb_act2(i):
        hd, c, kb = blocks[i]
        lo = 128 if kb == 2 * c + 1 else 0
        pz = i % 2
        wi = i % 4
        first = kb == 2 * c + 1
        P.op("act", lambda e: e.activation(out=Wr[wi][:, lo:256], in_=psb[2 + pz][:, lo:256], func=AF.Exp), reads=[bX[pz]], writes=[bWr[wi]])
        if first:
            P.op("dve", lambda e: e.memset(Wr[wi][:, 0:128], 0.0), writes=[bWr[wi]], accum=True)
        j = kb - 2 * c
        if j >= 0:
            P.op("dve", lambda e: e.tensor_tensor(out=Wr[wi][:, j * 128:(j + 1) * 128], in0=Wr[wi][:, j * 128:(j + 1) * 128], in1=strictm, op=ALU.mult),
                 reads=[bWr[wi], bC2], writes=[bWr[wi]])

    unit_ctr = [0]

    def sb_pe3(i):
        hd, c, kb = blocks[i]
        g, po = hd // 2, (hd % 2) * 64
        wi = i % 4
        first = kb == 2 * c + 1
        last = kb == 0
        up = (hd * 8 + c) % 2
        ob = psb[4 + up]
        P.op("pe", lambda e: e.matmul(ob[po:po + 64, 0:256], lhsT=VV[:, kb, hd * 64:(hd + 1) * 64], rhs=Wr[wi][:], start=first, stop=last),
             reads=[bWr[wi], bV[kb]], writes=[bOT[up]])
        if last:
            if (hd * 8 + c) % 2 == 0:
                P.op("act", lambda e: e.activation(out=attnT[po:po + 64, 4 + g, c * 256:(c + 1) * 256], in_=ob[po:po + 64, 0:256], func=AF.Copy),
                     reads=[bOT[up]], writes=[bAT[2 * c], bAT[2 * c + 1]], accum=True)
            else:
                P.op("dve", lambda e: e.tensor_copy(out=attnT[po:po + 64, 4 + g, c * 256:(c + 1) * 256], in_=ob[po:po + 64, 0:256]),
                     reads=[bOT[up]], writes=[bAT[2 * c], bAT[2 * c + 1]], accum=True)

    for s_ in range(NB + 2):
        if s_ < NB:
            sb_pe1(s_)
            sb_act1(s_)
        if 0 <= s_ - 1 < NB:
            sb_pe2(s_ - 1)
            sb_act2(s_ - 1)
        if 0 <= s_ - 2 < NB:
            sb_pe3(s_ - 2)

    if stop == "D":
        dbg["attnT"] = (attnT, [128, 8, S], BF16)
        return finish(nc, P, st, dbg)

    P.barrier()
    KB = 1024
    hres = view(0, [128, NT, D], F32)
    tT = view(96 * KB, [128, 8, S], BF16)
    wob = view(128 * KB, [128, 8, D], BF16)
    xt1 = view(144 * KB, [128, D], F32)
    tb1 = view(148 * KB, [128, D], BF16)
    junk2 = view(150 * KB, [128, D], BF16)
    tmpx = sbt("tmpx", [128, 4 * 512], F32)
    comb = sbt("comb", [128, NT * 32], F32)
    lgs = sbt("lgs", [128, 4 * 32], F32)
    mx8 = sbt("mx8", [128, 32], F32)
    ix8 = sbt("ix8", [128, 8], U32)
    mbt = sbt("mbt", [128, 96], BF16)
    bIx = B("ix8"); bMb = B("mskb"); bMacc = [B("macc0"), B("macc1")]; bXg = B("xg"); bH1d = B("h1d"); bMeta = B("meta")
    rwb = sbt("rwb", [128, 256], BF16)
    bH = [B("h%d" % i_) for i_ in range(NT)]
    bTT = [B("tT%d" % i_) for i_ in range(NT)]
    bWo = B("wo"); bX1 = B("x1"); bTb = B("tb1"); bJ2 = B("junk2"); bRwb = B("rwb"); bLg = B("lg"); bMx = B("mx"); bComb = B("comb")
    bPE = [B("pe%d" % i_) for i_ in range(8)]
    P.dma("pool", lambda e: e.dma_start(out=wob, in_=wout_d), writes=[bWo])
    P.dma("pool", lambda e: e.dma_start(out=rwb[:], in_=rw_d), writes=[bRwb])
    P.dma("sp", lambda e: e.dma_start(out=gainb[:], in_=gains_d[1].partition_broadcast(128)), writes=[bGain])

    def rms2(i_, src, srcbuf, n):
        c0 = i_ * 8
        P.op("act", lambda e: e.activation(out=junk2[:, 0:n], in_=src, func=AF.Square, accum_out=stats[:, c0:c0 + 1]), reads=[srcbuf], writes=[bJ2, bSt[i_]])
        P.op("dve", lambda e: e.tensor_scalar(out=stats[:, c0 + 1:c0 + 2], in0=stats[:, c0:c0 + 1], scalar1=1.0 / n, scalar2=EPS, op0=ALU.mult, op1=ALU.add), reads=[bSt[i_]], writes=[bSt[i_]])
        P.op("pool", lambda e: e.tensor_tensor(out=stats[:, c0 + 3:c0 + 4], in0=stats[:, c0 + 1:c0 + 2], in1=cst[:, 0:1], op=ALU.pow), reads=[bSt[i_], bCst], writes=[bSt[i_]])
        return stats[:, c0 + 3:c0 + 4]

    def phaseE(i_):
        P.dma("sp", lambda e: e.dma_start(out=xt1, in_=x_d[i_ * 128:(i_ + 1) * 128, :]), writes=[bX1])
        for half in range(2):
            bk = 2 * (i_ % 2) + half
            for c_ in range(8):
                P.op("pe", lambda e, c_=c_, bk=bk, half=half: e.matmul(psb[bk], lhsT=attnT[:, c_, i_ * 128:(i_ + 1) * 128], rhs=wob[:, c_, half * 512:(half + 1) * 512], start=(c_ == 0), stop=(c_ == 7)),
                     reads=[bAT[i_], bWo], writes=[bPE[bk]])
            P.op("dve", lambda e, bk=bk, half=half: e.tensor_tensor(out=hres[:, i_, half * 512:(half + 1) * 512], in0=psb[bk], in1=xt1[:, half * 512:(half + 1) * 512], op=ALU.add),
                 reads=[bPE[bk], bX1], writes=[bH[i_]], accum=(half == 1))
        rstd = rms2(i_, hres[:, i_, :], bH[i_], D)
        P.op("dve", lambda e: e.scalar_tensor_tensor(out=tb1, in0=hres[:, i_, :], scalar=rstd, in1=gainb[:], op0=ALU.mult, op1=ALU.mult), reads=[bH[i_], bSt[i_], bGain], writes=[bTb])
        pT = psb[4 + i_ % 2].bitcast(BF16)
        for c_ in range(8):
            P.op("pe", lambda e, c_=c_: e.transpose(pT[:, c_ * 128:(c_ + 1) * 128], tb1[:, c_ * 128:(c_ + 1) * 128], identb), reads=[bTb, bConst], writes=[bPE[4 + i_ % 2]])
        P.op("act", lambda e: e.activation(out=tT[:, :, i_ * 128:(i_ + 1) * 128], in_=pT.rearrange("p (c t) -> p c t", t=128), func=AF.Copy), reads=[bPE[4 + i_ % 2]], writes=[bTT[i_]])
        for c_ in range(8):
            P.op("pe", lambda e, c_=c_: e.matmul(psb[6][:, 0:32], lhsT=tT[:, c_, i_ * 128:(i_ + 1) * 128], rhs=rwb[:, c_ * 32:(c_ + 1) * 32], start=(c_ == 0), stop=(c_ == 7)),
                 reads=[bTT[i_], bRwb], writes=[bPE[6]])
        lg = lgs[:, 0:32]; exl = lgs[:, 32:64]; msk = lgs[:, 64:96]
        P.op("dve", lambda e: e.tensor_tensor(out=lg, in0=psb[6][:, 0:32], in1=rbb[:], op=ALU.add), reads=[bPE[6], bC8], writes=[bLg])
        P.op("dve", lambda e: e.max(out=mx8[:, 0:8], in_=lg), reads=[bLg], writes=[bMx])
        P.op("dve", lambda e: e.tensor_scalar(out=mx8[:, 8:9], in0=mx8[:, 0:1], scalar1=-1.0, scalar2=None, op0=ALU.mult), reads=[bMx], writes=[bMx])
        P.op("act", lambda e: e.activation(out=exl, in_=lg, func=AF.Exp, bias=mx8[:, 8:9]), reads=[bLg, bMx], writes=[bLg])
        P.op("dve", lambda e: e.tensor_scalar(out=msk, in0=lg, scalar1=mx8[:, 3:4], scalar2=None, op0=ALU.is_ge), reads=[bLg, bMx], writes=[bLg])
        P.op("dve", lambda e: e.tensor_tensor(out=exl, in0=exl, in1=msk, op=ALU.mult), reads=[bLg], writes=[bLg])
        P.op("dve", lambda e: e.tensor_reduce(out=mx8[:, 9:10], in_=exl, axis=AX.X, op=ALU.add), reads=[bLg], writes=[bMx])
        P.op("dve", lambda e: e.reciprocal(out=mx8[:, 10:11], in_=mx8[:, 9:10]), reads=[bMx], writes=[bMx])
        P.op("act", lambda e: e.activation(out=mx8[:, 16:20], in_=mx8[:, 0:4], func=AF.Exp, bias=mx8[:, 8:9]), reads=[bMx], writes=[bMx])
        P.op("dve", lambda e: e.tensor_scalar(out=meta_g[:, i_ * 4:(i_ + 1) * 4], in0=mx8[:, 16:20], scalar1=mx8[:, 10:11], scalar2=None, op0=ALU.mult), reads=[bMx], writes=[bMeta], accum=True)
        P.op("dve", lambda e: e.max_index(out=ix8[:], in_max=mx8[:, 0:8], in_values=lg), reads=[bLg, bMx], writes=[bIx])
        P.op("dve", lambda e: e.tensor_copy(out=mbt[:, 0:32], in_=msk), reads=[bLg], writes=[bMb])
        pfx = psb[7][:, 0:32]
        a_ = i_ % 2
        P.op("pe", lambda e: e.matmul(pfx, lhsT=ltm, rhs=mbt[:, 0:32], start=True, stop=(i_ == 0)), reads=[bMb, bConst], writes=[bPE[7]])
        if i_ > 0:
            P.op("pe", lambda e: e.matmul(pfx, lhsT=onesb, rhs=mbt[:, 32 + a_ * 32:64 + a_ * 32], start=False, stop=True), reads=[bMacc[a_], bConst], writes=[bPE[7]])
        if i_ == 0:
            P.op("dve", lambda e: e.tensor_copy(out=mbt[:, 64:96], in_=mbt[:, 0:32]), reads=[bMb], writes=[bMacc[1]])
        else:
            P.op("dve", lambda e: e.tensor_tensor(out=mbt[:, 32 + (1 - a_) * 32:64 + (1 - a_) * 32], in0=mbt[:, 32 + a_ * 32:64 + a_ * 32], in1=mbt[:, 0:32], op=ALU.add),
                 reads=[bMb, bMacc[a_]], writes=[bMacc[1 - a_]])
        oh = lgs[:, 96:128]
        for k_ in range(4):
            P.op("dve", lambda e, k_=k_: e.tensor_scalar(out=oh, in0=lg, scalar1=mx8[:, k_:k_ + 1], scalar2=None, op0=ALU.is_equal), reads=[bLg, bMx], writes=[bLg])
            P.op("dve", lambda e: e.tensor_tensor(out=oh, in0=oh, in1=pfx, op=ALU.mult), reads=[bLg, bPE[7]], writes=[bLg])
            P.op("dve", lambda e, k_=k_: e.tensor_reduce(out=mx8[:, 20 + k_:21 + k_], in_=oh, axis=AX.X, op=ALU.add), reads=[bLg], writes=[bMx])
        P.op("dve", lambda e: e.tensor_copy(out=mx8[:, 24:28], in_=ix8[:, 0:4]), reads=[bIx], writes=[bMx])
        P.op("dve", lambda e: e.scalar_tensor_tensor(out=mx8[:, 28:32], in0=mx8[:, 24:28], scalar=float(CAP), in1=mx8[:, 20:24], op0=ALU.mult, op1=ALU.add), reads=[bMx], writes=[bMx])
        P.op("dve", lambda e: e.tensor_copy(out=meta_dest[:, i_ * 4:(i_ + 1) * 4], in_=mx8[:, 28:32]), reads=[bMx], writes=[bMeta], accum=True)
        for k_ in range(4):
            P.dma("pool", lambda e, k_=k_: e.indirect_dma_start(out=xg_d, out_offset=bass.IndirectOffsetOnAxis(ap=meta_dest[:, i_ * 4 + k_:i_ * 4 + k_ + 1], axis=0),
                                                               in_=tb1, in_offset=None), reads=[bTb, bMeta], writes=[bXg])
        P.dma("sp", lambda e: e.dma_start(out=h1_d[i_ * 128:(i_ + 1) * 128, :], in_=hres[:, i_, :]), reads=[bH[i_]], writes=[bH1d])

    for i_ in range(NT):
        phaseE(i_)
    cnt_i = sbt("cnt_i", [128, 32], I32)
    bCnt = B("cnt")
    P.op("pe", lambda e: e.matmul(psb[7][:, 0:32], lhsT=onesb, rhs=mbt[:, 32:64], start=True, stop=True), reads=[bMacc[0], bConst], writes=[bPE[7]])
    P.op("dve", lambda e: e.tensor_copy(out=cnt_i[:], in_=psb[7][:, 0:32]), reads=[bPE[7]], writes=[bCnt])
    P.cnt_ap = lambda ex_: cnt_i[0:1, ex_:ex_ + 1]

    if stop == "E":
        dbg["h1"] = (hres, [128, NT, D], F32)
        dbg["tT"] = (tT, [128, 8, S], BF16)
        dbg["mdest"] = (meta_dest[:], [128, NT * 4], I32)
        dbg["mg"] = (meta_g[:], [128, NT * 4], F32)
        dbg["xg0"] = (xg_d[0:512, :], [512, D], BF16)
        return finish(nc, P, st, dbg)

    P.barrier()
    NTS = RC // 128
    xe = [view(0 + k_ * 6 * KB, [128, NTS, D], BF16) for k_ in range(2)]
    XeT = [view(12 * KB + k_ * 6 * KB, [128, 8, RC], BF16) for k_ in range(2)]
    actT = [view(24 * KB + k_ * 6 * KB, [128, 8, RC], BF16) for k_ in range(2)]
    wdb = [view(36 * KB + k_ * 16 * KB, [128, 8, D], BF16) for k_ in range(2)]
    wg = [view(68 * KB + k_ * 4 * KB, [128, 8, 256], BF16) for k_ in range(3)]
    yt = [view(80 * KB + k_ * 4 * KB, [128, D], F32) for k_ in range(2)]
    bdt = [view(88 * KB + k_ * 4 * KB, [128, D], F32) for k_ in range(2)]
    bXe = [B("xe%d" % k_) for k_ in range(2)]; bXT = [B("XeT%d" % k_) for k_ in range(2)]; bAc = [B("ac%d" % k_) for k_ in range(2)]
    bWd = [B("wd%d" % k_) for k_ in range(2)]; bWg = [B("wg%d" % k_) for k_ in range(3)]; bYt = [B("yt%d" % k_) for k_ in range(2)]; bBd = [B("bd%d" % k_) for k_ in range(2)]
    bYg = B("yg")
    bTg = B("tg"); bTs = B("ts"); bTl = B("tl")
    tg = tmpx[:, 0:RC]; ts = tmpx[:, 512:512 + RC]; tl = tmpx[:, 1024:1024 + RC]
    cnt = [0]; slabc = [0]; ytc = [0]; qc = [0]

    def gu_chunk(e_, j_, k_, q_):
        ba = 2 * (cnt[0] % 2)
        cnt[0] += 1
        for which in range(2):
            for c_ in range(8):
                P.op("pe", lambda e, c_=c_, which=which: e.matmul(psb[ba + which][:, 0:RC], lhsT=wg[k_][:, c_, which * 128:(which + 1) * 128], rhs=XeT[q_][:, c_, :], start=(c_ == 0), stop=(c_ == 7)),
                     reads=[bWg[k_], bXT[q_]], writes=[bPE[ba + which]])
        col = (e_ * 8 + j_) * 2
        P.op("dve", lambda e: e.tensor_scalar(out=tg, in0=psb[ba][:, 0:RC], scalar1=bgl[:, col:col + 1], scalar2=7.0, op0=ALU.add, op1=ALU.min), reads=[bPE[ba], bC9], writes=[bTg])
        P.op("act", lambda e: e.activation(out=ts, in_=tg, func=AF.Sigmoid, scale=1.702), reads=[bTg], writes=[bTs])
        P.op("dve", lambda e: e.tensor_scalar(out=tl, in0=psb[ba + 1][:, 0:RC], scalar1=bgl[:, col + 1:col + 2], scalar2=8.0, op0=ALU.add, op1=ALU.min), reads=[bPE[ba + 1], bC9], writes=[bTl])
        P.op("dve", lambda e: e.tensor_tensor(out=ts, in0=tg, in1=ts, op=ALU.mult), reads=[bTg, bTs], writes=[bTs])
        P.op("dve", lambda e: e.scalar_tensor_tensor(out=actT[q_][:, j_, :], in0=tl, scalar=-6.0, in1=ts, op0=ALU.max, op1=ALU.mult), reads=[bTl, bTs], writes=[bAc[q_]], accum=True)

    def down_tile(e_, q_, st_, half, yk):
        bk = 4 + cnt[0] % 2
        cnt[0] += 1
        for j_ in range(8):
            P.op("pe", lambda e, j_=j_: e.matmul(psb[bk], lhsT=actT[q_][:, j_, st_ * 128:(st_ + 1) * 128], rhs=wdb[q_][:, j_, half * 512:(half + 1) * 512], start=(j_ == 0), stop=(j_ == 7)),
                 reads=[bAc[q_], bWd[q_]], writes=[bPE[bk]])
        P.op("dve", lambda e: e.tensor_tensor(out=yt[yk][:, half * 512:(half + 1) * 512], in0=psb[bk], in1=bdt[q_][:, half * 512:(half + 1) * 512], op=ALU.add),
             reads=[bPE[bk], bBd[q_]], writes=[bYt[yk]], accum=(half == 1))

    def expert_chunk(e_, ch):
        q_ = qc[0] % 2
        qc[0] += 1
        row0 = e_ * CAP + min(ch * RC, CAP - RC)
        P.dma("sp", lambda e: e.dma_start(out=xe[q_], in_=xg_d[row0:row0 + RC, :].rearrange("(t p) d -> p t d", p=128)), reads=[bXg], writes=[bXe[q_]])
        P.dma("sp", lambda e: e.dma_start(out=bdt[q_], in_=bd_d[e_].partition_broadcast(128)), writes=[bBd[q_]])
        P.dma("pool", lambda e: e.dma_start(out=wdb[q_], in_=wd_d[e_]), writes=[bWd[q_]])
        for t_ in range(NTS):
            pT = psb[6 + t_ % 2].bitcast(BF16)
            for c_ in range(8):
                P.op("pe", lambda e, c_=c_, t_=t_, pT=pT: e.transpose(pT[:, c_ * 128:(c_ + 1) * 128], xe[q_][:, t_, c_ * 128:(c_ + 1) * 128], identb),
                     reads=[bXe[q_], bConst], writes=[bPE[6 + t_ % 2]])
            if t_ % 2 == 0:
                P.op("act", lambda e, t_=t_, pT=pT: e.activation(out=XeT[q_][:, :, t_ * 128:(t_ + 1) * 128], in_=pT.rearrange("p (c t) -> p c t", t=128), func=AF.Copy),
                     reads=[bPE[6 + t_ % 2]], writes=[bXT[q_]], accum=(t_ > 0))
            else:
                P.op("dve", lambda e, t_=t_, pT=pT: e.tensor_copy(out=XeT[q_][:, :, t_ * 128:(t_ + 1) * 128], in_=pT.rearrange("p (c t) -> p c t", t=128)),
                     reads=[bPE[6 + t_ % 2]], writes=[bXT[q_]], accum=True)
        for j_ in range(8):
            k_ = slabc[0] % 3
            slabc[0] += 1
            P.dma("pool", lambda e, j_=j_, k_=k_: e.dma_start(out=wg[k_], in_=wgu_d[e_, j_]), writes=[bWg[k_]])
            gu_chunk(e_, j_, k_, q_)
        for st_ in range(NTS):
            yk = ytc[0] % 2
            ytc[0] += 1
            for half in range(2):
                down_tile(e_, q_, st_, half, yk)
            P.dma("sp", lambda e, st_=st_, yk=yk: e.dma_start(out=yg_d[row0 + st_ * 128:row0 + (st_ + 1) * 128, :], in_=yt[yk]), reads=[bYt[yk]], writes=[bYg], sembuf=bYg)

    NEX = int(os.environ.get("KNEX", NE))
    NCHX = int(os.environ.get("KNCH", NCH))
    for e_ in range(NEX):
        expert_chunk(e_, 0)
        for ch in range(1, NCHX):
            P.guard_begin((e_, ch * RC))
            expert_chunk(e_, ch)
            P.guard_end()

    if stop == "G":
        dbg["cnt"] = (cnt_i[:], [128, 32], I32)
        return finish(nc, P, st, dbg)

    P.barrier()
    pgb = view(128 * KB, [128, 8, D], BF16)
    ppb = view(144 * KB, [128, 2, D], BF16)
    ptile = view(148 * KB, [128, 256], F32)
    pbf = view(149 * KB, [128, 256], BF16)
    pTs = view(149 * KB + 512, [128, 2, 128], BF16)
    pe32 = view(64 * KB, [128, D], F32)
    gate = view(68 * KB, [128, D], F32)
    hbf = view(72 * KB, [128, D], BF16)
    hT = view(74 * KB, [128, 8, 128], BF16)
    otile = [view(76 * KB + k_ * 4096, [128, D], F32) for k_ in range(2)]
    gfin = ebfix
    bPg = B("pg"); bPp = B("pp"); bPt = B("pt"); bPbf = B("pbf"); bPTs = B("pTs"); bPe32 = B("pe32"); bGate = B("gate"); bHbf = B("hbf"); bHT = B("hT")
    bOt = [B("ot%d" % k_) for k_ in range(2)]; bGf = B("gfin"); bOut = B("out")
    P.dma("pool", lambda e: e.dma_start(out=pgb, in_=pgate_d), writes=[bPg])
    P.dma("pool", lambda e: e.dma_start(out=ppb, in_=pproj_d), writes=[bPp])
    P.dma("sp", lambda e: e.dma_start(out=gainb[:], in_=gains_d[2].partition_broadcast(128)), writes=[bGain])
    P.dma("sp", lambda e: e.dma_start(out=gfin[:], in_=gains_d[3].partition_broadcast(128)), writes=[bGf])

    htl = [view(84 * KB + k_ * 4 * KB, [128, D], F32) for k_ in range(2)]
    ygk = [view(92 * KB + k_ * 4 * KB, [128, D], F32) for k_ in range(8)]
    bHt = [B("ht%d" % k_) for k_ in range(2)]
    bYk = [B("ygk%d" % k_) for k_ in range(8)]

    def phaseH(i_):
        k_ = i_ % 2
        hcur = htl[k_]
        P.dma("sp", lambda e: e.dma_start(out=hcur, in_=h1_d[i_ * 128:(i_ + 1) * 128, :]), reads=[bH1d], writes=[bHt[k_]])
        for kk in range(4):
            yb = k_ * 4 + kk
            P.dma("pool", lambda e, kk=kk, yb=yb: e.indirect_dma_start(out=ygk[yb], out_offset=None, in_=yg_d,
                                                                        in_offset=bass.IndirectOffsetOnAxis(ap=meta_dest[:, i_ * 4 + kk:i_ * 4 + kk + 1], axis=0)),
                  reads=[bYg, bMeta], writes=[bYk[yb]])
            P.op("dve", lambda e, kk=kk, yb=yb: e.scalar_tensor_tensor(out=hcur, in0=ygk[yb], scalar=meta_g[:, i_ * 4 + kk:i_ * 4 + kk + 1], in1=hcur, op0=ALU.mult, op1=ALU.add),
                 reads=[bYk[yb], bMeta, bHt[k_]], writes=[bHt[k_]])
        P.dma("sp", lambda e: e.dma_start(out=ptile, in_=p_d[i_ * 128:(i_ + 1) * 128, :]), writes=[bPt])
        P.op("dve", lambda e: e.tensor_copy(out=pbf, in_=ptile), reads=[bPt], writes=[bPbf])
        tq = psb[6].bitcast(BF16)
        for c_ in range(2):
            P.op("pe", lambda e, c_=c_: e.transpose(tq[:, c_ * 128:(c_ + 1) * 128], pbf[:, c_ * 128:(c_ + 1) * 128], identb), reads=[bPbf, bConst], writes=[bPE[6]])
        P.op("act", lambda e: e.activation(out=pTs, in_=tq[:, 0:256].rearrange("p (c t) -> p c t", t=128), func=AF.Copy), reads=[bPE[6]], writes=[bPTs])
        for half in range(2):
            for c_ in range(2):
                P.op("pe", lambda e, c_=c_, half=half: e.matmul(psb[half], lhsT=pTs[:, c_, :], rhs=ppb[:, c_, half * 512:(half + 1) * 512], start=(c_ == 0), stop=(c_ == 1)),
                     reads=[bPTs, bPp], writes=[bPE[half]])
            P.op("act", lambda e, half=half: e.activation(out=pe32[:, half * 512:(half + 1) * 512], in_=psb[half], func=AF.Copy), reads=[bPE[half]], writes=[bPe32], accum=(half == 1))
        rp = rms2(i_, pe32, bPe32, D)
        P.op("dve", lambda e: e.scalar_tensor_tensor(out=pe32, in0=pe32, scalar=rp, in1=gainb[:], op0=ALU.mult, op1=ALU.mult), reads=[bPe32, bSt[i_], bGain], writes=[bPe32])
        P.op("dve", lambda e: e.tensor_copy(out=hbf, in_=hcur), reads=[bHt[k_]], writes=[bHbf])
        tq2 = psb[7].bitcast(BF16)
        for c_ in range(8):
            P.op("pe", lambda e, c_=c_: e.transpose(tq2[:, c_ * 128:(c_ + 1) * 128], hbf[:, c_ * 128:(c_ + 1) * 128], identb), reads=[bHbf, bConst], writes=[bPE[7]])
        P.op("act", lambda e: e.activation(out=hT, in_=tq2.rearrange("p (c t) -> p c t", t=128), func=AF.Copy), reads=[bPE[7]], writes=[bHT])
        for half in range(2):
            for c_ in range(8):
                P.op("pe", lambda e, c_=c_, half=half: e.matmul(psb[2 + half], lhsT=hT[:, c_, :], rhs=pgb[:, c_, half * 512:(half + 1) * 512], start=(c_ == 0), stop=(c_ == 7)),
                     reads=[bHT, bPg], writes=[bPE[2 + half]])
            P.op("act", lambda e, half=half: e.activation(out=gate[:, half * 512:(half + 1) * 512], in_=psb[2 + half], func=AF.Sigmoid), reads=[bPE[2 + half]], writes=[bGate], accum=(half == 1))
        P.op("dve", lambda e: e.tensor_tensor(out=gate, in0=gate, in1=pe32, op=ALU.mult), reads=[bGate, bPe32], writes=[bGate])
        P.op("dve", lambda e: e.tensor_tensor(out=hcur, in0=hcur, in1=gate, op=ALU.add), reads=[bHt[k_], bGate], writes=[bHt[k_]])
        c0 = i_ * 8 + 4
        P.op("act", lambda e: e.activation(out=junk2, in_=hcur, func=AF.Square, accum_out=stats[:, c0:c0 + 1]), reads=[bHt[k_]], writes=[bJ2, bSt[i_]])
        P.op("dve", lambda e: e.tensor_scalar(out=stats[:, c0 + 1:c0 + 2], in0=stats[:, c0:c0 + 1], scalar1=1.0 / D, scalar2=EPS, op0=ALU.mult, op1=ALU.add), reads=[bSt[i_]], writes=[bSt[i_]])
        P.op("pool", lambda e: e.tensor_tensor(out=stats[:, c0 + 2:c0 + 3], in0=stats[:, c0 + 1:c0 + 2], in1=cst[:, 0:1], op=ALU.pow), reads=[bSt[i_], bCst], writes=[bSt[i_]])
        P.op("dve", lambda e: e.scalar_tensor_tensor(out=otile[k_], in0=hcur, scalar=stats[:, c0 + 2:c0 + 3], in1=gfin[:], op0=ALU.mult, op1=ALU.mult), reads=[bHt[k_], bSt[i_], bGf], writes=[bOt[k_]])
        P.dma("sp", lambda e: e.dma_start(out=out_d[i_ * 128:(i_ + 1) * 128, :], in_=otile[k_]), reads=[bOt[k_]], writes=[bOut], sembuf=bOt[k_])

    for i_ in range(NT):
        phaseH(i_)
    P.wait_all("sp", [bOut] + bOt)
    P.run(st)
    st.close()
    return nc


def finish(nc, P, st, dbg):
    P.barrier()
    outs = []
    for name, (ap, shape, dt) in dbg.items():
        d = nc.dram_tensor("dbg_" + name, shape, dt, kind="ExternalOutput").ap()
        b = Buf(P, "dbg_" + name)
        P.dma("sp", lambda e, d=d, ap=ap: e.dma_start(out=d, in_=ap), writes=[b])
        outs.append(b)
    P.wait_all("sp", outs)
    P.run(st)
    st.close()
    return nc


def _t5_bucket(rel):
    n = np.maximum(rel, 0)
    nf = np.maximum(n, 1).astype(np.float32)
    large = 16 + (np.log(nf / np.float32(16)) / np.float32(math.log(128 / 16)) * np.float32(16)).astype(np.int32)
    large = np.minimum(large, 31)
    return np.where(n < 16, n, large)


def prep_shared(inp):
    f32 = np.float32
    g = lambda k: np.asarray(inp[k], dtype=f32)
    w_in = g("w_in")[0]
    cols = []
    for h in range(4):
        cols += list(range(h * 64, h * 64 + 64)) + list(range(256 + h * 64, 256 + h * 64 + 64))
    for h in range(4):
        cols += list(range(512 + h * 64, 512 + h * 64 + 64)) + list(range(768 + h * 64, 768 + h * 64 + 64))
    cols += list(range(1024, 3072))
    w_in_p = w_in[:, cols]
    chunked = lambda w: np.ascontiguousarray(w.reshape(8, 128, -1).transpose(1, 0, 2))
    sh = {}
    sh["w_in"] = chunked(w_in_p)
    sh["w_out"] = chunked(g("w_out")[0])
    sh["gains"] = np.stack([g("attn_norm")[0], g("moe_norm")[0], g("ple_norm")[0], g("final_norm")], 0)
    sh["lamv"] = np.stack([g("lambda_q1")[0], g("lambda_k1")[0], g("lambda_q2")[0], g("lambda_k2")[0]], 0)
    sh["subln"] = g("subln")
    rb = g("rel_bias")
    k = np.arange(128)[:, None]
    q = np.arange(128)[None, :]
    bn = np.zeros((128, 4, 2, 128), f32)
    for d in range(2):
        bk = _t5_bucket(q + 128 * d - k)
        bn[:, :, d, :] = rb[bk].transpose(0, 2, 1)
    sh["bnear"] = bn.reshape(128, -1)
    sh["c31"] = np.ascontiguousarray(np.broadcast_to(rb[31][None, :], (128, 4)))
    sh["rw"] = chunked(g("router_w")[0]).reshape(128, -1)
    sh["rb"] = g("router_b")
    wgu = g("w_gate_up")[0]
    glu = wgu[:, :, 0::2].reshape(NE, 8, 128, 8, 128)
    lin = wgu[:, :, 1::2].reshape(NE, 8, 128, 8, 128)
    gl = np.concatenate([glu, lin], axis=-1)
    sh["wgu"] = np.ascontiguousarray(gl.transpose(0, 3, 2, 1, 4))
    bgu = g("b_gate_up")[0]
    bg = bgu[:, 0::2].reshape(NE, 8, 128)
    bl = bgu[:, 1::2].reshape(NE, 8, 128)
    sh["bgl"] = np.ascontiguousarray(np.stack([bg, bl], -1).transpose(2, 0, 1, 3)).reshape(128, -1)
    sh["wd"] = np.ascontiguousarray(g("w_down")[0].reshape(NE, 8, 128, D).transpose(0, 2, 1, 3))
    sh["bd"] = g("b_down")[0]
    sh["pproj"] = np.ascontiguousarray(g("ple_proj")[0].reshape(2, 128, D).transpose(1, 0, 2))
    sh["pgate"] = chunked(g("ple_gate")[0])
    ident = np.eye(128, dtype=f32)
    jj = np.arange(128)[:, None]
    kk = np.arange(128)[None, :]
    negtri = -(jj >= kk).astype(f32)
    lt = (jj < kk).astype(f32)
    sh["cbf"] = np.concatenate([ident, negtri, -np.ones((128, 128), f32), lt, np.ones((128, 128), f32)], 1).astype(ml_dtypes.bfloat16)
    sh["cf32"] = np.concatenate([ident, (kk >= jj).astype(f32), (kk > jj).astype(f32)], 1)
    return sh


_CACHE = {}


def kernel(**inputs):
    sh = prep_shared(inputs)
    x = np.asarray(inputs["x"], dtype=np.float32)
    p = np.asarray(inputs["p"], dtype=np.float32)[0]
    if "nc" not in _CACHE:
        _CACHE["nc"] = build()
    nc = _CACHE["nc"]
    in_maps = []
    for c in range(8):
        m = dict(sh)
        m["x"] = np.ascontiguousarray(x[c])
        m["p"] = np.ascontiguousarray(p[c])
        in_maps.append(m)
    res = run_bass_kernel_spmd(nc, in_maps, core_ids=list(range(8)))
    return np.stack([np.asarray(r["out"], dtype=np.float32) for r in res.results], 0)
```

```python
from contextlib import ExitStack
import math
import numpy as np
import ml_dtypes
import concourse.bass as bass
import concourse.mybir as mybir
from concourse.bass_utils import run_bass_kernel_spmd

F32 = mybir.dt.float32
BF16 = mybir.dt.bfloat16
U32 = mybir.dt.uint32
I32 = mybir.dt.int32
AF = mybir.ActivationFunctionType
ALU = mybir.AluOpType
AX = mybir.AxisListType

S = 2048
D = 1024
NT = 16
NE = 32
RC = 384
NCH = 6
CAP = 2048
EPS = 1e-6
ENG = {"pe": "tensor", "act": "scalar", "dve": "vector", "pool": "gpsimd", "sp": "sync"}


class Buf:
    __slots__ = ("name", "w", "r", "sem", "cnt")

    def __init__(self, P, name):
        self.name = name
        self.w = []
        self.r = []
        self.sem = None
        self.cnt = 0
        P.bufs.append(self)


class Prog:
    def __init__(self, nc):
        self.nc = nc
        self.recs = {e: [] for e in ENG}
        self.waited = {e: {} for e in ENG}
        self.dma_sems = []
        self.bufs = []

    def _filter(self, eng, deps):
        out = []
        for t in deps:
            if t[0] == "c":
                _, e2, idx = t
                if e2 == eng and eng in ("pe", "sp"):
                    continue
                k = ("c", e2)
                if self.waited[eng].get(k, -1) >= idx:
                    continue
                self.waited[eng][k] = idx
                self.recs[e2][idx]["sig"] = True
                out.append(t)
            else:
                _, b, val = t
                k = ("d", id(b))
                if self.waited[eng].get(k, -1) >= val:
                    continue
                self.waited[eng][k] = val
                out.append(t)
        return out

    def _deps(self, eng, reads, writes):
        deps = []
        for b in reads:
            deps += b.w
        for b in writes:
            deps += b.w
            deps += b.r
        return self._filter(eng, deps)

    def op(self, eng, fn, reads=(), writes=(), accum=False):
        waits = self._deps(eng, reads, writes)
        idx = len(self.recs[eng])
        self.recs[eng].append(dict(waits=waits, fn=fn, sig=False, dma=None))
        tok = ("c", eng, idx)
        for b in reads:
            b.r.append(tok)
        for b in writes:
            if accum:
                b.w.append(tok)
            else:
                b.w = [tok]
                b.r = []
        return tok

    def dma(self, eng, fn, reads=(), writes=(), sembuf=None):
        waits = self._deps(eng, reads, writes)
        sb = sembuf if sembuf is not None else (writes[0] if writes else reads[0])
        if sb.sem is None:
            sb.sem = True
            self.dma_sems.append(sb)
        sb.cnt += 16
        tok = ("d", sb, sb.cnt)
        self.recs[eng].append(dict(waits=waits, fn=fn, sig=False, dma=sb, dmaval=sb.cnt))
        for b in reads:
            b.r.append(tok)
        for b in writes:
            b.w = [tok]
            b.r = []
        return tok

    def barrier(self):
        toks = []
        for e in ENG:
            for i in range(len(self.recs[e]) - 1, -1, -1):
                r = self.recs[e][i]
                if r["fn"] is not None and r["dma"] is None:
                    toks.append(("c", e, i))
                    break
        for b in self.bufs:
            toks += [t for t in b.w + b.r if t[0] == "d"]
            b.w = []
            b.r = []
        for e in ENG:
            w = self._filter(e, [t for t in toks if not (t[0] == "c" and t[1] == e)])
            self.recs[e].append(dict(waits=w, fn=None, sig=False, dma=None))

    def guard_begin(self, key):
        self._saved_waited = {e: dict(self.waited[e]) for e in ENG}
        for e in ENG:
            self.recs[e].append(dict(waits=[], fn=None, sig=False, dma=None, gb=key))

    def guard_end(self):
        for e in ENG:
            self.recs[e].append(dict(waits=[], fn=None, sig=False, dma=None, ge=True))
        self.waited = self._saved_waited

    def wait_all(self, eng, bufs):
        waits = self._deps(eng, list(bufs), list(bufs))
        self.recs[eng].append(dict(waits=waits, fn=None, sig=False, dma=None))

    def run(self, stack):
        nc = self.nc
        esem = {e: stack.enter_context(nc.semaphore("s_" + e)) for e in ENG}
        for i, b in enumerate(self.dma_sems):
            b.sem = stack.enter_context(nc.semaphore("d%d" % i))
        cnts = {}
        for e in ENG:
            c = 0
            for i, r in enumerate(self.recs[e]):
                if r["sig"]:
                    c += 1
                    cnts[(e, i)] = c
        block = stack.enter_context(nc.Block())

        def body(e):
            def emit(engine, r):
                for t in r["waits"]:
                    if t[0] == "c":
                        engine.wait_ge(esem[t[1]], cnts[(t[1], t[2])])
                    else:
                        engine.wait_ge(t[1].sem, t[2])
                if r["fn"] is None:
                    return
                ins = r["fn"](engine)
                if r["dma"] is not None:
                    ins.then_inc(r["dma"].sem, 16)
                elif r["sig"]:
                    ins.then_inc(esem[e], 1)

            def f(engine):
                recs = self.recs[e]
                reg = rthr = None
                if any("gb" in q for q in recs):
                    reg = stack.enter_context(engine.register("rc_" + e))
                    rthr = stack.enter_context(engine.register("rt_" + e))
                i = 0
                csum = 0
                while i < len(recs):
                    r = recs[i]
                    if "gb" in r:
                        j = i + 1
                        while "ge" not in recs[j]:
                            j += 1
                        inner = recs[i + 1:j]
                        ex_, thr = r["gb"]
                        if any(q["fn"] is not None or q["waits"] for q in inner):
                            engine.reg_load(reg, self.cnt_ap(ex_))
                            engine.reg_mov(rthr, thr)
                            with engine.If_lt(rthr, reg):
                                for q in inner:
                                    emit(engine, q)
                            nsig = sum(1 for q in inner if q["sig"])
                            dmas = [q for q in inner if q["dma"] is not None]
                            if nsig or dmas:
                                with engine.Else():
                                    if nsig:
                                        if csum > 0:
                                            engine.wait_ge(esem[e], csum)
                                        engine.sem_inc(esem[e], nsig)
                                    for q in dmas:
                                        if q["dmaval"] - 16 > 0:
                                            engine.wait_ge(q["dma"].sem, q["dmaval"] - 16)
                                        engine.sem_inc(q["dma"].sem, 16)
                            csum += nsig
                        i = j + 1
                        continue
                    emit(engine, r)
                    if r["sig"]:
                        csum += 1
                    i += 1
            return f

        block.tensor(body("pe"))
        block.scalar(body("act"))
        block.vector(body("dve"))
        block.gpsimd(body("pool"))
        block.sync(body("sp"))


def build(stop=None):
    nc = bass.Bass("TRN2", target_bir_lowering=False, dynamic_dma_scratch_size=8192)
    dram = lambda n, s, d=F32, k="ExternalInput": nc.dram_tensor(n, s, d, kind=k).ap()
    x_d = dram("x", [S, D])
    p_d = dram("p", [S, 256])
    win_d = dram("w_in", [128, 8, 3072])
    wout_d = dram("w_out", [128, 8, D])
    gains_d = dram("gains", [4, D])
    lamv_d = dram("lamv", [4, 64])
    subln_d = dram("subln", [1, 128])
    bnear_d = dram("bnear", [128, 4 * 2 * 128])
    c31_d = dram("c31", [128, 4])
    rw_d = dram("rw", [128, 8 * 32])
    rb_d = dram("rb", [1, 32])
    wgu_d = dram("wgu", [NE, 8, 128, 8, 256])
    bgl_d = dram("bgl", [128, NE * 8 * 2])
    wd_d = dram("wd", [NE, 128, 8, D])
    bd_d = dram("bd", [NE, D])
    pproj_d = dram("pproj", [128, 2, D])
    pgate_d = dram("pgate", [128, 8, D])
    cb_d = dram("cbf", [128, 5 * 128], BF16)
    cf_d = dram("cf32", [128, 3 * 128])
    out_d = dram("out", [S, D], F32, "ExternalOutput")
    h1_d = dram("h1s", [S, D], F32, "Internal")
    xg_d = dram("xg", [NE * CAP, D], BF16, "Internal")
    yg_d = dram("yg", [NE * CAP, D], F32, "Internal")
    dbg = {}

    st = ExitStack()
    P = Prog(nc)
    B = lambda n: Buf(P, n)
    sbt = lambda n, s, d: st.enter_context(nc.sbuf_tensor("sb_" + n, s, d))
    pall = st.enter_context(nc.psum_tensor("pall", [128, 4096], F32))
    psb = [pall[:, i * 512:(i + 1) * 512] for i in range(8)]

    cb = sbt("cb", [128, 5 * 128], BF16)
    cf = sbt("cf", [128, 3 * 128], F32)
    identb, negtri, negones, ltm, onesb = [cb[:, i * 128:(i + 1) * 128] for i in range(5)]
    identf, mask0, strictm = [cf[:, i * 128:(i + 1) * 128] for i in range(3)]
    gainb = sbt("gainb", [128, D], F32)
    gain2 = sbt("gain2", [128, D], F32)
    ebfix = sbt("ebfix", [128, 4 * 2 * 128], F32)
    c31 = sbt("c31", [128, 4], F32)
    lamt = sbt("lamt", [128, 4 * 64], F32)
    lams = sbt("lams", [128, 8], F32)
    subg = sbt("subg", [128, 128], F32)
    rw = sbt("rw", [128, 8 * 32], F32)
    rbb = sbt("rbb", [128, 32], F32)
    bgl = sbt("bgl", [128, NE * 8 * 2], F32)
    cst = sbt("cst", [128, 8], F32)
    stats = sbt("stats", [128, NT * 8], F32)
    meta_dest = sbt("meta_dest", [128, NT * 4], I32)
    meta_g = sbt("meta_g", [128, NT * 4], F32)
    bConst = B("const")
    bGain = B("gain")
    bGain2 = B("gain2")

    AR = sbt("arena", [128, 152 * 1024], mybir.dt.uint8)
    K = 1024
    OFF_HN, OFF_QK, OFF_AT, OFF_W, OFF_V, OFF_T = 0, 32 * K, 64 * K, 96 * K, 128 * K, 145 * K

    def view(off, shape, dt):
        nb = {F32: 4, BF16: 2, I32: 4, U32: 4}[dt]
        n = 1
        for s_ in shape[1:]:
            n *= s_
        ap = AR[:, off:off + n * nb].bitcast(dt)
        if len(shape) == 3:
            ap = ap.rearrange("p (a b) -> p a b", b=shape[2])
        elif len(shape) == 4:
            ap = ap.rearrange("p (a b c) -> p a b c", b=shape[2], c=shape[3])
        return ap

    P.dma("sp", lambda e: e.dma_start(out=cb[:], in_=cb_d), writes=[bConst])
    bC2 = B("c2"); bC3 = B("c3"); bC4 = B("c4"); bC5 = B("c5"); bC6 = B("c6"); bC7 = B("c7"); bC8 = B("c8"); bC9 = B("c9")
    P.dma("sp", lambda e: e.dma_start(out=cf[:], in_=cf_d), writes=[bC2])
    P.dma("sp", lambda e: e.dma_start(out=ebfix[:], in_=bnear_d), writes=[bC3])
    P.dma("sp", lambda e: e.dma_start(out=c31[:], in_=c31_d), writes=[bC4])
    P.dma("sp", lambda e: e.dma_start(out=lamt[:], in_=lamv_d.rearrange("a b -> (a b)").partition_broadcast(128)), writes=[bC5])
    P.dma("sp", lambda e: e.dma_start(out=subg[:], in_=subln_d.rearrange("a b -> (a b)").partition_broadcast(128)), writes=[bC6])
    P.dma("sp", lambda e: e.dma_start(out=rw[:], in_=rw_d), writes=[bC7])
    P.dma("sp", lambda e: e.dma_start(out=rbb[:], in_=rb_d.rearrange("a b -> (a b)").partition_broadcast(128)), writes=[bC8])
    P.dma("sp", lambda e: e.dma_start(out=bgl[:], in_=bgl_d), writes=[bC9])
    P.dma("sp", lambda e: e.dma_start(out=gainb[:], in_=gains_d[0].partition_broadcast(128)), writes=[bGain])
    bCst = B("cst")
    P.op("dve", lambda e: e.memset(cst[:, 0:1], -0.5), writes=[bCst])
    for h in range(4):
        P.op("dve", lambda e, h=h: e.tensor_scalar(out=ebfix[:, h * 256:(h + 1) * 256], in0=ebfix[:, h * 256:(h + 1) * 256],
                                                   scalar1=c31[:, h:h + 1], scalar2=None, op0=ALU.subtract),
             reads=[bC3, bC4], writes=[bC3])
    P.op("act", lambda e: e.activation(out=ebfix[:], in_=ebfix[:], func=AF.Exp), reads=[bC3], writes=[bC3])
    for h in range(4):
        P.op("dve", lambda e, h=h: e.tensor_tensor(out=ebfix[:, h * 256:h * 256 + 128], in0=ebfix[:, h * 256:h * 256 + 128], in1=mask0, op=ALU.mult),
             reads=[bC3, bC2], writes=[bC3])
    P.op("dve", lambda e: e.tensor_tensor(out=lamt[:, 0:64], in0=lamt[:, 0:64], in1=lamt[:, 64:128], op=ALU.mult), reads=[bC5], writes=[bC5])
    P.op("dve", lambda e: e.tensor_tensor(out=lamt[:, 128:192], in0=lamt[:, 128:192], in1=lamt[:, 192:256], op=ALU.mult), reads=[bC5], writes=[bC5])
    P.op("dve", lambda e: e.tensor_reduce(out=lams[:, 0:1], in_=lamt[:, 0:64], axis=AX.X, op=ALU.add), reads=[bC5], writes=[bC5])
    P.op("dve", lambda e: e.tensor_reduce(out=lams[:, 1:2], in_=lamt[:, 128:192], axis=AX.X, op=ALU.add), reads=[bC5], writes=[bC5])
    P.op("act", lambda e: e.activation(out=lams[:, 2:4], in_=lams[:, 0:2], func=AF.Exp), reads=[bC5], writes=[bC5])
    P.op("dve", lambda e: e.tensor_tensor(out=lams[:, 4:5], in0=lams[:, 3:4], in1=lams[:, 2:3], op=ALU.subtract), reads=[bC5], writes=[bC5])
    P.op("dve", lambda e: e.tensor_scalar(out=lams[:, 4:5], in0=lams[:, 4:5], scalar1=-0.2, scalar2=None, op0=ALU.add), reads=[bC5], writes=[bC5])
    P.op("dve", lambda e: e.tensor_scalar(out=subg[:], in0=subg[:], scalar1=0.8, scalar2=None, op0=ALU.mult), reads=[bC6], writes=[bC6])
    bglv = bgl[:].rearrange("p (a t) -> p a t", t=2)
    P.op("dve", lambda e: e.tensor_scalar(out=bglv[:, :, 1:2], in0=bglv[:, :, 1:2], scalar1=1.0, scalar2=None, op0=ALU.add), reads=[bC9], writes=[bC9])
    neglam = lams[:, 4:5]

    hnT = view(OFF_HN, [128, 8, S], BF16)
    bHn = [B("hn%d" % i) for i in range(NT)]
    xt = [view(OFF_W + i * 4096, [128, D], F32) for i in range(2)]
    xs = [view(OFF_W + 8192 + i * 2048, [128, D], BF16) for i in range(2)]
    junkb = view(OFF_T, [128, D], BF16)
    bXt = [B("xt%d" % i) for i in range(2)]
    bXs = [B("xs%d" % i) for i in range(2)]
    bJ = B("junk")
    bSt = [B("st%d" % i) for i in range(NT)]
    bPs = [B("ps%d" % i) for i in range(8)]

    def rms_stats(i, src, srcbuf, n):
        c0 = i * 8
        P.op("act", lambda e: e.activation(out=junkb[:, 0:n], in_=src, func=AF.Square, accum_out=stats[:, c0:c0 + 1]),
             reads=[srcbuf], writes=[bJ, bSt[i]])
        P.op("dve", lambda e: e.tensor_scalar(out=stats[:, c0 + 1:c0 + 2], in0=stats[:, c0:c0 + 1], scalar1=1.0 / n, scalar2=EPS, op0=ALU.mult, op1=ALU.add),
             reads=[bSt[i]], writes=[bSt[i]])
        P.op("pool", lambda e: e.tensor_tensor(out=stats[:, c0 + 3:c0 + 4], in0=stats[:, c0 + 1:c0 + 2], in1=cst[:, 0:1], op=ALU.pow),
             reads=[bSt[i], bCst], writes=[bSt[i]])
        return stats[:, c0 + 3:c0 + 4]

    for i in range(NT):
        b = i % 2
        P.dma("sp", lambda e, i=i, b=b: e.dma_start(out=xt[b], in_=x_d[i * 128:(i + 1) * 128, :]), writes=[bXt[b]])
        rstd = rms_stats(i, xt[b], bXt[b], D)
        P.op("dve", lambda e, b=b, rstd=rstd: e.scalar_tensor_tensor(out=xs[b], in0=xt[b], scalar=rstd, in1=gainb[:], op0=ALU.mult, op1=ALU.mult),
             reads=[bXt[b], bSt[i], bGain], writes=[bXs[b]])
        pT = psb[b].bitcast(BF16)
        for c in range(8):
            P.op("pe", lambda e, c=c, b=b, pT=pT: e.transpose(pT[:, c * 128:(c + 1) * 128], xs[b][:, c * 128:(c + 1) * 128], identb),
                 reads=[bXs[b], bConst], writes=[bPs[b]])
        P.op("act", lambda e, i=i, pT=pT: e.activation(out=hnT[:, :, i * 128:(i + 1) * 128], in_=pT.rearrange("p (c t) -> p c t", t=128), func=AF.Copy),
             reads=[bPs[b]], writes=[bHn[i]])

    if stop == "A":
        dbg["hnT"] = (hnT, [128, 8, S], BF16)
        return finish(nc, P, st, dbg)

    QK = view(OFF_QK, [128, 8, S], BF16)
    VV = view(OFF_V, [128, NT, 516], BF16)
    wsl = [view(OFF_W + i * 4096, [128, 8, 256], BF16) for i in range(3)]
    bW = [B("wsl%d" % i) for i in range(3)]
    bQK = [B("qk%d" % i) for i in range(8)]
    bV = [B("v%d" % i) for i in range(NT)]
    slab_ctr = [0]
    evac_ctr = [0]

    def project(col0, kind):
        P.barrier()
        if kind == "diff":
            VD4 = VV.rearrange("p t (h c) -> p t h c", c=129)
            P.op("dve", lambda e: e.memset(VD4[:, :, :, 128:129], 1.0), writes=bV)
        for s in range(6):
            wi = slab_ctr[0] % 3
            slab_ctr[0] += 1
            c_lo = col0 + s * 256
            P.dma("pool", lambda e, wi=wi, c_lo=c_lo: e.dma_start(out=wsl[wi], in_=win_d[:, :, c_lo:c_lo + 256]), writes=[bW[wi]])
            if s < 4:
                for gg in range(2):
                    gi = s * 2 + gg
                    for tc in range(4):
                        bk = evac_ctr[0] % 4
                        evac_ctr[0] += 1
                        for c in range(8):
                            P.op("pe", lambda e, wi=wi, gg=gg, tc=tc, c=c, bk=bk: e.matmul(
                                psb[bk], lhsT=wsl[wi][:, c, gg * 128:(gg + 1) * 128], rhs=hnT[:, c, tc * 512:(tc + 1) * 512],
                                start=(c == 0), stop=(c == 7)),
                                reads=[bW[wi]] + bHn[tc * 4:tc * 4 + 4], writes=[bPs[bk]])
                        sc = 0.125 if s < 2 else 1.0
                        if evac_ctr[0] % 2 == 0:
                            P.op("act", lambda e, gi=gi, tc=tc, bk=bk, sc=sc: e.activation(out=QK[:, gi, tc * 512:(tc + 1) * 512], in_=psb[bk], func=AF.Copy, scale=sc),
                                 reads=[bPs[bk]], writes=[bQK[gi]], accum=True)
                        else:
                            P.op("dve", lambda e, gi=gi, tc=tc, bk=bk, sc=sc: e.tensor_scalar(out=QK[:, gi, tc * 512:(tc + 1) * 512], in0=psb[bk], scalar1=sc, scalar2=None, op0=ALU.mult),
                                 reads=[bPs[bk]], writes=[bQK[gi]], accum=True)
            else:
                vs = s - 4
                for i in range(NT):
                    bk = evac_ctr[0] % 4
                    evac_ctr[0] += 1
                    for c in range(8):
                        P.op("pe", lambda e, wi=wi, i=i, c=c, bk=bk: e.matmul(
                            psb[bk][:, 0:256], lhsT=hnT[:, c, i * 128:(i + 1) * 128], rhs=wsl[wi][:, c, :], start=(c == 0), stop=(c == 7)),
                            reads=[bW[wi], bHn[i]], writes=[bPs[bk]])
                    if kind == "diff":
                        dst = VV[:, i, vs * 258:(vs + 1) * 258].rearrange("p (h c) -> p h c", c=129)[:, :, 0:128]
                        src = psb[bk][:, 0:256].rearrange("p (h c) -> p h c", c=128)
                    else:
                        dst = VV[:, i, vs * 256:(vs + 1) * 256]
                        src = psb[bk][:, 0:256]
                    if evac_ctr[0] % 2 == 0:
                        P.op("act", lambda e, dst=dst, src=src: e.activation(out=dst, in_=src, func=AF.Copy), reads=[bPs[bk]], writes=[bV[i]], accum=True)
                    else:
                        P.op("dve", lambda e, dst=dst, src=src: e.tensor_copy(out=dst, in_=src), reads=[bPs[bk]], writes=[bV[i]], accum=True)

    project(0, "diff")
    if stop == "B":
        dbg["QK"] = (QK, [128, 8, S], BF16)
        dbg["VV"] = (VV, [128, NT, 516], BF16)
        return finish(nc, P, st, dbg)

    P.barrier()
    attnT = view(OFF_AT, [128, 8, S], BF16)
    bAT = [B("at%d" % i) for i in range(NT)]
    NSLOT = 32
    Er = [view(OFF_W + i * 1024, [128, 2, 256], BF16) for i in range(NSLOT)]
    bE = [B("E%d" % i) for i in range(NSLOT)]
    def tv(par, k):
        base = OFF_T + par * 1296
        if k == 0:
            return view(base, [128, 130], F32)
        if k == 1:
            return view(base + 520, [128, 130], F32)
        return view(base + 1040, [128, 128], BF16)
    bEp = [B("ep%d" % i) for i in range(4)]
    late = []
    bSS = [B("S%d" % i) for i in range(2)]
    bO = [B("O%d" % m) for m in range(2)]
    bTp = [B("tp%d" % i) for i in range(2)]
    VD4 = VV.rearrange("p t (h c) -> p t h c", c=129)
    ebv = ebfix[:].rearrange("p (h d q) -> p h d q", h=4, d=2)

    units = [(h, c) for h in range(4) for c in range(8)]
    import os
    if stop == 'C1':
        units = units[:1]
    if os.environ.get('KLIM'):
        units = units[:int(os.environ['KLIM'])]
    blk_ctr = [0]
    ep_ctr = [0]

    def av_items(h, c, slots):
        items = []
        for j in range(2):
            for m in range(2):
                kbs = list(range(0, 2 * c + j + 1))
                for kb in kbs:
                    items.append((j, m, kb, kb == 0, kb == kbs[-1]))
        return items

    def emit_av(h, c, slots, it):
        j, m, kb, first, last = it
        ob = psb[4 + m][:, 0:129]
        sl = slots[kb]
        P.op("pe", lambda e: e.matmul(ob, lhsT=Er[sl][:, m, j * 128:(j + 1) * 128], rhs=VD4[:, kb, h, :], start=first, stop=last),
             reads=[bE[sl], bV[kb]], writes=[bO[m]])
        if last:
            par = ep_ctr[0] % 4
            P.op("dve", lambda e: e.tensor_copy(out=tv(par, m)[:, 0:129], in_=ob), reads=[bO[m]], writes=[bEp[par]], accum=(m == 1))
            if m == 1:
                epilogue(h, c, j)

    def epilogue(h, c, j):
        qb = 2 * c + j
        par = ep_ctr[0] % 4
        ep_ctr[0] += 1
        si = qb
        c0 = si * 8
        o1 = tv(par, 0); o2 = tv(par, 1); obf = tv(par, 2)
        ep = [bEp[par]]
        P.op("dve", lambda e: e.reciprocal(out=stats[:, c0:c0 + 1], in_=o1[:, 128:129]), reads=ep, writes=[bSt[si]])
        P.op("dve", lambda e: e.reciprocal(out=stats[:, c0 + 1:c0 + 2], in_=o2[:, 128:129]), reads=ep, writes=[bSt[si]])
        P.op("dve", lambda e: e.tensor_scalar(out=stats[:, c0 + 2:c0 + 3], in0=stats[:, c0 + 1:c0 + 2], scalar1=neglam, scalar2=None, op0=ALU.mult),
             reads=[bSt[si], bC5], writes=[bSt[si]])
        P.op("dve", lambda e: e.tensor_scalar(out=o2[:, 0:128], in0=o2[:, 0:128], scalar1=stats[:, c0 + 2:c0 + 3], scalar2=None, op0=ALU.mult),
             reads=ep + [bSt[si]], writes=ep)
        P.op("dve", lambda e: e.scalar_tensor_tensor(out=o1[:, 0:128], in0=o1[:, 0:128], scalar=stats[:, c0:c0 + 1], in1=o2[:, 0:128], op0=ALU.mult, op1=ALU.add),
             reads=ep + [bSt[si]], writes=ep)
        P.op("dve", lambda e: e.tensor_tensor(out=o2[:, 0:128], in0=o1[:, 0:128], in1=o1[:, 0:128], op=ALU.mult), reads=ep, writes=ep)
        P.op("dve", lambda e: e.tensor_reduce(out=stats[:, c0 + 3:c0 + 4], in_=o2[:, 0:128], axis=AX.X, op=ALU.add), reads=ep, writes=[bSt[si]])
        P.op("dve", lambda e: e.tensor_scalar(out=stats[:, c0 + 4:c0 + 5], in0=stats[:, c0 + 3:c0 + 4], scalar1=1.0 / 128, scalar2=EPS, op0=ALU.mult, op1=ALU.add),
             reads=[bSt[si]], writes=[bSt[si]])
        P.op("pool", lambda e: e.tensor_tensor(out=stats[:, c0 + 5:c0 + 6], in0=stats[:, c0 + 4:c0 + 5], in1=cst[:, 0:1], op=ALU.pow),
             reads=[bSt[si], bCst], writes=[bSt[si]])
        P.op("dve", lambda e: e.scalar_tensor_tensor(out=obf, in0=o1[:, 0:128], scalar=stats[:, c0 + 5:c0 + 6], in1=subg[:], op0=ALU.mult, op1=ALU.mult),
             reads=ep + [bSt[si], bC6], writes=ep)
        tb = psb[6 + par % 2].bitcast(BF16)

        def fin():
            P.op("pe", lambda e: e.transpose(tb[:, 0:128], obf, identb), reads=[bEp[par], bConst], writes=[bTp[par % 2]])
            P.op("dve", lambda e: e.tensor_copy(out=attnT[:, h, qb * 128:(qb + 1) * 128], in_=tb[:, 0:128]), reads=[bTp[par % 2]], writes=[bAT[qb]], accum=True)
        late.append(fin)

    pending = []
    for ui, (h, c) in enumerate(units):
        nb = 2 * c + 2
        slots = {}
        per = (len(pending) + nb - 1) // nb if pending else 0
        late_now = list(late)
        del late[:]
        for kb in range(nb):
            if kb == 1:
                for f_ in late_now:
                    f_()
            sl = blk_ctr[0] % NSLOT
            blk_ctr[0] += 1
            slots[kb] = sl
            sp_ = kb % 2
            lo = 128 if kb == 2 * c + 1 else 0
            sb3 = pall[:, sp_ * 512:sp_ * 512 + 2048].rearrange("p (m r) -> p m r", m=2)
            for m in range(2):
                P.op("pe", lambda e, m=m, kb=kb, lo=lo, sp_=sp_, h=h, c=c: e.matmul(
                    psb[2 * m + sp_][:, lo:256], lhsT=QK[m * 64:(m + 1) * 64, 4 + h, kb * 128:(kb + 1) * 128],
                    rhs=QK[m * 64:(m + 1) * 64, h, c * 256 + lo:(c + 1) * 256], start=True, stop=True),
                    reads=[bQK[4 + h], bQK[h]], writes=[bSS[sp_]])
            P.op("act", lambda e, sl=sl, lo=lo, sb3=sb3: e.activation(out=Er[sl][:, :, lo:256], in_=sb3[:, :, lo:256], func=AF.Exp),
                 reads=[bSS[sp_]], writes=[bE[sl]])
            for j in range(2):
                d = 2 * c + j - kb
                if 0 <= d <= 1:
                    for m in range(2):
                        P.op("dve", lambda e, sl=sl, m=m, j=j, d=d, h=h: e.tensor_tensor(
                            out=Er[sl][:, m, j * 128:(j + 1) * 128], in0=Er[sl][:, m, j * 128:(j + 1) * 128], in1=ebv[:, h, d, :], op=ALU.mult),
                            reads=[bE[sl], bC3], writes=[bE[sl]])
            for _ in range(per):
                if pending:
                    emit_av(*pending.pop(0))
        while pending:
            emit_av(*pending.pop(0))
        pending = [(h, c, slots, it) for it in av_items(h, c, slots)]
    while pending:
        emit_av(*pending.pop(0))
    for f_ in late:
        f_()

    if stop == "C1":
        dbg["E0"] = (Er[0], [128, 2, 256], BF16)
        dbg["E1"] = (Er[1], [128, 2, 256], BF16)
        dbg["ebfix"] = (ebfix[:], [128, 1024], F32)
        dbg["lams"] = (lams[:], [128, 8], F32)
        dbg["o1s"] = (tv(0, 0), [128, 130], F32)
        dbg["obf"] = (tv(0, 2), [128, 128], BF16)
        dbg["at0"] = (attnT[:, 0, 0:256], [128, 256], BF16)
        dbg["stats"] = (stats[:], [128, 128], F32)
        return finish(nc, P, st, dbg)
    if stop == "C":
        dbg["attnT"] = (attnT, [128, 8, S], BF16)
        return finish(nc, P, st, dbg)

    project(1536, "sb")
    P.barrier()
    Wr = [view(OFF_W + i * 512, [128, 256], BF16) for i in range(4)]
    bWr = [B("Wr%d" % i) for i in range(4)]
    e32 = [view(OFF_W + 2048 + i * 1024, [128, 256], F32) for i in range(2)]
    Lb = [view(OFF_W + 4096 + i * 512, [128, 256], BF16) for i in range(2)]
    Rb = [view(OFF_W + 5120 + i * 512, [128, 256], BF16) for i in range(3)]
    be32 = [B("e32%d" % i) for i in range(2)]
    bL = [B("L%d" % i) for i in range(2)]
    bR = [B("R%d" % i) for i in range(3)]
    bZ = [B("Z%d" % i) for i in range(2)]
    bX = [B("X%d" % i) for i in range(2)]
    bOT = [B("OT%d" % i) for i in range(2)]

    blocks = []
    for hd in range(8):
        for c in range(8):
            for kb in range(2 * c + 1, -1, -1):
                blocks.append((hd, c, kb))
    NB = len(blocks)

    def sb_pe1(i):
        hd, c, kb = blocks[i]
        g, po = hd // 2, (hd % 2) * 64
        lo = 128 if kb == 2 * c + 1 else 0
        pz = i % 2
        P.op("pe", lambda e: e.matmul(psb[pz][:, lo:256], lhsT=QK[po:po + 64, 4 + g, kb * 128:(kb + 1) * 128],
                                      rhs=QK[po:po + 64, g, c * 256 + lo:(c + 1) * 256], start=True, stop=True),
             reads=[bQK[4 + g], bQK[g]], writes=[bZ[pz]])

    def sb_act1(i):
        hd, c, kb = blocks[i]
        lo = 128 if kb == 2 * c + 1 else 0
        pz = i % 2
        first = kb == 2 * c + 1
        P.op("act", lambda e: e.activation(out=e32[pz][:, lo:256], in_=psb[pz][:, lo:256], func=AF.Exp), reads=[bZ[pz]], writes=[be32[pz]])
        if first:
            P.op("dve", lambda e: e.memset(e32[pz][:, 0:128], 0.0), writes=[be32[pz]], accum=True)
        j = kb - 2 * c
        if j >= 0:
            P.op("dve", lambda e: e.tensor_tensor(out=e32[pz][:, j * 128:(j + 1) * 128], in0=e32[pz][:, j * 128:(j + 1) * 128], in1=strictm, op=ALU.mult),
                 reads=[be32[pz], bC2], writes=[be32[pz]])
        P.op("act", lambda e: e.activation(out=Lb[pz][:], in_=e32[pz][:], func=AF.Ln, bias=1.0), reads=[be32[pz]], writes=[bL[pz]])
        if kb > 0:
            if first:
                P.op("dve", lambda e: e.tensor_copy(out=Rb[(i + 1) % 3][:], in_=Lb[pz][:]), reads=[bL[pz]], writes=[bR[(i + 1) % 3]])
            else:
                P.op("dve", lambda e: e.tensor_tensor(out=Rb[(i + 1) % 3][:], in0=Rb[i % 3][:], in1=Lb[pz][:], op=ALU.add),
                     reads=[bL[pz], bR[i % 3]], writes=[bR[(i + 1) % 3]])

    def sb_pe2(i):
        hd, c, kb = blocks[i]
        g, po = hd // 2, (hd % 2) * 64
        lo = 128 if kb == 2 * c + 1 else 0
        pz = i % 2
        first = kb == 2 * c + 1
        xb = psb[2 + pz]
        P.op("pe", lambda e: e.matmul(xb[:, lo:256], lhsT=QK[po:po + 64, 4 + g, kb * 128:(kb + 1) * 128],
                                      rhs=QK[po:po + 64, g, c * 256 + lo:(c + 1) * 256], start=True, stop=False),
             reads=[bQK[4 + g], bQK[g]], writes=[bX[pz]])
        P.op("pe", lambda e: e.matmul(xb[:, lo:256], lhsT=negtri, rhs=Lb[pz][:, lo:256], start=False, stop=first),
             reads=[bL[pz], bConst], writes=[bX[pz]])
        if not first:
            P.op("pe", lambda e: e.matmul(xb[:, lo:256], lhsT=negones, rhs=Rb[i % 3][:, lo:256], start=False, stop=True),
                 reads=[bR[i % 3], bConst], writes=[bX[pz]])

    def sb_act2(i):
        hd, c, kb = blocks[i]
        lo = 128 if kb == 2 * c + 1 else 0
        pz = i % 2
        wi = i % 4
        first = kb == 2 * c + 1
        P.op("act", lambda e: e.activation(out=Wr[wi][:, lo:256], in_=psb[2 + pz][:, lo:256], func=AF.Exp), reads=[bX[pz]], writes=[bWr[wi]])
        if first:
            P.op("dve", lambda e: e.memset(Wr[wi][:, 0:128], 0.0), writes=[bWr[wi]], accum=True)
        j = kb - 2 * c
        if j >= 0:
            P.op("dve", lambda e: e.tensor_tensor(out=Wr[wi][:, j * 128:(j + 1) * 128], in0=Wr[wi][:, j * 128:(j + 1) * 128], in1=strictm, op=ALU.mult),
                 reads=[bWr[wi], bC2], writes=[bWr[wi]])

    unit_ctr = [0]

    def sb_pe3(i):
        hd, c, kb = blocks[i]
        g, po = hd // 2, (hd % 2) * 64
        wi = i % 4
        first = kb == 2 * c + 1
        last = kb == 0
        up = (hd * 8 + c) % 2
        ob = psb[4 + up]
        P.op("pe", lambda e: e.matmul(ob[po:po + 64, 0:256], lhsT=VV[:, kb, hd * 64:(hd + 1) * 64], rhs=Wr[wi][:], start=first, stop=last),
             reads=[bWr[wi], bV[kb]], writes=[bOT[up]])
        if last:
            if (hd * 8 + c) % 2 == 0:
                P.op("act", lambda e: e.activation(out=attnT[po:po + 64, 4 + g, c * 256:(c + 1) * 256], in_=ob[po:po + 64, 0:256], func=AF.Copy),
                     reads=[bOT[up]], writes=[bAT[2 * c], bAT[2 * c + 1]], accum=True)
            else:
                P.op("dve", lambda e: e.tensor_copy(out=attnT[po:po + 64, 4 + g, c * 256:(c + 1) * 256], in_=ob[po:po + 64, 0:256]),
                     reads=[bOT[up]], writes=[bAT[2 * c], bAT[2 * c + 1]], accum=True)

    for s_ in range(NB + 2):
        if s_ < NB:
            sb_pe1(s_)
            sb_act1(s_)
        if 0 <= s_ - 1 < NB:
            sb_pe2(s_ - 1)
            sb_act2(s_ - 1)
        if 0 <= s_ - 2 < NB:
            sb_pe3(s_ - 2)

    if stop == "D":
        dbg["attnT"] = (attnT, [128, 8, S], BF16)
        return finish(nc, P, st, dbg)

    P.barrier()
    KB = 1024
    hres = view(0, [128, NT, D], F32)
    tT = view(96 * KB, [128, 8, S], BF16)
    wob = view(128 * KB, [128, 8, D], BF16)
    xt1 = view(144 * KB, [128, D], F32)
    tb1 = view(148 * KB, [128, D], BF16)
    junk2 = view(150 * KB, [128, D], BF16)
    tmpx = sbt("tmpx", [128, 4 * 512], F32)
    comb = sbt("comb", [128, NT * 32], F32)
    lgs = sbt("lgs", [128, 4 * 32], F32)
    mx8 = sbt("mx8", [128, 32], F32)
    ix8 = sbt("ix8", [128, 8], U32)
    mbt = sbt("mbt", [128, 96], BF16)
    bIx = B("ix8"); bMb = B("mskb"); bMacc = [B("macc0"), B("macc1")]; bXg = B("xg"); bH1d = B("h1d"); bMeta = B("meta")
    rwb = sbt("rwb", [128, 256], BF16)
    bH = [B("h%d" % i_) for i_ in range(NT)]
    bTT = [B("tT%d" % i_) for i_ in range(NT)]
    bWo = B("wo"); bX1 = B("x1"); bTb = B("tb1"); bJ2 = B("junk2"); bRwb = B("rwb"); bLg = B("lg"); bMx = B("mx"); bComb = B("comb")
    bPE = [B("pe%d" % i_) for i_ in range(8)]
    P.dma("pool", lambda e: e.dma_start(out=wob, in_=wout_d), writes=[bWo])
    P.dma("pool", lambda e: e.dma_start(out=rwb[:], in_=rw_d), writes=[bRwb])
    P.dma("sp", lambda e: e.dma_start(out=gainb[:], in_=gains_d[1].partition_broadcast(128)), writes=[bGain])

    def rms2(i_, src, srcbuf, n):
        c0 = i_ * 8
        P.op("act", lambda e: e.activation(out=junk2[:, 0:n], in_=src, func=AF.Square, accum_out=stats[:, c0:c0 + 1]), reads=[srcbuf], writes=[bJ2, bSt[i_]])
        P.op("dve", lambda e: e.tensor_scalar(out=stats[:, c0 + 1:c0 + 2], in0=stats[:, c0:c0 + 1], scalar1=1.0 / n, scalar2=EPS, op0=ALU.mult, op1=ALU.add), reads=[bSt[i_]], writes=[bSt[i_]])
        P.op("pool", lambda e: e.tensor_tensor(out=stats[:, c0 + 3:c0 + 4], in0=stats[:, c0 + 1:c0 + 2], in1=cst[:, 0:1], op=ALU.pow), reads=[bSt[i_], bCst], writes=[bSt[i_]])
        return stats[:, c0 + 3:c0 + 4]

    def phaseE(i_):
        P.dma("sp", lambda e: e.dma_start(out=xt1, in_=x_d[i_ * 128:(i_ + 1) * 128, :]), writes=[bX1])
        for half in range(2):
            bk = 2 * (i_ % 2) + half
            for c_ in range(8):
                P.op("pe", lambda e, c_=c_, bk=bk, half=half: e.matmul(psb[bk], lhsT=attnT[:, c_, i_ * 128:(i_ + 1) * 128], rhs=wob[:, c_, half * 512:(half + 1) * 512], start=(c_ == 0), stop=(c_ == 7)),
                     reads=[bAT[i_], bWo], writes=[bPE[bk]])
            P.op("dve", lambda e, bk=bk, half=half: e.tensor_tensor(out=hres[:, i_, half * 512:(half + 1) * 512], in0=psb[bk], in1=xt1[:, half * 512:(half + 1) * 512], op=ALU.add),
                 reads=[bPE[bk], bX1], writes=[bH[i_]], accum=(half == 1))
        rstd = rms2(i_, hres[:, i_, :], bH[i_], D)
        P.op("dve", lambda e: e.scalar_tensor_tensor(out=tb1, in0=hres[:, i_, :], scalar=rstd, in1=gainb[:], op0=ALU.mult, op1=ALU.mult), reads=[bH[i_], bSt[i_], bGain], writes=[bTb])
        pT = psb[4 + i_ % 2].bitcast(BF16)
        for c_ in range(8):
            P.op("pe", lambda e, c_=c_: e.transpose(pT[:, c_ * 128:(c_ + 1) * 128], tb1[:, c_ * 128:(c_ + 1) * 128], identb), reads=[bTb, bConst], writes=[bPE[4 + i_ % 2]])
        P.op("act", lambda e: e.activation(out=tT[:, :, i_ * 128:(i_ + 1) * 128], in_=pT.rearrange("p (c t) -> p c t", t=128), func=AF.Copy), reads=[bPE[4 + i_ % 2]], writes=[bTT[i_]])
        for c_ in range(8):
            P.op("pe", lambda e, c_=c_: e.matmul(psb[6][:, 0:32], lhsT=tT[:, c_, i_ * 128:(i_ + 1) * 128], rhs=rwb[:, c_ * 32:(c_ + 1) * 32], start=(c_ == 0), stop=(c_ == 7)),
                 reads=[bTT[i_], bRwb], writes=[bPE[6]])
        lg = lgs[:, 0:32]; exl = lgs[:, 32:64]; msk = lgs[:, 64:96]
        P.op("dve", lambda e: e.tensor_tensor(out=lg, in0=psb[6][:, 0:32], in1=rbb[:], op=ALU.add), reads=[bPE[6], bC8], writes=[bLg])
        P.op("dve", lambda e: e.max(out=mx8[:, 0:8], in_=lg), reads=[bLg], writes=[bMx])
        P.op("dve", lambda e: e.tensor_scalar(out=mx8[:, 8:9], in0=mx8[:, 0:1], scalar1=-1.0, scalar2=None, op0=ALU.mult), reads=[bMx], writes=[bMx])
        P.op("act", lambda e: e.activation(out=exl, in_=lg, func=AF.Exp, bias=mx8[:, 8:9]), reads=[bLg, bMx], writes=[bLg])
        P.op("dve", lambda e: e.tensor_scalar(out=msk, in0=lg, scalar1=mx8[:, 3:4], scalar2=None, op0=ALU.is_ge), reads=[bLg, bMx], writes=[bLg])
        P.op("dve", lambda e: e.tensor_tensor(out=exl, in0=exl, in1=msk, op=ALU.mult), reads=[bLg], writes=[bLg])
        P.op("dve", lambda e: e.tensor_reduce(out=mx8[:, 9:10], in_=exl, axis=AX.X, op=ALU.add), reads=[bLg], writes=[bMx])
        P.op("dve", lambda e: e.reciprocal(out=mx8[:, 10:11], in_=mx8[:, 9:10]), reads=[bMx], writes=[bMx])
        P.op("act", lambda e: e.activation(out=mx8[:, 16:20], in_=mx8[:, 0:4], func=AF.Exp, bias=mx8[:, 8:9]), reads=[bMx], writes=[bMx])
        P.op("dve", lambda e: e.tensor_scalar(out=meta_g[:, i_ * 4:(i_ + 1) * 4], in0=mx8[:, 16:20], scalar1=mx8[:, 10:11], scalar2=None, op0=ALU.mult), reads=[bMx], writes=[bMeta], accum=True)
        P.op("dve", lambda e: e.max_index(out=ix8[:], in_max=mx8[:, 0:8], in_values=lg), reads=[bLg, bMx], writes=[bIx])
        P.op("dve", lambda e: e.tensor_copy(out=mbt[:, 0:32], in_=msk), reads=[bLg], writes=[bMb])
        pfx = psb[7][:, 0:32]
        a_ = i_ % 2
        P.op("pe", lambda e: e.matmul(pfx, lhsT=ltm, rhs=mbt[:, 0:32], start=True, stop=(i_ == 0)), reads=[bMb, bConst], writes=[bPE[7]])
        if i_ > 0:
            P.op("pe", lambda e: e.matmul(pfx, lhsT=onesb, rhs=mbt[:, 32 + a_ * 32:64 + a_ * 32], start=False, stop=True), reads=[bMacc[a_], bConst], writes=[bPE[7]])
        if i_ == 0:
            P.op("dve", lambda e: e.tensor_copy(out=mbt[:, 64:96], in_=mbt[:, 0:32]), reads=[bMb], writes=[bMacc[1]])
        else:
            P.op("dve", lambda e: e.tensor_tensor(out=mbt[:, 32 + (1 - a_) * 32:64 + (1 - a_) * 32], in0=mbt[:, 32 + a_ * 32:64 + a_ * 32], in1=mbt[:, 0:32], op=ALU.add),
                 reads=[bMb, bMacc[a_]], writes=[bMacc[1 - a_]])
        oh = lgs[:, 96:128]
        for k_ in range(4):
            P.op("dve", lambda e, k_=k_: e.tensor_scalar(out=oh, in0=lg, scalar1=mx8[:, k_:k_ + 1], scalar2=None, op0=ALU.is_equal), reads=[bLg, bMx], writes=[bLg])
            P.op("dve", lambda e: e.tensor_tensor(out=oh, in0=oh, in1=pfx, op=ALU.mult), reads=[bLg, bPE[7]], writes=[bLg])
            P.op("dve", lambda e, k_=k_: e.tensor_reduce(out=mx8[:, 20 + k_:21 + k_], in_=oh, axis=AX.X, op=ALU.add), reads=[bLg], writes=[bMx])
        P.op("dve", lambda e: e.tensor_copy(out=mx8[:, 24:28], in_=ix8[:, 0:4]), reads=[bIx], writes=[bMx])
        P.op("dve", lambda e: e.scalar_tensor_tensor(out=mx8[:, 28:32], in0=mx8[:, 24:28], scalar=float(CAP), in1=mx8[:, 20:24], op0=ALU.mult, op1=ALU.add), reads=[bMx], writes=[bMx])
        P.op("dve", lambda e: e.tensor_copy(out=meta_dest[:, i_ * 4:(i_ + 1) * 4], in_=mx8[:, 28:32]), reads=[bMx], writes=[bMeta], accum=True)
        for k_ in range(4):
            P.dma("pool", lambda e, k_=k_: e.indirect_dma_start(out=xg_d, out_offset=bass.IndirectOffsetOnAxis(ap=meta_dest[:, i_ * 4 + k_:i_ * 4 + k_ + 1], axis=0),
                                                               in_=tb1, in_offset=None), reads=[bTb, bMeta], writes=[bXg])
        P.dma("sp", lambda e: e.dma_start(out=h1_d[i_ * 128:(i_ + 1) * 128, :], in_=hres[:, i_, :]), reads=[bH[i_]], writes=[bH1d])

    for i_ in range(NT):
        phaseE(i_)
    cnt_i = sbt("cnt_i", [128, 32], I32)
    bCnt = B("cnt")
    P.op("pe", lambda e: e.matmul(psb[7][:, 0:32], lhsT=onesb, rhs=mbt[:, 32:64], start=True, stop=True), reads=[bMacc[0], bConst], writes=[bPE[7]])
    P.op("dve", lambda e: e.tensor_copy(out=cnt_i[:], in_=psb[7][:, 0:32]), reads=[bPE[7]], writes=[bCnt])
    P.cnt_ap = lambda ex_: cnt_i[0:1, ex_:ex_ + 1]

    if stop == "E":
        dbg["h1"] = (hres, [128, NT, D], F32)
        dbg["tT"] = (tT, [128, 8, S], BF16)
        dbg["mdest"] = (meta_dest[:], [128, NT * 4], I32)
        dbg["mg"] = (meta_g[:], [128, NT * 4], F32)
        dbg["xg0"] = (xg_d[0:512, :], [512, D], BF16)
        return finish(nc, P, st, dbg)

    P.barrier()
    NTS = RC // 128
    xe = [view(0 + k_ * 6 * KB, [128, NTS, D], BF16) for k_ in range(2)]
    XeT = [view(12 * KB + k_ * 6 * KB, [128, 8, RC], BF16) for k_ in range(2)]
    actT = [view(24 * KB + k_ * 6 * KB, [128, 8, RC], BF16) for k_ in range(2)]
    wdb = [view(36 * KB + k_ * 16 * KB, [128, 8, D], BF16) for k_ in range(2)]
    wg = [view(68 * KB + k_ * 4 * KB, [128, 8, 256], BF16) for k_ in range(3)]
    yt = [view(80 * KB + k_ * 4 * KB, [128, D], F32) for k_ in range(2)]
    bdt = [view(88 * KB + k_ * 4 * KB, [128, D], F32) for k_ in range(2)]
    bXe = [B("xe%d" % k_) for k_ in range(2)]; bXT = [B("XeT%d" % k_) for k_ in range(2)]; bAc = [B("ac%d" % k_) for k_ in range(2)]
    bWd = [B("wd%d" % k_) for k_ in range(2)]; bWg = [B("wg%d" % k_) for k_ in range(3)]; bYt = [B("yt%d" % k_) for k_ in range(2)]; bBd = [B("bd%d" % k_) for k_ in range(2)]
    bYg = B("yg")
    bTgs = [B("tg%d" % k_) for k_ in range(2)]; bTss = [B("ts%d" % k_) for k_ in range(2)]; bTls = [B("tl%d" % k_) for k_ in range(2)]
    tgs = [view(100 * KB + k_ * 1536, [128, RC], F32) for k_ in range(2)]
    tss = [view(104 * KB + k_ * 1536, [128, RC], F32) for k_ in range(2)]
    tls = [view(108 * KB + k_ * 1536, [128, RC], F32) for k_ in range(2)]
    cnt = [0]; slabc = [0]; ytc = [0]; qc = [0]

    def gu_chunk(e_, j_, k_, q_):
        ba = 2 * (cnt[0] % 2)
        tg = tgs[cnt[0] % 2]; ts = tss[cnt[0] % 2]; tl = tls[cnt[0] % 2]
        bTg = bTgs[cnt[0] % 2]; bTs = bTss[cnt[0] % 2]; bTl = bTls[cnt[0] % 2]
        cnt[0] += 1
        for which in range(2):
            for c_ in range(8):
                P.op("pe", lambda e, c_=c_, which=which: e.matmul(psb[ba + which][:, 0:RC], lhsT=wg[k_][:, c_, which * 128:(which + 1) * 128], rhs=XeT[q_][:, c_, :], start=(c_ == 0), stop=(c_ == 7)),
                     reads=[bWg[k_], bXT[q_]], writes=[bPE[ba + which]])
        col = (e_ * 8 + j_) * 2
        P.op("dve", lambda e: e.tensor_scalar(out=tg, in0=psb[ba][:, 0:RC], scalar1=bgl[:, col:col + 1], scalar2=7.0, op0=ALU.add, op1=ALU.min), reads=[bPE[ba], bC9], writes=[bTg])
        P.op("act", lambda e: e.activation(out=ts, in_=tg, func=AF.Sigmoid, scale=1.702), reads=[bTg], writes=[bTs])
        P.op("dve", lambda e: e.tensor_scalar(out=tl, in0=psb[ba + 1][:, 0:RC], scalar1=bgl[:, col + 1:col + 2], scalar2=8.0, op0=ALU.add, op1=ALU.min), reads=[bPE[ba + 1], bC9], writes=[bTl])
        P.op("dve", lambda e: e.tensor_tensor(out=ts, in0=tg, in1=ts, op=ALU.mult), reads=[bTg, bTs], writes=[bTs])
        P.op("dve", lambda e: e.scalar_tensor_tensor(out=actT[q_][:, j_, :], in0=tl, scalar=-6.0, in1=ts, op0=ALU.max, op1=ALU.mult), reads=[bTl, bTs], writes=[bAc[q_]], accum=True)

    def down_tile(e_, q_, st_, half, yk):
        bk = 4 + cnt[0] % 2
        cnt[0] += 1
        for j_ in range(8):
            P.op("pe", lambda e, j_=j_: e.matmul(psb[bk], lhsT=actT[q_][:, j_, st_ * 128:(st_ + 1) * 128], rhs=wdb[q_][:, j_, half * 512:(half + 1) * 512], start=(j_ == 0), stop=(j_ == 7)),
                 reads=[bAc[q_], bWd[q_]], writes=[bPE[bk]])
        P.op("dve", lambda e: e.tensor_tensor(out=yt[yk][:, half * 512:(half + 1) * 512], in0=psb[bk], in1=bdt[q_][:, half * 512:(half + 1) * 512], op=ALU.add),
             reads=[bPE[bk], bBd[q_]], writes=[bYt[yk]], accum=(half == 1))

    def expert_chunk(e_, ch):
        q_ = qc[0] % 2
        qc[0] += 1
        row0 = e_ * CAP + min(ch * RC, CAP - RC)
        P.dma("sp", lambda e: e.dma_start(out=xe[q_], in_=xg_d[row0:row0 + RC, :].rearrange("(t p) d -> p t d", p=128)), reads=[bXg], writes=[bXe[q_]])
        P.dma("sp", lambda e: e.dma_start(out=bdt[q_], in_=bd_d[e_].partition_broadcast(128)), writes=[bBd[q_]])
        P.dma("pool", lambda e: e.dma_start(out=wdb[q_], in_=wd_d[e_]), writes=[bWd[q_]])
        for t_ in range(NTS):
            pT = psb[6 + t_ % 2].bitcast(BF16)
            for c_ in range(8):
                P.op("pe", lambda e, c_=c_, t_=t_, pT=pT: e.transpose(pT[:, c_ * 128:(c_ + 1) * 128], xe[q_][:, t_, c_ * 128:(c_ + 1) * 128], identb),
                     reads=[bXe[q_], bConst], writes=[bPE[6 + t_ % 2]])
            if t_ % 2 == 0:
                P.op("act", lambda e, t_=t_, pT=pT: e.activation(out=XeT[q_][:, :, t_ * 128:(t_ + 1) * 128], in_=pT.rearrange("p (c t) -> p c t", t=128), func=AF.Copy),
                     reads=[bPE[6 + t_ % 2]], writes=[bXT[q_]], accum=(t_ > 0))
            else:
                P.op("dve", lambda e, t_=t_, pT=pT: e.tensor_copy(out=XeT[q_][:, :, t_ * 128:(t_ + 1) * 128], in_=pT.rearrange("p (c t) -> p c t", t=128)),
                     reads=[bPE[6 + t_ % 2]], writes=[bXT[q_]], accum=True)
        for j_ in range(8):
            k_ = slabc[0] % 3
            slabc[0] += 1
            P.dma("pool", lambda e, j_=j_, k_=k_: e.dma_start(out=wg[k_], in_=wgu_d[e_, j_]), writes=[bWg[k_]])
            gu_chunk(e_, j_, k_, q_)
        for st_ in range(NTS):
            yk = ytc[0] % 2
            ytc[0] += 1
            for half in range(2):
                down_tile(e_, q_, st_, half, yk)
            P.dma("sp", lambda e, st_=st_, yk=yk: e.dma_start(out=yg_d[row0 + st_ * 128:row0 + (st_ + 1) * 128, :], in_=yt[yk]), reads=[bYt[yk]], writes=[bYg], sembuf=bYg)

    NEX = int(os.environ.get("KNEX", NE))
    NCHX = int(os.environ.get("KNCH", NCH))
    for e_ in range(NEX):
        expert_chunk(e_, 0)
        for ch in range(1, NCHX):
            P.guard_begin((e_, ch * RC))
            expert_chunk(e_, ch)
            P.guard_end()

    if stop == "G":
        dbg["cnt"] = (cnt_i[:], [128, 32], I32)
        return finish(nc, P, st, dbg)

    P.barrier()
    pgb = view(128 * KB, [128, 8, D], BF16)
    ppb = view(144 * KB, [128, 2, D], BF16)
    ptile = view(148 * KB, [128, 256], F32)
    pbf = view(149 * KB, [128, 256], BF16)
    pTs = view(149 * KB + 512, [128, 2, 128], BF16)
    pe32 = view(64 * KB, [128, D], F32)
    gate = view(68 * KB, [128, D], F32)
    hbf = view(72 * KB, [128, D], BF16)
    hT = view(74 * KB, [128, 8, 128], BF16)
    otile = [view(76 * KB + k_ * 4096, [128, D], F32) for k_ in range(2)]
    gfin = ebfix
    bPg = B("pg"); bPp = B("pp"); bPt = B("pt"); bPbf = B("pbf"); bPTs = B("pTs"); bPe32 = B("pe32"); bGate = B("gate"); bHbf = B("hbf"); bHT = B("hT")
    bOt = [B("ot%d" % k_) for k_ in range(2)]; bGf = B("gfin"); bOut = B("out")
    P.dma("pool", lambda e: e.dma_start(out=pgb, in_=pgate_d), writes=[bPg])
    P.dma("pool", lambda e: e.dma_start(out=ppb, in_=pproj_d), writes=[bPp])
    P.dma("sp", lambda e: e.dma_start(out=gainb[:], in_=gains_d[2].partition_broadcast(128)), writes=[bGain])
    P.dma("sp", lambda e: e.dma_start(out=gfin[:], in_=gains_d[3].partition_broadcast(128)), writes=[bGf])

    htl = [view(84 * KB + k_ * 4 * KB, [128, D], F32) for k_ in range(2)]
    ygk = [view(92 * KB + k_ * 4 * KB, [128, D], F32) for k_ in range(8)]
    bHt = [B("ht%d" % k_) for k_ in range(2)]
    bYk = [B("ygk%d" % k_) for k_ in range(8)]

    def phaseH(i_):
        k_ = i_ % 2
        hcur = htl[k_]
        P.dma("sp", lambda e: e.dma_start(out=hcur, in_=h1_d[i_ * 128:(i_ + 1) * 128, :]), reads=[bH1d], writes=[bHt[k_]])
        for kk in range(4):
            yb = k_ * 4 + kk
            P.dma("pool", lambda e, kk=kk, yb=yb: e.indirect_dma_start(out=ygk[yb], out_offset=None, in_=yg_d,
                                                                        in_offset=bass.IndirectOffsetOnAxis(ap=meta_dest[:, i_ * 4 + kk:i_ * 4 + kk + 1], axis=0)),
                  reads=[bYg, bMeta], writes=[bYk[yb]])
            P.op("dve", lambda e, kk=kk, yb=yb: e.scalar_tensor_tensor(out=hcur, in0=ygk[yb], scalar=meta_g[:, i_ * 4 + kk:i_ * 4 + kk + 1], in1=hcur, op0=ALU.mult, op1=ALU.add),
                 reads=[bYk[yb], bMeta, bHt[k_]], writes=[bHt[k_]])
        P.dma("sp", lambda e: e.dma_start(out=ptile, in_=p_d[i_ * 128:(i_ + 1) * 128, :]), writes=[bPt])
        P.op("dve", lambda e: e.tensor_copy(out=pbf, in_=ptile), reads=[bPt], writes=[bPbf])
        tq = psb[6].bitcast(BF16)
        for c_ in range(2):
            P.op("pe", lambda e, c_=c_: e.transpose(tq[:, c_ * 128:(c_ + 1) * 128], pbf[:, c_ * 128:(c_ + 1) * 128], identb), reads=[bPbf, bConst], writes=[bPE[6]])
        P.op("act", lambda e: e.activation(out=pTs, in_=tq[:, 0:256].rearrange("p (c t) -> p c t", t=128), func=AF.Copy), reads=[bPE[6]], writes=[bPTs])
        for half in range(2):
            for c_ in range(2):
                P.op("pe", lambda e, c_=c_, half=half: e.matmul(psb[half], lhsT=pTs[:, c_, :], rhs=ppb[:, c_, half * 512:(half + 1) * 512], start=(c_ == 0), stop=(c_ == 1)),
                     reads=[bPTs, bPp], writes=[bPE[half]])
            P.op("act", lambda e, half=half: e.activation(out=pe32[:, half * 512:(half + 1) * 512], in_=psb[half], func=AF.Copy), reads=[bPE[half]], writes=[bPe32], accum=(half == 1))
        rp = rms2(i_, pe32, bPe32, D)
        P.op("dve", lambda e: e.scalar_tensor_tensor(out=pe32, in0=pe32, scalar=rp, in1=gainb[:], op0=ALU.mult, op1=ALU.mult), reads=[bPe32, bSt[i_], bGain], writes=[bPe32])
        P.op("dve", lambda e: e.tensor_copy(out=hbf, in_=hcur), reads=[bHt[k_]], writes=[bHbf])
        tq2 = psb[7].bitcast(BF16)
        for c_ in range(8):
            P.op("pe", lambda e, c_=c_: e.transpose(tq2[:, c_ * 128:(c_ + 1) * 128], hbf[:, c_ * 128:(c_ + 1) * 128], identb), reads=[bHbf, bConst], writes=[bPE[7]])
        P.op("act", lambda e: e.activation(out=hT, in_=tq2.rearrange("p (c t) -> p c t", t=128), func=AF.Copy), reads=[bPE[7]], writes=[bHT])
        for half in range(2):
            for c_ in range(8):
                P.op("pe", lambda e, c_=c_, half=half: e.matmul(psb[2 + half], lhsT=hT[:, c_, :], rhs=pgb[:, c_, half * 512:(half + 1) * 512], start=(c_ == 0), stop=(c_ == 7)),
                     reads=[bHT, bPg], writes=[bPE[2 + half]])
            P.op("act", lambda e, half=half: e.activation(out=gate[:, half * 512:(half + 1) * 512], in_=psb[2 + half], func=AF.Sigmoid), reads=[bPE[2 + half]], writes=[bGate], accum=(half == 1))
        P.op("dve", lambda e: e.tensor_tensor(out=gate, in0=gate, in1=pe32, op=ALU.mult), reads=[bGate, bPe32], writes=[bGate])
        P.op("dve", lambda e: e.tensor_tensor(out=hcur, in0=hcur, in1=gate, op=ALU.add), reads=[bHt[k_], bGate], writes=[bHt[k_]])
        c0 = i_ * 8 + 4
        P.op("act", lambda e: e.activation(out=junk2, in_=hcur, func=AF.Square, accum_out=stats[:, c0:c0 + 1]), reads=[bHt[k_]], writes=[bJ2, bSt[i_]])
        P.op("dve", lambda e: e.tensor_scalar(out=stats[:, c0 + 1:c0 + 2], in0=stats[:, c0:c0 + 1], scalar1=1.0 / D, scalar2=EPS, op0=ALU.mult, op1=ALU.add), reads=[bSt[i_]], writes=[bSt[i_]])
        P.op("pool", lambda e: e.tensor_tensor(out=stats[:, c0 + 2:c0 + 3], in0=stats[:, c0 + 1:c0 + 2], in1=cst[:, 0:1], op=ALU.pow), reads=[bSt[i_], bCst], writes=[bSt[i_]])
        P.op("dve", lambda e: e.scalar_tensor_tensor(out=otile[k_], in0=hcur, scalar=stats[:, c0 + 2:c0 + 3], in1=gfin[:], op0=ALU.mult, op1=ALU.mult), reads=[bHt[k_], bSt[i_], bGf], writes=[bOt[k_]])
        P.dma("sp", lambda e: e.dma_start(out=out_d[i_ * 128:(i_ + 1) * 128, :], in_=otile[k_]), reads=[bOt[k_]], writes=[bOut], sembuf=bOt[k_])

    for i_ in range(NT):
        phaseH(i_)
    P.wait_all("sp", [bOut] + bOt)
    P.run(st)
    st.close()
    return nc


def finish(nc, P, st, dbg):
    P.barrier()
    outs = []
    for name, (ap, shape, dt) in dbg.items():
        d = nc.dram_tensor("dbg_" + name, shape, dt, kind="ExternalOutput").ap()
        b = Buf(P, "dbg_" + name)
        P.dma("sp", lambda e, d=d, ap=ap: e.dma_start(out=d, in_=ap), writes=[b])
        outs.append(b)
    P.wait_all("sp", outs)
    P.run(st)
    st.close()
    return nc


def _t5_bucket(rel):
    n = np.maximum(rel, 0)
    nf = np.maximum(n, 1).astype(np.float32)
    large = 16 + (np.log(nf / np.float32(16)) / np.float32(math.log(128 / 16)) * np.float32(16)).astype(np.int32)
    large = np.minimum(large, 31)
    return np.where(n < 16, n, large)


def prep_shared(inp):
    f32 = np.float32
    g = lambda k: np.asarray(inp[k], dtype=f32)
    w_in = g("w_in")[0]
    cols = []
    for h in range(4):
        cols += list(range(h * 64, h * 64 + 64)) + list(range(256 + h * 64, 256 + h * 64 + 64))
    for h in range(4):
        cols += list(range(512 + h * 64, 512 + h * 64 + 64)) + list(range(768 + h * 64, 768 + h * 64 + 64))
    cols += list(range(1024, 3072))
    w_in_p = w_in[:, cols]
    chunked = lambda w: np.ascontiguousarray(w.reshape(8, 128, -1).transpose(1, 0, 2))
    sh = {}
    sh["w_in"] = chunked(w_in_p)
    sh["w_out"] = chunked(g("w_out")[0])
    sh["gains"] = np.stack([g("attn_norm")[0], g("moe_norm")[0], g("ple_norm")[0], g("final_norm")], 0)
    sh["lamv"] = np.stack([g("lambda_q1")[0], g("lambda_k1")[0], g("lambda_q2")[0], g("lambda_k2")[0]], 0)
    sh["subln"] = g("subln")
    rb = g("rel_bias")
    k = np.arange(128)[:, None]
    q = np.arange(128)[None, :]
    bn = np.zeros((128, 4, 2, 128), f32)
    for d in range(2):
        bk = _t5_bucket(q + 128 * d - k)
        bn[:, :, d, :] = rb[bk].transpose(0, 2, 1)
    sh["bnear"] = bn.reshape(128, -1)
    sh["c31"] = np.ascontiguousarray(np.broadcast_to(rb[31][None, :], (128, 4)))
    sh["rw"] = chunked(g("router_w")[0]).reshape(128, -1)
    sh["rb"] = g("router_b")
    wgu = g("w_gate_up")[0]
    glu = wgu[:, :, 0::2].reshape(NE, 8, 128, 8, 128)
    lin = wgu[:, :, 1::2].reshape(NE, 8, 128, 8, 128)
    gl = np.concatenate([glu, lin], axis=-1)
    sh["wgu"] = np.ascontiguousarray(gl.transpose(0, 3, 2, 1, 4))
    bgu = g("b_gate_up")[0]
    bg = bgu[:, 0::2].reshape(NE, 8, 128)
    bl = bgu[:, 1::2].reshape(NE, 8, 128)
    sh["bgl"] = np.ascontiguousarray(np.stack([bg, bl], -1).transpose(2, 0, 1, 3)).reshape(128, -1)
    sh["wd"] = np.ascontiguousarray(g("w_down")[0].reshape(NE, 8, 128, D).transpose(0, 2, 1, 3))
    sh["bd"] = g("b_down")[0]
    sh["pproj"] = np.ascontiguousarray(g("ple_proj")[0].reshape(2, 128, D).transpose(1, 0, 2))
    sh["pgate"] = chunked(g("ple_gate")[0])
    ident = np.eye(128, dtype=f32)
    jj = np.arange(128)[:, None]
    kk = np.arange(128)[None, :]
    negtri = -(jj >= kk).astype(f32)
    lt = (jj < kk).astype(f32)
    sh["cbf"] = np.concatenate([ident, negtri, -np.ones((128, 128), f32), lt, np.ones((128, 128), f32)], 1).astype(ml_dtypes.bfloat16)
    sh["cf32"] = np.concatenate([ident, (kk >= jj).astype(f32), (kk > jj).astype(f32)], 1)
    return sh


_CACHE = {}


def kernel(**inputs):
    sh = prep_shared(inputs)
    x = np.asarray(inputs["x"], dtype=np.float32)
    p = np.asarray(inputs["p"], dtype=np.float32)[0]
    if "nc" not in _CACHE:
        _CACHE["nc"] = build()
    nc = _CACHE["nc"]
    in_maps = []
    for c in range(8):
        m = dict(sh)
        m["x"] = np.ascontiguousarray(x[c])
        m["p"] = np.ascontiguousarray(p[c])
        in_maps.append(m)
    res = run_bass_kernel_spmd(nc, in_maps, core_ids=list(range(8)))
    return np.stack([np.asarray(r["out"], dtype=np.float32) for r in res.results], 0)
```

```python
from contextlib import ExitStack
import math
import numpy as np
import ml_dtypes
import concourse.bass as bass
import concourse.mybir as mybir
from concourse.bass_utils import run_bass_kernel_spmd

F32 = mybir.dt.float32
BF16 = mybir.dt.bfloat16
U32 = mybir.dt.uint32
I32 = mybir.dt.int32
AF = mybir.ActivationFunctionType
ALU = mybir.AluOpType
AX = mybir.AxisListType

S = 2048
D = 1024
NT = 16
NE = 32
RC = 384
NCH = 6
CAP = 2048
EPS = 1e-6
ENG = {"pe": "tensor", "act": "scalar", "dve": "vector", "pool": "gpsimd", "sp": "sync"}


class Buf:
    __slots__ = ("name", "w", "r", "sem", "cnt")

    def __init__(self, P, name):
        self.name = name
        self.w = []
        self.r = []
        self.sem = None
        self.cnt = 0
        P.bufs.append(self)


class Prog:
    def __init__(self, nc):
        self.nc = nc
        self.recs = {e: [] for e in ENG}
        self.waited = {e: {} for e in ENG}
        self.dma_sems = []
        self.bufs = []

    def _filter(self, eng, deps):
        out = []
        for t in deps:
            if t[0] == "c":
                _, e2, idx = t
                if e2 == eng and eng in ("pe", "sp"):
                    continue
                k = ("c", e2)
                if self.waited[eng].get(k, -1) >= idx:
                    continue
                self.waited[eng][k] = idx
                self.recs[e2][idx]["sig"] = True
                out.append(t)
            else:
                _, b, val = t
                k = ("d", id(b))
                if self.waited[eng].get(k, -1) >= val:
                    continue
                self.waited[eng][k] = val
                out.append(t)
        return out

    def _deps(self, eng, reads, writes):
        deps = []
        for b in reads:
            deps += b.w
        for b in writes:
            deps += b.w
            deps += b.r
        return self._filter(eng, deps)

    def op(self, eng, fn, reads=(), writes=(), accum=False):
        waits = self._deps(eng, reads, writes)
        idx = len(self.recs[eng])
        self.recs[eng].append(dict(waits=waits, fn=fn, sig=False, dma=None))
        tok = ("c", eng, idx)
        for b in reads:
            b.r.append(tok)
        for b in writes:
            if accum:
                b.w.append(tok)
            else:
                b.w = [tok]
                b.r = []
        return tok

    def dma(self, eng, fn, reads=(), writes=(), sembuf=None):
        waits = self._deps(eng, reads, writes)
        sb = sembuf if sembuf is not None else (writes[0] if writes else reads[0])
        if sb.sem is None:
            sb.sem = True
            self.dma_sems.append(sb)
        sb.cnt += 16
        tok = ("d", sb, sb.cnt)
        self.recs[eng].append(dict(waits=waits, fn=fn, sig=False, dma=sb, dmaval=sb.cnt))
        for b in reads:
            b.r.append(tok)
        for b in writes:
            b.w = [tok]
            b.r = []
        return tok

    def barrier(self):
        toks = []
        for e in ENG:
            for i in range(len(self.recs[e]) - 1, -1, -1):
                r = self.recs[e][i]
                if r["fn"] is not None and r["dma"] is None:
                    toks.append(("c", e, i))
                    break
        for b in self.bufs:
            toks += [t for t in b.w + b.r if t[0] == "d"]
            b.w = []
            b.r = []
        for e in ENG:
            w = self._filter(e, [t for t in toks if not (t[0] == "c" and t[1] == e)])
            self.recs[e].append(dict(waits=w, fn=None, sig=False, dma=None))

    def guard_begin(self, key):
        if not hasattr(self, "_wstack"):
            self._wstack = []
        self._wstack.append({e: dict(self.waited[e]) for e in ENG})
        for e in ENG:
            self.recs[e].append(dict(waits=[], fn=None, sig=False, dma=None, gb=key))

    def guard_end(self):
        for e in ENG:
            self.recs[e].append(dict(waits=[], fn=None, sig=False, dma=None, ge=True))
        self.waited = self._wstack.pop()

    def wait_all(self, eng, bufs):
        waits = self._deps(eng, list(bufs), list(bufs))
        self.recs[eng].append(dict(waits=waits, fn=None, sig=False, dma=None))

    def run(self, stack):
        nc = self.nc
        esem = {e: stack.enter_context(nc.semaphore("s_" + e)) for e in ENG}
        for i, b in enumerate(self.dma_sems):
            b.sem = stack.enter_context(nc.semaphore("d%d" % i))
        cnts = {}
        for e in ENG:
            c = 0
            for i, r in enumerate(self.recs[e]):
                if r["sig"]:
                    c += 1
                    cnts[(e, i)] = c
        block = stack.enter_context(nc.Block())

        def body(e):
            def emit(engine, r):
                for t in r["waits"]:
                    if t[0] == "c":
                        engine.wait_ge(esem[t[1]], cnts[(t[1], t[2])])
                    else:
                        engine.wait_ge(t[1].sem, t[2])
                if r["fn"] is None:
                    return
                ins = r["fn"](engine)
                if r["dma"] is not None:
                    ins.then_inc(r["dma"].sem, 16)
                elif r["sig"]:
                    ins.then_inc(esem[e], 1)

            def f(engine):
                recs = self.recs[e]
                reg = rthr = None
                if any("gb" in q for q in recs):
                    reg = stack.enter_context(engine.register("rc_" + e))
                    rthr = stack.enter_context(engine.register("rt_" + e))
                csum = [0]

                def match(i):
                    d_ = 0
                    j = i
                    while True:
                        if "gb" in recs[j]:
                            d_ += 1
                        elif "ge" in recs[j]:
                            d_ -= 1
                            if d_ == 0:
                                return j
                        j += 1

                def process(lo, hi):
                    i = lo
                    while i < hi:
                        r = recs[i]
                        if "gb" in r:
                            j = match(i)
                            inner = [q for q in recs[i + 1:j] if "gb" not in q and "ge" not in q]
                            ex_, thr = r["gb"]
                            if any(q["fn"] is not None or q["waits"] for q in inner):
                                c_before = csum[0]
                                engine.reg_load(reg, self.cnt_ap(ex_))
                                engine.reg_mov(rthr, thr)
                                with engine.If_lt(rthr, reg):
                                    process(i + 1, j)
                                csum[0] = c_before
                                nsig = sum(1 for q in inner if q["sig"])
                                dmas = [q for q in inner if q["dma"] is not None]
                                if nsig or dmas:
                                    with engine.Else():
                                        if nsig:
                                            if c_before > 0:
                                                engine.wait_ge(esem[e], c_before)
                                            engine.sem_inc(esem[e], nsig)
                                        for q in dmas:
                                            if q["dmaval"] - 16 > 0:
                                                engine.wait_ge(q["dma"].sem, q["dmaval"] - 16)
                                            engine.sem_inc(q["dma"].sem, 16)
                                csum[0] = c_before + nsig
                            i = j + 1
                            continue
                        emit(engine, r)
                        if r["sig"]:
                            csum[0] += 1
                        i += 1

                process(0, len(recs))
            return f

        block.tensor(body("pe"))
        block.scalar(body("act"))
        block.vector(body("dve"))
        block.gpsimd(body("pool"))
        block.sync(body("sp"))


def build(stop=None):
    nc = bass.Bass("TRN2", target_bir_lowering=False, dynamic_dma_scratch_size=8192)
    dram = lambda n, s, d=F32, k="ExternalInput": nc.dram_tensor(n, s, d, kind=k).ap()
    x_d = dram("x", [S, D])
    p_d = dram("p", [S, 256])
    win_d = dram("w_in", [128, 8, 3072])
    wout_d = dram("w_out", [128, 8, D])
    gains_d = dram("gains", [4, D])
    lamv_d = dram("lamv", [4, 64])
    subln_d = dram("subln", [1, 128])
    bnear_d = dram("bnear", [128, 4 * 2 * 128])
    c31_d = dram("c31", [128, 4])
    rw_d = dram("rw", [128, 8 * 32])
    rb_d = dram("rb", [1, 32])
    wgu_d = dram("wgu", [NE, 8, 128, 8, 256])
    bgl_d = dram("bgl", [128, NE * 8 * 2])
    wd_d = dram("wd", [NE, 128, 8, D])
    bd_d = dram("bd", [NE, D])
    pproj_d = dram("pproj", [128, 2, D])
    pgate_d = dram("pgate", [128, 8, D])
    cb_d = dram("cbf", [128, 5 * 128], BF16)
    cf_d = dram("cf32", [128, 3 * 128])
    out_d = dram("out", [S, D], F32, "ExternalOutput")
    h1_d = dram("h1s", [S, D], F32, "Internal")
    xg_d = dram("xg", [NE * CAP, D], BF16, "Internal")
    yg_d = dram("yg", [NE * CAP, D], F32, "Internal")
    dbg = {}

    st = ExitStack()
    P = Prog(nc)
    B = lambda n: Buf(P, n)
    sbt = lambda n, s, d: st.enter_context(nc.sbuf_tensor("sb_" + n, s, d))
    pall = st.enter_context(nc.psum_tensor("pall", [128, 4096], F32))
    psb = [pall[:, i * 512:(i + 1) * 512] for i in range(8)]

    cb = sbt("cb", [128, 5 * 128], BF16)
    cf = sbt("cf", [128, 3 * 128], F32)
    identb, negtri, negones, ltm, onesb = [cb[:, i * 128:(i + 1) * 128] for i in range(5)]
    identf, mask0, strictm = [cf[:, i * 128:(i + 1) * 128] for i in range(3)]
    gainb = sbt("gainb", [128, D], F32)
    gain2 = sbt("gain2", [128, D], F32)
    ebfix = sbt("ebfix", [128, 4 * 2 * 128], F32)
    c31 = sbt("c31", [128, 4], F32)
    lamt = sbt("lamt", [128, 4 * 64], F32)
    lams = sbt("lams", [128, 8], F32)
    subg = sbt("subg", [128, 128], F32)
    rw = sbt("rw", [128, 8 * 32], F32)
    rbb = sbt("rbb", [128, 32], F32)
    bgl = sbt("bgl", [128, NE * 8 * 2], F32)
    cst = sbt("cst", [128, 8], F32)
    stats = sbt("stats", [128, NT * 8], F32)
    meta_dest = sbt("meta_dest", [128, NT * 4], I32)
    meta_g = sbt("meta_g", [128, NT * 4], F32)
    bConst = B("const")
    bGain = B("gain")
    bGain2 = B("gain2")

    AR = sbt("arena", [128, 152 * 1024], mybir.dt.uint8)
    K = 1024
    OFF_HN, OFF_QK, OFF_AT, OFF_W, OFF_V, OFF_T = 0, 32 * K, 64 * K, 96 * K, 128 * K, 145 * K

    def view(off, shape, dt):
        nb = {F32: 4, BF16: 2, I32: 4, U32: 4}[dt]
        n = 1
        for s_ in shape[1:]:
            n *= s_
        ap = AR[:, off:off + n * nb].bitcast(dt)
        if len(shape) == 3:
            ap = ap.rearrange("p (a b) -> p a b", b=shape[2])
        elif len(shape) == 4:
            ap = ap.rearrange("p (a b c) -> p a b c", b=shape[2], c=shape[3])
        return ap

    P.dma("sp", lambda e: e.dma_start(out=cb[:], in_=cb_d), writes=[bConst])
    bC2 = B("c2"); bC3 = B("c3"); bC4 = B("c4"); bC5 = B("c5"); bC6 = B("c6"); bC7 = B("c7"); bC8 = B("c8"); bC9 = B("c9")
    P.dma("sp", lambda e: e.dma_start(out=cf[:], in_=cf_d), writes=[bC2])
    P.dma("sp", lambda e: e.dma_start(out=ebfix[:], in_=bnear_d), writes=[bC3])
    P.dma("sp", lambda e: e.dma_start(out=c31[:], in_=c31_d), writes=[bC4])
    P.dma("sp", lambda e: e.dma_start(out=lamt[:], in_=lamv_d.rearrange("a b -> (a b)").partition_broadcast(128)), writes=[bC5])
    P.dma("sp", lambda e: e.dma_start(out=subg[:], in_=subln_d.rearrange("a b -> (a b)").partition_broadcast(128)), writes=[bC6])
    P.dma("sp", lambda e: e.dma_start(out=rw[:], in_=rw_d), writes=[bC7])
    P.dma("sp", lambda e: e.dma_start(out=rbb[:], in_=rb_d.rearrange("a b -> (a b)").partition_broadcast(128)), writes=[bC8])
    P.dma("sp", lambda e: e.dma_start(out=bgl[:], in_=bgl_d), writes=[bC9])
    P.dma("sp", lambda e: e.dma_start(out=gainb[:], in_=gains_d[0].partition_broadcast(128)), writes=[bGain])
    bCst = B("cst")
    P.op("dve", lambda e: e.memset(cst[:, 0:1], -0.5), writes=[bCst])
    for h in range(4):
        P.op("dve", lambda e, h=h: e.tensor_scalar(out=ebfix[:, h * 256:(h + 1) * 256], in0=ebfix[:, h * 256:(h + 1) * 256],
                                                   scalar1=c31[:, h:h + 1], scalar2=None, op0=ALU.subtract),
             reads=[bC3, bC4], writes=[bC3])
    P.op("act", lambda e: e.activation(out=ebfix[:], in_=ebfix[:], func=AF.Exp), reads=[bC3], writes=[bC3])
    for h in range(4):
        P.op("dve", lambda e, h=h: e.tensor_tensor(out=ebfix[:, h * 256:h * 256 + 128], in0=ebfix[:, h * 256:h * 256 + 128], in1=mask0, op=ALU.mult),
             reads=[bC3, bC2], writes=[bC3])
    P.op("dve", lambda e: e.tensor_tensor(out=lamt[:, 0:64], in0=lamt[:, 0:64], in1=lamt[:, 64:128], op=ALU.mult), reads=[bC5], writes=[bC5])
    P.op("dve", lambda e: e.tensor_tensor(out=lamt[:, 128:192], in0=lamt[:, 128:192], in1=lamt[:, 192:256], op=ALU.mult), reads=[bC5], writes=[bC5])
    P.op("dve", lambda e: e.tensor_reduce(out=lams[:, 0:1], in_=lamt[:, 0:64], axis=AX.X, op=ALU.add), reads=[bC5], writes=[bC5])
    P.op("dve", lambda e: e.tensor_reduce(out=lams[:, 1:2], in_=lamt[:, 128:192], axis=AX.X, op=ALU.add), reads=[bC5], writes=[bC5])
    P.op("act", lambda e: e.activation(out=lams[:, 2:4], in_=lams[:, 0:2], func=AF.Exp), reads=[bC5], writes=[bC5])
    P.op("dve", lambda e: e.tensor_tensor(out=lams[:, 4:5], in0=lams[:, 3:4], in1=lams[:, 2:3], op=ALU.subtract), reads=[bC5], writes=[bC5])
    P.op("dve", lambda e: e.tensor_scalar(out=lams[:, 4:5], in0=lams[:, 4:5], scalar1=-0.2, scalar2=None, op0=ALU.add), reads=[bC5], writes=[bC5])
    P.op("dve", lambda e: e.tensor_scalar(out=subg[:], in0=subg[:], scalar1=0.8, scalar2=None, op0=ALU.mult), reads=[bC6], writes=[bC6])
    bglv = bgl[:].rearrange("p (a t) -> p a t", t=2)
    P.op("dve", lambda e: e.tensor_scalar(out=bglv[:, :, 1:2], in0=bglv[:, :, 1:2], scalar1=1.0, scalar2=None, op0=ALU.add), reads=[bC9], writes=[bC9])
    neglam = lams[:, 4:5]

    hnT = view(OFF_HN, [128, 8, S], BF16)
    bHn = [B("hn%d" % i) for i in range(NT)]
    xt = [view(OFF_W + i * 4096, [128, D], F32) for i in range(2)]
    xs = [view(OFF_W + 8192 + i * 2048, [128, D], BF16) for i in range(2)]
    junkb = view(OFF_T, [128, D], BF16)
    bXt = [B("xt%d" % i) for i in range(2)]
    bXs = [B("xs%d" % i) for i in range(2)]
    bJ = B("junk")
    bSt = [B("st%d" % i) for i in range(NT)]
    bPs = [B("ps%d" % i) for i in range(8)]

    def rms_stats(i, src, srcbuf, n):
        c0 = i * 8
        P.op("act", lambda e: e.activation(out=junkb[:, 0:n], in_=src, func=AF.Square, accum_out=stats[:, c0:c0 + 1]),
             reads=[srcbuf], writes=[bJ, bSt[i]])
        P.op("dve", lambda e: e.tensor_scalar(out=stats[:, c0 + 1:c0 + 2], in0=stats[:, c0:c0 + 1], scalar1=1.0 / n, scalar2=EPS, op0=ALU.mult, op1=ALU.add),
             reads=[bSt[i]], writes=[bSt[i]])
        P.op("pool", lambda e: e.tensor_tensor(out=stats[:, c0 + 3:c0 + 4], in0=stats[:, c0 + 1:c0 + 2], in1=cst[:, 0:1], op=ALU.pow),
             reads=[bSt[i], bCst], writes=[bSt[i]])
        return stats[:, c0 + 3:c0 + 4]

    for i in range(NT):
        b = i % 2
        P.dma("sp", lambda e, i=i, b=b: e.dma_start(out=xt[b], in_=x_d[i * 128:(i + 1) * 128, :]), writes=[bXt[b]])
        rstd = rms_stats(i, xt[b], bXt[b], D)
        P.op("dve", lambda e, b=b, rstd=rstd: e.scalar_tensor_tensor(out=xs[b], in0=xt[b], scalar=rstd, in1=gainb[:], op0=ALU.mult, op1=ALU.mult),
             reads=[bXt[b], bSt[i], bGain], writes=[bXs[b]])
        pT = psb[b].bitcast(BF16)
        for c in range(8):
            P.op("pe", lambda e, c=c, b=b, pT=pT: e.transpose(pT[:, c * 128:(c + 1) * 128], xs[b][:, c * 128:(c + 1) * 128], identb),
                 reads=[bXs[b], bConst], writes=[bPs[b]])
        P.op("act", lambda e, i=i, pT=pT: e.activation(out=hnT[:, :, i * 128:(i + 1) * 128], in_=pT.rearrange("p (c t) -> p c t", t=128), func=AF.Copy),
             reads=[bPs[b]], writes=[bHn[i]])

    if stop == "A":
        dbg["hnT"] = (hnT, [128, 8, S], BF16)
        return finish(nc, P, st, dbg)

    QK = view(OFF_QK, [128, 8, S], BF16)
    VV = view(OFF_V, [128, NT, 516], BF16)
    wsl = [view(OFF_W + i * 4096, [128, 8, 256], BF16) for i in range(3)]
    bW = [B("wsl%d" % i) for i in range(3)]
    bQK = [B("qk%d" % i) for i in range(8)]
    bV = [B("v%d" % i) for i in range(NT)]
    slab_ctr = [0]
    evac_ctr = [0]

    def project(col0, kind):
        P.barrier()
        if kind == "diff":
            VD4 = VV.rearrange("p t (h c) -> p t h c", c=129)
            P.op("dve", lambda e: e.memset(VD4[:, :, :, 128:129], 1.0), writes=bV)
        for s in range(6):
            wi = slab_ctr[0] % 3
            slab_ctr[0] += 1
            c_lo = col0 + s * 256
            P.dma("pool", lambda e, wi=wi, c_lo=c_lo: e.dma_start(out=wsl[wi], in_=win_d[:, :, c_lo:c_lo + 256]), writes=[bW[wi]])
            if s < 4:
                for gg in range(2):
                    gi = s * 2 + gg
                    for tc in range(4):
                        bk = evac_ctr[0] % 4
                        evac_ctr[0] += 1
                        for c in range(8):
                            P.op("pe", lambda e, wi=wi, gg=gg, tc=tc, c=c, bk=bk: e.matmul(
                                psb[bk], lhsT=wsl[wi][:, c, gg * 128:(gg + 1) * 128], rhs=hnT[:, c, tc * 512:(tc + 1) * 512],
                                start=(c == 0), stop=(c == 7)),
                                reads=[bW[wi]] + bHn[tc * 4:tc * 4 + 4], writes=[bPs[bk]])
                        sc = 0.125 if s < 2 else 1.0
                        if evac_ctr[0] % 2 == 0:
                            P.op("act", lambda e, gi=gi, tc=tc, bk=bk, sc=sc: e.activation(out=QK[:, gi, tc * 512:(tc + 1) * 512], in_=psb[bk], func=AF.Copy, scale=sc),
                                 reads=[bPs[bk]], writes=[bQK[gi]], accum=True)
                        else:
                            P.op("dve", lambda e, gi=gi, tc=tc, bk=bk, sc=sc: e.tensor_scalar(out=QK[:, gi, tc * 512:(tc + 1) * 512], in0=psb[bk], scalar1=sc, scalar2=None, op0=ALU.mult),
                                 reads=[bPs[bk]], writes=[bQK[gi]], accum=True)
            else:
                vs = s - 4
                for i in range(NT):
                    bk = evac_ctr[0] % 4
                    evac_ctr[0] += 1
                    for c in range(8):
                        P.op("pe", lambda e, wi=wi, i=i, c=c, bk=bk: e.matmul(
                            psb[bk][:, 0:256], lhsT=hnT[:, c, i * 128:(i + 1) * 128], rhs=wsl[wi][:, c, :], start=(c == 0), stop=(c == 7)),
                            reads=[bW[wi], bHn[i]], writes=[bPs[bk]])
                    if kind == "diff":
                        dst = VV[:, i, vs * 258:(vs + 1) * 258].rearrange("p (h c) -> p h c", c=129)[:, :, 0:128]
                        src = psb[bk][:, 0:256].rearrange("p (h c) -> p h c", c=128)
                    else:
                        dst = VV[:, i, vs * 256:(vs + 1) * 256]
                        src = psb[bk][:, 0:256]
                    if evac_ctr[0] % 2 == 0:
                        P.op("act", lambda e, dst=dst, src=src: e.activation(out=dst, in_=src, func=AF.Copy), reads=[bPs[bk]], writes=[bV[i]], accum=True)
                    else:
                        P.op("dve", lambda e, dst=dst, src=src: e.tensor_copy(out=dst, in_=src), reads=[bPs[bk]], writes=[bV[i]], accum=True)

    project(0, "diff")
    if stop == "B":
        dbg["QK"] = (QK, [128, 8, S], BF16)
        dbg["VV"] = (VV, [128, NT, 516], BF16)
        return finish(nc, P, st, dbg)

    P.barrier()
    attnT = view(OFF_AT, [128, 8, S], BF16)
    bAT = [B("at%d" % i) for i in range(NT)]
    NSLOT = 32
    Er = [view(OFF_W + i * 1024, [128, 2, 256], BF16) for i in range(NSLOT)]
    bE = [B("E%d" % i) for i in range(NSLOT)]
    def tv(par, k):
        base = OFF_T + par * 1296
        if k == 0:
            return view(base, [128, 130], F32)
        if k == 1:
            return view(base + 520, [128, 130], F32)
        return view(base + 1040, [128, 128], BF16)
    bEp = [B("ep%d" % i) for i in range(4)]
    late = []
    bSS = [B("S%d" % i) for i in range(2)]
    bO = [B("O%d" % m) for m in range(2)]
    bTp = [B("tp%d" % i) for i in range(2)]
    VD4 = VV.rearrange("p t (h c) -> p t h c", c=129)
    ebv = ebfix[:].rearrange("p (h d q) -> p h d q", h=4, d=2)

    units = [(h, c) for h in range(4) for c in range(8)]
    import os
    if stop == 'C1':
        units = units[:1]
    if os.environ.get('KLIM'):
        units = units[:int(os.environ['KLIM'])]
    blk_ctr = [0]
    ep_ctr = [0]

    def av_items(h, c, slots):
        items = []
        for j in range(2):
            for m in range(2):
                kbs = list(range(0, 2 * c + j + 1))
                for kb in kbs:
                    items.append((j, m, kb, kb == 0, kb == kbs[-1]))
        return items

    def emit_av(h, c, slots, it):
        j, m, kb, first, last = it
        ob = psb[4 + m][:, 0:129]
        sl = slots[kb]
        P.op("pe", lambda e: e.matmul(ob, lhsT=Er[sl][:, m, j * 128:(j + 1) * 128], rhs=VD4[:, kb, h, :], start=first, stop=last),
             reads=[bE[sl], bV[kb]], writes=[bO[m]])
        if last:
            par = ep_ctr[0] % 4
            P.op("dve", lambda e: e.tensor_copy(out=tv(par, m)[:, 0:129], in_=ob), reads=[bO[m]], writes=[bEp[par]], accum=(m == 1))
            if m == 1:
                epilogue(h, c, j)

    def epilogue(h, c, j):
        qb = 2 * c + j
        par = ep_ctr[0] % 4
        ep_ctr[0] += 1
        si = qb
        c0 = si * 8
        o1 = tv(par, 0); o2 = tv(par, 1); obf = tv(par, 2)
        ep = [bEp[par]]
        P.op("dve", lambda e: e.reciprocal(out=stats[:, c0:c0 + 1], in_=o1[:, 128:129]), reads=ep, writes=[bSt[si]])
        P.op("dve", lambda e: e.reciprocal(out=stats[:, c0 + 1:c0 + 2], in_=o2[:, 128:129]), reads=ep, writes=[bSt[si]])
        P.op("dve", lambda e: e.tensor_scalar(out=stats[:, c0 + 2:c0 + 3], in0=stats[:, c0 + 1:c0 + 2], scalar1=neglam, scalar2=None, op0=ALU.mult),
             reads=[bSt[si], bC5], writes=[bSt[si]])
        P.op("dve", lambda e: e.tensor_scalar(out=o2[:, 0:128], in0=o2[:, 0:128], scalar1=stats[:, c0 + 2:c0 + 3], scalar2=None, op0=ALU.mult),
             reads=ep + [bSt[si]], writes=ep)
        P.op("dve", lambda e: e.scalar_tensor_tensor(out=o1[:, 0:128], in0=o1[:, 0:128], scalar=stats[:, c0:c0 + 1], in1=o2[:, 0:128], op0=ALU.mult, op1=ALU.add),
             reads=ep + [bSt[si]], writes=ep)
        P.op("dve", lambda e: e.tensor_tensor(out=o2[:, 0:128], in0=o1[:, 0:128], in1=o1[:, 0:128], op=ALU.mult), reads=ep, writes=ep)
        P.op("dve", lambda e: e.tensor_reduce(out=stats[:, c0 + 3:c0 + 4], in_=o2[:, 0:128], axis=AX.X, op=ALU.add), reads=ep, writes=[bSt[si]])
        P.op("dve", lambda e: e.tensor_scalar(out=stats[:, c0 + 4:c0 + 5], in0=stats[:, c0 + 3:c0 + 4], scalar1=1.0 / 128, scalar2=EPS, op0=ALU.mult, op1=ALU.add),
             reads=[bSt[si]], writes=[bSt[si]])
        P.op("pool", lambda e: e.tensor_tensor(out=stats[:, c0 + 5:c0 + 6], in0=stats[:, c0 + 4:c0 + 5], in1=cst[:, 0:1], op=ALU.pow),
             reads=[bSt[si], bCst], writes=[bSt[si]])
        P.op("dve", lambda e: e.scalar_tensor_tensor(out=obf, in0=o1[:, 0:128], scalar=stats[:, c0 + 5:c0 + 6], in1=subg[:], op0=ALU.mult, op1=ALU.mult),
             reads=ep + [bSt[si], bC6], writes=ep)
        tb = psb[6 + par % 2].bitcast(BF16)

        def fin():
            P.op("pe", lambda e: e.transpose(tb[:, 0:128], obf, identb), reads=[bEp[par], bConst], writes=[bTp[par % 2]])
            P.op("dve", lambda e: e.tensor_copy(out=attnT[:, h, qb * 128:(qb + 1) * 128], in_=tb[:, 0:128]), reads=[bTp[par % 2]], writes=[bAT[qb]], accum=True)
        late.append(fin)

    pending = []
    for ui, (h, c) in enumerate(units):
        nb = 2 * c + 2
        slots = {}
        per = (len(pending) + nb - 1) // nb if pending else 0
        late_now = list(late)
        del late[:]
        for kb in range(nb):
            if kb == 1:
                for f_ in late_now:
                    f_()
            sl = blk_ctr[0] % NSLOT
            blk_ctr[0] += 1
            slots[kb] = sl
            sp_ = kb % 2
            lo = 128 if kb == 2 * c + 1 else 0
            sb3 = pall[:, sp_ * 512:sp_ * 512 + 2048].rearrange("p (m r) -> p m r", m=2)
            for m in range(2):
                P.op("pe", lambda e, m=m, kb=kb, lo=lo, sp_=sp_, h=h, c=c: e.matmul(
                    psb[2 * m + sp_][:, lo:256], lhsT=QK[m * 64:(m + 1) * 64, 4 + h, kb * 128:(kb + 1) * 128],
                    rhs=QK[m * 64:(m + 1) * 64, h, c * 256 + lo:(c + 1) * 256], start=True, stop=True),
                    reads=[bQK[4 + h], bQK[h]], writes=[bSS[sp_]])
            P.op("act", lambda e, sl=sl, lo=lo, sb3=sb3: e.activation(out=Er[sl][:, :, lo:256], in_=sb3[:, :, lo:256], func=AF.Exp),
                 reads=[bSS[sp_]], writes=[bE[sl]])
            for j in range(2):
                d = 2 * c + j - kb
                if 0 <= d <= 1:
                    for m in range(2):
                        P.op("dve", lambda e, sl=sl, m=m, j=j, d=d, h=h: e.tensor_tensor(
                            out=Er[sl][:, m, j * 128:(j + 1) * 128], in0=Er[sl][:, m, j * 128:(j + 1) * 128], in1=ebv[:, h, d, :], op=ALU.mult),
                            reads=[bE[sl], bC3], writes=[bE[sl]])
            for _ in range(per):
                if pending:
                    emit_av(*pending.pop(0))
        while pending:
            emit_av(*pending.pop(0))
        pending = [(h, c, slots, it) for it in av_items(h, c, slots)]
    while pending:
        emit_av(*pending.pop(0))
    for f_ in late:
        f_()

    if stop == "C1":
        dbg["E0"] = (Er[0], [128, 2, 256], BF16)
        dbg["E1"] = (Er[1], [128, 2, 256], BF16)
        dbg["ebfix"] = (ebfix[:], [128, 1024], F32)
        dbg["lams"] = (lams[:], [128, 8], F32)
        dbg["o1s"] = (tv(0, 0), [128, 130], F32)
        dbg["obf"] = (tv(0, 2), [128, 128], BF16)
        dbg["at0"] = (attnT[:, 0, 0:256], [128, 256], BF16)
        dbg["stats"] = (stats[:], [128, 128], F32)
        return finish(nc, P, st, dbg)
    if stop == "C":
        dbg["attnT"] = (attnT, [128, 8, S], BF16)
        return finish(nc, P, st, dbg)

    project(1536, "sb")
    P.barrier()
    Wr = [view(OFF_W + i * 512, [128, 256], BF16) for i in range(4)]
    bWr = [B("Wr%d" % i) for i in range(4)]
    e32 = [view(OFF_W + 2048 + i * 1024, [128, 256], F32) for i in range(2)]
    Lb = [view(OFF_W + 4096 + i * 512, [128, 256], BF16) for i in range(2)]
    Rb = [view(OFF_W + 5120 + i * 512, [128, 256], BF16) for i in range(3)]
    be32 = [B("e32%d" % i) for i in range(2)]
    bL = [B("L%d" % i) for i in range(2)]
    bR = [B("R%d" % i) for i in range(3)]
    bZ = [B("Z%d" % i) for i in range(2)]
    bX = [B("X%d" % i) for i in range(2)]
    bOT = [B("OT%d" % i) for i in range(2)]

    blocks = []
    for hd in range(8):
        for c in range(8):
            for kb in range(2 * c + 1, -1, -1):
                blocks.append((hd, c, kb))
    NB = len(blocks)

    def sb_pe1(i):
        hd, c, kb = blocks[i]
        g, po = hd // 2, (hd % 2) * 64
        lo = 128 if kb == 2 * c + 1 else 0
        pz = i % 2
        P.op("pe", lambda e: e.matmul(psb[pz][:, lo:256], lhsT=QK[po:po + 64, 4 + g, kb * 128:(kb + 1) * 128],
                                      rhs=QK[po:po + 64, g, c * 256 + lo:(c + 1) * 256], start=True, stop=True),
             reads=[bQK[4 + g], bQK[g]], writes=[bZ[pz]])

    def sb_act1(i):
        hd, c, kb = blocks[i]
        lo = 128 if kb == 2 * c + 1 else 0
        pz = i % 2
        first = kb == 2 * c + 1
        P.op("act", lambda e: e.activation(out=e32[pz][:, lo:256], in_=psb[pz][:, lo:256], func=AF.Exp), reads=[bZ[pz]], writes=[be32[pz]])
        if first:
            P.op("dve", lambda e: e.memset(e32[pz][:, 0:128], 0.0), writes=[be32[pz]], accum=True)
        j = kb - 2 * c
        if j >= 0:
            P.op("dve", lambda e: e.tensor_tensor(out=e32[pz][:, j * 128:(j + 1) * 128], in0=e32[pz][:, j * 128:(j + 1) * 128], in1=strictm, op=ALU.mult),
                 reads=[be32[pz], bC2], writes=[be32[pz]])
        P.op("act", lambda e: e.activation(out=Lb[pz][:], in_=e32[pz][:], func=AF.Ln, bias=1.0), reads=[be32[pz]], writes=[bL[pz]])
        if kb > 0:
            if first:
                P.op("dve", lambda e: e.tensor_copy(out=Rb[(i + 1) % 3][:], in_=Lb[pz][:]), reads=[bL[pz]], writes=[bR[(i + 1) % 3]])
            else:
                P.op("dve", lambda e: e.tensor_tensor(out=Rb[(i + 1) % 3][:], in0=Rb[i % 3][:], in1=Lb[pz][:], op=ALU.add),
                     reads=[bL[pz], bR[i % 3]], writes=[bR[(i + 1) % 3]])

    def sb_pe2(i):
        hd, c, kb = blocks[i]
        g, po = hd // 2, (hd % 2) * 64
        lo = 128 if kb == 2 * c + 1 else 0
        pz = i % 2
        first = kb == 2 * c + 1
        xb = psb[2 + pz]
        P.op("pe", lambda e: e.matmul(xb[:, lo:256], lhsT=QK[po:po + 64, 4 + g, kb * 128:(kb + 1) * 128],
                                      rhs=QK[po:po + 64, g, c * 256 + lo:(c + 1) * 256], start=True, stop=False),
             reads=[bQK[4 + g], bQK[g]], writes=[bX[pz]])
        P.op("pe", lambda e: e.matmul(xb[:, lo:256], lhsT=negtri, rhs=Lb[pz][:, lo:256], start=False, stop=first),
             reads=[bL[pz], bConst], writes=[bX[pz]])
        if not first:
            P.op("pe", lambda e: e.matmul(xb[:, lo:256], lhsT=negones, rhs=Rb[i % 3][:, lo:256], start=False, stop=True),
                 reads=[bR[i % 3], bConst], writes=[bX[pz]])

    def sb_act2(i):
        hd, c, kb = blocks[i]
        lo = 128 if kb == 2 * c + 1 else 0
        pz = i % 2
        wi = i % 4
        first = kb == 2 * c + 1
        P.op("act", lambda e: e.activation(out=Wr[wi][:, lo:256], in_=psb[2 + pz][:, lo:256], func=AF.Exp), reads=[bX[pz]], writes=[bWr[wi]])
        if first:
            P.op("dve", lambda e: e.memset(Wr[wi][:, 0:128], 0.0), writes=[bWr[wi]], accum=True)
        j = kb - 2 * c
        if j >= 0:
            P.op("dve", lambda e: e.tensor_tensor(out=Wr[wi][:, j * 128:(j + 1) * 128], in0=Wr[wi][:, j * 128:(j + 1) * 128], in1=strictm, op=ALU.mult),
                 reads=[bWr[wi], bC2], writes=[bWr[wi]])

    unit_ctr = [0]

    def sb_pe3(i):
        hd, c, kb = blocks[i]
        g, po = hd // 2, (hd % 2) * 64
        wi = i % 4
        first = kb == 2 * c + 1
        last = kb == 0
        up = (hd * 8 + c) % 2
        ob = psb[4 + up]
        P.op("pe", lambda e: e.matmul(ob[po:po + 64, 0:256], lhsT=VV[:, kb, hd * 64:(hd + 1) * 64], rhs=Wr[wi][:], start=first, stop=last),
             reads=[bWr[wi], bV[kb]], writes=[bOT[up]])
        if last:
            if (hd * 8 + c) % 2 == 0:
                P.op("act", lambda e: e.activation(out=attnT[po:po + 64, 4 + g, c * 256:(c + 1) * 256], in_=ob[po:po + 64, 0:256], func=AF.Copy),
                     reads=[bOT[up]], writes=[bAT[2 * c], bAT[2 * c + 1]], accum=True)
            else:
                P.op("dve", lambda e: e.tensor_copy(out=attnT[po:po + 64, 4 + g, c * 256:(c + 1) * 256], in_=ob[po:po + 64, 0:256]),
                     reads=[bOT[up]], writes=[bAT[2 * c], bAT[2 * c + 1]], accum=True)

    for s_ in range(NB + 2):
        if s_ < NB:
            sb_pe1(s_)
            sb_act1(s_)
        if 0 <= s_ - 1 < NB:
            sb_pe2(s_ - 1)
            sb_act2(s_ - 1)
        if 0 <= s_ - 2 < NB:
            sb_pe3(s_ - 2)

    if stop == "D":
        dbg["attnT"] = (attnT, [128, 8, S], BF16)
        return finish(nc, P, st, dbg)

    P.barrier()
    KB = 1024
    hres = view(0, [128, NT, D], F32)
    tT = view(96 * KB, [128, 8, S], BF16)
    wob = view(128 * KB, [128, 8, D], BF16)
    xt1 = view(144 * KB, [128, D], F32)
    tb1 = view(148 * KB, [128, D], BF16)
    junk2 = view(150 * KB, [128, D], BF16)
    tmpx = sbt("tmpx", [128, 4 * 512], F32)
    comb = sbt("comb", [128, NT * 32], F32)
    lgs = sbt("lgs", [128, 4 * 32], F32)
    mx8 = sbt("mx8", [128, 32], F32)
    ix8 = sbt("ix8", [128, 8], U32)
    mbt = sbt("mbt", [128, 96], BF16)
    bIx = B("ix8"); bMb = B("mskb"); bMacc = [B("macc0"), B("macc1")]; bXg = B("xg"); bH1d = B("h1d"); bMeta = B("meta")
    rwb = sbt("rwb", [128, 256], BF16)
    bH = [B("h%d" % i_) for i_ in range(NT)]
    bTT = [B("tT%d" % i_) for i_ in range(NT)]
    bWo = B("wo"); bX1 = B("x1"); bTb = B("tb1"); bJ2 = B("junk2"); bRwb = B("rwb"); bLg = B("lg"); bMx = B("mx"); bComb = B("comb")
    bPE = [B("pe%d" % i_) for i_ in range(8)]
    P.dma("pool", lambda e: e.dma_start(out=wob, in_=wout_d), writes=[bWo])
    P.dma("pool", lambda e: e.dma_start(out=rwb[:], in_=rw_d), writes=[bRwb])
    P.dma("sp", lambda e: e.dma_start(out=gainb[:], in_=gains_d[1].partition_broadcast(128)), writes=[bGain])

    def rms2(i_, src, srcbuf, n):
        c0 = i_ * 8
        P.op("act", lambda e: e.activation(out=junk2[:, 0:n], in_=src, func=AF.Square, accum_out=stats[:, c0:c0 + 1]), reads=[srcbuf], writes=[bJ2, bSt[i_]])
        P.op("dve", lambda e: e.tensor_scalar(out=stats[:, c0 + 1:c0 + 2], in0=stats[:, c0:c0 + 1], scalar1=1.0 / n, scalar2=EPS, op0=ALU.mult, op1=ALU.add), reads=[bSt[i_]], writes=[bSt[i_]])
        P.op("pool", lambda e: e.tensor_tensor(out=stats[:, c0 + 3:c0 + 4], in0=stats[:, c0 + 1:c0 + 2], in1=cst[:, 0:1], op=ALU.pow), reads=[bSt[i_], bCst], writes=[bSt[i_]])
        return stats[:, c0 + 3:c0 + 4]

    xt1s = [xt1, tmpx[:, 0:1024]]
    tb1s = [tb1, tmpx[:, 1024:1536].bitcast(BF16)]
    bX1s = [bX1, B("x1b")]
    bTbs = [bTb, B("tb1b")]

    def phaseE(i_):
        xt1 = xt1s[i_ % 2]; tb1 = tb1s[i_ % 2]; bX1 = bX1s[i_ % 2]; bTb = bTbs[i_ % 2]
        P.dma("sp", lambda e: e.dma_start(out=xt1, in_=x_d[i_ * 128:(i_ + 1) * 128, :]), writes=[bX1])
        for half in range(2):
            bk = 2 * (i_ % 2) + half
            for c_ in range(8):
                P.op("pe", lambda e, c_=c_, bk=bk, half=half: e.matmul(psb[bk], lhsT=attnT[:, c_, i_ * 128:(i_ + 1) * 128], rhs=wob[:, c_, half * 512:(half + 1) * 512], start=(c_ == 0), stop=(c_ == 7)),
                     reads=[bAT[i_], bWo], writes=[bPE[bk]])
            P.op("dve", lambda e, bk=bk, half=half: e.tensor_tensor(out=hres[:, i_, half * 512:(half + 1) * 512], in0=psb[bk], in1=xt1[:, half * 512:(half + 1) * 512], op=ALU.add),
                 reads=[bPE[bk], bX1], writes=[bH[i_]], accum=(half == 1))
        rstd = rms2(i_, hres[:, i_, :], bH[i_], D)
        P.op("dve", lambda e: e.scalar_tensor_tensor(out=tb1, in0=hres[:, i_, :], scalar=rstd, in1=gainb[:], op0=ALU.mult, op1=ALU.mult), reads=[bH[i_], bSt[i_], bGain], writes=[bTb])
        pT = psb[4 + i_ % 2].bitcast(BF16)
        for c_ in range(8):
            P.op("pe", lambda e, c_=c_: e.transpose(pT[:, c_ * 128:(c_ + 1) * 128], tb1[:, c_ * 128:(c_ + 1) * 128], identb), reads=[bTb, bConst], writes=[bPE[4 + i_ % 2]])
        P.op("act", lambda e: e.activation(out=tT[:, :, i_ * 128:(i_ + 1) * 128], in_=pT.rearrange("p (c t) -> p c t", t=128), func=AF.Copy), reads=[bPE[4 + i_ % 2]], writes=[bTT[i_]])
        for c_ in range(8):
            P.op("pe", lambda e, c_=c_: e.matmul(psb[6][:, 0:32], lhsT=tT[:, c_, i_ * 128:(i_ + 1) * 128], rhs=rwb[:, c_ * 32:(c_ + 1) * 32], start=(c_ == 0), stop=(c_ == 7)),
                 reads=[bTT[i_], bRwb], writes=[bPE[6]])
        lg = lgs[:, 0:32]; exl = lgs[:, 32:64]; msk = lgs[:, 64:96]
        P.op("dve", lambda e: e.tensor_tensor(out=lg, in0=psb[6][:, 0:32], in1=rbb[:], op=ALU.add), reads=[bPE[6], bC8], writes=[bLg])
        P.op("dve", lambda e: e.max(out=mx8[:, 0:8], in_=lg), reads=[bLg], writes=[bMx])
        P.op("dve", lambda e: e.tensor_scalar(out=mx8[:, 8:9], in0=mx8[:, 0:1], scalar1=-1.0, scalar2=None, op0=ALU.mult), reads=[bMx], writes=[bMx])
        P.op("act", lambda e: e.activation(out=exl, in_=lg, func=AF.Exp, bias=mx8[:, 8:9]), reads=[bLg, bMx], writes=[bLg])
        P.op("dve", lambda e: e.tensor_scalar(out=msk, in0=lg, scalar1=mx8[:, 3:4], scalar2=None, op0=ALU.is_ge), reads=[bLg, bMx], writes=[bLg])
        P.op("dve", lambda e: e.tensor_tensor(out=exl, in0=exl, in1=msk, op=ALU.mult), reads=[bLg], writes=[bLg])
        P.op("dve", lambda e: e.tensor_reduce(out=mx8[:, 9:10], in_=exl, axis=AX.X, op=ALU.add), reads=[bLg], writes=[bMx])
        P.op("dve", lambda e: e.reciprocal(out=mx8[:, 10:11], in_=mx8[:, 9:10]), reads=[bMx], writes=[bMx])
        P.op("act", lambda e: e.activation(out=mx8[:, 16:20], in_=mx8[:, 0:4], func=AF.Exp, bias=mx8[:, 8:9]), reads=[bMx], writes=[bMx])
        P.op("dve", lambda e: e.tensor_scalar(out=meta_g[:, i_ * 4:(i_ + 1) * 4], in0=mx8[:, 16:20], scalar1=mx8[:, 10:11], scalar2=None, op0=ALU.mult), reads=[bMx], writes=[bMeta], accum=True)
        P.op("dve", lambda e: e.max_index(out=ix8[:], in_max=mx8[:, 0:8], in_values=lg), reads=[bLg, bMx], writes=[bIx])
        P.op("dve", lambda e: e.tensor_copy(out=mbt[:, 0:32], in_=msk), reads=[bLg], writes=[bMb])
        pfx = psb[7][:, 0:32]
        a_ = i_ % 2
        P.op("pe", lambda e: e.matmul(pfx, lhsT=ltm, rhs=mbt[:, 0:32], start=True, stop=(i_ == 0)), reads=[bMb, bConst], writes=[bPE[7]])
        if i_ > 0:
            P.op("pe", lambda e: e.matmul(pfx, lhsT=onesb, rhs=mbt[:, 32 + a_ * 32:64 + a_ * 32], start=False, stop=True), reads=[bMacc[a_], bConst], writes=[bPE[7]])
        if i_ == 0:
            P.op("dve", lambda e: e.tensor_copy(out=mbt[:, 64:96], in_=mbt[:, 0:32]), reads=[bMb], writes=[bMacc[1]])
        else:
            P.op("dve", lambda e: e.tensor_tensor(out=mbt[:, 32 + (1 - a_) * 32:64 + (1 - a_) * 32], in0=mbt[:, 32 + a_ * 32:64 + a_ * 32], in1=mbt[:, 0:32], op=ALU.add),
                 reads=[bMb, bMacc[a_]], writes=[bMacc[1 - a_]])
        oh = lgs[:, 96:128]
        for k_ in range(4):
            P.op("dve", lambda e, k_=k_: e.tensor_scalar(out=oh, in0=lg, scalar1=mx8[:, k_:k_ + 1], scalar2=None, op0=ALU.is_equal), reads=[bLg, bMx], writes=[bLg])
            P.op("dve", lambda e: e.tensor_tensor(out=oh, in0=oh, in1=pfx, op=ALU.mult), reads=[bLg, bPE[7]], writes=[bLg])
            P.op("dve", lambda e, k_=k_: e.tensor_reduce(out=mx8[:, 20 + k_:21 + k_], in_=oh, axis=AX.X, op=ALU.add), reads=[bLg], writes=[bMx])
        P.op("dve", lambda e: e.tensor_copy(out=mx8[:, 24:28], in_=ix8[:, 0:4]), reads=[bIx], writes=[bMx])
        P.op("dve", lambda e: e.scalar_tensor_tensor(out=mx8[:, 28:32], in0=mx8[:, 24:28], scalar=float(CAP), in1=mx8[:, 20:24], op0=ALU.mult, op1=ALU.add), reads=[bMx], writes=[bMx])
        P.op("dve", lambda e: e.tensor_copy(out=meta_dest[:, i_ * 4:(i_ + 1) * 4], in_=mx8[:, 28:32]), reads=[bMx], writes=[bMeta], accum=True)
        for k_ in range(4):
            P.dma("pool", lambda e, k_=k_: e.indirect_dma_start(out=xg_d, out_offset=bass.IndirectOffsetOnAxis(ap=meta_dest[:, i_ * 4 + k_:i_ * 4 + k_ + 1], axis=0),
                                                               in_=tb1, in_offset=None), reads=[bTb, bMeta], writes=[bXg])
        P.dma("sp", lambda e: e.dma_start(out=h1_d[i_ * 128:(i_ + 1) * 128, :], in_=hres[:, i_, :]), reads=[bH[i_]], writes=[bH1d])

    for i_ in range(NT):
        phaseE(i_)
    cnt_i = sbt("cnt_i", [128, 32], I32)
    bCnt = B("cnt")
    P.op("pe", lambda e: e.matmul(psb[7][:, 0:32], lhsT=onesb, rhs=mbt[:, 32:64], start=True, stop=True), reads=[bMacc[0], bConst], writes=[bPE[7]])
    P.op("dve", lambda e: e.tensor_copy(out=cnt_i[:], in_=psb[7][:, 0:32]), reads=[bPE[7]], writes=[bCnt])
    P.cnt_ap = lambda ex_: cnt_i[0:1, ex_:ex_ + 1]

    if stop == "E":
        dbg["h1"] = (hres, [128, NT, D], F32)
        dbg["tT"] = (tT, [128, 8, S], BF16)
        dbg["mdest"] = (meta_dest[:], [128, NT * 4], I32)
        dbg["mg"] = (meta_g[:], [128, NT * 4], F32)
        dbg["xg0"] = (xg_d[0:512, :], [512, D], BF16)
        return finish(nc, P, st, dbg)

    P.barrier()
    NTS = RC // 128
    xe = [view(0 + k_ * 6 * KB, [128, NTS, D], BF16) for k_ in range(2)]
    XeT = [view(12 * KB + k_ * 6 * KB, [128, 8, RC], BF16) for k_ in range(2)]
    actT = [view(24 * KB + k_ * 6 * KB, [128, 8, RC], BF16) for k_ in range(2)]
    wdb = [view(36 * KB + k_ * 16 * KB, [128, 8, D], BF16) for k_ in range(2)]
    wg = [view(112 * KB + k_ * 4 * KB, [128, 8, 256], BF16) for k_ in range(6)]
    yt = [view(80 * KB + k_ * 4 * KB, [128, D], F32) for k_ in range(2)]
    bdt = [view(88 * KB + k_ * 4 * KB, [128, D], F32) for k_ in range(2)]
    bXe = [B("xe%d" % k_) for k_ in range(2)]; bXT = [B("XeT%d" % k_) for k_ in range(2)]; bAc = [B("ac%d" % k_) for k_ in range(2)]
    bWd = [B("wd%d" % k_) for k_ in range(2)]; bWg = [B("wg%d" % k_) for k_ in range(6)]; bYt = [B("yt%d" % k_) for k_ in range(2)]; bBd = [B("bd%d" % k_) for k_ in range(2)]
    bYg = B("yg")
    bTgs = [B("tg%d" % k_) for k_ in range(3)]; bTss = [B("ts%d" % k_) for k_ in range(3)]; bTls = [B("tl%d" % k_) for k_ in range(3)]
    tgs = [view(96 * KB + k_ * 1536, [128, RC], F32) for k_ in range(3)]
    tss = [view(101 * KB + k_ * 1536, [128, RC], F32) for k_ in range(3)]
    tls = [view(106 * KB + k_ * 1536, [128, RC], F32) for k_ in range(3)]
    cnt = [0]; gcnt = [0]; slabc = [0]; ytc = [0]; qc = [0]

    def gu_chunk(e_, j_, k_, q_):
        ba = 2 * (gcnt[0] % 3)
        tg = tgs[gcnt[0] % 3]; ts = tss[gcnt[0] % 3]; tl = tls[gcnt[0] % 3]
        bTg = bTgs[gcnt[0] % 3]; bTs = bTss[gcnt[0] % 3]; bTl = bTls[gcnt[0] % 3]
        gcnt[0] += 1
        KC = int(os.environ.get("KKC", 8))
        for which in range(2):
            for c_ in range(KC):
                P.op("pe", lambda e, c_=c_, which=which: e.matmul(psb[ba + which][:, 0:RC], lhsT=wg[k_][:, c_, which * 128:(which + 1) * 128], rhs=XeT[q_][:, c_, :], start=(c_ == 0), stop=(c_ == KC - 1)),
                     reads=[bWg[k_], bXT[q_]], writes=[bPE[ba + which]])
        col = (e_ * 8 + j_) * 2
        P.op("dve", lambda e: e.tensor_scalar(out=tg, in0=psb[ba][:, 0:RC], scalar1=bgl[:, col:col + 1], scalar2=7.0, op0=ALU.add, op1=ALU.min), reads=[bPE[ba], bC9], writes=[bTg])
        P.op("act", lambda e: e.activation(out=ts, in_=tg, func=AF.Silu, scale=1.702), reads=[bTg], writes=[bTs])
        P.op("dve", lambda e: e.tensor_scalar(out=tl, in0=psb[ba + 1][:, 0:RC], scalar1=bgl[:, col + 1:col + 2], scalar2=8.0, op0=ALU.add, op1=ALU.min), reads=[bPE[ba + 1], bC9], writes=[bTl])
        P.op("dve", lambda e: e.scalar_tensor_tensor(out=actT[q_][:, j_, :], in0=tl, scalar=-6.0, in1=ts, op0=ALU.max, op1=ALU.mult), reads=[bTl, bTs], writes=[bAc[q_]], accum=True)

    def down_tile(e_, q_, st_, half, yk):
        bk = 6 + cnt[0] % 2
        cnt[0] += 1
        for j_ in range(8):
            P.op("pe", lambda e, j_=j_: e.matmul(psb[bk], lhsT=actT[q_][:, j_, st_ * 128:(st_ + 1) * 128], rhs=wdb[q_][:, j_, half * 512:(half + 1) * 512], start=(j_ == 0), stop=(j_ == 7)),
                 reads=[bAc[q_], bWd[q_]], writes=[bPE[bk]])
        P.op("dve", lambda e: e.scalar_tensor_tensor(out=yt[yk][:, half * 512:(half + 1) * 512], in0=psb[bk], scalar=1.0 / 1.702, in1=bdt[q_][:, half * 512:(half + 1) * 512], op0=ALU.mult, op1=ALU.add),
             reads=[bPE[bk], bBd[q_]], writes=[bYt[yk]], accum=(half == 1))

    def expert_chunk(e_, ch):
        q_ = qc[0] % 2
        qc[0] += 1
        row0 = e_ * CAP + min(ch * RC, CAP - RC)
        P.dma("sp", lambda e: e.dma_start(out=xe[q_], in_=xg_d[row0:row0 + RC, :].rearrange("(t p) d -> p t d", p=128)), reads=[bXg], writes=[bXe[q_]])
        P.dma("sp", lambda e: e.dma_start(out=bdt[q_], in_=bd_d[e_].partition_broadcast(128)), writes=[bBd[q_]])
        P.dma("pool", lambda e: e.dma_start(out=wdb[q_], in_=wd_d[e_]), writes=[bWd[q_]])
        for t_ in range(NTS):
            pT = psb[6 + t_ % 2].bitcast(BF16)
            for c_ in range(8):
                P.op("pe", lambda e, c_=c_, t_=t_, pT=pT: e.transpose(pT[:, c_ * 128:(c_ + 1) * 128], xe[q_][:, t_, c_ * 128:(c_ + 1) * 128], identb),
                     reads=[bXe[q_], bConst], writes=[bPE[6 + t_ % 2]])
            if t_ % 2 == 0:
                P.op("act", lambda e, t_=t_, pT=pT: e.activation(out=XeT[q_][:, :, t_ * 128:(t_ + 1) * 128], in_=pT.rearrange("p (c t) -> p c t", t=128), func=AF.Copy),
                     reads=[bPE[6 + t_ % 2]], writes=[bXT[q_]], accum=(t_ > 0))
            else:
                P.op("dve", lambda e, t_=t_, pT=pT: e.tensor_copy(out=XeT[q_][:, :, t_ * 128:(t_ + 1) * 128], in_=pT.rearrange("p (c t) -> p c t", t=128)),
                     reads=[bPE[6 + t_ % 2]], writes=[bXT[q_]], accum=True)
        for j_ in range(8):
            k_ = slabc[0] % 6
            slabc[0] += 1
            P.dma("pool", lambda e, j_=j_, k_=k_: e.dma_start(out=wg[k_], in_=wgu_d[e_, j_]), writes=[bWg[k_]])
            gu_chunk(e_, j_, k_, q_)
        for st_ in range(NTS):
            yk = ytc[0] % 2
            ytc[0] += 1
            for half in range(2):
                down_tile(e_, q_, st_, half, yk)
            P.dma("sp", lambda e, st_=st_, yk=yk: e.dma_start(out=yg_d[row0 + st_ * 128:row0 + (st_ + 1) * 128, :], in_=yt[yk]), reads=[bYt[yk]], writes=[bYg], sembuf=bYg)

    NEX = int(os.environ.get("KNEX", NE))
    NCHX = int(os.environ.get("KNCH", NCH))
    for e_ in range(NEX):
        expert_chunk(e_, 0)
    for e_ in range(NEX):
        for ch in range(1, NCHX):
            P.guard_begin((e_, ch * RC))
            expert_chunk(e_, ch)
        for ch in range(1, NCHX):
            P.guard_end()

    if stop == "G":
        dbg["cnt"] = (cnt_i[:], [128, 32], I32)
        return finish(nc, P, st, dbg)

    P.barrier()
    pgb = view(128 * KB, [128, 8, D], BF16)
    ppb = view(144 * KB, [128, 2, D], BF16)
    ptile = view(148 * KB, [128, 256], F32)
    pbf = view(149 * KB, [128, 256], BF16)
    pTs = view(149 * KB + 512, [128, 2, 128], BF16)
    pe32 = view(64 * KB, [128, D], F32)
    gate = view(68 * KB, [128, D], F32)
    hbf = view(72 * KB, [128, D], BF16)
    hT = view(74 * KB, [128, 8, 128], BF16)
    otile = [view(76 * KB + k_ * 4096, [128, D], F32) for k_ in range(2)]
    gfin = ebfix
    bPg = B("pg"); bPp = B("pp"); bPt = B("pt"); bPbf = B("pbf"); bPTs = B("pTs"); bPe32 = B("pe32"); bGate = B("gate"); bHbf = B("hbf"); bHT = B("hT")
    bOt = [B("ot%d" % k_) for k_ in range(2)]; bGf = B("gfin"); bOut = B("out")
    P.dma("pool", lambda e: e.dma_start(out=pgb, in_=pgate_d), writes=[bPg])
    P.dma("pool", lambda e: e.dma_start(out=ppb, in_=pproj_d), writes=[bPp])
    P.dma("sp", lambda e: e.dma_start(out=gainb[:], in_=gains_d[2].partition_broadcast(128)), writes=[bGain])
    P.dma("sp", lambda e: e.dma_start(out=gfin[:], in_=gains_d[3].partition_broadcast(128)), writes=[bGf])

    htl = [view(84 * KB + k_ * 4 * KB, [128, D], F32) for k_ in range(2)]
    ygk = [view(92 * KB + k_ * 4 * KB, [128, D], F32) for k_ in range(8)]
    bHt = [B("ht%d" % k_) for k_ in range(2)]
    bYk = [B("ygk%d" % k_) for k_ in range(8)]

    def phaseH(i_):
        k_ = i_ % 2
        hcur = htl[k_]
        P.dma("sp", lambda e: e.dma_start(out=hcur, in_=h1_d[i_ * 128:(i_ + 1) * 128, :]), reads=[bH1d], writes=[bHt[k_]])
        for kk in range(4):
            yb = k_ * 4 + kk
            P.dma("pool", lambda e, kk=kk, yb=yb: e.indirect_dma_start(out=ygk[yb], out_offset=None, in_=yg_d,
                                                                        in_offset=bass.IndirectOffsetOnAxis(ap=meta_dest[:, i_ * 4 + kk:i_ * 4 + kk + 1], axis=0)),
                  reads=[bYg, bMeta], writes=[bYk[yb]])
            P.op("dve", lambda e, kk=kk, yb=yb: e.scalar_tensor_tensor(out=hcur, in0=ygk[yb], scalar=meta_g[:, i_ * 4 + kk:i_ * 4 + kk + 1], in1=hcur, op0=ALU.mult, op1=ALU.add),
                 reads=[bYk[yb], bMeta, bHt[k_]], writes=[bHt[k_]])
        P.dma("sp", lambda e: e.dma_start(out=ptile, in_=p_d[i_ * 128:(i_ + 1) * 128, :]), writes=[bPt])
        P.op("dve", lambda e: e.tensor_copy(out=pbf, in_=ptile), reads=[bPt], writes=[bPbf])
        tq = psb[6].bitcast(BF16)
        for c_ in range(2):
            P.op("pe", lambda e, c_=c_: e.transpose(tq[:, c_ * 128:(c_ + 1) * 128], pbf[:, c_ * 128:(c_ + 1) * 128], identb), reads=[bPbf, bConst], writes=[bPE[6]])
        P.op("act", lambda e: e.activation(out=pTs, in_=tq[:, 0:256].rearrange("p (c t) -> p c t", t=128), func=AF.Copy), reads=[bPE[6]], writes=[bPTs])
        for half in range(2):
            for c_ in range(2):
                P.op("pe", lambda e, c_=c_, half=half: e.matmul(psb[half], lhsT=pTs[:, c_, :], rhs=ppb[:, c_, half * 512:(half + 1) * 512], start=(c_ == 0), stop=(c_ == 1)),
                     reads=[bPTs, bPp], writes=[bPE[half]])
            P.op("act", lambda e, half=half: e.activation(out=pe32[:, half * 512:(half + 1) * 512], in_=psb[half], func=AF.Copy), reads=[bPE[half]], writes=[bPe32], accum=(half == 1))
        rp = rms2(i_, pe32, bPe32, D)
        P.op("dve", lambda e: e.scalar_tensor_tensor(out=pe32, in0=pe32, scalar=rp, in1=gainb[:], op0=ALU.mult, op1=ALU.mult), reads=[bPe32, bSt[i_], bGain], writes=[bPe32])
        P.op("dve", lambda e: e.tensor_copy(out=hbf, in_=hcur), reads=[bHt[k_]], writes=[bHbf])
        tq2 = psb[7].bitcast(BF16)
        for c_ in range(8):
            P.op("pe", lambda e, c_=c_: e.transpose(tq2[:, c_ * 128:(c_ + 1) * 128], hbf[:, c_ * 128:(c_ + 1) * 128], identb), reads=[bHbf, bConst], writes=[bPE[7]])
        P.op("act", lambda e: e.activation(out=hT, in_=tq2.rearrange("p (c t) -> p c t", t=128), func=AF.Copy), reads=[bPE[7]], writes=[bHT])
        for half in range(2):
            for c_ in range(8):
                P.op("pe", lambda e, c_=c_, half=half: e.matmul(psb[2 + half], lhsT=hT[:, c_, :], rhs=pgb[:, c_, half * 512:(half + 1) * 512], start=(c_ == 0), stop=(c_ == 7)),
                     reads=[bHT, bPg], writes=[bPE[2 + half]])
            P.op("act", lambda e, half=half: e.activation(out=gate[:, half * 512:(half + 1) * 512], in_=psb[2 + half], func=AF.Sigmoid), reads=[bPE[2 + half]], writes=[bGate], accum=(half == 1))
        P.op("dve", lambda e: e.tensor_tensor(out=gate, in0=gate, in1=pe32, op=ALU.mult), reads=[bGate, bPe32], writes=[bGate])
        P.op("dve", lambda e: e.tensor_tensor(out=hcur, in0=hcur, in1=gate, op=ALU.add), reads=[bHt[k_], bGate], writes=[bHt[k_]])
        c0 = i_ * 8 + 4
        P.op("act", lambda e: e.activation(out=junk2, in_=hcur, func=AF.Square, accum_out=stats[:, c0:c0 + 1]), reads=[bHt[k_]], writes=[bJ2, bSt[i_]])
        P.op("dve", lambda e: e.tensor_scalar(out=stats[:, c0 + 1:c0 + 2], in0=stats[:, c0:c0 + 1], scalar1=1.0 / D, scalar2=EPS, op0=ALU.mult, op1=ALU.add), reads=[bSt[i_]], writes=[bSt[i_]])
        P.op("pool", lambda e: e.tensor_tensor(out=stats[:, c0 + 2:c0 + 3], in0=stats[:, c0 + 1:c0 + 2], in1=cst[:, 0:1], op=ALU.pow), reads=[bSt[i_], bCst], writes=[bSt[i_]])
        P.op("dve", lambda e: e.scalar_tensor_tensor(out=otile[k_], in0=hcur, scalar=stats[:, c0 + 2:c0 + 3], in1=gfin[:], op0=ALU.mult, op1=ALU.mult), reads=[bHt[k_], bSt[i_], bGf], writes=[bOt[k_]])
        P.dma("sp", lambda e: e.dma_start(out=out_d[i_ * 128:(i_ + 1) * 128, :], in_=otile[k_]), reads=[bOt[k_]], writes=[bOut], sembuf=bOt[k_])

    for i_ in range(NT):
        phaseH(i_)
    P.wait_all("sp", [bOut] + bOt)
    P.run(st)
    st.close()
    return nc


def finish(nc, P, st, dbg):
    P.barrier()
    outs = []
    for name, (ap, shape, dt) in dbg.items():
        d = nc.dram_tensor("dbg_" + name, shape, dt, kind="ExternalOutput").ap()
        b = Buf(P, "dbg_" + name)
        P.dma("sp", lambda e, d=d, ap=ap: e.dma_start(out=d, in_=ap), writes=[b])
        outs.append(b)
    P.wait_all("sp", outs)
    P.run(st)
    st.close()
    return nc


def _t5_bucket(rel):
    n = np.maximum(rel, 0)
    nf = np.maximum(n, 1).astype(np.float32)
    large = 16 + (np.log(nf / np.float32(16)) / np.float32(math.log(128 / 16)) * np.float32(16)).astype(np.int32)
    large = np.minimum(large, 31)
    return np.where(n < 16, n, large)


def prep_shared(inp):
    f32 = np.float32
    g = lambda k: np.asarray(inp[k], dtype=f32)
    w_in = g("w_in")[0]
    cols = []
    for h in range(4):
        cols += list(range(h * 64, h * 64 + 64)) + list(range(256 + h * 64, 256 + h * 64 + 64))
    for h in range(4):
        cols += list(range(512 + h * 64, 512 + h * 64 + 64)) + list(range(768 + h * 64, 768 + h * 64 + 64))
    cols += list(range(1024, 3072))
    w_in_p = w_in[:, cols]
    chunked = lambda w: np.ascontiguousarray(w.reshape(8, 128, -1).transpose(1, 0, 2))
    sh = {}
    sh["w_in"] = chunked(w_in_p)
    sh["w_out"] = chunked(g("w_out")[0])
    sh["gains"] = np.stack([g("attn_norm")[0], g("moe_norm")[0], g("ple_norm")[0], g("final_norm")], 0)
    sh["lamv"] = np.stack([g("lambda_q1")[0], g("lambda_k1")[0], g("lambda_q2")[0], g("lambda_k2")[0]], 0)
    sh["subln"] = g("subln")
    rb = g("rel_bias")
    k = np.arange(128)[:, None]
    q = np.arange(128)[None, :]
    bn = np.zeros((128, 4, 2, 128), f32)
    for d in range(2):
        bk = _t5_bucket(q + 128 * d - k)
        bn[:, :, d, :] = rb[bk].transpose(0, 2, 1)
    sh["bnear"] = bn.reshape(128, -1)
    sh["c31"] = np.ascontiguousarray(np.broadcast_to(rb[31][None, :], (128, 4)))
    sh["rw"] = chunked(g("router_w")[0]).reshape(128, -1)
    sh["rb"] = g("router_b")
    wgu = g("w_gate_up")[0]
    glu = wgu[:, :, 0::2].reshape(NE, 8, 128, 8, 128)
    lin = wgu[:, :, 1::2].reshape(NE, 8, 128, 8, 128)
    gl = np.concatenate([glu, lin], axis=-1)
    sh["wgu"] = np.ascontiguousarray(gl.transpose(0, 3, 2, 1, 4))
    bgu = g("b_gate_up")[0]
    bg = bgu[:, 0::2].reshape(NE, 8, 128)
    bl = bgu[:, 1::2].reshape(NE, 8, 128)
    sh["bgl"] = np.ascontiguousarray(np.stack([bg, bl], -1).transpose(2, 0, 1, 3)).reshape(128, -1)
    sh["wd"] = np.ascontiguousarray(g("w_down")[0].reshape(NE, 8, 128, D).transpose(0, 2, 1, 3))
    sh["bd"] = g("b_down")[0]
    sh["pproj"] = np.ascontiguousarray(g("ple_proj")[0].reshape(2, 128, D).transpose(1, 0, 2))
    sh["pgate"] = chunked(g("ple_gate")[0])
    ident = np.eye(128, dtype=f32)
    jj = np.arange(128)[:, None]
    kk = np.arange(128)[None, :]
    negtri = -(jj >= kk).astype(f32)
    lt = (jj < kk).astype(f32)
    sh["cbf"] = np.concatenate([ident, negtri, -np.ones((128, 128), f32), lt, np.ones((128, 128), f32)], 1).astype(ml_dtypes.bfloat16)
    sh["cf32"] = np.concatenate([ident, (kk >= jj).astype(f32), (kk > jj).astype(f32)], 1)
    return sh


_CACHE = {}


def kernel(**inputs):
    sh = prep_shared(inputs)
    x = np.asarray(inputs["x"], dtype=np.float32)
    p = np.asarray(inputs["p"], dtype=np.float32)[0]
    if "nc" not in _CACHE:
        _CACHE["nc"] = build()
    nc = _CACHE["nc"]
    in_maps = []
    for c in range(8):
        m = dict(sh)
        m["x"] = np.ascontiguousarray(x[c])
        m["p"] = np.ascontiguousarray(p[c])
        in_maps.append(m)
    res = run_bass_kernel_spmd(nc, in_maps, core_ids=list(range(8)))
    return np.stack([np.asarray(r["out"], dtype=np.float32) for r in res.results], 0)
```

```python
from contextlib import ExitStack
import math
import numpy as np
import ml_dtypes
import concourse.bass as bass
import concourse.mybir as mybir
from concourse.bass_utils import run_bass_kernel_spmd

F32 = mybir.dt.float32
BF16 = mybir.dt.bfloat16
U32 = mybir.dt.uint32
I32 = mybir.dt.int32
AF = mybir.ActivationFunctionType
ALU = mybir.AluOpType
AX = mybir.AxisListType

S = 2048
D = 1024
NT = 16
NE = 32
RC = 384
NCH = 6
CAP = 2048
EPS = 1e-6
ENG = {"pe": "tensor", "act": "scalar", "dve": "vector", "pool": "gpsimd", "sp": "sync"}


class Buf:
    __slots__ = ("name", "w", "r", "sem", "cnt")

    def __init__(self, P, name):
        self.name = name
        self.w = []
        self.r = []
        self.sem = None
        self.cnt = 0
        P.bufs.append(self)


class Prog:
    def __init__(self, nc):
        self.nc = nc
        self.recs = {e: [] for e in ENG}
        self.waited = {e: {} for e in ENG}
        self.dma_sems = []
        self.bufs = []

    def _filter(self, eng, deps):
        out = []
        for t in deps:
            if t[0] == "c":
                _, e2, idx = t
                if e2 == eng and eng in ("pe", "sp"):
                    continue
                k = ("c", e2)
                if self.waited[eng].get(k, -1) >= idx:
                    continue
                self.waited[eng][k] = idx
                self.recs[e2][idx]["sig"] = True
                out.append(t)
            else:
                _, b, val = t
                k = ("d", id(b))
                if self.waited[eng].get(k, -1) >= val:
                    continue
                self.waited[eng][k] = val
                out.append(t)
        return out

    def _deps(self, eng, reads, writes):
        deps = []
        for b in reads:
            deps += b.w
        for b in writes:
            deps += b.w
            deps += b.r
        return self._filter(eng, deps)

    def op(self, eng, fn, reads=(), writes=(), accum=False):
        waits = self._deps(eng, reads, writes)
        idx = len(self.recs[eng])
        self.recs[eng].append(dict(waits=waits, fn=fn, sig=False, dma=None))
        tok = ("c", eng, idx)
        for b in reads:
            b.r.append(tok)
        for b in writes:
            if accum:
                b.w.append(tok)
            else:
                b.w = [tok]
                b.r = []
        return tok

    def dma(self, eng, fn, reads=(), writes=(), sembuf=None):
        waits = self._deps(eng, reads, writes)
        sb = sembuf if sembuf is not None else (writes[0] if writes else reads[0])
        if sb.sem is None:
            sb.sem = True
            self.dma_sems.append(sb)
        sb.cnt += 16
        tok = ("d", sb, sb.cnt)
        self.recs[eng].append(dict(waits=waits, fn=fn, sig=False, dma=sb, dmaval=sb.cnt))
        for b in reads:
            b.r.append(tok)
        for b in writes:
            b.w = [tok]
            b.r = []
        return tok

    def barrier(self):
        toks = []
        for e in ENG:
            for i in range(len(self.recs[e]) - 1, -1, -1):
                r = self.recs[e][i]
                if r["fn"] is not None and r["dma"] is None:
                    toks.append(("c", e, i))
                    break
        for b in self.bufs:
            toks += [t for t in b.w + b.r if t[0] == "d"]
            b.w = []
            b.r = []
        for e in ENG:
            w = self._filter(e, [t for t in toks if not (t[0] == "c" and t[1] == e)])
            self.recs[e].append(dict(waits=w, fn=None, sig=False, dma=None))

    def guard_begin(self, key):
        if not hasattr(self, "_wstack"):
            self._wstack = []
        self._wstack.append({e: dict(self.waited[e]) for e in ENG})
        for e in ENG:
            self.recs[e].append(dict(waits=[], fn=None, sig=False, dma=None, gb=key))

    def guard_end(self):
        for e in ENG:
            self.recs[e].append(dict(waits=[], fn=None, sig=False, dma=None, ge=True))
        self.waited = self._wstack.pop()

    def wait_all(self, eng, bufs):
        waits = self._deps(eng, list(bufs), list(bufs))
        self.recs[eng].append(dict(waits=waits, fn=None, sig=False, dma=None))

    def run(self, stack):
        nc = self.nc
        esem = {e: stack.enter_context(nc.semaphore("s_" + e)) for e in ENG}
        for i, b in enumerate(self.dma_sems):
            b.sem = stack.enter_context(nc.semaphore("d%d" % i))
        cnts = {}
        for e in ENG:
            c = 0
            for i, r in enumerate(self.recs[e]):
                if r["sig"]:
                    c += 1
                    cnts[(e, i)] = c
        block = stack.enter_context(nc.Block())

        def body(e):
            def emit(engine, r):
                for t in r["waits"]:
                    if t[0] == "c":
                        engine.wait_ge(esem[t[1]], cnts[(t[1], t[2])])
                    else:
                        engine.wait_ge(t[1].sem, t[2])
                if r["fn"] is None:
                    return
                ins = r["fn"](engine)
                if r["dma"] is not None:
                    ins.then_inc(r["dma"].sem, 16)
                elif r["sig"]:
                    ins.then_inc(esem[e], 1)

            def f(engine):
                recs = self.recs[e]
                reg = rthr = None
                if any("gb" in q for q in recs):
                    reg = stack.enter_context(engine.register("rc_" + e))
                    rthr = stack.enter_context(engine.register("rt_" + e))
                csum = [0]

                def match(i):
                    d_ = 0
                    j = i
                    while True:
                        if "gb" in recs[j]:
                            d_ += 1
                        elif "ge" in recs[j]:
                            d_ -= 1
                            if d_ == 0:
                                return j
                        j += 1

                def process(lo, hi):
                    i = lo
                    while i < hi:
                        r = recs[i]
                        if "gb" in r:
                            j = match(i)
                            inner = [q for q in recs[i + 1:j] if "gb" not in q and "ge" not in q]
                            ex_, thr = r["gb"]
                            if any(q["fn"] is not None or q["waits"] for q in inner):
                                c_before = csum[0]
                                engine.reg_load(reg, self.cnt_ap(ex_))
                                engine.reg_mov(rthr, thr)
                                with engine.If_lt(rthr, reg):
                                    process(i + 1, j)
                                csum[0] = c_before
                                nsig = sum(1 for q in inner if q["sig"])
                                dmas = [q for q in inner if q["dma"] is not None]
                                if nsig or dmas:
                                    with engine.Else():
                                        if nsig:
                                            if c_before > 0:
                                                engine.wait_ge(esem[e], c_before)
                                            engine.sem_inc(esem[e], nsig)
                                        for q in dmas:
                                            if q["dmaval"] - 16 > 0:
                                                engine.wait_ge(q["dma"].sem, q["dmaval"] - 16)
                                            engine.sem_inc(q["dma"].sem, 16)
                                csum[0] = c_before + nsig
                            i = j + 1
                            continue
                        emit(engine, r)
                        if r["sig"]:
                            csum[0] += 1
                        i += 1

                process(0, len(recs))
            return f

        block.tensor(body("pe"))
        block.scalar(body("act"))
        block.vector(body("dve"))
        block.gpsimd(body("pool"))
        block.sync(body("sp"))


def build(stop=None):
    nc = bass.Bass("TRN2", target_bir_lowering=False, dynamic_dma_scratch_size=8192)
    dram = lambda n, s, d=F32, k="ExternalInput": nc.dram_tensor(n, s, d, kind=k).ap()
    x_d = dram("x", [S, D])
    p_d = dram("p", [S, 256])
    win_d = dram("w_in", [128, 8, 3072])
    wout_d = dram("w_out", [128, 8, D])
    gains_d = dram("gains", [4, D])
    lamv_d = dram("lamv", [4, 64])
    subln_d = dram("subln", [1, 128])
    bnear_d = dram("bnear", [128, 4 * 2 * 128])
    c31_d = dram("c31", [128, 4])
    rw_d = dram("rw", [128, 8 * 32])
    rb_d = dram("rb", [1, 32])
    wgu_d = dram("wgu", [NE, 8, 128, 8, 256])
    bgl_d = dram("bgl", [128, NE * 8 * 2])
    wd_d = dram("wd", [NE, 128, 8, D])
    bd_d = dram("bd", [NE, D])
    pproj_d = dram("pproj", [128, 2, D])
    pgate_d = dram("pgate", [128, 8, D])
    cb_d = dram("cbf", [128, 5 * 128], BF16)
    cf_d = dram("cf32", [128, 3 * 128])
    out_d = dram("out", [S, D], F32, "ExternalOutput")
    h1_d = dram("h1s", [S, D], F32, "Internal")
    xg_d = dram("xg", [NE * CAP, D], BF16, "Internal")
    yg_d = dram("yg", [NE * CAP, D], F32, "Internal")
    dbg = {}

    st = ExitStack()
    P = Prog(nc)
    B = lambda n: Buf(P, n)
    sbt = lambda n, s, d: st.enter_context(nc.sbuf_tensor("sb_" + n, s, d))
    pall = st.enter_context(nc.psum_tensor("pall", [128, 4096], F32))
    psb = [pall[:, i * 512:(i + 1) * 512] for i in range(8)]

    cb = sbt("cb", [128, 5 * 128], BF16)
    cf = sbt("cf", [128, 3 * 128], F32)
    identb, negtri, negones, ltm, onesb = [cb[:, i * 128:(i + 1) * 128] for i in range(5)]
    identf, mask0, strictm = [cf[:, i * 128:(i + 1) * 128] for i in range(3)]
    gainb = sbt("gainb", [128, D], F32)
    gain2 = sbt("gain2", [128, D], F32)
    ebfix = sbt("ebfix", [128, 4 * 2 * 128], F32)
    c31 = sbt("c31", [128, 4], F32)
    lamt = sbt("lamt", [128, 4 * 64], F32)
    lams = sbt("lams", [128, 8], F32)
    subg = sbt("subg", [128, 128], F32)
    rw = sbt("rw", [128, 8 * 32], F32)
    rbb = sbt("rbb", [128, 32], F32)
    bgl = sbt("bgl", [128, NE * 8 * 2], F32)
    cst = sbt("cst", [128, 8], F32)
    stats = sbt("stats", [128, NT * 8], F32)
    meta_dest = sbt("meta_dest", [128, NT * 4], I32)
    meta_g = sbt("meta_g", [128, NT * 4], F32)
    bConst = B("const")
    bGain = B("gain")
    bGain2 = B("gain2")

    AR = sbt("arena", [128, 152 * 1024], mybir.dt.uint8)
    K = 1024
    OFF_HN, OFF_QK, OFF_AT, OFF_W, OFF_V, OFF_T = 0, 32 * K, 64 * K, 96 * K, 128 * K, 145 * K

    def view(off, shape, dt):
        nb = {F32: 4, BF16: 2, I32: 4, U32: 4}[dt]
        n = 1
        for s_ in shape[1:]:
            n *= s_
        ap = AR[:, off:off + n * nb].bitcast(dt)
        if len(shape) == 3:
            ap = ap.rearrange("p (a b) -> p a b", b=shape[2])
        elif len(shape) == 4:
            ap = ap.rearrange("p (a b c) -> p a b c", b=shape[2], c=shape[3])
        return ap

    P.dma("sp", lambda e: e.dma_start(out=cb[:], in_=cb_d), writes=[bConst])
    bC2 = B("c2"); bC3 = B("c3"); bC4 = B("c4"); bC5 = B("c5"); bC6 = B("c6"); bC7 = B("c7"); bC8 = B("c8"); bC9 = B("c9")
    P.dma("sp", lambda e: e.dma_start(out=cf[:], in_=cf_d), writes=[bC2])
    P.dma("sp", lambda e: e.dma_start(out=ebfix[:], in_=bnear_d), writes=[bC3])
    P.dma("sp", lambda e: e.dma_start(out=c31[:], in_=c31_d), writes=[bC4])
    P.dma("sp", lambda e: e.dma_start(out=lamt[:], in_=lamv_d.rearrange("a b -> (a b)").partition_broadcast(128)), writes=[bC5])
    P.dma("sp", lambda e: e.dma_start(out=subg[:], in_=subln_d.rearrange("a b -> (a b)").partition_broadcast(128)), writes=[bC6])
    P.dma("sp", lambda e: e.dma_start(out=rw[:], in_=rw_d), writes=[bC7])
    P.dma("sp", lambda e: e.dma_start(out=rbb[:], in_=rb_d.rearrange("a b -> (a b)").partition_broadcast(128)), writes=[bC8])
    P.dma("sp", lambda e: e.dma_start(out=bgl[:], in_=bgl_d), writes=[bC9])
    P.dma("sp", lambda e: e.dma_start(out=gainb[:], in_=gains_d[0].partition_broadcast(128)), writes=[bGain])
    bCst = B("cst")
    P.op("dve", lambda e: e.memset(cst[:, 0:1], -0.5), writes=[bCst])
    for h in range(4):
        P.op("dve", lambda e, h=h: e.tensor_scalar(out=ebfix[:, h * 256:(h + 1) * 256], in0=ebfix[:, h * 256:(h + 1) * 256],
                                                   scalar1=c31[:, h:h + 1], scalar2=None, op0=ALU.subtract),
             reads=[bC3, bC4], writes=[bC3])
    P.op("act", lambda e: e.activation(out=ebfix[:], in_=ebfix[:], func=AF.Exp), reads=[bC3], writes=[bC3])
    for h in range(4):
        P.op("dve", lambda e, h=h: e.tensor_tensor(out=ebfix[:, h * 256:h * 256 + 128], in0=ebfix[:, h * 256:h * 256 + 128], in1=mask0, op=ALU.mult),
             reads=[bC3, bC2], writes=[bC3])
    P.op("dve", lambda e: e.tensor_tensor(out=lamt[:, 0:64], in0=lamt[:, 0:64], in1=lamt[:, 64:128], op=ALU.mult), reads=[bC5], writes=[bC5])
    P.op("dve", lambda e: e.tensor_tensor(out=lamt[:, 128:192], in0=lamt[:, 128:192], in1=lamt[:, 192:256], op=ALU.mult), reads=[bC5], writes=[bC5])
    P.op("dve", lambda e: e.tensor_reduce(out=lams[:, 0:1], in_=lamt[:, 0:64], axis=AX.X, op=ALU.add), reads=[bC5], writes=[bC5])
    P.op("dve", lambda e: e.tensor_reduce(out=lams[:, 1:2], in_=lamt[:, 128:192], axis=AX.X, op=ALU.add), reads=[bC5], writes=[bC5])
    P.op("act", lambda e: e.activation(out=lams[:, 2:4], in_=lams[:, 0:2], func=AF.Exp), reads=[bC5], writes=[bC5])
    P.op("dve", lambda e: e.tensor_tensor(out=lams[:, 4:5], in0=lams[:, 3:4], in1=lams[:, 2:3], op=ALU.subtract), reads=[bC5], writes=[bC5])
    P.op("dve", lambda e: e.tensor_scalar(out=lams[:, 4:5], in0=lams[:, 4:5], scalar1=-0.2, scalar2=None, op0=ALU.add), reads=[bC5], writes=[bC5])
    P.op("dve", lambda e: e.tensor_scalar(out=subg[:], in0=subg[:], scalar1=0.8, scalar2=None, op0=ALU.mult), reads=[bC6], writes=[bC6])
    bglv = bgl[:].rearrange("p (a t) -> p a t", t=2)
    P.op("dve", lambda e: e.tensor_scalar(out=bglv[:, :, 1:2], in0=bglv[:, :, 1:2], scalar1=1.0, scalar2=None, op0=ALU.add), reads=[bC9], writes=[bC9])
    neglam = lams[:, 4:5]

    hnT = view(OFF_HN, [128, 8, S], BF16)
    bHn = [B("hn%d" % i) for i in range(NT)]
    xt = [view(OFF_W + i * 4096, [128, D], F32) for i in range(2)]
    xs = [view(OFF_W + 8192 + i * 2048, [128, D], BF16) for i in range(2)]
    junkb = view(OFF_T, [128, D], BF16)
    bXt = [B("xt%d" % i) for i in range(2)]
    bXs = [B("xs%d" % i) for i in range(2)]
    bJ = B("junk")
    bSt = [B("st%d" % i) for i in range(NT)]
    bPs = [B("ps%d" % i) for i in range(8)]

    def rms_stats(i, src, srcbuf, n):
        c0 = i * 8
        P.op("act", lambda e: e.activation(out=junkb[:, 0:n], in_=src, func=AF.Square, accum_out=stats[:, c0:c0 + 1]),
             reads=[srcbuf], writes=[bJ, bSt[i]])
        P.op("dve", lambda e: e.tensor_scalar(out=stats[:, c0 + 1:c0 + 2], in0=stats[:, c0:c0 + 1], scalar1=1.0 / n, scalar2=EPS, op0=ALU.mult, op1=ALU.add),
             reads=[bSt[i]], writes=[bSt[i]])
        P.op("pool", lambda e: e.tensor_tensor(out=stats[:, c0 + 3:c0 + 4], in0=stats[:, c0 + 1:c0 + 2], in1=cst[:, 0:1], op=ALU.pow),
             reads=[bSt[i], bCst], writes=[bSt[i]])
        return stats[:, c0 + 3:c0 + 4]

    for i in range(NT):
        b = i % 2
        P.dma("sp", lambda e, i=i, b=b: e.dma_start(out=xt[b], in_=x_d[i * 128:(i + 1) * 128, :]), writes=[bXt[b]])
        rstd = rms_stats(i, xt[b], bXt[b], D)
        P.op("dve", lambda e, b=b, rstd=rstd: e.scalar_tensor_tensor(out=xs[b], in0=xt[b], scalar=rstd, in1=gainb[:], op0=ALU.mult, op1=ALU.mult),
             reads=[bXt[b], bSt[i], bGain], writes=[bXs[b]])
        pT = psb[b].bitcast(BF16)
        for c in range(8):
            P.op("pe", lambda e, c=c, b=b, pT=pT: e.transpose(pT[:, c * 128:(c + 1) * 128], xs[b][:, c * 128:(c + 1) * 128], identb),
                 reads=[bXs[b], bConst], writes=[bPs[b]])
        P.op("act", lambda e, i=i, pT=pT: e.activation(out=hnT[:, :, i * 128:(i + 1) * 128], in_=pT.rearrange("p (c t) -> p c t", t=128), func=AF.Copy),
             reads=[bPs[b]], writes=[bHn[i]])

    if stop == "A":
        dbg["hnT"] = (hnT, [128, 8, S], BF16)
        return finish(nc, P, st, dbg)

    QK = view(OFF_QK, [128, 8, S], BF16)
    VV = view(OFF_V, [128, NT, 516], BF16)
    wsl = [view(OFF_W + i * 4096, [128, 8, 256], BF16) for i in range(3)]
    bW = [B("wsl%d" % i) for i in range(3)]
    bQK = [B("qk%d" % i) for i in range(8)]
    bV = [B("v%d" % i) for i in range(NT)]
    slab_ctr = [0]
    evac_ctr = [0]

    def project(col0, kind):
        P.barrier()
        if kind == "diff":
            VD4 = VV.rearrange("p t (h c) -> p t h c", c=129)
            P.op("dve", lambda e: e.memset(VD4[:, :, :, 128:129], 1.0), writes=bV)
        for s in range(6):
            wi = slab_ctr[0] % 3
            slab_ctr[0] += 1
            c_lo = col0 + s * 256
            P.dma("pool", lambda e, wi=wi, c_lo=c_lo: e.dma_start(out=wsl[wi], in_=win_d[:, :, c_lo:c_lo + 256]), writes=[bW[wi]])
            if s < 4:
                for gg in range(2):
                    gi = s * 2 + gg
                    for tc in range(4):
                        bk = evac_ctr[0] % 4
                        evac_ctr[0] += 1
                        for c in range(8):
                            P.op("pe", lambda e, wi=wi, gg=gg, tc=tc, c=c, bk=bk: e.matmul(
                                psb[bk], lhsT=wsl[wi][:, c, gg * 128:(gg + 1) * 128], rhs=hnT[:, c, tc * 512:(tc + 1) * 512],
                                start=(c == 0), stop=(c == 7)),
                                reads=[bW[wi]] + bHn[tc * 4:tc * 4 + 4], writes=[bPs[bk]])
                        sc = 0.125 if s < 2 else 1.0
                        if evac_ctr[0] % 2 == 0:
                            P.op("act", lambda e, gi=gi, tc=tc, bk=bk, sc=sc: e.activation(out=QK[:, gi, tc * 512:(tc + 1) * 512], in_=psb[bk], func=AF.Copy, scale=sc),
                                 reads=[bPs[bk]], writes=[bQK[gi]], accum=True)
                        else:
                            P.op("dve", lambda e, gi=gi, tc=tc, bk=bk, sc=sc: e.tensor_scalar(out=QK[:, gi, tc * 512:(tc + 1) * 512], in0=psb[bk], scalar1=sc, scalar2=None, op0=ALU.mult),
                                 reads=[bPs[bk]], writes=[bQK[gi]], accum=True)
            else:
                vs = s - 4
                for i in range(NT):
                    bk = evac_ctr[0] % 4
                    evac_ctr[0] += 1
                    for c in range(8):
                        P.op("pe", lambda e, wi=wi, i=i, c=c, bk=bk: e.matmul(
                            psb[bk][:, 0:256], lhsT=hnT[:, c, i * 128:(i + 1) * 128], rhs=wsl[wi][:, c, :], start=(c == 0), stop=(c == 7)),
                            reads=[bW[wi], bHn[i]], writes=[bPs[bk]])
                    if kind == "diff":
                        dst = VV[:, i, vs * 258:(vs + 1) * 258].rearrange("p (h c) -> p h c", c=129)[:, :, 0:128]
                        src = psb[bk][:, 0:256].rearrange("p (h c) -> p h c", c=128)
                    else:
                        dst = VV[:, i, vs * 256:(vs + 1) * 256]
                        src = psb[bk][:, 0:256]
                    if evac_ctr[0] % 2 == 0:
                        P.op("act", lambda e, dst=dst, src=src: e.activation(out=dst, in_=src, func=AF.Copy), reads=[bPs[bk]], writes=[bV[i]], accum=True)
                    else:
                        P.op("dve", lambda e, dst=dst, src=src: e.tensor_copy(out=dst, in_=src), reads=[bPs[bk]], writes=[bV[i]], accum=True)

    project(0, "diff")
    if stop == "B":
        dbg["QK"] = (QK, [128, 8, S], BF16)
        dbg["VV"] = (VV, [128, NT, 516], BF16)
        return finish(nc, P, st, dbg)

    P.barrier()
    attnT = view(OFF_AT, [128, 8, S], BF16)
    bAT = [B("at%d" % i) for i in range(NT)]
    NSLOT = 32
    Er = [view(OFF_W + i * 1024, [128, 2, 256], BF16) for i in range(NSLOT)]
    bE = [B("E%d" % i) for i in range(NSLOT)]
    def tv(par, k):
        base = OFF_T + par * 1296
        if k == 0:
            return view(base, [128, 130], F32)
        if k == 1:
            return view(base + 520, [128, 130], F32)
        return view(base + 1040, [128, 128], BF16)
    bEp = [B("ep%d" % i) for i in range(4)]
    late = []
    bSS = [B("S%d" % i) for i in range(2)]
    bO = [B("O%d" % m) for m in range(2)]
    bTp = [B("tp%d" % i) for i in range(2)]
    VD4 = VV.rearrange("p t (h c) -> p t h c", c=129)
    ebv = ebfix[:].rearrange("p (h d q) -> p h d q", h=4, d=2)

    units = [(h, c) for h in range(4) for c in range(8)]
    import os
    if stop == 'C1':
        units = units[:1]
    if os.environ.get('KLIM'):
        units = units[:int(os.environ['KLIM'])]
    blk_ctr = [0]
    ep_ctr = [0]

    def av_items(h, c, slots):
        items = []
        for j in range(2):
            for m in range(2):
                kbs = list(range(0, 2 * c + j + 1))
                for kb in kbs:
                    items.append((j, m, kb, kb == 0, kb == kbs[-1]))
        return items

    def emit_av(h, c, slots, it):
        j, m, kb, first, last = it
        ob = psb[4 + m][:, 0:129]
        sl = slots[kb]
        P.op("pe", lambda e: e.matmul(ob, lhsT=Er[sl][:, m, j * 128:(j + 1) * 128], rhs=VD4[:, kb, h, :], start=first, stop=last),
             reads=[bE[sl], bV[kb]], writes=[bO[m]])
        if last:
            par = ep_ctr[0] % 4
            P.op("dve", lambda e: e.tensor_copy(out=tv(par, m)[:, 0:129], in_=ob), reads=[bO[m]], writes=[bEp[par]], accum=(m == 1))
            if m == 1:
                epilogue(h, c, j)

    def epilogue(h, c, j):
        qb = 2 * c + j
        par = ep_ctr[0] % 4
        ep_ctr[0] += 1
        si = qb
        c0 = si * 8
        o1 = tv(par, 0); o2 = tv(par, 1); obf = tv(par, 2)
        ep = [bEp[par]]
        P.op("dve", lambda e: e.reciprocal(out=stats[:, c0:c0 + 1], in_=o1[:, 128:129]), reads=ep, writes=[bSt[si]])
        P.op("dve", lambda e: e.reciprocal(out=stats[:, c0 + 1:c0 + 2], in_=o2[:, 128:129]), reads=ep, writes=[bSt[si]])
        P.op("dve", lambda e: e.tensor_scalar(out=stats[:, c0 + 2:c0 + 3], in0=stats[:, c0 + 1:c0 + 2], scalar1=neglam, scalar2=None, op0=ALU.mult),
             reads=[bSt[si], bC5], writes=[bSt[si]])
        P.op("dve", lambda e: e.tensor_scalar(out=o2[:, 0:128], in0=o2[:, 0:128], scalar1=stats[:, c0 + 2:c0 + 3], scalar2=None, op0=ALU.mult),
             reads=ep + [bSt[si]], writes=ep)
        P.op("dve", lambda e: e.scalar_tensor_tensor(out=o1[:, 0:128], in0=o1[:, 0:128], scalar=stats[:, c0:c0 + 1], in1=o2[:, 0:128], op0=ALU.mult, op1=ALU.add),
             reads=ep + [bSt[si]], writes=ep)
        P.op("dve", lambda e: e.tensor_tensor(out=o2[:, 0:128], in0=o1[:, 0:128], in1=o1[:, 0:128], op=ALU.mult), reads=ep, writes=ep)
        P.op("dve", lambda e: e.tensor_reduce(out=stats[:, c0 + 3:c0 + 4], in_=o2[:, 0:128], axis=AX.X, op=ALU.add), reads=ep, writes=[bSt[si]])
        P.op("dve", lambda e: e.tensor_scalar(out=stats[:, c0 + 4:c0 + 5], in0=stats[:, c0 + 3:c0 + 4], scalar1=1.0 / 128, scalar2=EPS, op0=ALU.mult, op1=ALU.add),
             reads=[bSt[si]], writes=[bSt[si]])
        P.op("pool", lambda e: e.tensor_tensor(out=stats[:, c0 + 5:c0 + 6], in0=stats[:, c0 + 4:c0 + 5], in1=cst[:, 0:1], op=ALU.pow),
             reads=[bSt[si], bCst], writes=[bSt[si]])
        P.op("dve", lambda e: e.scalar_tensor_tensor(out=obf, in0=o1[:, 0:128], scalar=stats[:, c0 + 5:c0 + 6], in1=subg[:], op0=ALU.mult, op1=ALU.mult),
             reads=ep + [bSt[si], bC6], writes=ep)
        tb = psb[6 + par % 2].bitcast(BF16)

        def fin():
            P.op("pe", lambda e: e.transpose(tb[:, 0:128], obf, identb), reads=[bEp[par], bConst], writes=[bTp[par % 2]])
            P.op("dve", lambda e: e.tensor_copy(out=attnT[:, h, qb * 128:(qb + 1) * 128], in_=tb[:, 0:128]), reads=[bTp[par % 2]], writes=[bAT[qb]], accum=True)
        late.append(fin)

    pending = []
    for ui, (h, c) in enumerate(units):
        nb = 2 * c + 2
        slots = {}
        per = (len(pending) + nb - 1) // nb if pending else 0
        late_now = list(late)
        del late[:]
        for kb in range(nb):
            if kb == 1:
                for f_ in late_now:
                    f_()
            sl = blk_ctr[0] % NSLOT
            blk_ctr[0] += 1
            slots[kb] = sl
            sp_ = kb % 2
            lo = 128 if kb == 2 * c + 1 else 0
            sb3 = pall[:, sp_ * 512:sp_ * 512 + 2048].rearrange("p (m r) -> p m r", m=2)
            for m in range(2):
                P.op("pe", lambda e, m=m, kb=kb, lo=lo, sp_=sp_, h=h, c=c: e.matmul(
                    psb[2 * m + sp_][:, lo:256], lhsT=QK[m * 64:(m + 1) * 64, 4 + h, kb * 128:(kb + 1) * 128],
                    rhs=QK[m * 64:(m + 1) * 64, h, c * 256 + lo:(c + 1) * 256], start=True, stop=True),
                    reads=[bQK[4 + h], bQK[h]], writes=[bSS[sp_]])
            P.op("act", lambda e, sl=sl, lo=lo, sb3=sb3: e.activation(out=Er[sl][:, :, lo:256], in_=sb3[:, :, lo:256], func=AF.Exp),
                 reads=[bSS[sp_]], writes=[bE[sl]])
            for j in range(2):
                d = 2 * c + j - kb
                if 0 <= d <= 1:
                    for m in range(2):
                        P.op("dve", lambda e, sl=sl, m=m, j=j, d=d, h=h: e.tensor_tensor(
                            out=Er[sl][:, m, j * 128:(j + 1) * 128], in0=Er[sl][:, m, j * 128:(j + 1) * 128], in1=ebv[:, h, d, :], op=ALU.mult),
                            reads=[bE[sl], bC3], writes=[bE[sl]])
            for _ in range(per):
                if pending:
                    emit_av(*pending.pop(0))
        while pending:
            emit_av(*pending.pop(0))
        pending = [(h, c, slots, it) for it in av_items(h, c, slots)]
    while pending:
        emit_av(*pending.pop(0))
    for f_ in late:
        f_()

    if stop == "C1":
        dbg["E0"] = (Er[0], [128, 2, 256], BF16)
        dbg["E1"] = (Er[1], [128, 2, 256], BF16)
        dbg["ebfix"] = (ebfix[:], [128, 1024], F32)
        dbg["lams"] = (lams[:], [128, 8], F32)
        dbg["o1s"] = (tv(0, 0), [128, 130], F32)
        dbg["obf"] = (tv(0, 2), [128, 128], BF16)
        dbg["at0"] = (attnT[:, 0, 0:256], [128, 256], BF16)
        dbg["stats"] = (stats[:], [128, 128], F32)
        return finish(nc, P, st, dbg)
    if stop == "C":
        dbg["attnT"] = (attnT, [128, 8, S], BF16)
        return finish(nc, P, st, dbg)

    project(1536, "sb")
    P.barrier()
    Wr = [view(OFF_W + i * 512, [128, 256], BF16) for i in range(4)]
    bWr = [B("Wr%d" % i) for i in range(4)]
    e32 = [view(OFF_W + 2048 + i * 1024, [128, 256], F32) for i in range(2)]
    Lb = [view(OFF_W + 4096 + i * 512, [128, 256], BF16) for i in range(2)]
    Rb = [view(OFF_W + 5120 + i * 512, [128, 256], BF16) for i in range(3)]
    be32 = [B("e32%d" % i) for i in range(2)]
    bL = [B("L%d" % i) for i in range(2)]
    bR = [B("R%d" % i) for i in range(3)]
    bZ = [B("Z%d" % i) for i in range(2)]
    bX = [B("X%d" % i) for i in range(2)]
    bOT = [B("OT%d" % i) for i in range(2)]

    blocks = []
    for hd in range(8):
        for c in range(8):
            for kb in range(2 * c + 1, -1, -1):
                blocks.append((hd, c, kb))
    NB = len(blocks)

    def sb_pe1(i):
        hd, c, kb = blocks[i]
        g, po = hd // 2, (hd % 2) * 64
        lo = 128 if kb == 2 * c + 1 else 0
        pz = i % 2
        P.op("pe", lambda e: e.matmul(psb[pz][:, lo:256], lhsT=QK[po:po + 64, 4 + g, kb * 128:(kb + 1) * 128],
                                      rhs=QK[po:po + 64, g, c * 256 + lo:(c + 1) * 256], start=True, stop=True),
             reads=[bQK[4 + g], bQK[g]], writes=[bZ[pz]])

    def sb_act1(i):
        hd, c, kb = blocks[i]
        lo = 128 if kb == 2 * c + 1 else 0
        pz = i % 2
        first = kb == 2 * c + 1
        P.op("act", lambda e: e.activation(out=e32[pz][:, lo:256], in_=psb[pz][:, lo:256], func=AF.Exp), reads=[bZ[pz]], writes=[be32[pz]])
        if first:
            P.op("dve", lambda e: e.memset(e32[pz][:, 0:128], 0.0), writes=[be32[pz]], accum=True)
        j = kb - 2 * c
        if j >= 0:
            P.op("dve", lambda e: e.tensor_tensor(out=e32[pz][:, j * 128:(j + 1) * 128], in0=e32[pz][:, j * 128:(j + 1) * 128], in1=strictm, op=ALU.mult),
                 reads=[be32[pz], bC2], writes=[be32[pz]])
        P.op("act", lambda e: e.activation(out=Lb[pz][:], in_=e32[pz][:], func=AF.Ln, bias=1.0), reads=[be32[pz]], writes=[bL[pz]])
        if kb > 0:
            if first:
                P.op("dve", lambda e: e.tensor_copy(out=Rb[(i + 1) % 3][:], in_=Lb[pz][:]), reads=[bL[pz]], writes=[bR[(i + 1) % 3]])
            else:
                P.op("dve", lambda e: e.tensor_tensor(out=Rb[(i + 1) % 3][:], in0=Rb[i % 3][:], in1=Lb[pz][:], op=ALU.add),
                     reads=[bL[pz], bR[i % 3]], writes=[bR[(i + 1) % 3]])

    def sb_pe2(i):
        hd, c, kb = blocks[i]
        g, po = hd // 2, (hd % 2) * 64
        lo = 128 if kb == 2 * c + 1 else 0
        pz = i % 2
        first = kb == 2 * c + 1
        xb = psb[2 + pz]
        P.op("pe", lambda e: e.matmul(xb[:, lo:256], lhsT=QK[po:po + 64, 4 + g, kb * 128:(kb + 1) * 128],
                                      rhs=QK[po:po + 64, g, c * 256 + lo:(c + 1) * 256], start=True, stop=False),
             reads=[bQK[4 + g], bQK[g]], writes=[bX[pz]])
        P.op("pe", lambda e: e.matmul(xb[:, lo:256], lhsT=negtri, rhs=Lb[pz][:, lo:256], start=False, stop=first),
             reads=[bL[pz], bConst], writes=[bX[pz]])
        if not first:
            P.op("pe", lambda e: e.matmul(xb[:, lo:256], lhsT=negones, rhs=Rb[i % 3][:, lo:256], start=False, stop=True),
                 reads=[bR[i % 3], bConst], writes=[bX[pz]])

    def sb_act2(i):
        hd, c, kb = blocks[i]
        lo = 128 if kb == 2 * c + 1 else 0
        pz = i % 2
        wi = i % 4
        first = kb == 2 * c + 1
        P.op("act", lambda e: e.activation(out=Wr[wi][:, lo:256], in_=psb[2 + pz][:, lo:256], func=AF.Exp), reads=[bX[pz]], writes=[bWr[wi]])
        if first:
            P.op("dve", lambda e: e.memset(Wr[wi][:, 0:128], 0.0), writes=[bWr[wi]], accum=True)
        j = kb - 2 * c
        if j >= 0:
            P.op("dve", lambda e: e.tensor_tensor(out=Wr[wi][:, j * 128:(j + 1) * 128], in0=Wr[wi][:, j * 128:(j + 1) * 128], in1=strictm, op=ALU.mult),
                 reads=[bWr[wi], bC2], writes=[bWr[wi]])

    unit_ctr = [0]

    def sb_pe3(i):
        hd, c, kb = blocks[i]
        g, po = hd // 2, (hd % 2) * 64
        wi = i % 4
        first = kb == 2 * c + 1
        last = kb == 0
        up = (hd * 8 + c) % 2
        ob = psb[4 + up]
        P.op("pe", lambda e: e.matmul(ob[po:po + 64, 0:256], lhsT=VV[:, kb, hd * 64:(hd + 1) * 64], rhs=Wr[wi][:], start=first, stop=last),
             reads=[bWr[wi], bV[kb]], writes=[bOT[up]])
        if last:
            if (hd * 8 + c) % 2 == 0:
                P.op("act", lambda e: e.activation(out=attnT[po:po + 64, 4 + g, c * 256:(c + 1) * 256], in_=ob[po:po + 64, 0:256], func=AF.Copy),
                     reads=[bOT[up]], writes=[bAT[2 * c], bAT[2 * c + 1]], accum=True)
            else:
                P.op("dve", lambda e: e.tensor_copy(out=attnT[po:po + 64, 4 + g, c * 256:(c + 1) * 256], in_=ob[po:po + 64, 0:256]),
                     reads=[bOT[up]], writes=[bAT[2 * c], bAT[2 * c + 1]], accum=True)

    for s_ in range(NB + 2):
        if s_ < NB:
            sb_pe1(s_)
            sb_act1(s_)
        if 0 <= s_ - 1 < NB:
            sb_pe2(s_ - 1)
            sb_act2(s_ - 1)
        if 0 <= s_ - 2 < NB:
            sb_pe3(s_ - 2)

    if stop == "D":
        dbg["attnT"] = (attnT, [128, 8, S], BF16)
        return finish(nc, P, st, dbg)

    P.barrier()
    KB = 1024
    hres = view(0, [128, NT, D], F32)
    tT = view(96 * KB, [128, 8, S], BF16)
    wob = view(128 * KB, [128, 8, D], BF16)
    xt1 = view(144 * KB, [128, D], F32)
    tb1 = view(148 * KB, [128, D], BF16)
    junk2 = view(150 * KB, [128, D], BF16)
    tmpx = sbt("tmpx", [128, 4 * 512], F32)
    comb = sbt("comb", [128, NT * 32], F32)
    lgs = sbt("lgs", [128, 4 * 32], F32)
    mx8 = sbt("mx8", [128, 32], F32)
    ix8 = sbt("ix8", [128, 8], U32)
    mbt = sbt("mbt", [128, 96], BF16)
    bIx = B("ix8"); bMb = B("mskb"); bMacc = [B("macc0"), B("macc1")]; bXg = B("xg"); bH1d = B("h1d"); bMeta = B("meta")
    rwb = sbt("rwb", [128, 256], BF16)
    bH = [B("h%d" % i_) for i_ in range(NT)]
    bTT = [B("tT%d" % i_) for i_ in range(NT)]
    bWo = B("wo"); bX1 = B("x1"); bTb = B("tb1"); bJ2 = B("junk2"); bRwb = B("rwb"); bLg = B("lg"); bMx = B("mx"); bComb = B("comb")
    bPE = [B("pe%d" % i_) for i_ in range(8)]
    P.dma("pool", lambda e: e.dma_start(out=wob, in_=wout_d), writes=[bWo])
    P.dma("pool", lambda e: e.dma_start(out=rwb[:], in_=rw_d), writes=[bRwb])
    P.dma("sp", lambda e: e.dma_start(out=gainb[:], in_=gains_d[1].partition_broadcast(128)), writes=[bGain])

    def rms2(i_, src, srcbuf, n):
        c0 = i_ * 8
        P.op("act", lambda e: e.activation(out=junk2[:, 0:n], in_=src, func=AF.Square, accum_out=stats[:, c0:c0 + 1]), reads=[srcbuf], writes=[bJ2, bSt[i_]])
        P.op("dve", lambda e: e.tensor_scalar(out=stats[:, c0 + 1:c0 + 2], in0=stats[:, c0:c0 + 1], scalar1=1.0 / n, scalar2=EPS, op0=ALU.mult, op1=ALU.add), reads=[bSt[i_]], writes=[bSt[i_]])
        P.op("pool", lambda e: e.tensor_tensor(out=stats[:, c0 + 3:c0 + 4], in0=stats[:, c0 + 1:c0 + 2], in1=cst[:, 0:1], op=ALU.pow), reads=[bSt[i_], bCst], writes=[bSt[i_]])
        return stats[:, c0 + 3:c0 + 4]

    xt1s = [xt1, tmpx[:, 0:1024]]
    tb1s = [tb1, tmpx[:, 1024:1536].bitcast(BF16)]
    bX1s = [bX1, B("x1b")]
    bTbs = [bTb, B("tb1b")]

    def phaseE(i_):
        xt1 = xt1s[i_ % 2]; tb1 = tb1s[i_ % 2]; bX1 = bX1s[i_ % 2]; bTb = bTbs[i_ % 2]
        P.dma("sp", lambda e: e.dma_start(out=xt1, in_=x_d[i_ * 128:(i_ + 1) * 128, :]), writes=[bX1])
        for half in range(2):
            bk = 2 * (i_ % 2) + half
            for c_ in range(8):
                P.op("pe", lambda e, c_=c_, bk=bk, half=half: e.matmul(psb[bk], lhsT=attnT[:, c_, i_ * 128:(i_ + 1) * 128], rhs=wob[:, c_, half * 512:(half + 1) * 512], start=(c_ == 0), stop=(c_ == 7)),
                     reads=[bAT[i_], bWo], writes=[bPE[bk]])
            P.op("dve", lambda e, bk=bk, half=half: e.tensor_tensor(out=hres[:, i_, half * 512:(half + 1) * 512], in0=psb[bk], in1=xt1[:, half * 512:(half + 1) * 512], op=ALU.add),
                 reads=[bPE[bk], bX1], writes=[bH[i_]], accum=(half == 1))
        rstd = rms2(i_, hres[:, i_, :], bH[i_], D)
        P.op("dve", lambda e: e.scalar_tensor_tensor(out=tb1, in0=hres[:, i_, :], scalar=rstd, in1=gainb[:], op0=ALU.mult, op1=ALU.mult), reads=[bH[i_], bSt[i_], bGain], writes=[bTb])
        pT = psb[4 + i_ % 2].bitcast(BF16)
        for c_ in range(8):
            P.op("pe", lambda e, c_=c_: e.transpose(pT[:, c_ * 128:(c_ + 1) * 128], tb1[:, c_ * 128:(c_ + 1) * 128], identb), reads=[bTb, bConst], writes=[bPE[4 + i_ % 2]])
        P.op("act", lambda e: e.activation(out=tT[:, :, i_ * 128:(i_ + 1) * 128], in_=pT.rearrange("p (c t) -> p c t", t=128), func=AF.Copy), reads=[bPE[4 + i_ % 2]], writes=[bTT[i_]])
        for c_ in range(8):
            P.op("pe", lambda e, c_=c_: e.matmul(psb[6][:, 0:32], lhsT=tT[:, c_, i_ * 128:(i_ + 1) * 128], rhs=rwb[:, c_ * 32:(c_ + 1) * 32], start=(c_ == 0), stop=(c_ == 7)),
                 reads=[bTT[i_], bRwb], writes=[bPE[6]])
        lg = lgs[:, 0:32]; exl = lgs[:, 32:64]; msk = lgs[:, 64:96]
        P.op("dve", lambda e: e.tensor_tensor(out=lg, in0=psb[6][:, 0:32], in1=rbb[:], op=ALU.add), reads=[bPE[6], bC8], writes=[bLg])
        P.op("dve", lambda e: e.max(out=mx8[:, 0:8], in_=lg), reads=[bLg], writes=[bMx])
        P.op("dve", lambda e: e.tensor_scalar(out=mx8[:, 8:9], in0=mx8[:, 0:1], scalar1=-1.0, scalar2=None, op0=ALU.mult), reads=[bMx], writes=[bMx])
        P.op("act", lambda e: e.activation(out=exl, in_=lg, func=AF.Exp, bias=mx8[:, 8:9]), reads=[bLg, bMx], writes=[bLg])
        P.op("dve", lambda e: e.tensor_scalar(out=msk, in0=lg, scalar1=mx8[:, 3:4], scalar2=None, op0=ALU.is_ge), reads=[bLg, bMx], writes=[bLg])
        P.op("dve", lambda e: e.tensor_tensor(out=exl, in0=exl, in1=msk, op=ALU.mult), reads=[bLg], writes=[bLg])
        P.op("dve", lambda e: e.tensor_reduce(out=mx8[:, 9:10], in_=exl, axis=AX.X, op=ALU.add), reads=[bLg], writes=[bMx])
        P.op("dve", lambda e: e.reciprocal(out=mx8[:, 10:11], in_=mx8[:, 9:10]), reads=[bMx], writes=[bMx])
        P.op("act", lambda e: e.activation(out=mx8[:, 16:20], in_=mx8[:, 0:4], func=AF.Exp, bias=mx8[:, 8:9]), reads=[bMx], writes=[bMx])
        P.op("dve", lambda e: e.tensor_scalar(out=meta_g[:, i_ * 4:(i_ + 1) * 4], in0=mx8[:, 16:20], scalar1=mx8[:, 10:11], scalar2=None, op0=ALU.mult), reads=[bMx], writes=[bMeta], accum=True)
        P.op("dve", lambda e: e.max_index(out=ix8[:], in_max=mx8[:, 0:8], in_values=lg), reads=[bLg, bMx], writes=[bIx])
        P.op("dve", lambda e: e.tensor_copy(out=mbt[:, 0:32], in_=msk), reads=[bLg], writes=[bMb])
        pfx = psb[7][:, 0:32]
        a_ = i_ % 2
        P.op("pe", lambda e: e.matmul(pfx, lhsT=ltm, rhs=mbt[:, 0:32], start=True, stop=(i_ == 0)), reads=[bMb, bConst], writes=[bPE[7]])
        if i_ > 0:
            P.op("pe", lambda e: e.matmul(pfx, lhsT=onesb, rhs=mbt[:, 32 + a_ * 32:64 + a_ * 32], start=False, stop=True), reads=[bMacc[a_], bConst], writes=[bPE[7]])
        if i_ == 0:
            P.op("dve", lambda e: e.tensor_copy(out=mbt[:, 64:96], in_=mbt[:, 0:32]), reads=[bMb], writes=[bMacc[1]])
        else:
            P.op("dve", lambda e: e.tensor_tensor(out=mbt[:, 32 + (1 - a_) * 32:64 + (1 - a_) * 32], in0=mbt[:, 32 + a_ * 32:64 + a_ * 32], in1=mbt[:, 0:32], op=ALU.add),
                 reads=[bMb, bMacc[a_]], writes=[bMacc[1 - a_]])
        oh = lgs[:, 96:128]
        for k_ in range(4):
            P.op("dve", lambda e, k_=k_: e.tensor_scalar(out=oh, in0=lg, scalar1=mx8[:, k_:k_ + 1], scalar2=None, op0=ALU.is_equal), reads=[bLg, bMx], writes=[bLg])
            P.op("dve", lambda e: e.tensor_tensor(out=oh, in0=oh, in1=pfx, op=ALU.mult), reads=[bLg, bPE[7]], writes=[bLg])
            P.op("dve", lambda e, k_=k_: e.tensor_reduce(out=mx8[:, 20 + k_:21 + k_], in_=oh, axis=AX.X, op=ALU.add), reads=[bLg], writes=[bMx])
        P.op("dve", lambda e: e.tensor_copy(out=mx8[:, 24:28], in_=ix8[:, 0:4]), reads=[bIx], writes=[bMx])
        P.op("dve", lambda e: e.scalar_tensor_tensor(out=mx8[:, 28:32], in0=mx8[:, 24:28], scalar=float(CAP), in1=mx8[:, 20:24], op0=ALU.mult, op1=ALU.add), reads=[bMx], writes=[bMx])
        P.op("dve", lambda e: e.tensor_copy(out=meta_dest[:, i_ * 4:(i_ + 1) * 4], in_=mx8[:, 28:32]), reads=[bMx], writes=[bMeta], accum=True)
        for k_ in range(4):
            P.dma("pool", lambda e, k_=k_: e.indirect_dma_start(out=xg_d, out_offset=bass.IndirectOffsetOnAxis(ap=meta_dest[:, i_ * 4 + k_:i_ * 4 + k_ + 1], axis=0),
                                                               in_=tb1, in_offset=None), reads=[bTb, bMeta], writes=[bXg])
        P.dma("sp", lambda e: e.dma_start(out=h1_d[i_ * 128:(i_ + 1) * 128, :], in_=hres[:, i_, :]), reads=[bH[i_]], writes=[bH1d])

    for i_ in range(NT):
        phaseE(i_)
    cnt_i = sbt("cnt_i", [128, 32], I32)
    bCnt = B("cnt")
    P.op("pe", lambda e: e.matmul(psb[7][:, 0:32], lhsT=onesb, rhs=mbt[:, 32:64], start=True, stop=True), reads=[bMacc[0], bConst], writes=[bPE[7]])
    P.op("dve", lambda e: e.tensor_copy(out=cnt_i[:], in_=psb[7][:, 0:32]), reads=[bPE[7]], writes=[bCnt])
    P.cnt_ap = lambda ex_: cnt_i[0:1, ex_:ex_ + 1]

    if stop == "E":
        dbg["h1"] = (hres, [128, NT, D], F32)
        dbg["tT"] = (tT, [128, 8, S], BF16)
        dbg["mdest"] = (meta_dest[:], [128, NT * 4], I32)
        dbg["mg"] = (meta_g[:], [128, NT * 4], F32)
        dbg["xg0"] = (xg_d[0:512, :], [512, D], BF16)
        return finish(nc, P, st, dbg)

    P.barrier()
    NTS = RC // 128
    xe = [view(0 + k_ * 6 * KB, [128, NTS, D], BF16) for k_ in range(2)]
    XeT = [view(12 * KB + k_ * 6 * KB, [128, 8, RC], BF16) for k_ in range(2)]
    actT = [view(24 * KB + k_ * 6 * KB, [128, 8, RC], BF16) for k_ in range(2)]
    wdb = [view(36 * KB + k_ * 16 * KB, [128, 8, D], BF16) for k_ in range(2)]
    wg = [view(112 * KB + k_ * 4 * KB, [128, 8, 256], BF16) for k_ in range(6)]
    yt = [view(80 * KB + k_ * 4 * KB, [128, D], F32) for k_ in range(2)]
    bdt = [view(88 * KB + k_ * 4 * KB, [128, D], F32) for k_ in range(2)]
    bXe = [B("xe%d" % k_) for k_ in range(2)]; bXT = [B("XeT%d" % k_) for k_ in range(2)]; bAc = [B("ac%d" % k_) for k_ in range(2)]
    bWd = [B("wd%d" % k_) for k_ in range(2)]; bWg = [B("wg%d" % k_) for k_ in range(6)]; bYt = [B("yt%d" % k_) for k_ in range(2)]; bBd = [B("bd%d" % k_) for k_ in range(2)]
    bYg = B("yg")
    bTgs = [B("tg%d" % k_) for k_ in range(3)]; bTss = [B("ts%d" % k_) for k_ in range(3)]; bTls = [B("tl%d" % k_) for k_ in range(3)]
    tgs = [view(96 * KB + k_ * 1536, [128, RC], F32) for k_ in range(3)]
    tss = [view(101 * KB + k_ * 1536, [128, RC], F32) for k_ in range(3)]
    tls = [view(106 * KB + k_ * 1536, [128, RC], F32) for k_ in range(3)]
    cnt = [0]; gcnt = [0]; slabc = [0]; ytc = [0]; qc = [0]

    def gu_chunk(e_, j_, k_, q_):
        ba = 2 * (gcnt[0] % 2)
        tg = tgs[gcnt[0] % 3]; ts = tss[gcnt[0] % 3]; tl = tls[gcnt[0] % 3]
        bTg = bTgs[gcnt[0] % 3]; bTs = bTss[gcnt[0] % 3]; bTl = bTls[gcnt[0] % 3]
        gcnt[0] += 1
        KC = int(os.environ.get("KKC", 8))
        for which in range(2):
            for c_ in range(KC):
                P.op("pe", lambda e, c_=c_, which=which: e.matmul(psb[ba + which][:, 0:RC], lhsT=wg[k_][:, c_, which * 128:(which + 1) * 128], rhs=XeT[q_][:, c_, :], start=(c_ == 0), stop=(c_ == KC - 1)),
                     reads=[bWg[k_], bXT[q_]], writes=[bPE[ba + which]])
        col = (e_ * 8 + j_) * 2
        P.op("dve", lambda e: e.tensor_scalar(out=tg, in0=psb[ba][:, 0:RC], scalar1=bgl[:, col:col + 1], scalar2=7.0, op0=ALU.add, op1=ALU.min), reads=[bPE[ba], bC9], writes=[bTg])
        P.op("act", lambda e: e.activation(out=ts, in_=tg, func=AF.Silu, scale=1.702), reads=[bTg], writes=[bTs])
        P.op("dve", lambda e: e.tensor_scalar(out=tl, in0=psb[ba + 1][:, 0:RC], scalar1=bgl[:, col + 1:col + 2], scalar2=8.0, op0=ALU.add, op1=ALU.min), reads=[bPE[ba + 1], bC9], writes=[bTl])
        P.op("dve", lambda e: e.scalar_tensor_tensor(out=actT[q_][:, j_, :], in0=tl, scalar=-6.0, in1=ts, op0=ALU.max, op1=ALU.mult), reads=[bTl, bTs], writes=[bAc[q_]], accum=True)

    def down_tile(e_, q_, st_, half, yk):
        bk = 6 + cnt[0] % 2
        cnt[0] += 1
        for j_ in range(8):
            P.op("pe", lambda e, j_=j_: e.matmul(psb[bk], lhsT=actT[q_][:, j_, st_ * 128:(st_ + 1) * 128], rhs=wdb[q_][:, j_, half * 512:(half + 1) * 512], start=(j_ == 0), stop=(j_ == 7)),
                 reads=[bAc[q_], bWd[q_]], writes=[bPE[bk]])
        P.op("dve", lambda e: e.scalar_tensor_tensor(out=yt[yk][:, half * 512:(half + 1) * 512], in0=psb[bk], scalar=1.0 / 1.702, in1=bdt[q_][:, half * 512:(half + 1) * 512], op0=ALU.mult, op1=ALU.add),
             reads=[bPE[bk], bBd[q_]], writes=[bYt[yk]], accum=(half == 1))

    def chunk_prep(e_, ch):
        q_ = qc[0] % 2
        qc[0] += 1
        row0 = e_ * CAP + min(ch * RC, CAP - RC)
        P.dma("sp", lambda e: e.dma_start(out=xe[q_], in_=xg_d[row0:row0 + RC, :].rearrange("(t p) d -> p t d", p=128)), reads=[bXg], writes=[bXe[q_]])
        P.dma("sp", lambda e: e.dma_start(out=bdt[q_], in_=bd_d[e_].partition_broadcast(128)), writes=[bBd[q_]])
        P.dma("pool", lambda e: e.dma_start(out=wdb[q_], in_=wd_d[e_]), writes=[bWd[q_]])
        for t_ in range(NTS):
            pT = psb[4 + t_ % 2].bitcast(BF16)
            for c_ in range(8):
                P.op("pe", lambda e, c_=c_, t_=t_, pT=pT: e.transpose(pT[:, c_ * 128:(c_ + 1) * 128], xe[q_][:, t_, c_ * 128:(c_ + 1) * 128], identb),
                     reads=[bXe[q_], bConst], writes=[bPE[4 + t_ % 2]])
            if t_ % 2 == 0:
                P.op("act", lambda e, t_=t_, pT=pT: e.activation(out=XeT[q_][:, :, t_ * 128:(t_ + 1) * 128], in_=pT.rearrange("p (c t) -> p c t", t=128), func=AF.Copy),
                     reads=[bPE[4 + t_ % 2]], writes=[bXT[q_]], accum=(t_ > 0))
            else:
                P.op("dve", lambda e, t_=t_, pT=pT: e.tensor_copy(out=XeT[q_][:, :, t_ * 128:(t_ + 1) * 128], in_=pT.rearrange("p (c t) -> p c t", t=128)),
                     reads=[bPE[4 + t_ % 2]], writes=[bXT[q_]], accum=True)
        return (e_, q_, row0)

    def chunk_gate(stt):
        e_, q_, row0 = stt
        for j_ in range(8):
            k_ = slabc[0] % 6
            slabc[0] += 1
            P.dma("pool", lambda e, j_=j_, k_=k_: e.dma_start(out=wg[k_], in_=wgu_d[e_, j_]), writes=[bWg[k_]])
            gu_chunk(e_, j_, k_, q_)

    def chunk_down(stt):
        e_, q_, row0 = stt
        for st_ in range(NTS):
            yk = ytc[0] % 2
            ytc[0] += 1
            for half in range(2):
                down_tile(e_, q_, st_, half, yk)
            P.dma("sp", lambda e, st_=st_, yk=yk: e.dma_start(out=yg_d[row0 + st_ * 128:row0 + (st_ + 1) * 128, :], in_=yt[yk]), reads=[bYt[yk]], writes=[bYg], sembuf=bYg)

    def expert_chunk(e_, ch):
        stt = chunk_prep(e_, ch)
        chunk_gate(stt)
        chunk_down(stt)

    NEX = int(os.environ.get("KNEX", NE))
    NCHX = int(os.environ.get("KNCH", NCH))
    stt_ = chunk_prep(0, 0)
    for e_ in range(NEX):
        chunk_gate(stt_)
        nxt = chunk_prep(e_ + 1, 0) if e_ + 1 < NEX else None
        chunk_down(stt_)
        stt_ = nxt
    for e_ in range(NEX):
        for ch in range(1, NCHX):
            P.guard_begin((e_, ch * RC))
            expert_chunk(e_, ch)
        for ch in range(1, NCHX):
            P.guard_end()

    if stop == "G":
        dbg["cnt"] = (cnt_i[:], [128, 32], I32)
        return finish(nc, P, st, dbg)

    P.barrier()
    pgb = view(128 * KB, [128, 8, D], BF16)
    ppb = view(144 * KB, [128, 2, D], BF16)
    ptile = view(148 * KB, [128, 256], F32)
    pbf = view(149 * KB, [128, 256], BF16)
    pTs = view(149 * KB + 512, [128, 2, 128], BF16)
    pe32 = view(64 * KB, [128, D], F32)
    gate = view(68 * KB, [128, D], F32)
    hbf = view(72 * KB, [128, D], BF16)
    hT = view(74 * KB, [128, 8, 128], BF16)
    otile = [view(76 * KB + k_ * 4096, [128, D], F32) for k_ in range(2)]
    gfin = ebfix
    bPg = B("pg"); bPp = B("pp"); bPt = B("pt"); bPbf = B("pbf"); bPTs = B("pTs"); bPe32 = B("pe32"); bGate = B("gate"); bHbf = B("hbf"); bHT = B("hT")
    bOt = [B("ot%d" % k_) for k_ in range(2)]; bGf = B("gfin"); bOut = B("out")
    P.dma("pool", lambda e: e.dma_start(out=pgb, in_=pgate_d), writes=[bPg])
    P.dma("pool", lambda e: e.dma_start(out=ppb, in_=pproj_d), writes=[bPp])
    P.dma("sp", lambda e: e.dma_start(out=gainb[:], in_=gains_d[2].partition_broadcast(128)), writes=[bGain])
    P.dma("sp", lambda e: e.dma_start(out=gfin[:], in_=gains_d[3].partition_broadcast(128)), writes=[bGf])

    htl = [view(84 * KB + k_ * 4 * KB, [128, D], F32) for k_ in range(2)]
    ygk = [view(92 * KB + k_ * 4 * KB, [128, D], F32) for k_ in range(8)]
    bHt = [B("ht%d" % k_) for k_ in range(2)]
    bYk = [B("ygk%d" % k_) for k_ in range(8)]

    def phaseH(i_):
        k_ = i_ % 2
        hcur = htl[k_]
        P.dma("sp", lambda e: e.dma_start(out=hcur, in_=h1_d[i_ * 128:(i_ + 1) * 128, :]), reads=[bH1d], writes=[bHt[k_]])
        for kk in range(4):
            yb = k_ * 4 + kk
            P.dma("pool", lambda e, kk=kk, yb=yb: e.indirect_dma_start(out=ygk[yb], out_offset=None, in_=yg_d,
                                                                        in_offset=bass.IndirectOffsetOnAxis(ap=meta_dest[:, i_ * 4 + kk:i_ * 4 + kk + 1], axis=0)),
                  reads=[bYg, bMeta], writes=[bYk[yb]])
            P.op("dve", lambda e, kk=kk, yb=yb: e.scalar_tensor_tensor(out=hcur, in0=ygk[yb], scalar=meta_g[:, i_ * 4 + kk:i_ * 4 + kk + 1], in1=hcur, op0=ALU.mult, op1=ALU.add),
                 reads=[bYk[yb], bMeta, bHt[k_]], writes=[bHt[k_]])
        P.dma("sp", lambda e: e.dma_start(out=ptile, in_=p_d[i_ * 128:(i_ + 1) * 128, :]), writes=[bPt])
        P.op("dve", lambda e: e.tensor_copy(out=pbf, in_=ptile), reads=[bPt], writes=[bPbf])
        tq = psb[6].bitcast(BF16)
        for c_ in range(2):
            P.op("pe", lambda e, c_=c_: e.transpose(tq[:, c_ * 128:(c_ + 1) * 128], pbf[:, c_ * 128:(c_ + 1) * 128], identb), reads=[bPbf, bConst], writes=[bPE[6]])
        P.op("act", lambda e: e.activation(out=pTs, in_=tq[:, 0:256].rearrange("p (c t) -> p c t", t=128), func=AF.Copy), reads=[bPE[6]], writes=[bPTs])
        for half in range(2):
            for c_ in range(2):
                P.op("pe", lambda e, c_=c_, half=half: e.matmul(psb[half], lhsT=pTs[:, c_, :], rhs=ppb[:, c_, half * 512:(half + 1) * 512], start=(c_ == 0), stop=(c_ == 1)),
                     reads=[bPTs, bPp], writes=[bPE[half]])
            P.op("act", lambda e, half=half: e.activation(out=pe32[:, half * 512:(half + 1) * 512], in_=psb[half], func=AF.Copy), reads=[bPE[half]], writes=[bPe32], accum=(half == 1))
        rp = rms2(i_, pe32, bPe32, D)
        P.op("dve", lambda e: e.scalar_tensor_tensor(out=pe32, in0=pe32, scalar=rp, in1=gainb[:], op0=ALU.mult, op1=ALU.mult), reads=[bPe32, bSt[i_], bGain], writes=[bPe32])
        P.op("dve", lambda e: e.tensor_copy(out=hbf, in_=hcur), reads=[bHt[k_]], writes=[bHbf])
        tq2 = psb[7].bitcast(BF16)
        for c_ in range(8):
            P.op("pe", lambda e, c_=c_: e.transpose(tq2[:, c_ * 128:(c_ + 1) * 128], hbf[:, c_ * 128:(c_ + 1) * 128], identb), reads=[bHbf, bConst], writes=[bPE[7]])
        P.op("act", lambda e: e.activation(out=hT, in_=tq2.rearrange("p (c t) -> p c t", t=128), func=AF.Copy), reads=[bPE[7]], writes=[bHT])
        for half in range(2):
            for c_ in range(8):
                P.op("pe", lambda e, c_=c_, half=half: e.matmul(psb[2 + half], lhsT=hT[:, c_, :], rhs=pgb[:, c_, half * 512:(half + 1) * 512], start=(c_ == 0), stop=(c_ == 7)),
                     reads=[bHT, bPg], writes=[bPE[2 + half]])
            P.op("act", lambda e, half=half: e.activation(out=gate[:, half * 512:(half + 1) * 512], in_=psb[2 + half], func=AF.Sigmoid), reads=[bPE[2 + half]], writes=[bGate], accum=(half == 1))
        P.op("dve", lambda e: e.tensor_tensor(out=gate, in0=gate, in1=pe32, op=ALU.mult), reads=[bGate, bPe32], writes=[bGate])
        P.op("dve", lambda e: e.tensor_tensor(out=hcur, in0=hcur, in1=gate, op=ALU.add), reads=[bHt[k_], bGate], writes=[bHt[k_]])
        c0 = i_ * 8 + 4
        P.op("act", lambda e: e.activation(out=junk2, in_=hcur, func=AF.Square, accum_out=stats[:, c0:c0 + 1]), reads=[bHt[k_]], writes=[bJ2, bSt[i_]])
        P.op("dve", lambda e: e.tensor_scalar(out=stats[:, c0 + 1:c0 + 2], in0=stats[:, c0:c0 + 1], scalar1=1.0 / D, scalar2=EPS, op0=ALU.mult, op1=ALU.add), reads=[bSt[i_]], writes=[bSt[i_]])
        P.op("pool", lambda e: e.tensor_tensor(out=stats[:, c0 + 2:c0 + 3], in0=stats[:, c0 + 1:c0 + 2], in1=cst[:, 0:1], op=ALU.pow), reads=[bSt[i_], bCst], writes=[bSt[i_]])
        P.op("dve", lambda e: e.scalar_tensor_tensor(out=otile[k_], in0=hcur, scalar=stats[:, c0 + 2:c0 + 3], in1=gfin[:], op0=ALU.mult, op1=ALU.mult), reads=[bHt[k_], bSt[i_], bGf], writes=[bOt[k_]])
        P.dma("sp", lambda e: e.dma_start(out=out_d[i_ * 128:(i_ + 1) * 128, :], in_=otile[k_]), reads=[bOt[k_]], writes=[bOut], sembuf=bOt[k_])

    for i_ in range(NT):
        phaseH(i_)
    P.wait_all("sp", [bOut] + bOt)
    P.run(st)
    st.close()
    return nc


def finish(nc, P, st, dbg):
    P.barrier()
    outs = []
    for name, (ap, shape, dt) in dbg.items():
        d = nc.dram_tensor("dbg_" + name, shape, dt, kind="ExternalOutput").ap()
        b = Buf(P, "dbg_" + name)
        P.dma("sp", lambda e, d=d, ap=ap: e.dma_start(out=d, in_=ap), writes=[b])
        outs.append(b)
    P.wait_all("sp", outs)
    P.run(st)
    st.close()
    return nc


def _t5_bucket(rel):
    n = np.maximum(rel, 0)
    nf = np.maximum(n, 1).astype(np.float32)
    large = 16 + (np.log(nf / np.float32(16)) / np.float32(math.log(128 / 16)) * np.float32(16)).astype(np.int32)
    large = np.minimum(large, 31)
    return np.where(n < 16, n, large)


def prep_shared(inp):
    f32 = np.float32
    g = lambda k: np.asarray(inp[k], dtype=f32)
    w_in = g("w_in")[0]
    cols = []
    for h in range(4):
        cols += list(range(h * 64, h * 64 + 64)) + list(range(256 + h * 64, 256 + h * 64 + 64))
    for h in range(4):
        cols += list(range(512 + h * 64, 512 + h * 64 + 64)) + list(range(768 + h * 64, 768 + h * 64 + 64))
    cols += list(range(1024, 3072))
    w_in_p = w_in[:, cols]
    chunked = lambda w: np.ascontiguousarray(w.reshape(8, 128, -1).transpose(1, 0, 2))
    sh = {}
    sh["w_in"] = chunked(w_in_p)
    sh["w_out"] = chunked(g("w_out")[0])
    sh["gains"] = np.stack([g("attn_norm")[0], g("moe_norm")[0], g("ple_norm")[0], g("final_norm")], 0)
    sh["lamv"] = np.stack([g("lambda_q1")[0], g("lambda_k1")[0], g("lambda_q2")[0], g("lambda_k2")[0]], 0)
    sh["subln"] = g("subln")
    rb = g("rel_bias")
    k = np.arange(128)[:, None]
    q = np.arange(128)[None, :]
    bn = np.zeros((128, 4, 2, 128), f32)
    for d in range(2):
        bk = _t5_bucket(q + 128 * d - k)
        bn[:, :, d, :] = rb[bk].transpose(0, 2, 1)
    sh["bnear"] = bn.reshape(128, -1)
    sh["c31"] = np.ascontiguousarray(np.broadcast_to(rb[31][None, :], (128, 4)))
    sh["rw"] = chunked(g("router_w")[0]).reshape(128, -1)
    sh["rb"] = g("router_b")
    wgu = g("w_gate_up")[0]
    glu = wgu[:, :, 0::2].reshape(NE, 8, 128, 8, 128)
    lin = wgu[:, :, 1::2].reshape(NE, 8, 128, 8, 128)
    gl = np.concatenate([glu, lin], axis=-1)
    sh["wgu"] = np.ascontiguousarray(gl.transpose(0, 3, 2, 1, 4))
    bgu = g("b_gate_up")[0]
    bg = bgu[:, 0::2].reshape(NE, 8, 128)
    bl = bgu[:, 1::2].reshape(NE, 8, 128)
    sh["bgl"] = np.ascontiguousarray(np.stack([bg, bl], -1).transpose(2, 0, 1, 3)).reshape(128, -1)
    sh["wd"] = np.ascontiguousarray(g("w_down")[0].reshape(NE, 8, 128, D).transpose(0, 2, 1, 3))
    sh["bd"] = g("b_down")[0]
    sh["pproj"] = np.ascontiguousarray(g("ple_proj")[0].reshape(2, 128, D).transpose(1, 0, 2))
    sh["pgate"] = chunked(g("ple_gate")[0])
    ident = np.eye(128, dtype=f32)
    jj = np.arange(128)[:, None]
    kk = np.arange(128)[None, :]
    negtri = -(jj >= kk).astype(f32)
    lt = (jj < kk).astype(f32)
    sh["cbf"] = np.concatenate([ident, negtri, -np.ones((128, 128), f32), lt, np.ones((128, 128), f32)], 1).astype(ml_dtypes.bfloat16)
    sh["cf32"] = np.concatenate([ident, (kk >= jj).astype(f32), (kk > jj).astype(f32)], 1)
    return sh


_CACHE = {}


def kernel(**inputs):
    sh = prep_shared(inputs)
    x = np.asarray(inputs["x"], dtype=np.float32)
    p = np.asarray(inputs["p"], dtype=np.float32)[0]
    if "nc" not in _CACHE:
        _CACHE["nc"] = build()
    nc = _CACHE["nc"]
    in_maps = []
    for c in range(8):
        m = dict(sh)
        m["x"] = np.ascontiguousarray(x[c])
        m["p"] = np.ascontiguousarray(p[c])
        in_maps.append(m)
    res = run_bass_kernel_spmd(nc, in_maps, core_ids=list(range(8)))
    return np.stack([np.asarray(r["out"], dtype=np.float32) for r in res.results], 0)
```

```python
from contextlib import ExitStack
import math
import numpy as np
import ml_dtypes
import concourse.bass as bass
import concourse.mybir as mybir
from concourse.bass_utils import run_bass_kernel_spmd

F32 = mybir.dt.float32
BF16 = mybir.dt.bfloat16
U32 = mybir.dt.uint32
I32 = mybir.dt.int32
AF = mybir.ActivationFunctionType
ALU = mybir.AluOpType
AX = mybir.AxisListType

S = 2048
D = 1024
NT = 16
NE = 32
RC = 384
NCH = 6
CAP = 2048
EPS = 1e-6
ENG = {"pe": "tensor", "act": "scalar", "dve": "vector", "pool": "gpsimd", "sp": "sync"}


class Buf:
    __slots__ = ("name", "w", "r", "sem", "cnt")

    def __init__(self, P, name):
        self.name = name
        self.w = []
        self.r = []
        self.sem = None
        self.cnt = 0
        P.bufs.append(self)


class Prog:
    def __init__(self, nc):
        self.nc = nc
        self.recs = {e: [] for e in ENG}
        self.waited = {e: {} for e in ENG}
        self.dma_sems = []
        self.bufs = []

    def _filter(self, eng, deps):
        out = []
        for t in deps:
            if t[0] == "c":
                _, e2, idx = t
                if e2 == eng and eng in ("pe", "sp"):
                    continue
                k = ("c", e2)
                if self.waited[eng].get(k, -1) >= idx:
                    continue
                self.waited[eng][k] = idx
                self.recs[e2][idx]["sig"] = True
                out.append(t)
            else:
                _, b, val = t
                k = ("d", id(b))
                if self.waited[eng].get(k, -1) >= val:
                    continue
                self.waited[eng][k] = val
                out.append(t)
        return out

    def _deps(self, eng, reads, writes):
        deps = []
        for b in reads:
            deps += b.w
        for b in writes:
            deps += b.w
            deps += b.r
        return self._filter(eng, deps)

    def op(self, eng, fn, reads=(), writes=(), accum=False):
        waits = self._deps(eng, reads, writes)
        idx = len(self.recs[eng])
        self.recs[eng].append(dict(waits=waits, fn=fn, sig=False, dma=None))
        tok = ("c", eng, idx)
        for b in reads:
            b.r.append(tok)
        for b in writes:
            if accum:
                b.w.append(tok)
            else:
                b.w = [tok]
                b.r = []
        return tok

    def dma(self, eng, fn, reads=(), writes=(), sembuf=None):
        waits = self._deps(eng, reads, writes)
        sb = sembuf if sembuf is not None else (writes[0] if writes else reads[0])
        if sb.sem is None:
            sb.sem = True
            self.dma_sems.append(sb)
        sb.cnt += 16
        tok = ("d", sb, sb.cnt)
        self.recs[eng].append(dict(waits=waits, fn=fn, sig=False, dma=sb, dmaval=sb.cnt))
        for b in reads:
            b.r.append(tok)
        for b in writes:
            b.w = [tok]
            b.r = []
        return tok

    def barrier(self):
        toks = []
        for e in ENG:
            for i in range(len(self.recs[e]) - 1, -1, -1):
                r = self.recs[e][i]
                if r["fn"] is not None and r["dma"] is None:
                    toks.append(("c", e, i))
                    break
        for b in self.bufs:
            toks += [t for t in b.w + b.r if t[0] == "d"]
            b.w = []
            b.r = []
        for e in ENG:
            w = self._filter(e, [t for t in toks if not (t[0] == "c" and t[1] == e)])
            self.recs[e].append(dict(waits=w, fn=None, sig=False, dma=None))

    def guard_begin(self, key):
        if not hasattr(self, "_wstack"):
            self._wstack = []
        self._wstack.append({e: dict(self.waited[e]) for e in ENG})
        for e in ENG:
            self.recs[e].append(dict(waits=[], fn=None, sig=False, dma=None, gb=key))

    def guard_end(self):
        for e in ENG:
            self.recs[e].append(dict(waits=[], fn=None, sig=False, dma=None, ge=True))
        self.waited = self._wstack.pop()

    def wait_all(self, eng, bufs):
        waits = self._deps(eng, list(bufs), list(bufs))
        self.recs[eng].append(dict(waits=waits, fn=None, sig=False, dma=None))

    def run(self, stack):
        nc = self.nc
        esem = {e: stack.enter_context(nc.semaphore("s_" + e)) for e in ENG}
        for i, b in enumerate(self.dma_sems):
            b.sem = stack.enter_context(nc.semaphore("d%d" % i))
        cnts = {}
        for e in ENG:
            c = 0
            for i, r in enumerate(self.recs[e]):
                if r["sig"]:
                    c += 1
                    cnts[(e, i)] = c
        block = stack.enter_context(nc.Block())

        def body(e):
            def emit(engine, r):
                for t in r["waits"]:
                    if t[0] == "c":
                        engine.wait_ge(esem[t[1]], cnts[(t[1], t[2])])
                    else:
                        engine.wait_ge(t[1].sem, t[2])
                if r["fn"] is None:
                    return
                ins = r["fn"](engine)
                if r["dma"] is not None:
                    ins.then_inc(r["dma"].sem, 16)
                elif r["sig"]:
                    ins.then_inc(esem[e], 1)

            def f(engine):
                recs = self.recs[e]
                reg = rthr = None
                if any("gb" in q for q in recs):
                    reg = stack.enter_context(engine.register("rc_" + e))
                    rthr = stack.enter_context(engine.register("rt_" + e))
                csum = [0]

                def match(i):
                    d_ = 0
                    j = i
                    while True:
                        if "gb" in recs[j]:
                            d_ += 1
                        elif "ge" in recs[j]:
                            d_ -= 1
                            if d_ == 0:
                                return j
                        j += 1

                def process(lo, hi):
                    i = lo
                    while i < hi:
                        r = recs[i]
                        if "gb" in r:
                            j = match(i)
                            inner = [q for q in recs[i + 1:j] if "gb" not in q and "ge" not in q]
                            ex_, thr = r["gb"]
                            if any(q["fn"] is not None or q["waits"] for q in inner):
                                c_before = csum[0]
                                engine.reg_load(reg, self.cnt_ap(ex_))
                                engine.reg_mov(rthr, thr)
                                with engine.If_lt(rthr, reg):
                                    process(i + 1, j)
                                csum[0] = c_before
                                nsig = sum(1 for q in inner if q["sig"])
                                dmas = [q for q in inner if q["dma"] is not None]
                                if nsig or dmas:
                                    with engine.Else():
                                        if nsig:
                                            if c_before > 0:
                                                engine.wait_ge(esem[e], c_before)
                                            engine.sem_inc(esem[e], nsig)
                                        for q in dmas:
                                            if q["dmaval"] - 16 > 0:
                                                engine.wait_ge(q["dma"].sem, q["dmaval"] - 16)
                                            engine.sem_inc(q["dma"].sem, 16)
                                csum[0] = c_before + nsig
                            i = j + 1
                            continue
                        emit(engine, r)
                        if r["sig"]:
                            csum[0] += 1
                        i += 1

                process(0, len(recs))
            return f

        block.tensor(body("pe"))
        block.scalar(body("act"))
        block.vector(body("dve"))
        block.gpsimd(body("pool"))
        block.sync(body("sp"))


def build(stop=None):
    nc = bass.Bass("TRN2", target_bir_lowering=False, dynamic_dma_scratch_size=8192)
    dram = lambda n, s, d=F32, k="ExternalInput": nc.dram_tensor(n, s, d, kind=k).ap()
    x_d = dram("x", [S, D])
    p_d = dram("p", [S, 256])
    win_d = dram("w_in", [128, 8, 3072])
    wout_d = dram("w_out", [128, 8, D])
    gains_d = dram("gains", [4, D])
    lamv_d = dram("lamv", [4, 64])
    subln_d = dram("subln", [1, 128])
    bnear_d = dram("bnear", [128, 4 * 2 * 128])
    c31_d = dram("c31", [128, 4])
    rw_d = dram("rw", [128, 8 * 32])
    rb_d = dram("rb", [1, 32])
    wgu_d = dram("wgu", [NE, 8, 128, 8, 256])
    bgl_d = dram("bgl", [128, NE * 8 * 2])
    wd_d = dram("wd", [NE, 128, 8, D])
    bd_d = dram("bd", [NE, D])
    pproj_d = dram("pproj", [128, 2, D])
    pgate_d = dram("pgate", [128, 8, D])
    cb_d = dram("cbf", [128, 5 * 128], BF16)
    cf_d = dram("cf32", [128, 3 * 128])
    out_d = dram("out", [S, D], F32, "ExternalOutput")
    h1_d = dram("h1s", [S, D], F32, "Internal")
    xg_d = dram("xg", [NE * CAP, D], BF16, "Internal")
    yg_d = dram("yg", [NE * CAP, D], F32, "Internal")
    dbg = {}

    st = ExitStack()
    P = Prog(nc)
    B = lambda n: Buf(P, n)
    sbt = lambda n, s, d: st.enter_context(nc.sbuf_tensor("sb_" + n, s, d))
    pall = st.enter_context(nc.psum_tensor("pall", [128, 4096], F32))
    psb = [pall[:, i * 512:(i + 1) * 512] for i in range(8)]

    cb = sbt("cb", [128, 5 * 128], BF16)
    cf = sbt("cf", [128, 3 * 128], F32)
    identb, negtri, negones, ltm, onesb = [cb[:, i * 128:(i + 1) * 128] for i in range(5)]
    identf, mask0, strictm = [cf[:, i * 128:(i + 1) * 128] for i in range(3)]
    gainb = sbt("gainb", [128, D], F32)
    gain2 = sbt("gain2", [128, D], F32)
    ebfix = sbt("ebfix", [128, 4 * 2 * 128], F32)
    c31 = sbt("c31", [128, 4], F32)
    lamt = sbt("lamt", [128, 4 * 64], F32)
    lams = sbt("lams", [128, 8], F32)
    subg = sbt("subg", [128, 128], F32)
    rw = sbt("rw", [128, 8 * 32], F32)
    rbb = sbt("rbb", [128, 32], F32)
    bgl = sbt("bgl", [128, NE * 8 * 2], F32)
    cst = sbt("cst", [128, 8], F32)
    stats = sbt("stats", [128, NT * 8], F32)
    meta_dest = sbt("meta_dest", [128, NT * 4], I32)
    meta_g = sbt("meta_g", [128, NT * 4], F32)
    bConst = B("const")
    bGain = B("gain")
    bGain2 = B("gain2")

    AR = sbt("arena", [128, 152 * 1024], mybir.dt.uint8)
    K = 1024
    OFF_HN, OFF_QK, OFF_AT, OFF_W, OFF_V, OFF_T = 0, 32 * K, 64 * K, 96 * K, 128 * K, 145 * K

    def view(off, shape, dt):
        nb = {F32: 4, BF16: 2, I32: 4, U32: 4}[dt]
        n = 1
        for s_ in shape[1:]:
            n *= s_
        ap = AR[:, off:off + n * nb].bitcast(dt)
        if len(shape) == 3:
            ap = ap.rearrange("p (a b) -> p a b", b=shape[2])
        elif len(shape) == 4:
            ap = ap.rearrange("p (a b c) -> p a b c", b=shape[2], c=shape[3])
        return ap

    P.dma("sp", lambda e: e.dma_start(out=cb[:], in_=cb_d), writes=[bConst])
    bC2 = B("c2"); bC3 = B("c3"); bC4 = B("c4"); bC5 = B("c5"); bC6 = B("c6"); bC7 = B("c7"); bC8 = B("c8"); bC9 = B("c9")
    P.dma("sp", lambda e: e.dma_start(out=cf[:], in_=cf_d), writes=[bC2])
    P.dma("sp", lambda e: e.dma_start(out=ebfix[:], in_=bnear_d), writes=[bC3])
    P.dma("sp", lambda e: e.dma_start(out=c31[:], in_=c31_d), writes=[bC4])
    P.dma("sp", lambda e: e.dma_start(out=lamt[:], in_=lamv_d.rearrange("a b -> (a b)").partition_broadcast(128)), writes=[bC5])
    P.dma("sp", lambda e: e.dma_start(out=subg[:], in_=subln_d.rearrange("a b -> (a b)").partition_broadcast(128)), writes=[bC6])
    P.dma("sp", lambda e: e.dma_start(out=rw[:], in_=rw_d), writes=[bC7])
    P.dma("sp", lambda e: e.dma_start(out=rbb[:], in_=rb_d.rearrange("a b -> (a b)").partition_broadcast(128)), writes=[bC8])
    P.dma("sp", lambda e: e.dma_start(out=bgl[:], in_=bgl_d), writes=[bC9])
    P.dma("sp", lambda e: e.dma_start(out=gainb[:], in_=gains_d[0].partition_broadcast(128)), writes=[bGain])
    bCst = B("cst")
    P.op("dve", lambda e: e.memset(cst[:, 0:1], -0.5), writes=[bCst])
    for h in range(4):
        P.op("dve", lambda e, h=h: e.tensor_scalar(out=ebfix[:, h * 256:(h + 1) * 256], in0=ebfix[:, h * 256:(h + 1) * 256],
                                                   scalar1=c31[:, h:h + 1], scalar2=None, op0=ALU.subtract),
             reads=[bC3, bC4], writes=[bC3])
    P.op("act", lambda e: e.activation(out=ebfix[:], in_=ebfix[:], func=AF.Exp), reads=[bC3], writes=[bC3])
    for h in range(4):
        P.op("dve", lambda e, h=h: e.tensor_tensor(out=ebfix[:, h * 256:h * 256 + 128], in0=ebfix[:, h * 256:h * 256 + 128], in1=mask0, op=ALU.mult),
             reads=[bC3, bC2], writes=[bC3])
    P.op("dve", lambda e: e.tensor_tensor(out=lamt[:, 0:64], in0=lamt[:, 0:64], in1=lamt[:, 64:128], op=ALU.mult), reads=[bC5], writes=[bC5])
    P.op("dve", lambda e: e.tensor_tensor(out=lamt[:, 128:192], in0=lamt[:, 128:192], in1=lamt[:, 192:256], op=ALU.mult), reads=[bC5], writes=[bC5])
    P.op("dve", lambda e: e.tensor_reduce(out=lams[:, 0:1], in_=lamt[:, 0:64], axis=AX.X, op=ALU.add), reads=[bC5], writes=[bC5])
    P.op("dve", lambda e: e.tensor_reduce(out=lams[:, 1:2], in_=lamt[:, 128:192], axis=AX.X, op=ALU.add), reads=[bC5], writes=[bC5])
    P.op("act", lambda e: e.activation(out=lams[:, 2:4], in_=lams[:, 0:2], func=AF.Exp), reads=[bC5], writes=[bC5])
    P.op("dve", lambda e: e.tensor_tensor(out=lams[:, 4:5], in0=lams[:, 3:4], in1=lams[:, 2:3], op=ALU.subtract), reads=[bC5], writes=[bC5])
    P.op("dve", lambda e: e.tensor_scalar(out=lams[:, 4:5], in0=lams[:, 4:5], scalar1=-0.2, scalar2=None, op0=ALU.add), reads=[bC5], writes=[bC5])
    P.op("dve", lambda e: e.tensor_scalar(out=subg[:], in0=subg[:], scalar1=0.8, scalar2=None, op0=ALU.mult), reads=[bC6], writes=[bC6])
    bglv = bgl[:].rearrange("p (a t) -> p a t", t=2)
    P.op("dve", lambda e: e.tensor_scalar(out=bglv[:, :, 1:2], in0=bglv[:, :, 1:2], scalar1=1.0, scalar2=None, op0=ALU.add), reads=[bC9], writes=[bC9])
    neglam = lams[:, 4:5]

    hnT = view(OFF_HN, [128, 8, S], BF16)
    bHn = [B("hn%d" % i) for i in range(NT)]
    xt = [view(OFF_W + i * 4096, [128, D], F32) for i in range(2)]
    xs = [view(OFF_W + 8192 + i * 2048, [128, D], BF16) for i in range(2)]
    junkb = view(OFF_T, [128, D], BF16)
    bXt = [B("xt%d" % i) for i in range(2)]
    bXs = [B("xs%d" % i) for i in range(2)]
    bJ = B("junk")
    bSt = [B("st%d" % i) for i in range(NT)]
    bPs = [B("ps%d" % i) for i in range(8)]

    def rms_stats(i, src, srcbuf, n):
        c0 = i * 8
        P.op("act", lambda e: e.activation(out=junkb[:, 0:n], in_=src, func=AF.Square, accum_out=stats[:, c0:c0 + 1]),
             reads=[srcbuf], writes=[bJ, bSt[i]])
        P.op("dve", lambda e: e.tensor_scalar(out=stats[:, c0 + 1:c0 + 2], in0=stats[:, c0:c0 + 1], scalar1=1.0 / n, scalar2=EPS, op0=ALU.mult, op1=ALU.add),
             reads=[bSt[i]], writes=[bSt[i]])
        P.op("pool", lambda e: e.tensor_tensor(out=stats[:, c0 + 3:c0 + 4], in0=stats[:, c0 + 1:c0 + 2], in1=cst[:, 0:1], op=ALU.pow),
             reads=[bSt[i], bCst], writes=[bSt[i]])
        return stats[:, c0 + 3:c0 + 4]

    for i in range(NT):
        b = i % 2
        P.dma("sp", lambda e, i=i, b=b: e.dma_start(out=xt[b], in_=x_d[i * 128:(i + 1) * 128, :]), writes=[bXt[b]])
        rstd = rms_stats(i, xt[b], bXt[b], D)
        P.op("dve", lambda e, b=b, rstd=rstd: e.scalar_tensor_tensor(out=xs[b], in0=xt[b], scalar=rstd, in1=gainb[:], op0=ALU.mult, op1=ALU.mult),
             reads=[bXt[b], bSt[i], bGain], writes=[bXs[b]])
        pT = psb[b].bitcast(BF16)
        for c in range(8):
            P.op("pe", lambda e, c=c, b=b, pT=pT: e.transpose(pT[:, c * 128:(c + 1) * 128], xs[b][:, c * 128:(c + 1) * 128], identb),
                 reads=[bXs[b], bConst], writes=[bPs[b]])
        P.op("act", lambda e, i=i, pT=pT: e.activation(out=hnT[:, :, i * 128:(i + 1) * 128], in_=pT.rearrange("p (c t) -> p c t", t=128), func=AF.Copy),
             reads=[bPs[b]], writes=[bHn[i]])

    if stop == "A":
        dbg["hnT"] = (hnT, [128, 8, S], BF16)
        return finish(nc, P, st, dbg)

    QK = view(OFF_QK, [128, 8, S], BF16)
    VV = view(OFF_V, [128, NT, 516], BF16)
    wsl = [view(OFF_W + i * 4096, [128, 8, 256], BF16) for i in range(3)]
    bW = [B("wsl%d" % i) for i in range(3)]
    bQK = [B("qk%d" % i) for i in range(8)]
    bV = [B("v%d" % i) for i in range(NT)]
    slab_ctr = [0]
    evac_ctr = [0]

    def project(col0, kind):
        P.barrier()
        if kind == "diff":
            VD4 = VV.rearrange("p t (h c) -> p t h c", c=129)
            P.op("dve", lambda e: e.memset(VD4[:, :, :, 128:129], 1.0), writes=bV)
        for s in range(6):
            wi = slab_ctr[0] % 3
            slab_ctr[0] += 1
            c_lo = col0 + s * 256
            P.dma("pool", lambda e, wi=wi, c_lo=c_lo: e.dma_start(out=wsl[wi], in_=win_d[:, :, c_lo:c_lo + 256]), writes=[bW[wi]])
            if s < 4:
                for gg in range(2):
                    gi = s * 2 + gg
                    for tc in range(4):
                        bk = evac_ctr[0] % 4
                        evac_ctr[0] += 1
                        for c in range(8):
                            P.op("pe", lambda e, wi=wi, gg=gg, tc=tc, c=c, bk=bk: e.matmul(
                                psb[bk], lhsT=wsl[wi][:, c, gg * 128:(gg + 1) * 128], rhs=hnT[:, c, tc * 512:(tc + 1) * 512],
                                start=(c == 0), stop=(c == 7)),
                                reads=[bW[wi]] + bHn[tc * 4:tc * 4 + 4], writes=[bPs[bk]])
                        sc = 0.125 if s < 2 else 1.0
                        if evac_ctr[0] % 2 == 0:
                            P.op("act", lambda e, gi=gi, tc=tc, bk=bk, sc=sc: e.activation(out=QK[:, gi, tc * 512:(tc + 1) * 512], in_=psb[bk], func=AF.Copy, scale=sc),
                                 reads=[bPs[bk]], writes=[bQK[gi]], accum=True)
                        else:
                            P.op("dve", lambda e, gi=gi, tc=tc, bk=bk, sc=sc: e.tensor_scalar(out=QK[:, gi, tc * 512:(tc + 1) * 512], in0=psb[bk], scalar1=sc, scalar2=None, op0=ALU.mult),
                                 reads=[bPs[bk]], writes=[bQK[gi]], accum=True)
            else:
                vs = s - 4
                for i in range(NT):
                    bk = evac_ctr[0] % 4
                    evac_ctr[0] += 1
                    for c in range(8):
                        P.op("pe", lambda e, wi=wi, i=i, c=c, bk=bk: e.matmul(
                            psb[bk][:, 0:256], lhsT=hnT[:, c, i * 128:(i + 1) * 128], rhs=wsl[wi][:, c, :], start=(c == 0), stop=(c == 7)),
                            reads=[bW[wi], bHn[i]], writes=[bPs[bk]])
                    if kind == "diff":
                        dst = VV[:, i, vs * 258:(vs + 1) * 258].rearrange("p (h c) -> p h c", c=129)[:, :, 0:128]
                        src = psb[bk][:, 0:256].rearrange("p (h c) -> p h c", c=128)
                    else:
                        dst = VV[:, i, vs * 256:(vs + 1) * 256]
                        src = psb[bk][:, 0:256]
                    if evac_ctr[0] % 2 == 0:
                        P.op("act", lambda e, dst=dst, src=src: e.activation(out=dst, in_=src, func=AF.Copy), reads=[bPs[bk]], writes=[bV[i]], accum=True)
                    else:
                        P.op("dve", lambda e, dst=dst, src=src: e.tensor_copy(out=dst, in_=src), reads=[bPs[bk]], writes=[bV[i]], accum=True)

    project(0, "diff")
    if stop == "B":
        dbg["QK"] = (QK, [128, 8, S], BF16)
        dbg["VV"] = (VV, [128, NT, 516], BF16)
        return finish(nc, P, st, dbg)

    P.barrier()
    attnT = view(OFF_AT, [128, 8, S], BF16)
    bAT = [B("at%d" % i) for i in range(NT)]
    NSLOT = 32
    Er = [view(OFF_W + i * 1024, [128, 2, 256], BF16) for i in range(NSLOT)]
    bE = [B("E%d" % i) for i in range(NSLOT)]
    def tv(par, k):
        base = OFF_T + par * 1296
        if k == 0:
            return view(base, [128, 130], F32)
        if k == 1:
            return view(base + 520, [128, 130], F32)
        return view(base + 1040, [128, 128], BF16)
    bEp = [B("ep%d" % i) for i in range(4)]
    late = []
    bSS = [B("S%d" % i) for i in range(2)]
    bO = [B("O%d" % m) for m in range(2)]
    bTp = [B("tp%d" % i) for i in range(2)]
    VD4 = VV.rearrange("p t (h c) -> p t h c", c=129)
    ebv = ebfix[:].rearrange("p (h d q) -> p h d q", h=4, d=2)

    units = [(h, c) for h in range(4) for c in range(8)]
    import os
    if stop == 'C1':
        units = units[:1]
    if os.environ.get('KLIM'):
        units = units[:int(os.environ['KLIM'])]
    blk_ctr = [0]
    ep_ctr = [0]

    def av_items(h, c, slots):
        items = []
        for j in range(2):
            for m in range(2):
                kbs = list(range(0, 2 * c + j + 1))
                for kb in kbs:
                    items.append((j, m, kb, kb == 0, kb == kbs[-1]))
        return items

    def emit_av(h, c, slots, it):
        j, m, kb, first, last = it
        ob = psb[4 + m][:, 0:129]
        sl = slots[kb]
        P.op("pe", lambda e: e.matmul(ob, lhsT=Er[sl][:, m, j * 128:(j + 1) * 128], rhs=VD4[:, kb, h, :], start=first, stop=last),
             reads=[bE[sl], bV[kb]], writes=[bO[m]])
        if last:
            par = ep_ctr[0] % 4
            P.op("dve", lambda e: e.tensor_copy(out=tv(par, m)[:, 0:129], in_=ob), reads=[bO[m]], writes=[bEp[par]], accum=(m == 1))
            if m == 1:
                epilogue(h, c, j)

    def epilogue(h, c, j):
        qb = 2 * c + j
        par = ep_ctr[0] % 4
        ep_ctr[0] += 1
        si = qb
        c0 = si * 8
        o1 = tv(par, 0); o2 = tv(par, 1); obf = tv(par, 2)
        ep = [bEp[par]]
        P.op("dve", lambda e: e.reciprocal(out=stats[:, c0:c0 + 1], in_=o1[:, 128:129]), reads=ep, writes=[bSt[si]])
        P.op("dve", lambda e: e.reciprocal(out=stats[:, c0 + 1:c0 + 2], in_=o2[:, 128:129]), reads=ep, writes=[bSt[si]])
        P.op("dve", lambda e: e.tensor_scalar(out=stats[:, c0 + 2:c0 + 3], in0=stats[:, c0 + 1:c0 + 2], scalar1=neglam, scalar2=None, op0=ALU.mult),
             reads=[bSt[si], bC5], writes=[bSt[si]])
        P.op("dve", lambda e: e.tensor_scalar(out=o2[:, 0:128], in0=o2[:, 0:128], scalar1=stats[:, c0 + 2:c0 + 3], scalar2=None, op0=ALU.mult),
             reads=ep + [bSt[si]], writes=ep)
        P.op("dve", lambda e: e.scalar_tensor_tensor(out=o1[:, 0:128], in0=o1[:, 0:128], scalar=stats[:, c0:c0 + 1], in1=o2[:, 0:128], op0=ALU.mult, op1=ALU.add),
             reads=ep + [bSt[si]], writes=ep)
        P.op("dve", lambda e: e.tensor_tensor(out=o2[:, 0:128], in0=o1[:, 0:128], in1=o1[:, 0:128], op=ALU.mult), reads=ep, writes=ep)
        P.op("dve", lambda e: e.tensor_reduce(out=stats[:, c0 + 3:c0 + 4], in_=o2[:, 0:128], axis=AX.X, op=ALU.add), reads=ep, writes=[bSt[si]])
        P.op("dve", lambda e: e.tensor_scalar(out=stats[:, c0 + 4:c0 + 5], in0=stats[:, c0 + 3:c0 + 4], scalar1=1.0 / 128, scalar2=EPS, op0=ALU.mult, op1=ALU.add),
             reads=[bSt[si]], writes=[bSt[si]])
        P.op("pool", lambda e: e.tensor_tensor(out=stats[:, c0 + 5:c0 + 6], in0=stats[:, c0 + 4:c0 + 5], in1=cst[:, 0:1], op=ALU.pow),
             reads=[bSt[si], bCst], writes=[bSt[si]])
        P.op("dve", lambda e: e.scalar_tensor_tensor(out=obf, in0=o1[:, 0:128], scalar=stats[:, c0 + 5:c0 + 6], in1=subg[:], op0=ALU.mult, op1=ALU.mult),
             reads=ep + [bSt[si], bC6], writes=ep)
        tb = psb[6 + par % 2].bitcast(BF16)

        def fin():
            P.op("pe", lambda e: e.transpose(tb[:, 0:128], obf, identb), reads=[bEp[par], bConst], writes=[bTp[par % 2]])
            P.op("dve", lambda e: e.tensor_copy(out=attnT[:, h, qb * 128:(qb + 1) * 128], in_=tb[:, 0:128]), reads=[bTp[par % 2]], writes=[bAT[qb]], accum=True)
        late.append(fin)

    pending = []
    for ui, (h, c) in enumerate(units):
        nb = 2 * c + 2
        slots = {}
        per = (len(pending) + nb - 1) // nb if pending else 0
        late_now = list(late)
        del late[:]
        for kb in range(nb):
            if kb == 1:
                for f_ in late_now:
                    f_()
            sl = blk_ctr[0] % NSLOT
            blk_ctr[0] += 1
            slots[kb] = sl
            sp_ = kb % 2
            lo = 128 if kb == 2 * c + 1 else 0
            sb3 = pall[:, sp_ * 512:sp_ * 512 + 2048].rearrange("p (m r) -> p m r", m=2)
            for m in range(2):
                P.op("pe", lambda e, m=m, kb=kb, lo=lo, sp_=sp_, h=h, c=c: e.matmul(
                    psb[2 * m + sp_][:, lo:256], lhsT=QK[m * 64:(m + 1) * 64, 4 + h, kb * 128:(kb + 1) * 128],
                    rhs=QK[m * 64:(m + 1) * 64, h, c * 256 + lo:(c + 1) * 256], start=True, stop=True),
                    reads=[bQK[4 + h], bQK[h]], writes=[bSS[sp_]])
            P.op("act", lambda e, sl=sl, lo=lo, sb3=sb3: e.activation(out=Er[sl][:, :, lo:256], in_=sb3[:, :, lo:256], func=AF.Exp),
                 reads=[bSS[sp_]], writes=[bE[sl]])
            for j in range(2):
                d = 2 * c + j - kb
                if 0 <= d <= 1:
                    for m in range(2):
                        P.op("dve", lambda e, sl=sl, m=m, j=j, d=d, h=h: e.tensor_tensor(
                            out=Er[sl][:, m, j * 128:(j + 1) * 128], in0=Er[sl][:, m, j * 128:(j + 1) * 128], in1=ebv[:, h, d, :], op=ALU.mult),
                            reads=[bE[sl], bC3], writes=[bE[sl]])
            for _ in range(per):
                if pending:
                    emit_av(*pending.pop(0))
        while pending:
            emit_av(*pending.pop(0))
        pending = [(h, c, slots, it) for it in av_items(h, c, slots)]
    while pending:
        emit_av(*pending.pop(0))
    for f_ in late:
        f_()

    if stop == "C1":
        dbg["E0"] = (Er[0], [128, 2, 256], BF16)
        dbg["E1"] = (Er[1], [128, 2, 256], BF16)
        dbg["ebfix"] = (ebfix[:], [128, 1024], F32)
        dbg["lams"] = (lams[:], [128, 8], F32)
        dbg["o1s"] = (tv(0, 0), [128, 130], F32)
        dbg["obf"] = (tv(0, 2), [128, 128], BF16)
        dbg["at0"] = (attnT[:, 0, 0:256], [128, 256], BF16)
        dbg["stats"] = (stats[:], [128, 128], F32)
        return finish(nc, P, st, dbg)
    if stop == "C":
        dbg["attnT"] = (attnT, [128, 8, S], BF16)
        return finish(nc, P, st, dbg)

    project(1536, "sb")
    P.barrier()
    Wr = [view(OFF_W + i * 512, [128, 256], BF16) for i in range(4)]
    bWr = [B("Wr%d" % i) for i in range(4)]
    e32 = [view(OFF_W + 2048 + i * 1024, [128, 256], F32) for i in range(2)]
    Lb = [view(OFF_W + 4096 + i * 512, [128, 256], BF16) for i in range(2)]
    Rb = [view(OFF_W + 5120 + i * 512, [128, 256], BF16) for i in range(3)]
    be32 = [B("e32%d" % i) for i in range(2)]
    bL = [B("L%d" % i) for i in range(2)]
    bR = [B("R%d" % i) for i in range(3)]
    bZ = [B("Z%d" % i) for i in range(4)]
    bX = [B("X%d" % i) for i in range(2)]
    bOT = [B("OT%d" % i) for i in range(2)]

    blocks = []
    for hd in range(8):
        for c in range(8):
            for kb in range(2 * c + 1, -1, -1):
                blocks.append((hd, c, kb))
    NB = len(blocks)

    def sb_pe1(i):
        hd, c, kb = blocks[i]
        g, po = hd // 2, (hd % 2) * 64
        lo = 128 if kb == 2 * c + 1 else 0
        pz = i % 2
        P.op("pe", lambda e: e.matmul(psb[i % 4][:, lo:256], lhsT=QK[po:po + 64, 4 + g, kb * 128:(kb + 1) * 128],
                                      rhs=QK[po:po + 64, g, c * 256 + lo:(c + 1) * 256], start=True, stop=False),
             reads=[bQK[4 + g], bQK[g]], writes=[bZ[i % 4]])

    def sb_act1(i):
        hd, c, kb = blocks[i]
        lo = 128 if kb == 2 * c + 1 else 0
        pz = i % 2
        first = kb == 2 * c + 1
        P.op("act", lambda e: e.activation(out=e32[pz][:, lo:256], in_=psb[i % 4][:, lo:256], func=AF.Exp), reads=[bZ[i % 4]], writes=[be32[pz]])
        if first:
            P.op("dve", lambda e: e.memset(e32[pz][:, 0:128], 0.0), writes=[be32[pz]], accum=True)
        j = kb - 2 * c
        if j >= 0:
            P.op("dve", lambda e: e.tensor_tensor(out=e32[pz][:, j * 128:(j + 1) * 128], in0=e32[pz][:, j * 128:(j + 1) * 128], in1=strictm, op=ALU.mult),
                 reads=[be32[pz], bC2], writes=[be32[pz]])
        P.op("act", lambda e: e.activation(out=Lb[pz][:], in_=e32[pz][:], func=AF.Ln, bias=1.0), reads=[be32[pz]], writes=[bL[pz]])
        if kb > 0:
            if first:
                P.op("dve", lambda e: e.tensor_copy(out=Rb[(i + 1) % 3][:], in_=Lb[pz][:]), reads=[bL[pz]], writes=[bR[(i + 1) % 3]])
            else:
                P.op("dve", lambda e: e.tensor_tensor(out=Rb[(i + 1) % 3][:], in0=Rb[i % 3][:], in1=Lb[pz][:], op=ALU.add),
                     reads=[bL[pz], bR[i % 3]], writes=[bR[(i + 1) % 3]])

    def sb_pe2(i):
        hd, c, kb = blocks[i]
        lo = 128 if kb == 2 * c + 1 else 0
        pz = i % 2
        first = kb == 2 * c + 1
        xb = psb[i % 4]
        P.op("pe", lambda e: e.matmul(xb[:, lo:256], lhsT=negtri, rhs=Lb[pz][:, lo:256], start=False, stop=first),
             reads=[bL[pz], bConst, bZ[i % 4]], writes=[bZ[i % 4]])
        if not first:
            P.op("pe", lambda e: e.matmul(xb[:, lo:256], lhsT=negones, rhs=Rb[i % 3][:, lo:256], start=False, stop=True),
                 reads=[bR[i % 3], bConst], writes=[bZ[i % 4]])

    def sb_act2(i):
        hd, c, kb = blocks[i]
        lo = 128 if kb == 2 * c + 1 else 0
        pz = i % 2
        wi = i % 4
        first = kb == 2 * c + 1
        P.op("act", lambda e: e.activation(out=Wr[wi][:, lo:256], in_=psb[i % 4][:, lo:256], func=AF.Exp), reads=[bZ[i % 4]], writes=[bWr[wi]])
        if first:
            P.op("dve", lambda e: e.memset(Wr[wi][:, 0:128], 0.0), writes=[bWr[wi]], accum=True)
        j = kb - 2 * c
        if j >= 0:
            P.op("dve", lambda e: e.tensor_tensor(out=Wr[wi][:, j * 128:(j + 1) * 128], in0=Wr[wi][:, j * 128:(j + 1) * 128], in1=strictm, op=ALU.mult),
                 reads=[bWr[wi], bC2], writes=[bWr[wi]])

    unit_ctr = [0]

    def sb_pe3(i):
        hd, c, kb = blocks[i]
        g, po = hd // 2, (hd % 2) * 64
        wi = i % 4
        first = kb == 2 * c + 1
        last = kb == 0
        up = (hd * 8 + c) % 2
        ob = psb[4 + up]
        P.op("pe", lambda e: e.matmul(ob[po:po + 64, 0:256], lhsT=VV[:, kb, hd * 64:(hd + 1) * 64], rhs=Wr[wi][:], start=first, stop=last),
             reads=[bWr[wi], bV[kb]], writes=[bOT[up]])
        if last:
            if (hd * 8 + c) % 2 == 0:
                P.op("act", lambda e: e.activation(out=attnT[po:po + 64, 4 + g, c * 256:(c + 1) * 256], in_=ob[po:po + 64, 0:256], func=AF.Copy),
                     reads=[bOT[up]], writes=[bAT[2 * c], bAT[2 * c + 1]], accum=True)
            else:
                P.op("dve", lambda e: e.tensor_copy(out=attnT[po:po + 64, 4 + g, c * 256:(c + 1) * 256], in_=ob[po:po + 64, 0:256]),
                     reads=[bOT[up]], writes=[bAT[2 * c], bAT[2 * c + 1]], accum=True)

    for s_ in range(NB + 2):
        if s_ < NB:
            sb_pe1(s_)
            sb_act1(s_)
        if 0 <= s_ - 1 < NB:
            sb_pe2(s_ - 1)
            sb_act2(s_ - 1)
        if 0 <= s_ - 2 < NB:
            sb_pe3(s_ - 2)

    if stop == "D":
        dbg["attnT"] = (attnT, [128, 8, S], BF16)
        return finish(nc, P, st, dbg)

    P.barrier()
    KB = 1024
    hres = view(0, [128, NT, D], F32)
    tT = view(96 * KB, [128, 8, S], BF16)
    wob = view(128 * KB, [128, 8, D], BF16)
    xt1 = view(144 * KB, [128, D], F32)
    tb1 = view(148 * KB, [128, D], BF16)
    junk2 = view(150 * KB, [128, D], BF16)
    tmpx = sbt("tmpx", [128, 4 * 512], F32)
    comb = sbt("comb", [128, NT * 32], F32)
    lgs = sbt("lgs", [128, 4 * 32], F32)
    mx8 = sbt("mx8", [128, 32], F32)
    ix8 = sbt("ix8", [128, 8], U32)
    mbt = sbt("mbt", [128, 96], BF16)
    bIx = B("ix8"); bMb = B("mskb"); bMacc = [B("macc0"), B("macc1")]; bXg = B("xg"); bH1d = B("h1d"); bMeta = B("meta")
    rwb = sbt("rwb", [128, 256], BF16)
    bH = [B("h%d" % i_) for i_ in range(NT)]
    bTT = [B("tT%d" % i_) for i_ in range(NT)]
    bWo = B("wo"); bX1 = B("x1"); bTb = B("tb1"); bJ2 = B("junk2"); bRwb = B("rwb"); bLg = B("lg"); bMx = B("mx"); bComb = B("comb")
    bPE = [B("pe%d" % i_) for i_ in range(8)]
    P.dma("pool", lambda e: e.dma_start(out=wob, in_=wout_d), writes=[bWo])
    P.dma("pool", lambda e: e.dma_start(out=rwb[:], in_=rw_d), writes=[bRwb])
    P.dma("sp", lambda e: e.dma_start(out=gainb[:], in_=gains_d[1].partition_broadcast(128)), writes=[bGain])

    def rms2(i_, src, srcbuf, n):
        c0 = i_ * 8
        P.op("act", lambda e: e.activation(out=junk2[:, 0:n], in_=src, func=AF.Square, accum_out=stats[:, c0:c0 + 1]), reads=[srcbuf], writes=[bJ2, bSt[i_]])
        P.op("dve", lambda e: e.tensor_scalar(out=stats[:, c0 + 1:c0 + 2], in0=stats[:, c0:c0 + 1], scalar1=1.0 / n, scalar2=EPS, op0=ALU.mult, op1=ALU.add), reads=[bSt[i_]], writes=[bSt[i_]])
        P.op("pool", lambda e: e.tensor_tensor(out=stats[:, c0 + 3:c0 + 4], in0=stats[:, c0 + 1:c0 + 2], in1=cst[:, 0:1], op=ALU.pow), reads=[bSt[i_], bCst], writes=[bSt[i_]])
        return stats[:, c0 + 3:c0 + 4]

    xt1s = [xt1, tmpx[:, 0:1024]]
    tb1s = [tb1, tmpx[:, 1024:1536].bitcast(BF16)]
    bX1s = [bX1, B("x1b")]
    bTbs = [bTb, B("tb1b")]

    def phaseE(i_):
        xt1 = xt1s[i_ % 2]; tb1 = tb1s[i_ % 2]; bX1 = bX1s[i_ % 2]; bTb = bTbs[i_ % 2]
        P.dma("sp", lambda e: e.dma_start(out=xt1, in_=x_d[i_ * 128:(i_ + 1) * 128, :]), writes=[bX1])
        for half in range(2):
            bk = 2 * (i_ % 2) + half
            for c_ in range(8):
                P.op("pe", lambda e, c_=c_, bk=bk, half=half: e.matmul(psb[bk], lhsT=attnT[:, c_, i_ * 128:(i_ + 1) * 128], rhs=wob[:, c_, half * 512:(half + 1) * 512], start=(c_ == 0), stop=(c_ == 7)),
                     reads=[bAT[i_], bWo], writes=[bPE[bk]])
            P.op("dve", lambda e, bk=bk, half=half: e.tensor_tensor(out=hres[:, i_, half * 512:(half + 1) * 512], in0=psb[bk], in1=xt1[:, half * 512:(half + 1) * 512], op=ALU.add),
                 reads=[bPE[bk], bX1], writes=[bH[i_]], accum=(half == 1))
        rstd = rms2(i_, hres[:, i_, :], bH[i_], D)
        P.op("dve", lambda e: e.scalar_tensor_tensor(out=tb1, in0=hres[:, i_, :], scalar=rstd, in1=gainb[:], op0=ALU.mult, op1=ALU.mult), reads=[bH[i_], bSt[i_], bGain], writes=[bTb])
        pT = psb[4 + i_ % 2].bitcast(BF16)
        for c_ in range(8):
            P.op("pe", lambda e, c_=c_: e.transpose(pT[:, c_ * 128:(c_ + 1) * 128], tb1[:, c_ * 128:(c_ + 1) * 128], identb), reads=[bTb, bConst], writes=[bPE[4 + i_ % 2]])
        P.op("act", lambda e: e.activation(out=tT[:, :, i_ * 128:(i_ + 1) * 128], in_=pT.rearrange("p (c t) -> p c t", t=128), func=AF.Copy), reads=[bPE[4 + i_ % 2]], writes=[bTT[i_]])
        for c_ in range(8):
            P.op("pe", lambda e, c_=c_: e.matmul(psb[6][:, 0:32], lhsT=tT[:, c_, i_ * 128:(i_ + 1) * 128], rhs=rwb[:, c_ * 32:(c_ + 1) * 32], start=(c_ == 0), stop=(c_ == 7)),
                 reads=[bTT[i_], bRwb], writes=[bPE[6]])
        lg = lgs[:, 0:32]; exl = lgs[:, 32:64]; msk = lgs[:, 64:96]
        P.op("dve", lambda e: e.tensor_tensor(out=lg, in0=psb[6][:, 0:32], in1=rbb[:], op=ALU.add), reads=[bPE[6], bC8], writes=[bLg])
        P.op("dve", lambda e: e.max(out=mx8[:, 0:8], in_=lg), reads=[bLg], writes=[bMx])
        P.op("dve", lambda e: e.tensor_scalar(out=mx8[:, 8:9], in0=mx8[:, 0:1], scalar1=-1.0, scalar2=None, op0=ALU.mult), reads=[bMx], writes=[bMx])
        P.op("act", lambda e: e.activation(out=exl, in_=lg, func=AF.Exp, bias=mx8[:, 8:9]), reads=[bLg, bMx], writes=[bLg])
        P.op("dve", lambda e: e.tensor_scalar(out=msk, in0=lg, scalar1=mx8[:, 3:4], scalar2=None, op0=ALU.is_ge), reads=[bLg, bMx], writes=[bLg])
        P.op("dve", lambda e: e.tensor_tensor(out=exl, in0=exl, in1=msk, op=ALU.mult), reads=[bLg], writes=[bLg])
        P.op("dve", lambda e: e.tensor_reduce(out=mx8[:, 9:10], in_=exl, axis=AX.X, op=ALU.add), reads=[bLg], writes=[bMx])
        P.op("dve", lambda e: e.reciprocal(out=mx8[:, 10:11], in_=mx8[:, 9:10]), reads=[bMx], writes=[bMx])
        P.op("act", lambda e: e.activation(out=mx8[:, 16:20], in_=mx8[:, 0:4], func=AF.Exp, bias=mx8[:, 8:9]), reads=[bMx], writes=[bMx])
        P.op("dve", lambda e: e.tensor_scalar(out=meta_g[:, i_ * 4:(i_ + 1) * 4], in0=mx8[:, 16:20], scalar1=mx8[:, 10:11], scalar2=None, op0=ALU.mult), reads=[bMx], writes=[bMeta], accum=True)
        P.op("dve", lambda e: e.max_index(out=ix8[:], in_max=mx8[:, 0:8], in_values=lg), reads=[bLg, bMx], writes=[bIx])
        P.op("dve", lambda e: e.tensor_copy(out=mbt[:, 0:32], in_=msk), reads=[bLg], writes=[bMb])
        pfx = psb[7][:, 0:32]
        a_ = i_ % 2
        P.op("pe", lambda e: e.matmul(pfx, lhsT=ltm, rhs=mbt[:, 0:32], start=True, stop=(i_ == 0)), reads=[bMb, bConst], writes=[bPE[7]])
        if i_ > 0:
            P.op("pe", lambda e: e.matmul(pfx, lhsT=onesb, rhs=mbt[:, 32 + a_ * 32:64 + a_ * 32], start=False, stop=True), reads=[bMacc[a_], bConst], writes=[bPE[7]])
        if i_ == 0:
            P.op("dve", lambda e: e.tensor_copy(out=mbt[:, 64:96], in_=mbt[:, 0:32]), reads=[bMb], writes=[bMacc[1]])
        else:
            P.op("dve", lambda e: e.tensor_tensor(out=mbt[:, 32 + (1 - a_) * 32:64 + (1 - a_) * 32], in0=mbt[:, 32 + a_ * 32:64 + a_ * 32], in1=mbt[:, 0:32], op=ALU.add),
                 reads=[bMb, bMacc[a_]], writes=[bMacc[1 - a_]])
        oh = lgs[:, 96:128]
        for k_ in range(4):
            P.op("dve", lambda e, k_=k_: e.tensor_scalar(out=oh, in0=lg, scalar1=mx8[:, k_:k_ + 1], scalar2=None, op0=ALU.is_equal), reads=[bLg, bMx], writes=[bLg])
            P.op("dve", lambda e: e.tensor_tensor(out=oh, in0=oh, in1=pfx, op=ALU.mult), reads=[bLg, bPE[7]], writes=[bLg])
            P.op("dve", lambda e, k_=k_: e.tensor_reduce(out=mx8[:, 20 + k_:21 + k_], in_=oh, axis=AX.X, op=ALU.add), reads=[bLg], writes=[bMx])
        P.op("dve", lambda e: e.tensor_copy(out=mx8[:, 24:28], in_=ix8[:, 0:4]), reads=[bIx], writes=[bMx])
        P.op("dve", lambda e: e.scalar_tensor_tensor(out=mx8[:, 28:32], in0=mx8[:, 24:28], scalar=float(CAP), in1=mx8[:, 20:24], op0=ALU.mult, op1=ALU.add), reads=[bMx], writes=[bMx])
        P.op("dve", lambda e: e.tensor_copy(out=meta_dest[:, i_ * 4:(i_ + 1) * 4], in_=mx8[:, 28:32]), reads=[bMx], writes=[bMeta], accum=True)
        for k_ in range(4):
            P.dma("pool", lambda e, k_=k_: e.indirect_dma_start(out=xg_d, out_offset=bass.IndirectOffsetOnAxis(ap=meta_dest[:, i_ * 4 + k_:i_ * 4 + k_ + 1], axis=0),
                                                               in_=tb1, in_offset=None), reads=[bTb, bMeta], writes=[bXg])
        P.dma("sp", lambda e: e.dma_start(out=h1_d[i_ * 128:(i_ + 1) * 128, :], in_=hres[:, i_, :]), reads=[bH[i_]], writes=[bH1d])

    for i_ in range(NT):
        phaseE(i_)
    cnt_i = sbt("cnt_i", [128, 32], I32)
    bCnt = B("cnt")
    P.op("pe", lambda e: e.matmul(psb[7][:, 0:32], lhsT=onesb, rhs=mbt[:, 32:64], start=True, stop=True), reads=[bMacc[0], bConst], writes=[bPE[7]])
    P.op("dve", lambda e: e.tensor_copy(out=cnt_i[:], in_=psb[7][:, 0:32]), reads=[bPE[7]], writes=[bCnt])
    P.cnt_ap = lambda ex_: cnt_i[0:1, ex_:ex_ + 1]

    if stop == "E":
        dbg["h1"] = (hres, [128, NT, D], F32)
        dbg["tT"] = (tT, [128, 8, S], BF16)
        dbg["mdest"] = (meta_dest[:], [128, NT * 4], I32)
        dbg["mg"] = (meta_g[:], [128, NT * 4], F32)
        dbg["xg0"] = (xg_d[0:512, :], [512, D], BF16)
        return finish(nc, P, st, dbg)

    P.barrier()
    NTS = RC // 128
    xe = [view(0 + k_ * 6 * KB, [128, NTS, D], BF16) for k_ in range(2)]
    XeT = [view(12 * KB + k_ * 6 * KB, [128, 8, RC], BF16) for k_ in range(2)]
    actT = [view(24 * KB + k_ * 6 * KB, [128, 8, RC], BF16) for k_ in range(2)]
    wdb = [view(36 * KB + k_ * 16 * KB, [128, 8, D], BF16) for k_ in range(2)]
    wg = [view(112 * KB + k_ * 4 * KB, [128, 8, 256], BF16) for k_ in range(6)]
    yt = [view(80 * KB + k_ * 4 * KB, [128, D], F32) for k_ in range(2)]
    bdt = [view(88 * KB + k_ * 4 * KB, [128, D], F32) for k_ in range(2)]
    bXe = [B("xe%d" % k_) for k_ in range(2)]; bXT = [B("XeT%d" % k_) for k_ in range(2)]; bAc = [B("ac%d" % k_) for k_ in range(2)]
    bWd = [B("wd%d" % k_) for k_ in range(2)]; bWg = [B("wg%d" % k_) for k_ in range(6)]; bYt = [B("yt%d" % k_) for k_ in range(2)]; bBd = [B("bd%d" % k_) for k_ in range(2)]
    bYg = B("yg")
    bTgs = [B("tg%d" % k_) for k_ in range(3)]; bTss = [B("ts%d" % k_) for k_ in range(3)]; bTls = [B("tl%d" % k_) for k_ in range(3)]
    tgs = [view(96 * KB + k_ * 1536, [128, RC], F32) for k_ in range(3)]
    tss = [view(101 * KB + k_ * 1536, [128, RC], F32) for k_ in range(3)]
    tls = [view(106 * KB + k_ * 1536, [128, RC], F32) for k_ in range(3)]
    cnt = [0]; gcnt = [0]; slabc = [0]; ytc = [0]; qc = [0]

    def gu_chunk(e_, j_, k_, q_):
        ba = 2 * (gcnt[0] % 2)
        tg = tgs[gcnt[0] % 3]; ts = tss[gcnt[0] % 3]; tl = tls[gcnt[0] % 3]
        bTg = bTgs[gcnt[0] % 3]; bTs = bTss[gcnt[0] % 3]; bTl = bTls[gcnt[0] % 3]
        gcnt[0] += 1
        KC = int(os.environ.get("KKC", 8))
        for which in range(2):
            for c_ in range(KC):
                P.op("pe", lambda e, c_=c_, which=which: e.matmul(psb[ba + which][:, 0:RC], lhsT=wg[k_][:, c_, which * 128:(which + 1) * 128], rhs=XeT[q_][:, c_, :], start=(c_ == 0), stop=(c_ == KC - 1)),
                     reads=[bWg[k_], bXT[q_]], writes=[bPE[ba + which]])
        col = (e_ * 8 + j_) * 2
        P.op("dve", lambda e: e.tensor_scalar(out=tg, in0=psb[ba][:, 0:RC], scalar1=bgl[:, col:col + 1], scalar2=7.0, op0=ALU.add, op1=ALU.min), reads=[bPE[ba], bC9], writes=[bTg])
        P.op("act", lambda e: e.activation(out=ts, in_=tg, func=AF.Silu, scale=1.702), reads=[bTg], writes=[bTs])
        P.op("dve", lambda e: e.tensor_scalar(out=tl, in0=psb[ba + 1][:, 0:RC], scalar1=bgl[:, col + 1:col + 2], scalar2=8.0, op0=ALU.add, op1=ALU.min), reads=[bPE[ba + 1], bC9], writes=[bTl])
        P.op("dve", lambda e: e.scalar_tensor_tensor(out=actT[q_][:, j_, :], in0=tl, scalar=-6.0, in1=ts, op0=ALU.max, op1=ALU.mult), reads=[bTl, bTs], writes=[bAc[q_]], accum=True)

    def down_tile(e_, q_, st_, half, yk):
        bk = 6 + cnt[0] % 2
        cnt[0] += 1
        for j_ in range(8):
            P.op("pe", lambda e, j_=j_: e.matmul(psb[bk], lhsT=actT[q_][:, j_, st_ * 128:(st_ + 1) * 128], rhs=wdb[q_][:, j_, half * 512:(half + 1) * 512], start=(j_ == 0), stop=(j_ == 7)),
                 reads=[bAc[q_], bWd[q_]], writes=[bPE[bk]])
        P.op("dve", lambda e: e.scalar_tensor_tensor(out=yt[yk][:, half * 512:(half + 1) * 512], in0=psb[bk], scalar=1.0 / 1.702, in1=bdt[q_][:, half * 512:(half + 1) * 512], op0=ALU.mult, op1=ALU.add),
             reads=[bPE[bk], bBd[q_]], writes=[bYt[yk]], accum=(half == 1))

    def chunk_prep(e_, ch):
        q_ = qc[0] % 2
        qc[0] += 1
        row0 = e_ * CAP + min(ch * RC, CAP - RC)
        P.dma("sp", lambda e: e.dma_start(out=xe[q_], in_=xg_d[row0:row0 + RC, :].rearrange("(t p) d -> p t d", p=128)), reads=[bXg], writes=[bXe[q_]])
        P.dma("sp", lambda e: e.dma_start(out=bdt[q_], in_=bd_d[e_].partition_broadcast(128)), writes=[bBd[q_]])
        P.dma("pool", lambda e: e.dma_start(out=wdb[q_], in_=wd_d[e_]), writes=[bWd[q_]])
        for t_ in range(NTS):
            pT = psb[4 + t_ % 2].bitcast(BF16)
            for c_ in range(8):
                P.op("pe", lambda e, c_=c_, t_=t_, pT=pT: e.transpose(pT[:, c_ * 128:(c_ + 1) * 128], xe[q_][:, t_, c_ * 128:(c_ + 1) * 128], identb),
                     reads=[bXe[q_], bConst], writes=[bPE[4 + t_ % 2]])
            if t_ % 2 == 0:
                P.op("act", lambda e, t_=t_, pT=pT: e.activation(out=XeT[q_][:, :, t_ * 128:(t_ + 1) * 128], in_=pT.rearrange("p (c t) -> p c t", t=128), func=AF.Copy),
                     reads=[bPE[4 + t_ % 2]], writes=[bXT[q_]], accum=(t_ > 0))
            else:
                P.op("dve", lambda e, t_=t_, pT=pT: e.tensor_copy(out=XeT[q_][:, :, t_ * 128:(t_ + 1) * 128], in_=pT.rearrange("p (c t) -> p c t", t=128)),
                     reads=[bPE[4 + t_ % 2]], writes=[bXT[q_]], accum=True)
        return (e_, q_, row0)

    def chunk_gate(stt):
        e_, q_, row0 = stt
        for j_ in range(8):
            k_ = slabc[0] % 6
            slabc[0] += 1
            P.dma("pool", lambda e, j_=j_, k_=k_: e.dma_start(out=wg[k_], in_=wgu_d[e_, j_]), writes=[bWg[k_]])
            gu_chunk(e_, j_, k_, q_)

    def chunk_down(stt):
        e_, q_, row0 = stt
        for st_ in range(NTS):
            yk = ytc[0] % 2
            ytc[0] += 1
            for half in range(2):
                down_tile(e_, q_, st_, half, yk)
            P.dma("sp", lambda e, st_=st_, yk=yk: e.dma_start(out=yg_d[row0 + st_ * 128:row0 + (st_ + 1) * 128, :], in_=yt[yk]), reads=[bYt[yk]], writes=[bYg], sembuf=bYg)

    def expert_chunk(e_, ch):
        stt = chunk_prep(e_, ch)
        chunk_gate(stt)
        chunk_down(stt)

    NEX = int(os.environ.get("KNEX", NE))
    NCHX = int(os.environ.get("KNCH", NCH))
    stt_ = chunk_prep(0, 0)
    for e_ in range(NEX):
        chunk_gate(stt_)
        nxt = chunk_prep(e_ + 1, 0) if e_ + 1 < NEX else None
        chunk_down(stt_)
        stt_ = nxt
    for e_ in range(NEX):
        for ch in range(1, NCHX):
            P.guard_begin((e_, ch * RC))
            expert_chunk(e_, ch)
        for ch in range(1, NCHX):
            P.guard_end()

    if stop == "G":
        dbg["cnt"] = (cnt_i[:], [128, 32], I32)
        return finish(nc, P, st, dbg)

    P.barrier()
    pgb = view(128 * KB, [128, 8, D], BF16)
    ppb = view(144 * KB, [128, 2, D], BF16)
    ptile_ = [view(148 * KB + k_ * 2 * KB, [128, 256], F32) for k_ in range(2)]
    pbf_ = [view(149 * KB + k_ * 2 * KB, [128, 256], BF16) for k_ in range(2)]
    pTs_ = [view(149 * KB + 512 + k_ * 2 * KB, [128, 2, 128], BF16) for k_ in range(2)]
    pe32_ = [view(k_ * 4 * KB, [128, D], F32) for k_ in range(2)]
    gate_ = [view(8 * KB + k_ * 4 * KB, [128, D], F32) for k_ in range(2)]
    hbf_ = [view(16 * KB + k_ * 2 * KB, [128, D], BF16) for k_ in range(2)]
    hT_ = [view(20 * KB + k_ * 2 * KB, [128, 8, 128], BF16) for k_ in range(2)]
    otile = [view(76 * KB + k_ * 4096, [128, D], F32) for k_ in range(2)]
    gfin = ebfix
    bPg = B("pg"); bPp = B("pp"); bPt_ = [B("pt0"), B("pt1")]; bPbf_ = [B("pbf0"), B("pbf1")]; bPTs_ = [B("pTs0"), B("pTs1")]; bPe32_ = [B("pe320"), B("pe321")]; bGate_ = [B("gate0"), B("gate1")]; bHbf_ = [B("hbf0"), B("hbf1")]; bHT_ = [B("hT0"), B("hT1")]
    bOt = [B("ot%d" % k_) for k_ in range(2)]; bGf = B("gfin"); bOut = B("out")
    P.dma("pool", lambda e: e.dma_start(out=pgb, in_=pgate_d), writes=[bPg])
    P.dma("pool", lambda e: e.dma_start(out=ppb, in_=pproj_d), writes=[bPp])
    P.dma("sp", lambda e: e.dma_start(out=gainb[:], in_=gains_d[2].partition_broadcast(128)), writes=[bGain])
    P.dma("sp", lambda e: e.dma_start(out=gfin[:], in_=gains_d[3].partition_broadcast(128)), writes=[bGf])

    htl = [view(84 * KB + k_ * 4 * KB, [128, D], F32) for k_ in range(2)]
    ygk = [view(92 * KB + k_ * 4 * KB, [128, D], F32) for k_ in range(8)]
    bHt = [B("ht%d" % k_) for k_ in range(2)]
    bYk = [B("ygk%d" % k_) for k_ in range(8)]

    junkH = view(24 * KB, [128, D], BF16)

    def rmsH(i_, src, srcbuf, n):
        c0 = i_ * 8
        P.op("act", lambda e: e.activation(out=junkH[:, 0:n], in_=src, func=AF.Square, accum_out=stats[:, c0:c0 + 1]), reads=[srcbuf], writes=[bJ2, bSt[i_]])
        P.op("dve", lambda e: e.tensor_scalar(out=stats[:, c0 + 1:c0 + 2], in0=stats[:, c0:c0 + 1], scalar1=1.0 / n, scalar2=EPS, op0=ALU.mult, op1=ALU.add), reads=[bSt[i_]], writes=[bSt[i_]])
        P.op("pool", lambda e: e.tensor_tensor(out=stats[:, c0 + 3:c0 + 4], in0=stats[:, c0 + 1:c0 + 2], in1=cst[:, 0:1], op=ALU.pow), reads=[bSt[i_], bCst], writes=[bSt[i_]])
        return stats[:, c0 + 3:c0 + 4]

    def phaseH(i_):
        k_ = i_ % 2
        ptile = ptile_[k_]; pbf = pbf_[k_]; pTs = pTs_[k_]; pe32 = pe32_[k_]; gate = gate_[k_]; hbf = hbf_[k_]; hT = hT_[k_]
        bPt = bPt_[k_]; bPbf = bPbf_[k_]; bPTs = bPTs_[k_]; bPe32 = bPe32_[k_]; bGate = bGate_[k_]; bHbf = bHbf_[k_]; bHT = bHT_[k_]
        hcur = htl[k_]
        P.dma("sp", lambda e: e.dma_start(out=hcur, in_=h1_d[i_ * 128:(i_ + 1) * 128, :]), reads=[bH1d], writes=[bHt[k_]])
        for kk in range(4):
            yb = k_ * 4 + kk
            P.dma("pool", lambda e, kk=kk, yb=yb: e.indirect_dma_start(out=ygk[yb], out_offset=None, in_=yg_d,
                                                                        in_offset=bass.IndirectOffsetOnAxis(ap=meta_dest[:, i_ * 4 + kk:i_ * 4 + kk + 1], axis=0)),
                  reads=[bYg, bMeta], writes=[bYk[yb]])
            P.op("dve", lambda e, kk=kk, yb=yb: e.scalar_tensor_tensor(out=hcur, in0=ygk[yb], scalar=meta_g[:, i_ * 4 + kk:i_ * 4 + kk + 1], in1=hcur, op0=ALU.mult, op1=ALU.add),
                 reads=[bYk[yb], bMeta, bHt[k_]], writes=[bHt[k_]])
        P.dma("sp", lambda e: e.dma_start(out=ptile, in_=p_d[i_ * 128:(i_ + 1) * 128, :]), writes=[bPt])
        P.op("dve", lambda e: e.tensor_copy(out=pbf, in_=ptile), reads=[bPt], writes=[bPbf])
        tq = psb[6].bitcast(BF16)
        for c_ in range(2):
            P.op("pe", lambda e, c_=c_: e.transpose(tq[:, c_ * 128:(c_ + 1) * 128], pbf[:, c_ * 128:(c_ + 1) * 128], identb), reads=[bPbf, bConst], writes=[bPE[6]])
        P.op("act", lambda e: e.activation(out=pTs, in_=tq[:, 0:256].rearrange("p (c t) -> p c t", t=128), func=AF.Copy), reads=[bPE[6]], writes=[bPTs])
        for half in range(2):
            for c_ in range(2):
                P.op("pe", lambda e, c_=c_, half=half: e.matmul(psb[half], lhsT=pTs[:, c_, :], rhs=ppb[:, c_, half * 512:(half + 1) * 512], start=(c_ == 0), stop=(c_ == 1)),
                     reads=[bPTs, bPp], writes=[bPE[half]])
            P.op("act", lambda e, half=half: e.activation(out=pe32[:, half * 512:(half + 1) * 512], in_=psb[half], func=AF.Copy), reads=[bPE[half]], writes=[bPe32], accum=(half == 1))
        rp = rmsH(i_, pe32, bPe32, D)
        P.op("dve", lambda e: e.scalar_tensor_tensor(out=pe32, in0=pe32, scalar=rp, in1=gainb[:], op0=ALU.mult, op1=ALU.mult), reads=[bPe32, bSt[i_], bGain], writes=[bPe32])
        P.op("dve", lambda e: e.tensor_copy(out=hbf, in_=hcur), reads=[bHt[k_]], writes=[bHbf])
        tq2 = psb[7].bitcast(BF16)
        for c_ in range(8):
            P.op("pe", lambda e, c_=c_: e.transpose(tq2[:, c_ * 128:(c_ + 1) * 128], hbf[:, c_ * 128:(c_ + 1) * 128], identb), reads=[bHbf, bConst], writes=[bPE[7]])
        P.op("act", lambda e: e.activation(out=hT, in_=tq2.rearrange("p (c t) -> p c t", t=128), func=AF.Copy), reads=[bPE[7]], writes=[bHT])
        for half in range(2):
            for c_ in range(8):
                P.op("pe", lambda e, c_=c_, half=half: e.matmul(psb[2 + half], lhsT=hT[:, c_, :], rhs=pgb[:, c_, half * 512:(half + 1) * 512], start=(c_ == 0), stop=(c_ == 7)),
                     reads=[bHT, bPg], writes=[bPE[2 + half]])
            P.op("act", lambda e, half=half: e.activation(out=gate[:, half * 512:(half + 1) * 512], in_=psb[2 + half], func=AF.Sigmoid), reads=[bPE[2 + half]], writes=[bGate], accum=(half == 1))
        P.op("dve", lambda e: e.tensor_tensor(out=gate, in0=gate, in1=pe32, op=ALU.mult), reads=[bGate, bPe32], writes=[bGate])
        P.op("dve", lambda e: e.tensor_tensor(out=hcur, in0=hcur, in1=gate, op=ALU.add), reads=[bHt[k_], bGate], writes=[bHt[k_]])
        c0 = i_ * 8 + 4
        P.op("act", lambda e: e.activation(out=junkH, in_=hcur, func=AF.Square, accum_out=stats[:, c0:c0 + 1]), reads=[bHt[k_]], writes=[bJ2, bSt[i_]])
        P.op("dve", lambda e: e.tensor_scalar(out=stats[:, c0 + 1:c0 + 2], in0=stats[:, c0:c0 + 1], scalar1=1.0 / D, scalar2=EPS, op0=ALU.mult, op1=ALU.add), reads=[bSt[i_]], writes=[bSt[i_]])
        P.op("pool", lambda e: e.tensor_tensor(out=stats[:, c0 + 2:c0 + 3], in0=stats[:, c0 + 1:c0 + 2], in1=cst[:, 0:1], op=ALU.pow), reads=[bSt[i_], bCst], writes=[bSt[i_]])
        P.op("dve", lambda e: e.scalar_tensor_tensor(out=otile[k_], in0=hcur, scalar=stats[:, c0 + 2:c0 + 3], in1=gfin[:], op0=ALU.mult, op1=ALU.mult), reads=[bHt[k_], bSt[i_], bGf], writes=[bOt[k_]])
        P.dma("sp", lambda e: e.dma_start(out=out_d[i_ * 128:(i_ + 1) * 128, :], in_=otile[k_]), reads=[bOt[k_]], writes=[bOut], sembuf=bOt[k_])

    for i_ in range(NT):
        phaseH(i_)
    P.wait_all("sp", [bOut] + bOt)
    P.run(st)
    st.close()
    return nc


def finish(nc, P, st, dbg):
    P.barrier()
    outs = []
    for name, (ap, shape, dt) in dbg.items():
        d = nc.dram_tensor("dbg_" + name, shape, dt, kind="ExternalOutput").ap()
        b = Buf(P, "dbg_" + name)
        P.dma("sp", lambda e, d=d, ap=ap: e.dma_start(out=d, in_=ap), writes=[b])
        outs.append(b)
    P.wait_all("sp", outs)
    P.run(st)
    st.close()
    return nc


def _t5_bucket(rel):
    n = np.maximum(rel, 0)
    nf = np.maximum(n, 1).astype(np.float32)
    large = 16 + (np.log(nf / np.float32(16)) / np.float32(math.log(128 / 16)) * np.float32(16)).astype(np.int32)
    large = np.minimum(large, 31)
    return np.where(n < 16, n, large)


def prep_shared(inp):
    f32 = np.float32
    g = lambda k: np.asarray(inp[k], dtype=f32)
    w_in = g("w_in")[0]
    cols = []
    for h in range(4):
        cols += list(range(h * 64, h * 64 + 64)) + list(range(256 + h * 64, 256 + h * 64 + 64))
    for h in range(4):
        cols += list(range(512 + h * 64, 512 + h * 64 + 64)) + list(range(768 + h * 64, 768 + h * 64 + 64))
    cols += list(range(1024, 3072))
    w_in_p = w_in[:, cols]
    chunked = lambda w: np.ascontiguousarray(w.reshape(8, 128, -1).transpose(1, 0, 2))
    sh = {}
    sh["w_in"] = chunked(w_in_p)
    sh["w_out"] = chunked(g("w_out")[0])
    sh["gains"] = np.stack([g("attn_norm")[0], g("moe_norm")[0], g("ple_norm")[0], g("final_norm")], 0)
    sh["lamv"] = np.stack([g("lambda_q1")[0], g("lambda_k1")[0], g("lambda_q2")[0], g("lambda_k2")[0]], 0)
    sh["subln"] = g("subln")
    rb = g("rel_bias")
    k = np.arange(128)[:, None]
    q = np.arange(128)[None, :]
    bn = np.zeros((128, 4, 2, 128), f32)
    for d in range(2):
        bk = _t5_bucket(q + 128 * d - k)
        bn[:, :, d, :] = rb[bk].transpose(0, 2, 1)
    sh["bnear"] = bn.reshape(128, -1)
    sh["c31"] = np.ascontiguousarray(np.broadcast_to(rb[31][None, :], (128, 4)))
    sh["rw"] = chunked(g("router_w")[0]).reshape(128, -1)
    sh["rb"] = g("router_b")
    wgu = g("w_gate_up")[0]
    glu = wgu[:, :, 0::2].reshape(NE, 8, 128, 8, 128)
    lin = wgu[:, :, 1::2].reshape(NE, 8, 128, 8, 128)
    gl = np.concatenate([glu, lin], axis=-1)
    sh["wgu"] = np.ascontiguousarray(gl.transpose(0, 3, 2, 1, 4))
    bgu = g("b_gate_up")[0]
    bg = bgu[:, 0::2].reshape(NE, 8, 128)
    bl = bgu[:, 1::2].reshape(NE, 8, 128)
    sh["bgl"] = np.ascontiguousarray(np.stack([bg, bl], -1).transpose(2, 0, 1, 3)).reshape(128, -1)
    sh["wd"] = np.ascontiguousarray(g("w_down")[0].reshape(NE, 8, 128, D).transpose(0, 2, 1, 3))
    sh["bd"] = g("b_down")[0]
    sh["pproj"] = np.ascontiguousarray(g("ple_proj")[0].reshape(2, 128, D).transpose(1, 0, 2))
    sh["pgate"] = chunked(g("ple_gate")[0])
    ident = np.eye(128, dtype=f32)
    jj = np.arange(128)[:, None]
    kk = np.arange(128)[None, :]
    negtri = -(jj >= kk).astype(f32)
    lt = (jj < kk).astype(f32)
    sh["cbf"] = np.concatenate([ident, negtri, -np.ones((128, 128), f32), lt, np.ones((128, 128), f32)], 1).astype(ml_dtypes.bfloat16)
    sh["cf32"] = np.concatenate([ident, (kk >= jj).astype(f32), (kk > jj).astype(f32)], 1)
    return sh


_CACHE = {}


def kernel(**inputs):
    sh = prep_shared(inputs)
    x = np.asarray(inputs["x"], dtype=np.float32)
    p = np.asarray(inputs["p"], dtype=np.float32)[0]
    if "nc" not in _CACHE:
        _CACHE["nc"] = build()
    nc = _CACHE["nc"]
    in_maps = []
    for c in range(8):
        m = dict(sh)
        m["x"] = np.ascontiguousarray(x[c])
        m["p"] = np.ascontiguousarray(p[c])
        in_maps.append(m)
    res = run_bass_kernel_spmd(nc, in_maps, core_ids=list(range(8)))
    return np.stack([np.asarray(r["out"], dtype=np.float32) for r in res.results], 0)
```

```python
from contextlib import ExitStack
import math
import numpy as np
import ml_dtypes
import concourse.bass as bass
import concourse.mybir as mybir
from concourse.bass_utils import run_bass_kernel_spmd

F32 = mybir.dt.float32
BF16 = mybir.dt.bfloat16
U32 = mybir.dt.uint32
I32 = mybir.dt.int32
AF = mybir.ActivationFunctionType
ALU = mybir.AluOpType
AX = mybir.AxisListType

S = 2048
D = 1024
NT = 16
NE = 32
RC = 384
NCH = 6
CAP = 2048
EPS = 1e-6
ENG = {"pe": "tensor", "act": "scalar", "dve": "vector", "pool": "gpsimd", "sp": "sync"}


class Buf:
    __slots__ = ("name", "w", "r", "sem", "cnt")

    def __init__(self, P, name):
        self.name = name
        self.w = []
        self.r = []
        self.sem = None
        self.cnt = 0
        P.bufs.append(self)


class Prog:
    def __init__(self, nc):
        self.nc = nc
        self.recs = {e: [] for e in ENG}
        self.waited = {e: {} for e in ENG}
        self.dma_sems = []
        self.bufs = []

    def _filter(self, eng, deps):
        out = []
        for t in deps:
            if t[0] == "c":
                _, e2, idx = t
                if e2 == eng and eng in ("pe", "sp"):
                    continue
                k = ("c", e2)
                if self.waited[eng].get(k, -1) >= idx:
                    continue
                self.waited[eng][k] = idx
                self.recs[e2][idx]["sig"] = True
                out.append(t)
            else:
                _, b, val = t
                k = ("d", id(b))
                if self.waited[eng].get(k, -1) >= val:
                    continue
                self.waited[eng][k] = val
                out.append(t)
        return out

    def _deps(self, eng, reads, writes):
        deps = []
        for b in reads:
            deps += b.w
        for b in writes:
            deps += b.w
            deps += b.r
        return self._filter(eng, deps)

    def op(self, eng, fn, reads=(), writes=(), accum=False):
        waits = self._deps(eng, reads, writes)
        idx = len(self.recs[eng])
        self.recs[eng].append(dict(waits=waits, fn=fn, sig=False, dma=None))
        tok = ("c", eng, idx)
        for b in reads:
            b.r.append(tok)
        for b in writes:
            if accum:
                b.w.append(tok)
            else:
                b.w = [tok]
                b.r = []
        return tok

    def dma(self, eng, fn, reads=(), writes=(), sembuf=None):
        waits = self._deps(eng, reads, writes)
        sb = sembuf if sembuf is not None else (writes[0] if writes else reads[0])
        if sb.sem is None:
            sb.sem = True
            self.dma_sems.append(sb)
        sb.cnt += 16
        tok = ("d", sb, sb.cnt)
        self.recs[eng].append(dict(waits=waits, fn=fn, sig=False, dma=sb, dmaval=sb.cnt))
        for b in reads:
            b.r.append(tok)
        for b in writes:
            b.w = [tok]
            b.r = []
        return tok

    def barrier(self):
        toks = []
        for e in ENG:
            for i in range(len(self.recs[e]) - 1, -1, -1):
                r = self.recs[e][i]
                if r["fn"] is not None and r["dma"] is None:
                    toks.append(("c", e, i))
                    break
        for b in self.bufs:
            toks += [t for t in b.w + b.r if t[0] == "d"]
            b.w = []
            b.r = []
        for e in ENG:
            w = self._filter(e, [t for t in toks if not (t[0] == "c" and t[1] == e)])
            self.recs[e].append(dict(waits=w, fn=None, sig=False, dma=None))

    def guard_begin(self, key):
        if not hasattr(self, "_wstack"):
            self._wstack = []
        self._wstack.append({e: dict(self.waited[e]) for e in ENG})
        for e in ENG:
            self.recs[e].append(dict(waits=[], fn=None, sig=False, dma=None, gb=key))

    def guard_end(self):
        for e in ENG:
            self.recs[e].append(dict(waits=[], fn=None, sig=False, dma=None, ge=True))
        self.waited = self._wstack.pop()

    def wait_all(self, eng, bufs):
        waits = self._deps(eng, list(bufs), list(bufs))
        self.recs[eng].append(dict(waits=waits, fn=None, sig=False, dma=None))

    def run(self, stack):
        nc = self.nc
        esem = {e: stack.enter_context(nc.semaphore("s_" + e)) for e in ENG}
        for i, b in enumerate(self.dma_sems):
            b.sem = stack.enter_context(nc.semaphore("d%d" % i))
        cnts = {}
        for e in ENG:
            c = 0
            for i, r in enumerate(self.recs[e]):
                if r["sig"]:
                    c += 1
                    cnts[(e, i)] = c
        block = stack.enter_context(nc.Block())

        def body(e):
            def emit(engine, r):
                for t in r["waits"]:
                    if t[0] == "c":
                        engine.wait_ge(esem[t[1]], cnts[(t[1], t[2])])
                    else:
                        engine.wait_ge(t[1].sem, t[2])
                if r["fn"] is None:
                    return
                ins = r["fn"](engine)
                if r["dma"] is not None:
                    ins.then_inc(r["dma"].sem, 16)
                elif r["sig"]:
                    ins.then_inc(esem[e], 1)

            def f(engine):
                recs = self.recs[e]
                reg = rthr = None
                if any("gb" in q for q in recs):
                    reg = stack.enter_context(engine.register("rc_" + e))
                    rthr = stack.enter_context(engine.register("rt_" + e))
                csum = [0]

                def match(i):
                    d_ = 0
                    j = i
                    while True:
                        if "gb" in recs[j]:
                            d_ += 1
                        elif "ge" in recs[j]:
                            d_ -= 1
                            if d_ == 0:
                                return j
                        j += 1

                def process(lo, hi):
                    i = lo
                    while i < hi:
                        r = recs[i]
                        if "gb" in r:
                            j = match(i)
                            inner = [q for q in recs[i + 1:j] if "gb" not in q and "ge" not in q]
                            ex_, thr = r["gb"]
                            if any(q["fn"] is not None or q["waits"] for q in inner):
                                c_before = csum[0]
                                engine.reg_load(reg, self.cnt_ap(ex_))
                                engine.reg_mov(rthr, thr)
                                with engine.If_lt(rthr, reg):
                                    process(i + 1, j)
                                csum[0] = c_before
                                nsig = sum(1 for q in inner if q["sig"])
                                dmas = [q for q in inner if q["dma"] is not None]
                                if nsig or dmas:
                                    with engine.Else():
                                        if nsig:
                                            if c_before > 0:
                                                engine.wait_ge(esem[e], c_before)
                                            engine.sem_inc(esem[e], nsig)
                                        for q in dmas:
                                            if q["dmaval"] - 16 > 0:
                                                engine.wait_ge(q["dma"].sem, q["dmaval"] - 16)
                                            engine.sem_inc(q["dma"].sem, 16)
                                csum[0] = c_before + nsig
                            i = j + 1
                            continue
                        emit(engine, r)
                        if r["sig"]:
                            csum[0] += 1
                        i += 1

                process(0, len(recs))
            return f

        block.tensor(body("pe"))
        block.scalar(body("act"))
        block.vector(body("dve"))
        block.gpsimd(body("pool"))
        block.sync(body("sp"))


def build(stop=None):
    nc = bass.Bass("TRN2", target_bir_lowering=False, dynamic_dma_scratch_size=8192)
    dram = lambda n, s, d=F32, k="ExternalInput": nc.dram_tensor(n, s, d, kind=k).ap()
    x_d = dram("x", [S, D])
    p_d = dram("p", [S, 256])
    win_d = dram("w_in", [128, 8, 3072])
    wout_d = dram("w_out", [128, 8, D])
    gains_d = dram("gains", [4, D])
    lamv_d = dram("lamv", [4, 64])
    subln_d = dram("subln", [1, 128])
    bnear_d = dram("bnear", [128, 4 * 2 * 128])
    c31_d = dram("c31", [128, 4])
    rw_d = dram("rw", [128, 8 * 32])
    rb_d = dram("rb", [1, 32])
    wgu_d = dram("wgu", [NE, 8, 128, 8, 256])
    bgl_d = dram("bgl", [128, NE * 8 * 2])
    wd_d = dram("wd", [NE, 128, 8, D])
    bd_d = dram("bd", [NE, D])
    pproj_d = dram("pproj", [128, 2, D])
    pgate_d = dram("pgate", [128, 8, D])
    cb_d = dram("cbf", [128, 5 * 128], BF16)
    cf_d = dram("cf32", [128, 3 * 128])
    out_d = dram("out", [S, D], F32, "ExternalOutput")
    h1_d = dram("h1s", [S, D], F32, "Internal")
    xg_d = dram("xg", [NE * CAP, D], BF16, "Internal")
    yg_d = dram("yg", [NE * CAP, D], F32, "Internal")
    dbg = {}

    st = ExitStack()
    P = Prog(nc)
    B = lambda n: Buf(P, n)
    sbt = lambda n, s, d: st.enter_context(nc.sbuf_tensor("sb_" + n, s, d))
    pall = st.enter_context(nc.psum_tensor("pall", [128, 4096], F32))
    psb = [pall[:, i * 512:(i + 1) * 512] for i in range(8)]

    cb = sbt("cb", [128, 5 * 128], BF16)
    cf = sbt("cf", [128, 3 * 128], F32)
    identb, negtri, negones, ltm, onesb = [cb[:, i * 128:(i + 1) * 128] for i in range(5)]
    identf, mask0, strictm = [cf[:, i * 128:(i + 1) * 128] for i in range(3)]
    gainb = sbt("gainb", [128, D], F32)
    gain2 = sbt("gain2", [128, D], F32)
    ebfix = sbt("ebfix", [128, 4 * 2 * 128], F32)
    c31 = sbt("c31", [128, 4], F32)
    lamt = sbt("lamt", [128, 4 * 64], F32)
    lams = sbt("lams", [128, 8], F32)
    subg = sbt("subg", [128, 128], F32)
    rw = sbt("rw", [128, 8 * 32], F32)
    rbb = sbt("rbb", [128, 32], F32)
    bgl = sbt("bgl", [128, NE * 8 * 2], F32)
    cst = sbt("cst", [128, 8], F32)
    stats = sbt("stats", [128, NT * 8], F32)
    meta_dest = sbt("meta_dest", [128, NT * 4], I32)
    meta_g = sbt("meta_g", [128, NT * 4], F32)
    bConst = B("const")
    bGain = B("gain")
    bGain2 = B("gain2")

    AR = sbt("arena", [128, 152 * 1024], mybir.dt.uint8)
    K = 1024
    OFF_HN, OFF_QK, OFF_AT, OFF_W, OFF_V, OFF_T = 0, 32 * K, 64 * K, 96 * K, 128 * K, 145 * K

    def view(off, shape, dt):
        nb = {F32: 4, BF16: 2, I32: 4, U32: 4}[dt]
        n = 1
        for s_ in shape[1:]:
            n *= s_
        ap = AR[:, off:off + n * nb].bitcast(dt)
        if len(shape) == 3:
            ap = ap.rearrange("p (a b) -> p a b", b=shape[2])
        elif len(shape) == 4:
            ap = ap.rearrange("p (a b c) -> p a b c", b=shape[2], c=shape[3])
        return ap

    P.dma("sp", lambda e: e.dma_start(out=cb[:], in_=cb_d), writes=[bConst])
    bC2 = B("c2"); bC3 = B("c3"); bC4 = B("c4"); bC5 = B("c5"); bC6 = B("c6"); bC7 = B("c7"); bC8 = B("c8"); bC9 = B("c9")
    P.dma("sp", lambda e: e.dma_start(out=cf[:], in_=cf_d), writes=[bC2])
    P.dma("sp", lambda e: e.dma_start(out=ebfix[:], in_=bnear_d), writes=[bC3])
    P.dma("sp", lambda e: e.dma_start(out=c31[:], in_=c31_d), writes=[bC4])
    P.dma("sp", lambda e: e.dma_start(out=lamt[:], in_=lamv_d.rearrange("a b -> (a b)").partition_broadcast(128)), writes=[bC5])
    P.dma("sp", lambda e: e.dma_start(out=subg[:], in_=subln_d.rearrange("a b -> (a b)").partition_broadcast(128)), writes=[bC6])
    P.dma("sp", lambda e: e.dma_start(out=rw[:], in_=rw_d), writes=[bC7])
    P.dma("sp", lambda e: e.dma_start(out=rbb[:], in_=rb_d.rearrange("a b -> (a b)").partition_broadcast(128)), writes=[bC8])
    P.dma("sp", lambda e: e.dma_start(out=bgl[:], in_=bgl_d), writes=[bC9])
    P.dma("sp", lambda e: e.dma_start(out=gainb[:], in_=gains_d[0].partition_broadcast(128)), writes=[bGain])
    bCst = B("cst")
    P.op("dve", lambda e: e.memset(cst[:, 0:1], -0.5), writes=[bCst])
    for h in range(4):
        P.op("dve", lambda e, h=h: e.tensor_scalar(out=ebfix[:, h * 256:(h + 1) * 256], in0=ebfix[:, h * 256:(h + 1) * 256],
                                                   scalar1=c31[:, h:h + 1], scalar2=None, op0=ALU.subtract),
             reads=[bC3, bC4], writes=[bC3])
    P.op("act", lambda e: e.activation(out=ebfix[:], in_=ebfix[:], func=AF.Exp), reads=[bC3], writes=[bC3])
    for h in range(4):
        P.op("dve", lambda e, h=h: e.tensor_tensor(out=ebfix[:, h * 256:h * 256 + 128], in0=ebfix[:, h * 256:h * 256 + 128], in1=mask0, op=ALU.mult),
             reads=[bC3, bC2], writes=[bC3])
    P.op("dve", lambda e: e.tensor_tensor(out=lamt[:, 0:64], in0=lamt[:, 0:64], in1=lamt[:, 64:128], op=ALU.mult), reads=[bC5], writes=[bC5])
    P.op("dve", lambda e: e.tensor_tensor(out=lamt[:, 128:192], in0=lamt[:, 128:192], in1=lamt[:, 192:256], op=ALU.mult), reads=[bC5], writes=[bC5])
    P.op("dve", lambda e: e.tensor_reduce(out=lams[:, 0:1], in_=lamt[:, 0:64], axis=AX.X, op=ALU.add), reads=[bC5], writes=[bC5])
    P.op("dve", lambda e: e.tensor_reduce(out=lams[:, 1:2], in_=lamt[:, 128:192], axis=AX.X, op=ALU.add), reads=[bC5], writes=[bC5])
    P.op("act", lambda e: e.activation(out=lams[:, 2:4], in_=lams[:, 0:2], func=AF.Exp), reads=[bC5], writes=[bC5])
    P.op("dve", lambda e: e.tensor_tensor(out=lams[:, 4:5], in0=lams[:, 3:4], in1=lams[:, 2:3], op=ALU.subtract), reads=[bC5], writes=[bC5])
    P.op("dve", lambda e: e.tensor_scalar(out=lams[:, 4:5], in0=lams[:, 4:5], scalar1=-0.2, scalar2=None, op0=ALU.add), reads=[bC5], writes=[bC5])
    P.op("dve", lambda e: e.tensor_scalar(out=subg[:], in0=subg[:], scalar1=0.8, scalar2=None, op0=ALU.mult), reads=[bC6], writes=[bC6])
    bglv = bgl[:].rearrange("p (a t) -> p a t", t=2)
    P.op("dve", lambda e: e.tensor_scalar(out=bglv[:, :, 1:2], in0=bglv[:, :, 1:2], scalar1=1.0, scalar2=None, op0=ALU.add), reads=[bC9], writes=[bC9])
    neglam = lams[:, 4:5]

    hnT = view(OFF_HN, [128, 8, S], BF16)
    bHn = [B("hn%d" % i) for i in range(NT)]
    xt = [view(OFF_W + i * 4096, [128, D], F32) for i in range(2)]
    xs = [view(OFF_W + 8192 + i * 2048, [128, D], BF16) for i in range(2)]
    junkb = view(OFF_T, [128, D], BF16)
    bXt = [B("xt%d" % i) for i in range(2)]
    bXs = [B("xs%d" % i) for i in range(2)]
    bJ = B("junk")
    bSt = [B("st%d" % i) for i in range(NT)]
    bPs = [B("ps%d" % i) for i in range(8)]

    def rms_stats(i, src, srcbuf, n):
        c0 = i * 8
        P.op("act", lambda e: e.activation(out=junkb[:, 0:n], in_=src, func=AF.Square, accum_out=stats[:, c0:c0 + 1]),
             reads=[srcbuf], writes=[bJ, bSt[i]])
        P.op("dve", lambda e: e.tensor_scalar(out=stats[:, c0 + 1:c0 + 2], in0=stats[:, c0:c0 + 1], scalar1=1.0 / n, scalar2=EPS, op0=ALU.mult, op1=ALU.add),
             reads=[bSt[i]], writes=[bSt[i]])
        P.op("pool", lambda e: e.tensor_tensor(out=stats[:, c0 + 3:c0 + 4], in0=stats[:, c0 + 1:c0 + 2], in1=cst[:, 0:1], op=ALU.pow),
             reads=[bSt[i], bCst], writes=[bSt[i]])
        return stats[:, c0 + 3:c0 + 4]

    for i in range(NT):
        b = i % 2
        P.dma("sp", lambda e, i=i, b=b: e.dma_start(out=xt[b], in_=x_d[i * 128:(i + 1) * 128, :]), writes=[bXt[b]])
        rstd = rms_stats(i, xt[b], bXt[b], D)
        P.op("dve", lambda e, b=b, rstd=rstd: e.scalar_tensor_tensor(out=xs[b], in0=xt[b], scalar=rstd, in1=gainb[:], op0=ALU.mult, op1=ALU.mult),
             reads=[bXt[b], bSt[i], bGain], writes=[bXs[b]])
        pT = psb[b].bitcast(BF16)
        for c in range(8):
            P.op("pe", lambda e, c=c, b=b, pT=pT: e.transpose(pT[:, c * 128:(c + 1) * 128], xs[b][:, c * 128:(c + 1) * 128], identb),
                 reads=[bXs[b], bConst], writes=[bPs[b]])
        P.op("act", lambda e, i=i, pT=pT: e.activation(out=hnT[:, :, i * 128:(i + 1) * 128], in_=pT.rearrange("p (c t) -> p c t", t=128), func=AF.Copy),
             reads=[bPs[b]], writes=[bHn[i]])

    if stop == "A":
        dbg["hnT"] = (hnT, [128, 8, S], BF16)
        return finish(nc, P, st, dbg)

    QK = view(OFF_QK, [128, 8, S], BF16)
    VV = view(OFF_V, [128, NT, 516], BF16)
    wsl = [view(OFF_W + i * 4096, [128, 8, 256], BF16) for i in range(3)]
    bW = [B("wsl%d" % i) for i in range(3)]
    bQK = [B("qk%d" % i) for i in range(8)]
    bV = [B("v%d" % i) for i in range(NT)]
    slab_ctr = [0]
    evac_ctr = [0]

    def project(col0, kind):
        P.barrier()
        if kind == "diff":
            VD4 = VV.rearrange("p t (h c) -> p t h c", c=129)
            P.op("dve", lambda e: e.memset(VD4[:, :, :, 128:129], 1.0), writes=bV)
        for s in range(6):
            wi = slab_ctr[0] % 3
            slab_ctr[0] += 1
            c_lo = col0 + s * 256
            P.dma("pool", lambda e, wi=wi, c_lo=c_lo: e.dma_start(out=wsl[wi], in_=win_d[:, :, c_lo:c_lo + 256]), writes=[bW[wi]])
            if s < 4:
                for gg in range(2):
                    gi = s * 2 + gg
                    for tc in range(4):
                        bk = evac_ctr[0] % 4
                        evac_ctr[0] += 1
                        for c in range(8):
                            P.op("pe", lambda e, wi=wi, gg=gg, tc=tc, c=c, bk=bk: e.matmul(
                                psb[bk], lhsT=wsl[wi][:, c, gg * 128:(gg + 1) * 128], rhs=hnT[:, c, tc * 512:(tc + 1) * 512],
                                start=(c == 0), stop=(c == 7)),
                                reads=[bW[wi]] + bHn[tc * 4:tc * 4 + 4], writes=[bPs[bk]])
                        sc = 0.125 if s < 2 else 1.0
                        if evac_ctr[0] % 2 == 0:
                            P.op("act", lambda e, gi=gi, tc=tc, bk=bk, sc=sc: e.activation(out=QK[:, gi, tc * 512:(tc + 1) * 512], in_=psb[bk], func=AF.Copy, scale=sc),
                                 reads=[bPs[bk]], writes=[bQK[gi]], accum=True)
                        else:
                            P.op("dve", lambda e, gi=gi, tc=tc, bk=bk, sc=sc: e.tensor_scalar(out=QK[:, gi, tc * 512:(tc + 1) * 512], in0=psb[bk], scalar1=sc, scalar2=None, op0=ALU.mult),
                                 reads=[bPs[bk]], writes=[bQK[gi]], accum=True)
            else:
                vs = s - 4
                for i in range(NT):
                    bk = evac_ctr[0] % 4
                    evac_ctr[0] += 1
                    for c in range(8):
                        P.op("pe", lambda e, wi=wi, i=i, c=c, bk=bk: e.matmul(
                            psb[bk][:, 0:256], lhsT=hnT[:, c, i * 128:(i + 1) * 128], rhs=wsl[wi][:, c, :], start=(c == 0), stop=(c == 7)),
                            reads=[bW[wi], bHn[i]], writes=[bPs[bk]])
                    if kind == "diff":
                        dst = VV[:, i, vs * 258:(vs + 1) * 258].rearrange("p (h c) -> p h c", c=129)[:, :, 0:128]
                        src = psb[bk][:, 0:256].rearrange("p (h c) -> p h c", c=128)
                    else:
                        dst = VV[:, i, vs * 256:(vs + 1) * 256]
                        src = psb[bk][:, 0:256]
                    if evac_ctr[0] % 2 == 0:
                        P.op("act", lambda e, dst=dst, src=src: e.activation(out=dst, in_=src, func=AF.Copy), reads=[bPs[bk]], writes=[bV[i]], accum=True)
                    else:
                        P.op("dve", lambda e, dst=dst, src=src: e.tensor_copy(out=dst, in_=src), reads=[bPs[bk]], writes=[bV[i]], accum=True)

    project(0, "diff")
    if stop == "B":
        dbg["QK"] = (QK, [128, 8, S], BF16)
        dbg["VV"] = (VV, [128, NT, 516], BF16)
        return finish(nc, P, st, dbg)

    P.barrier()
    attnT = view(OFF_AT, [128, 8, S], BF16)
    bAT = [B("at%d" % i) for i in range(NT)]
    NSLOT = 32
    Er = [view(OFF_W + i * 1024, [128, 2, 256], BF16) for i in range(NSLOT)]
    bE = [B("E%d" % i) for i in range(NSLOT)]
    def tv(par, k):
        base = OFF_T + par * 1296
        if k == 0:
            return view(base, [128, 130], F32)
        if k == 1:
            return view(base + 520, [128, 130], F32)
        return view(base + 1040, [128, 128], BF16)
    bEp = [B("ep%d" % i) for i in range(4)]
    late = []
    bSS = [B("S%d" % i) for i in range(2)]
    bO = [B("O%d" % m) for m in range(2)]
    bTp = [B("tp%d" % i) for i in range(2)]
    VD4 = VV.rearrange("p t (h c) -> p t h c", c=129)
    ebv = ebfix[:].rearrange("p (h d q) -> p h d q", h=4, d=2)

    units = [(h, c) for h in range(4) for c in range(8)]
    import os
    if stop == 'C1':
        units = units[:1]
    if os.environ.get('KLIM'):
        units = units[:int(os.environ['KLIM'])]
    blk_ctr = [0]
    ep_ctr = [0]

    def av_items(h, c, slots):
        items = []
        for j in range(2):
            for m in range(2):
                kbs = list(range(0, 2 * c + j + 1))
                for kb in kbs:
                    items.append((j, m, kb, kb == 0, kb == kbs[-1]))
        return items

    def emit_av(h, c, slots, it):
        j, m, kb, first, last = it
        ob = psb[4 + m][:, 0:129]
        sl = slots[kb]
        P.op("pe", lambda e: e.matmul(ob, lhsT=Er[sl][:, m, j * 128:(j + 1) * 128], rhs=VD4[:, kb, h, :], start=first, stop=last),
             reads=[bE[sl], bV[kb]], writes=[bO[m]])
        if last:
            par = ep_ctr[0] % 4
            P.op("dve", lambda e: e.tensor_copy(out=tv(par, m)[:, 0:129], in_=ob), reads=[bO[m]], writes=[bEp[par]], accum=(m == 1))
            if m == 1:
                epilogue(h, c, j)

    def epilogue(h, c, j):
        qb = 2 * c + j
        par = ep_ctr[0] % 4
        ep_ctr[0] += 1
        si = qb
        c0 = si * 8
        o1 = tv(par, 0); o2 = tv(par, 1); obf = tv(par, 2)
        ep = [bEp[par]]
        P.op("dve", lambda e: e.reciprocal(out=stats[:, c0:c0 + 1], in_=o1[:, 128:129]), reads=ep, writes=[bSt[si]])
        P.op("dve", lambda e: e.reciprocal(out=stats[:, c0 + 1:c0 + 2], in_=o2[:, 128:129]), reads=ep, writes=[bSt[si]])
        P.op("dve", lambda e: e.tensor_scalar(out=stats[:, c0 + 2:c0 + 3], in0=stats[:, c0 + 1:c0 + 2], scalar1=neglam, scalar2=None, op0=ALU.mult),
             reads=[bSt[si], bC5], writes=[bSt[si]])
        P.op("dve", lambda e: e.tensor_scalar(out=o2[:, 0:128], in0=o2[:, 0:128], scalar1=stats[:, c0 + 2:c0 + 3], scalar2=None, op0=ALU.mult),
             reads=ep + [bSt[si]], writes=ep)
        P.op("dve", lambda e: e.scalar_tensor_tensor(out=o1[:, 0:128], in0=o1[:, 0:128], scalar=stats[:, c0:c0 + 1], in1=o2[:, 0:128], op0=ALU.mult, op1=ALU.add),
             reads=ep + [bSt[si]], writes=ep)
        P.op("dve", lambda e: e.tensor_tensor(out=o2[:, 0:128], in0=o1[:, 0:128], in1=o1[:, 0:128], op=ALU.mult), reads=ep, writes=ep)
        P.op("dve", lambda e: e.tensor_reduce(out=stats[:, c0 + 3:c0 + 4], in_=o2[:, 0:128], axis=AX.X, op=ALU.add), reads=ep, writes=[bSt[si]])
        P.op("dve", lambda e: e.tensor_scalar(out=stats[:, c0 + 4:c0 + 5], in0=stats[:, c0 + 3:c0 + 4], scalar1=1.0 / 128, scalar2=EPS, op0=ALU.mult, op1=ALU.add),
             reads=[bSt[si]], writes=[bSt[si]])
        P.op("pool", lambda e: e.tensor_tensor(out=stats[:, c0 + 5:c0 + 6], in0=stats[:, c0 + 4:c0 + 5], in1=cst[:, 0:1], op=ALU.pow),
             reads=[bSt[si], bCst], writes=[bSt[si]])
        P.op("dve", lambda e: e.scalar_tensor_tensor(out=obf, in0=o1[:, 0:128], scalar=stats[:, c0 + 5:c0 + 6], in1=subg[:], op0=ALU.mult, op1=ALU.mult),
             reads=ep + [bSt[si], bC6], writes=ep)
        tb = psb[6 + par % 2].bitcast(BF16)

        def fin():
            P.op("pe", lambda e: e.transpose(tb[:, 0:128], obf, identb), reads=[bEp[par], bConst], writes=[bTp[par % 2]])
            P.op("dve", lambda e: e.tensor_copy(out=attnT[:, h, qb * 128:(qb + 1) * 128], in_=tb[:, 0:128]), reads=[bTp[par % 2]], writes=[bAT[qb]], accum=True)
        late.append(fin)

    pending = []
    for ui, (h, c) in enumerate(units):
        nb = 2 * c + 2
        slots = {}
        per = (len(pending) + nb - 1) // nb if pending else 0
        late_now = list(late)
        del late[:]
        for kb in range(nb):
            if kb == 1:
                for f_ in late_now:
                    f_()
            sl = blk_ctr[0] % NSLOT
            blk_ctr[0] += 1
            slots[kb] = sl
            sp_ = kb % 2
            lo = 128 if kb == 2 * c + 1 else 0
            sb3 = pall[:, sp_ * 512:sp_ * 512 + 2048].rearrange("p (m r) -> p m r", m=2)
            for m in range(2):
                P.op("pe", lambda e, m=m, kb=kb, lo=lo, sp_=sp_, h=h, c=c: e.matmul(
                    psb[2 * m + sp_][:, lo:256], lhsT=QK[m * 64:(m + 1) * 64, 4 + h, kb * 128:(kb + 1) * 128],
                    rhs=QK[m * 64:(m + 1) * 64, h, c * 256 + lo:(c + 1) * 256], start=True, stop=True),
                    reads=[bQK[4 + h], bQK[h]], writes=[bSS[sp_]])
            P.op("act", lambda e, sl=sl, lo=lo, sb3=sb3: e.activation(out=Er[sl][:, :, lo:256], in_=sb3[:, :, lo:256], func=AF.Exp),
                 reads=[bSS[sp_]], writes=[bE[sl]])
            for j in range(2):
                d = 2 * c + j - kb
                if 0 <= d <= 1:
                    for m in range(2):
                        P.op("dve", lambda e, sl=sl, m=m, j=j, d=d, h=h: e.tensor_tensor(
                            out=Er[sl][:, m, j * 128:(j + 1) * 128], in0=Er[sl][:, m, j * 128:(j + 1) * 128], in1=ebv[:, h, d, :], op=ALU.mult),
                            reads=[bE[sl], bC3], writes=[bE[sl]])
            for _ in range(per):
                if pending:
                    emit_av(*pending.pop(0))
        while pending:
            emit_av(*pending.pop(0))
        pending = [(h, c, slots, it) for it in av_items(h, c, slots)]
    while pending:
        emit_av(*pending.pop(0))
    for f_ in late:
        f_()

    if stop == "C1":
        dbg["E0"] = (Er[0], [128, 2, 256], BF16)
        dbg["E1"] = (Er[1], [128, 2, 256], BF16)
        dbg["ebfix"] = (ebfix[:], [128, 1024], F32)
        dbg["lams"] = (lams[:], [128, 8], F32)
        dbg["o1s"] = (tv(0, 0), [128, 130], F32)
        dbg["obf"] = (tv(0, 2), [128, 128], BF16)
        dbg["at0"] = (attnT[:, 0, 0:256], [128, 256], BF16)
        dbg["stats"] = (stats[:], [128, 128], F32)
        return finish(nc, P, st, dbg)
    if stop == "C":
        dbg["attnT"] = (attnT, [128, 8, S], BF16)
        return finish(nc, P, st, dbg)

    project(1536, "sb")
    P.barrier()
    Wr = [view(OFF_W + i * 512, [128, 256], BF16) for i in range(4)]
    bWr = [B("Wr%d" % i) for i in range(4)]
    e32 = [view(OFF_W + 2048 + i * 1024, [128, 256], F32) for i in range(2)]
    Lb = [view(OFF_W + 4096 + i * 512, [128, 256], BF16) for i in range(2)]
    Rb = [view(OFF_W + 5120 + i * 512, [128, 256], BF16) for i in range(3)]
    be32 = [B("e32%d" % i) for i in range(2)]
    bL = [B("L%d" % i) for i in range(2)]
    bR = [B("R%d" % i) for i in range(3)]
    bZ = [B("Z%d" % i) for i in range(4)]
    bX = [B("X%d" % i) for i in range(2)]
    bOT = [B("OT%d" % i) for i in range(2)]

    blocks = []
    for hd in range(8):
        for c in range(8):
            for kb in range(2 * c + 1, -1, -1):
                blocks.append((hd, c, kb))
    NB = len(blocks)

    def sb_pe1(i):
        hd, c, kb = blocks[i]
        g, po = hd // 2, (hd % 2) * 64
        lo = 128 if kb == 2 * c + 1 else 0
        pz = i % 2
        P.op("pe", lambda e: e.matmul(psb[i % 4][:, lo:256], lhsT=QK[po:po + 64, 4 + g, kb * 128:(kb + 1) * 128],
                                      rhs=QK[po:po + 64, g, c * 256 + lo:(c + 1) * 256], start=True, stop=True),
             reads=[bQK[4 + g], bQK[g]], writes=[bZ[i % 4]])

    def sb_act1(i):
        hd, c, kb = blocks[i]
        lo = 128 if kb == 2 * c + 1 else 0
        pz = i % 2
        first = kb == 2 * c + 1
        P.op("act", lambda e: e.activation(out=e32[pz][:, lo:256], in_=psb[i % 4][:, lo:256], func=AF.Exp), reads=[bZ[i % 4]], writes=[be32[pz]])
        if first:
            P.op("dve", lambda e: e.memset(e32[pz][:, 0:128], 0.0), writes=[be32[pz]], accum=True)
        j = kb - 2 * c
        if j >= 0:
            P.op("dve", lambda e: e.tensor_tensor(out=e32[pz][:, j * 128:(j + 1) * 128], in0=e32[pz][:, j * 128:(j + 1) * 128], in1=strictm, op=ALU.mult),
                 reads=[be32[pz], bC2], writes=[be32[pz]])
        P.op("act", lambda e: e.activation(out=Lb[pz][:], in_=e32[pz][:], func=AF.Ln, bias=1.0), reads=[be32[pz]], writes=[bL[pz]])
        if kb > 0:
            if first:
                P.op("dve", lambda e: e.tensor_copy(out=Rb[(i + 1) % 3][:], in_=Lb[pz][:]), reads=[bL[pz]], writes=[bR[(i + 1) % 3]])
            else:
                P.op("dve", lambda e: e.tensor_tensor(out=Rb[(i + 1) % 3][:], in0=Rb[i % 3][:], in1=Lb[pz][:], op=ALU.add),
                     reads=[bL[pz], bR[i % 3]], writes=[bR[(i + 1) % 3]])

    def sb_pe2(i):
        hd, c, kb = blocks[i]
        lo = 128 if kb == 2 * c + 1 else 0
        pz = i % 2
        first = kb == 2 * c + 1
        xb = psb[i % 4]
        P.op("pe", lambda e: e.matmul(xb[:, lo:256], lhsT=negtri, rhs=Lb[pz][:, lo:256], start=False, stop=first, skip_group_check=True),
             reads=[bL[pz], bConst, bZ[i % 4]], writes=[bZ[i % 4]])
        if not first:
            P.op("pe", lambda e: e.matmul(xb[:, lo:256], lhsT=negones, rhs=Rb[i % 3][:, lo:256], start=False, stop=True, skip_group_check=True),
                 reads=[bR[i % 3], bConst], writes=[bZ[i % 4]])

    def sb_act2(i):
        hd, c, kb = blocks[i]
        lo = 128 if kb == 2 * c + 1 else 0
        pz = i % 2
        wi = i % 4
        first = kb == 2 * c + 1
        P.op("act", lambda e: e.activation(out=Wr[wi][:, lo:256], in_=psb[i % 4][:, lo:256], func=AF.Exp), reads=[bZ[i % 4]], writes=[bWr[wi]])
        if first:
            P.op("dve", lambda e: e.memset(Wr[wi][:, 0:128], 0.0), writes=[bWr[wi]], accum=True)
        j = kb - 2 * c
        if j >= 0:
            P.op("dve", lambda e: e.tensor_tensor(out=Wr[wi][:, j * 128:(j + 1) * 128], in0=Wr[wi][:, j * 128:(j + 1) * 128], in1=strictm, op=ALU.mult),
                 reads=[bWr[wi], bC2], writes=[bWr[wi]])

    unit_ctr = [0]

    def sb_pe3(i):
        hd, c, kb = blocks[i]
        g, po = hd // 2, (hd % 2) * 64
        wi = i % 4
        first = kb == 2 * c + 1
        last = kb == 0
        up = (hd * 8 + c) % 2
        ob = psb[4 + up]
        P.op("pe", lambda e: e.matmul(ob[po:po + 64, 0:256], lhsT=VV[:, kb, hd * 64:(hd + 1) * 64], rhs=Wr[wi][:], start=first, stop=last),
             reads=[bWr[wi], bV[kb]], writes=[bOT[up]])
        if last:
            if (hd * 8 + c) % 2 == 0:
                P.op("act", lambda e: e.activation(out=attnT[po:po + 64, 4 + g, c * 256:(c + 1) * 256], in_=ob[po:po + 64, 0:256], func=AF.Copy),
                     reads=[bOT[up]], writes=[bAT[2 * c], bAT[2 * c + 1]], accum=True)
            else:
                P.op("dve", lambda e: e.tensor_copy(out=attnT[po:po + 64, 4 + g, c * 256:(c + 1) * 256], in_=ob[po:po + 64, 0:256]),
                     reads=[bOT[up]], writes=[bAT[2 * c], bAT[2 * c + 1]], accum=True)

    for s_ in range(NB + 2):
        if s_ < NB:
            sb_pe1(s_)
            sb_act1(s_)
        if 0 <= s_ - 1 < NB:
            sb_pe2(s_ - 1)
            sb_act2(s_ - 1)
        if 0 <= s_ - 2 < NB:
            sb_pe3(s_ - 2)

    if stop == "D":
        dbg["attnT"] = (attnT, [128, 8, S], BF16)
        return finish(nc, P, st, dbg)

    P.barrier()
    KB = 1024
    hres = view(0, [128, NT, D], F32)
    tT = view(96 * KB, [128, 8, S], BF16)
    wob = view(128 * KB, [128, 8, D], BF16)
    xt1 = view(144 * KB, [128, D], F32)
    tb1 = view(148 * KB, [128, D], BF16)
    junk2 = view(150 * KB, [128, D], BF16)
    tmpx = sbt("tmpx", [128, 4 * 512], F32)
    comb = sbt("comb", [128, NT * 32], F32)
    lgs = sbt("lgs", [128, 4 * 32], F32)
    mx8 = sbt("mx8", [128, 32], F32)
    ix8 = sbt("ix8", [128, 8], U32)
    mbt = sbt("mbt", [128, 96], BF16)
    bIx = B("ix8"); bMb = B("mskb"); bMacc = [B("macc0"), B("macc1")]; bXg = B("xg"); bH1d = B("h1d"); bMeta = B("meta")
    rwb = sbt("rwb", [128, 256], BF16)
    bH = [B("h%d" % i_) for i_ in range(NT)]
    bTT = [B("tT%d" % i_) for i_ in range(NT)]
    bWo = B("wo"); bX1 = B("x1"); bTb = B("tb1"); bJ2 = B("junk2"); bRwb = B("rwb"); bLg = B("lg"); bMx = B("mx"); bComb = B("comb")
    bPE = [B("pe%d" % i_) for i_ in range(8)]
    P.dma("pool", lambda e: e.dma_start(out=wob, in_=wout_d), writes=[bWo])
    P.dma("pool", lambda e: e.dma_start(out=rwb[:], in_=rw_d), writes=[bRwb])
    P.dma("sp", lambda e: e.dma_start(out=gainb[:], in_=gains_d[1].partition_broadcast(128)), writes=[bGain])

    def rms2(i_, src, srcbuf, n):
        c0 = i_ * 8
        P.op("act", lambda e: e.activation(out=junk2[:, 0:n], in_=src, func=AF.Square, accum_out=stats[:, c0:c0 + 1]), reads=[srcbuf], writes=[bJ2, bSt[i_]])
        P.op("dve", lambda e: e.tensor_scalar(out=stats[:, c0 + 1:c0 + 2], in0=stats[:, c0:c0 + 1], scalar1=1.0 / n, scalar2=EPS, op0=ALU.mult, op1=ALU.add), reads=[bSt[i_]], writes=[bSt[i_]])
        P.op("pool", lambda e: e.tensor_tensor(out=stats[:, c0 + 3:c0 + 4], in0=stats[:, c0 + 1:c0 + 2], in1=cst[:, 0:1], op=ALU.pow), reads=[bSt[i_], bCst], writes=[bSt[i_]])
        return stats[:, c0 + 3:c0 + 4]

    xt1s = [xt1, tmpx[:, 0:1024]]
    tb1s = [tb1, tmpx[:, 1024:1536].bitcast(BF16)]
    bX1s = [bX1, B("x1b")]
    bTbs = [bTb, B("tb1b")]

    def phaseE(i_):
        xt1 = xt1s[i_ % 2]; tb1 = tb1s[i_ % 2]; bX1 = bX1s[i_ % 2]; bTb = bTbs[i_ % 2]
        P.dma("sp", lambda e: e.dma_start(out=xt1, in_=x_d[i_ * 128:(i_ + 1) * 128, :]), writes=[bX1])
        for half in range(2):
            bk = 2 * (i_ % 2) + half
            for c_ in range(8):
                P.op("pe", lambda e, c_=c_, bk=bk, half=half: e.matmul(psb[bk], lhsT=attnT[:, c_, i_ * 128:(i_ + 1) * 128], rhs=wob[:, c_, half * 512:(half + 1) * 512], start=(c_ == 0), stop=(c_ == 7)),
                     reads=[bAT[i_], bWo], writes=[bPE[bk]])
            P.op("dve", lambda e, bk=bk, half=half: e.tensor_tensor(out=hres[:, i_, half * 512:(half + 1) * 512], in0=psb[bk], in1=xt1[:, half * 512:(half + 1) * 512], op=ALU.add),
                 reads=[bPE[bk], bX1], writes=[bH[i_]], accum=(half == 1))
        rstd = rms2(i_, hres[:, i_, :], bH[i_], D)
        P.op("dve", lambda e: e.scalar_tensor_tensor(out=tb1, in0=hres[:, i_, :], scalar=rstd, in1=gainb[:], op0=ALU.mult, op1=ALU.mult), reads=[bH[i_], bSt[i_], bGain], writes=[bTb])
        pT = psb[4 + i_ % 2].bitcast(BF16)
        for c_ in range(8):
            P.op("pe", lambda e, c_=c_: e.transpose(pT[:, c_ * 128:(c_ + 1) * 128], tb1[:, c_ * 128:(c_ + 1) * 128], identb), reads=[bTb, bConst], writes=[bPE[4 + i_ % 2]])
        P.op("act", lambda e: e.activation(out=tT[:, :, i_ * 128:(i_ + 1) * 128], in_=pT.rearrange("p (c t) -> p c t", t=128), func=AF.Copy), reads=[bPE[4 + i_ % 2]], writes=[bTT[i_]])
        for c_ in range(8):
            P.op("pe", lambda e, c_=c_: e.matmul(psb[6][:, 0:32], lhsT=tT[:, c_, i_ * 128:(i_ + 1) * 128], rhs=rwb[:, c_ * 32:(c_ + 1) * 32], start=(c_ == 0), stop=(c_ == 7)),
                 reads=[bTT[i_], bRwb], writes=[bPE[6]])
        lg = lgs[:, 0:32]; exl = lgs[:, 32:64]; msk = lgs[:, 64:96]
        P.op("dve", lambda e: e.tensor_tensor(out=lg, in0=psb[6][:, 0:32], in1=rbb[:], op=ALU.add), reads=[bPE[6], bC8], writes=[bLg])
        P.op("dve", lambda e: e.max(out=mx8[:, 0:8], in_=lg), reads=[bLg], writes=[bMx])
        P.op("dve", lambda e: e.tensor_scalar(out=mx8[:, 8:9], in0=mx8[:, 0:1], scalar1=-1.0, scalar2=None, op0=ALU.mult), reads=[bMx], writes=[bMx])
        P.op("act", lambda e: e.activation(out=exl, in_=lg, func=AF.Exp, bias=mx8[:, 8:9]), reads=[bLg, bMx], writes=[bLg])
        P.op("dve", lambda e: e.tensor_scalar(out=msk, in0=lg, scalar1=mx8[:, 3:4], scalar2=None, op0=ALU.is_ge), reads=[bLg, bMx], writes=[bLg])
        P.op("dve", lambda e: e.tensor_tensor(out=exl, in0=exl, in1=msk, op=ALU.mult), reads=[bLg], writes=[bLg])
        P.op("dve", lambda e: e.tensor_reduce(out=mx8[:, 9:10], in_=exl, axis=AX.X, op=ALU.add), reads=[bLg], writes=[bMx])
        P.op("dve", lambda e: e.reciprocal(out=mx8[:, 10:11], in_=mx8[:, 9:10]), reads=[bMx], writes=[bMx])
        P.op("act", lambda e: e.activation(out=mx8[:, 16:20], in_=mx8[:, 0:4], func=AF.Exp, bias=mx8[:, 8:9]), reads=[bMx], writes=[bMx])
        P.op("dve", lambda e: e.tensor_scalar(out=meta_g[:, i_ * 4:(i_ + 1) * 4], in0=mx8[:, 16:20], scalar1=mx8[:, 10:11], scalar2=None, op0=ALU.mult), reads=[bMx], writes=[bMeta], accum=True)
        P.op("dve", lambda e: e.max_index(out=ix8[:], in_max=mx8[:, 0:8], in_values=lg), reads=[bLg, bMx], writes=[bIx])
        P.op("dve", lambda e: e.tensor_copy(out=mbt[:, 0:32], in_=msk), reads=[bLg], writes=[bMb])
        pfx = psb[7][:, 0:32]
        a_ = i_ % 2
        P.op("pe", lambda e: e.matmul(pfx, lhsT=ltm, rhs=mbt[:, 0:32], start=True, stop=(i_ == 0)), reads=[bMb, bConst], writes=[bPE[7]])
        if i_ > 0:
            P.op("pe", lambda e: e.matmul(pfx, lhsT=onesb, rhs=mbt[:, 32 + a_ * 32:64 + a_ * 32], start=False, stop=True), reads=[bMacc[a_], bConst], writes=[bPE[7]])
        if i_ == 0:
            P.op("dve", lambda e: e.tensor_copy(out=mbt[:, 64:96], in_=mbt[:, 0:32]), reads=[bMb], writes=[bMacc[1]])
        else:
            P.op("dve", lambda e: e.tensor_tensor(out=mbt[:, 32 + (1 - a_) * 32:64 + (1 - a_) * 32], in0=mbt[:, 32 + a_ * 32:64 + a_ * 32], in1=mbt[:, 0:32], op=ALU.add),
                 reads=[bMb, bMacc[a_]], writes=[bMacc[1 - a_]])
        oh = lgs[:, 96:128]
        for k_ in range(4):
            P.op("dve", lambda e, k_=k_: e.tensor_scalar(out=oh, in0=lg, scalar1=mx8[:, k_:k_ + 1], scalar2=None, op0=ALU.is_equal), reads=[bLg, bMx], writes=[bLg])
            P.op("dve", lambda e: e.tensor_tensor(out=oh, in0=oh, in1=pfx, op=ALU.mult), reads=[bLg, bPE[7]], writes=[bLg])
            P.op("dve", lambda e, k_=k_: e.tensor_reduce(out=mx8[:, 20 + k_:21 + k_], in_=oh, axis=AX.X, op=ALU.add), reads=[bLg], writes=[bMx])
        P.op("dve", lambda e: e.tensor_copy(out=mx8[:, 24:28], in_=ix8[:, 0:4]), reads=[bIx], writes=[bMx])
        P.op("dve", lambda e: e.scalar_tensor_tensor(out=mx8[:, 28:32], in0=mx8[:, 24:28], scalar=float(CAP), in1=mx8[:, 20:24], op0=ALU.mult, op1=ALU.add), reads=[bMx], writes=[bMx])
        P.op("dve", lambda e: e.tensor_copy(out=meta_dest[:, i_ * 4:(i_ + 1) * 4], in_=mx8[:, 28:32]), reads=[bMx], writes=[bMeta], accum=True)
        for k_ in range(4):
            P.dma("pool", lambda e, k_=k_: e.indirect_dma_start(out=xg_d, out_offset=bass.IndirectOffsetOnAxis(ap=meta_dest[:, i_ * 4 + k_:i_ * 4 + k_ + 1], axis=0),
                                                               in_=tb1, in_offset=None), reads=[bTb, bMeta], writes=[bXg])
        P.dma("sp", lambda e: e.dma_start(out=h1_d[i_ * 128:(i_ + 1) * 128, :], in_=hres[:, i_, :]), reads=[bH[i_]], writes=[bH1d])

    for i_ in range(NT):
        phaseE(i_)
    cnt_i = sbt("cnt_i", [128, 32], I32)
    bCnt = B("cnt")
    P.op("pe", lambda e: e.matmul(psb[7][:, 0:32], lhsT=onesb, rhs=mbt[:, 32:64], start=True, stop=True), reads=[bMacc[0], bConst], writes=[bPE[7]])
    P.op("dve", lambda e: e.tensor_copy(out=cnt_i[:], in_=psb[7][:, 0:32]), reads=[bPE[7]], writes=[bCnt])
    P.cnt_ap = lambda ex_: cnt_i[0:1, ex_:ex_ + 1]

    if stop == "E":
        dbg["h1"] = (hres, [128, NT, D], F32)
        dbg["tT"] = (tT, [128, 8, S], BF16)
        dbg["mdest"] = (meta_dest[:], [128, NT * 4], I32)
        dbg["mg"] = (meta_g[:], [128, NT * 4], F32)
        dbg["xg0"] = (xg_d[0:512, :], [512, D], BF16)
        return finish(nc, P, st, dbg)

    P.barrier()
    NTS = RC // 128
    xe = [view(0 + k_ * 6 * KB, [128, NTS, D], BF16) for k_ in range(2)]
    XeT = [view(12 * KB + k_ * 6 * KB, [128, 8, RC], BF16) for k_ in range(2)]
    actT = [view(24 * KB + k_ * 6 * KB, [128, 8, RC], BF16) for k_ in range(2)]
    wdb = [view(36 * KB + k_ * 16 * KB, [128, 8, D], BF16) for k_ in range(2)]
    wg = [view(112 * KB + k_ * 4 * KB, [128, 8, 256], BF16) for k_ in range(6)]
    yt = [view(80 * KB + k_ * 4 * KB, [128, D], F32) for k_ in range(2)]
    bdt = [view(88 * KB + k_ * 4 * KB, [128, D], F32) for k_ in range(2)]
    bXe = [B("xe%d" % k_) for k_ in range(2)]; bXT = [B("XeT%d" % k_) for k_ in range(2)]; bAc = [B("ac%d" % k_) for k_ in range(2)]
    bWd = [B("wd%d" % k_) for k_ in range(2)]; bWg = [B("wg%d" % k_) for k_ in range(6)]; bYt = [B("yt%d" % k_) for k_ in range(2)]; bBd = [B("bd%d" % k_) for k_ in range(2)]
    bYg = B("yg")
    bTgs = [B("tg%d" % k_) for k_ in range(3)]; bTss = [B("ts%d" % k_) for k_ in range(3)]; bTls = [B("tl%d" % k_) for k_ in range(3)]
    tgs = [view(96 * KB + k_ * 1536, [128, RC], F32) for k_ in range(3)]
    tss = [view(101 * KB + k_ * 1536, [128, RC], F32) for k_ in range(3)]
    tls = [view(106 * KB + k_ * 1536, [128, RC], F32) for k_ in range(3)]
    cnt = [0]; gcnt = [0]; slabc = [0]; ytc = [0]; qc = [0]

    def gu_chunk(e_, j_, k_, q_):
        ba = 2 * (gcnt[0] % 2)
        tg = tgs[gcnt[0] % 3]; ts = tss[gcnt[0] % 3]; tl = tls[gcnt[0] % 3]
        bTg = bTgs[gcnt[0] % 3]; bTs = bTss[gcnt[0] % 3]; bTl = bTls[gcnt[0] % 3]
        gcnt[0] += 1
        KC = int(os.environ.get("KKC", 8))
        for which in range(2):
            for c_ in range(KC):
                P.op("pe", lambda e, c_=c_, which=which: e.matmul(psb[ba + which][:, 0:RC], lhsT=wg[k_][:, c_, which * 128:(which + 1) * 128], rhs=XeT[q_][:, c_, :], start=(c_ == 0), stop=(c_ == KC - 1)),
                     reads=[bWg[k_], bXT[q_]], writes=[bPE[ba + which]])
        col = (e_ * 8 + j_) * 2
        P.op("dve", lambda e: e.tensor_scalar(out=tg, in0=psb[ba][:, 0:RC], scalar1=bgl[:, col:col + 1], scalar2=7.0, op0=ALU.add, op1=ALU.min), reads=[bPE[ba], bC9], writes=[bTg])
        P.op("act", lambda e: e.activation(out=ts, in_=tg, func=AF.Silu, scale=1.702), reads=[bTg], writes=[bTs])
        P.op("dve", lambda e: e.tensor_scalar(out=tl, in0=psb[ba + 1][:, 0:RC], scalar1=bgl[:, col + 1:col + 2], scalar2=8.0, op0=ALU.add, op1=ALU.min), reads=[bPE[ba + 1], bC9], writes=[bTl])
        P.op("dve", lambda e: e.scalar_tensor_tensor(out=actT[q_][:, j_, :], in0=tl, scalar=-6.0, in1=ts, op0=ALU.max, op1=ALU.mult), reads=[bTl, bTs], writes=[bAc[q_]], accum=True)

    def down_tile(e_, q_, st_, half, yk):
        bk = 6 + cnt[0] % 2
        cnt[0] += 1
        for j_ in range(8):
            P.op("pe", lambda e, j_=j_: e.matmul(psb[bk], lhsT=actT[q_][:, j_, st_ * 128:(st_ + 1) * 128], rhs=wdb[q_][:, j_, half * 512:(half + 1) * 512], start=(j_ == 0), stop=(j_ == 7)),
                 reads=[bAc[q_], bWd[q_]], writes=[bPE[bk]])
        P.op("dve", lambda e: e.scalar_tensor_tensor(out=yt[yk][:, half * 512:(half + 1) * 512], in0=psb[bk], scalar=1.0 / 1.702, in1=bdt[q_][:, half * 512:(half + 1) * 512], op0=ALU.mult, op1=ALU.add),
             reads=[bPE[bk], bBd[q_]], writes=[bYt[yk]], accum=(half == 1))

    def chunk_prep(e_, ch):
        q_ = qc[0] % 2
        qc[0] += 1
        row0 = e_ * CAP + min(ch * RC, CAP - RC)
        P.dma("sp", lambda e: e.dma_start(out=xe[q_], in_=xg_d[row0:row0 + RC, :].rearrange("(t p) d -> p t d", p=128)), reads=[bXg], writes=[bXe[q_]])
        P.dma("sp", lambda e: e.dma_start(out=bdt[q_], in_=bd_d[e_].partition_broadcast(128)), writes=[bBd[q_]])
        P.dma("pool", lambda e: e.dma_start(out=wdb[q_], in_=wd_d[e_]), writes=[bWd[q_]])
        for t_ in range(NTS):
            pT = psb[4 + t_ % 2].bitcast(BF16)
            for c_ in range(8):
                P.op("pe", lambda e, c_=c_, t_=t_, pT=pT: e.transpose(pT[:, c_ * 128:(c_ + 1) * 128], xe[q_][:, t_, c_ * 128:(c_ + 1) * 128], identb),
                     reads=[bXe[q_], bConst], writes=[bPE[4 + t_ % 2]])
            if t_ % 2 == 0:
                P.op("act", lambda e, t_=t_, pT=pT: e.activation(out=XeT[q_][:, :, t_ * 128:(t_ + 1) * 128], in_=pT.rearrange("p (c t) -> p c t", t=128), func=AF.Copy),
                     reads=[bPE[4 + t_ % 2]], writes=[bXT[q_]], accum=(t_ > 0))
            else:
                P.op("dve", lambda e, t_=t_, pT=pT: e.tensor_copy(out=XeT[q_][:, :, t_ * 128:(t_ + 1) * 128], in_=pT.rearrange("p (c t) -> p c t", t=128)),
                     reads=[bPE[4 + t_ % 2]], writes=[bXT[q_]], accum=True)
        return (e_, q_, row0)

    def chunk_gate(stt):
        e_, q_, row0 = stt
        for j_ in range(8):
            k_ = slabc[0] % 6
            slabc[0] += 1
            P.dma("pool", lambda e, j_=j_, k_=k_: e.dma_start(out=wg[k_], in_=wgu_d[e_, j_]), writes=[bWg[k_]])
            gu_chunk(e_, j_, k_, q_)

    def chunk_down(stt):
        e_, q_, row0 = stt
        for st_ in range(NTS):
            yk = ytc[0] % 2
            ytc[0] += 1
            for half in range(2):
                down_tile(e_, q_, st_, half, yk)
            P.dma("sp", lambda e, st_=st_, yk=yk: e.dma_start(out=yg_d[row0 + st_ * 128:row0 + (st_ + 1) * 128, :], in_=yt[yk]), reads=[bYt[yk]], writes=[bYg], sembuf=bYg)

    def expert_chunk(e_, ch):
        stt = chunk_prep(e_, ch)
        chunk_gate(stt)
        chunk_down(stt)

    NEX = int(os.environ.get("KNEX", NE))
    NCHX = int(os.environ.get("KNCH", NCH))
    stt_ = chunk_prep(0, 0)
    for e_ in range(NEX):
        chunk_gate(stt_)
        nxt = chunk_prep(e_ + 1, 0) if e_ + 1 < NEX else None
        chunk_down(stt_)
        stt_ = nxt
    for e_ in range(NEX):
        for ch in range(1, NCHX):
            P.guard_begin((e_, ch * RC))
            expert_chunk(e_, ch)
        for ch in range(1, NCHX):
            P.guard_end()

    if stop == "G":
        dbg["cnt"] = (cnt_i[:], [128, 32], I32)
        return finish(nc, P, st, dbg)

    P.barrier()
    pgb = view(128 * KB, [128, 8, D], BF16)
    ppb = view(144 * KB, [128, 2, D], BF16)
    ptile_ = [view(148 * KB + k_ * 2 * KB, [128, 256], F32) for k_ in range(2)]
    pbf_ = [view(149 * KB + k_ * 2 * KB, [128, 256], BF16) for k_ in range(2)]
    pTs_ = [view(149 * KB + 512 + k_ * 2 * KB, [128, 2, 128], BF16) for k_ in range(2)]
    pe32_ = [view(k_ * 4 * KB, [128, D], F32) for k_ in range(2)]
    gate_ = [view(8 * KB + k_ * 4 * KB, [128, D], F32) for k_ in range(2)]
    hbf_ = [view(16 * KB + k_ * 2 * KB, [128, D], BF16) for k_ in range(2)]
    hT_ = [view(20 * KB + k_ * 2 * KB, [128, 8, 128], BF16) for k_ in range(2)]
    otile = [view(76 * KB + k_ * 4096, [128, D], F32) for k_ in range(2)]
    gfin = ebfix
    bPg = B("pg"); bPp = B("pp"); bPt_ = [B("pt0"), B("pt1")]; bPbf_ = [B("pbf0"), B("pbf1")]; bPTs_ = [B("pTs0"), B("pTs1")]; bPe32_ = [B("pe320"), B("pe321")]; bGate_ = [B("gate0"), B("gate1")]; bHbf_ = [B("hbf0"), B("hbf1")]; bHT_ = [B("hT0"), B("hT1")]
    bOt = [B("ot%d" % k_) for k_ in range(2)]; bGf = B("gfin"); bOut = B("out")
    P.dma("pool", lambda e: e.dma_start(out=pgb, in_=pgate_d), writes=[bPg])
    P.dma("pool", lambda e: e.dma_start(out=ppb, in_=pproj_d), writes=[bPp])
    P.dma("sp", lambda e: e.dma_start(out=gainb[:], in_=gains_d[2].partition_broadcast(128)), writes=[bGain])
    P.dma("sp", lambda e: e.dma_start(out=gfin[:], in_=gains_d[3].partition_broadcast(128)), writes=[bGf])

    htl = [view(84 * KB + k_ * 4 * KB, [128, D], F32) for k_ in range(2)]
    ygk = [view(92 * KB + k_ * 4 * KB, [128, D], F32) for k_ in range(8)]
    bHt = [B("ht%d" % k_) for k_ in range(2)]
    bYk = [B("ygk%d" % k_) for k_ in range(8)]

    junkH = view(24 * KB, [128, D], BF16)

    def rmsH(i_, src, srcbuf, n):
        c0 = i_ * 8
        P.op("act", lambda e: e.activation(out=junkH[:, 0:n], in_=src, func=AF.Square, accum_out=stats[:, c0:c0 + 1]), reads=[srcbuf], writes=[bJ2, bSt[i_]])
        P.op("dve", lambda e: e.tensor_scalar(out=stats[:, c0 + 1:c0 + 2], in0=stats[:, c0:c0 + 1], scalar1=1.0 / n, scalar2=EPS, op0=ALU.mult, op1=ALU.add), reads=[bSt[i_]], writes=[bSt[i_]])
        P.op("pool", lambda e: e.tensor_tensor(out=stats[:, c0 + 3:c0 + 4], in0=stats[:, c0 + 1:c0 + 2], in1=cst[:, 0:1], op=ALU.pow), reads=[bSt[i_], bCst], writes=[bSt[i_]])
        return stats[:, c0 + 3:c0 + 4]

    def phaseH(i_):
        k_ = i_ % 2
        ptile = ptile_[k_]; pbf = pbf_[k_]; pTs = pTs_[k_]; pe32 = pe32_[k_]; gate = gate_[k_]; hbf = hbf_[k_]; hT = hT_[k_]
        bPt = bPt_[k_]; bPbf = bPbf_[k_]; bPTs = bPTs_[k_]; bPe32 = bPe32_[k_]; bGate = bGate_[k_]; bHbf = bHbf_[k_]; bHT = bHT_[k_]
        hcur = htl[k_]
        P.dma("sp", lambda e: e.dma_start(out=hcur, in_=h1_d[i_ * 128:(i_ + 1) * 128, :]), reads=[bH1d], writes=[bHt[k_]])
        for kk in range(4):
            yb = k_ * 4 + kk
            P.dma("pool", lambda e, kk=kk, yb=yb: e.indirect_dma_start(out=ygk[yb], out_offset=None, in_=yg_d,
                                                                        in_offset=bass.IndirectOffsetOnAxis(ap=meta_dest[:, i_ * 4 + kk:i_ * 4 + kk + 1], axis=0)),
                  reads=[bYg, bMeta], writes=[bYk[yb]])
            P.op("dve", lambda e, kk=kk, yb=yb: e.scalar_tensor_tensor(out=hcur, in0=ygk[yb], scalar=meta_g[:, i_ * 4 + kk:i_ * 4 + kk + 1], in1=hcur, op0=ALU.mult, op1=ALU.add),
                 reads=[bYk[yb], bMeta, bHt[k_]], writes=[bHt[k_]])
        P.dma("sp", lambda e: e.dma_start(out=ptile, in_=p_d[i_ * 128:(i_ + 1) * 128, :]), writes=[bPt])
        P.op("dve", lambda e: e.tensor_copy(out=pbf, in_=ptile), reads=[bPt], writes=[bPbf])
        tq = psb[6].bitcast(BF16)
        for c_ in range(2):
            P.op("pe", lambda e, c_=c_: e.transpose(tq[:, c_ * 128:(c_ + 1) * 128], pbf[:, c_ * 128:(c_ + 1) * 128], identb), reads=[bPbf, bConst], writes=[bPE[6]])
        P.op("act", lambda e: e.activation(out=pTs, in_=tq[:, 0:256].rearrange("p (c t) -> p c t", t=128), func=AF.Copy), reads=[bPE[6]], writes=[bPTs])
        for half in range(2):
            for c_ in range(2):
                P.op("pe", lambda e, c_=c_, half=half: e.matmul(psb[half], lhsT=pTs[:, c_, :], rhs=ppb[:, c_, half * 512:(half + 1) * 512], start=(c_ == 0), stop=(c_ == 1)),
                     reads=[bPTs, bPp], writes=[bPE[half]])
            P.op("act", lambda e, half=half: e.activation(out=pe32[:, half * 512:(half + 1) * 512], in_=psb[half], func=AF.Copy), reads=[bPE[half]], writes=[bPe32], accum=(half == 1))
        rp = rmsH(i_, pe32, bPe32, D)
        P.op("dve", lambda e: e.scalar_tensor_tensor(out=pe32, in0=pe32, scalar=rp, in1=gainb[:], op0=ALU.mult, op1=ALU.mult), reads=[bPe32, bSt[i_], bGain], writes=[bPe32])
        P.op("dve", lambda e: e.tensor_copy(out=hbf, in_=hcur), reads=[bHt[k_]], writes=[bHbf])
        tq2 = psb[7].bitcast(BF16)
        for c_ in range(8):
            P.op("pe", lambda e, c_=c_: e.transpose(tq2[:, c_ * 128:(c_ + 1) * 128], hbf[:, c_ * 128:(c_ + 1) * 128], identb), reads=[bHbf, bConst], writes=[bPE[7]])
        P.op("act", lambda e: e.activation(out=hT, in_=tq2.rearrange("p (c t) -> p c t", t=128), func=AF.Copy), reads=[bPE[7]], writes=[bHT])
        for half in range(2):
            for c_ in range(8):
                P.op("pe", lambda e, c_=c_, half=half: e.matmul(psb[2 + half], lhsT=hT[:, c_, :], rhs=pgb[:, c_, half * 512:(half + 1) * 512], start=(c_ == 0), stop=(c_ == 7)),
                     reads=[bHT, bPg], writes=[bPE[2 + half]])
            P.op("act", lambda e, half=half: e.activation(out=gate[:, half * 512:(half + 1) * 512], in_=psb[2 + half], func=AF.Sigmoid), reads=[bPE[2 + half]], writes=[bGate], accum=(half == 1))
        P.op("dve", lambda e: e.tensor_tensor(out=gate, in0=gate, in1=pe32, op=ALU.mult), reads=[bGate, bPe32], writes=[bGate])
        P.op("dve", lambda e: e.tensor_tensor(out=hcur, in0=hcur, in1=gate, op=ALU.add), reads=[bHt[k_], bGate], writes=[bHt[k_]])
        c0 = i_ * 8 + 4
        P.op("act", lambda e: e.activation(out=junkH, in_=hcur, func=AF.Square, accum_out=stats[:, c0:c0 + 1]), reads=[bHt[k_]], writes=[bJ2, bSt[i_]])
        P.op("dve", lambda e: e.tensor_scalar(out=stats[:, c0 + 1:c0 + 2], in0=stats[:, c0:c0 + 1], scalar1=1.0 / D, scalar2=EPS, op0=ALU.mult, op1=ALU.add), reads=[bSt[i_]], writes=[bSt[i_]])
        P.op("pool", lambda e: e.tensor_tensor(out=stats[:, c0 + 2:c0 + 3], in0=stats[:, c0 + 1:c0 + 2], in1=cst[:, 0:1], op=ALU.pow), reads=[bSt[i_], bCst], writes=[bSt[i_]])
        P.op("dve", lambda e: e.scalar_tensor_tensor(out=otile[k_], in0=hcur, scalar=stats[:, c0 + 2:c0 + 3], in1=gfin[:], op0=ALU.mult, op1=ALU.mult), reads=[bHt[k_], bSt[i_], bGf], writes=[bOt[k_]])
        P.dma("sp", lambda e: e.dma_start(out=out_d[i_ * 128:(i_ + 1) * 128, :], in_=otile[k_]), reads=[bOt[k_]], writes=[bOut], sembuf=bOt[k_])

    for i_ in range(NT):
        phaseH(i_)
    P.wait_all("sp", [bOut] + bOt)
    P.run(st)
    st.close()
    return nc


def finish(nc, P, st, dbg):
    P.barrier()
    outs = []
    for name, (ap, shape, dt) in dbg.items():
        d = nc.dram_tensor("dbg_" + name, shape, dt, kind="ExternalOutput").ap()
        b = Buf(P, "dbg_" + name)
        P.dma("sp", lambda e, d=d, ap=ap: e.dma_start(out=d, in_=ap), writes=[b])
        outs.append(b)
    P.wait_all("sp", outs)
    P.run(st)
    st.close()
    return nc


def _t5_bucket(rel):
    n = np.maximum(rel, 0)
    nf = np.maximum(n, 1).astype(np.float32)
    large = 16 + (np.log(nf / np.float32(16)) / np.float32(math.log(128 / 16)) * np.float32(16)).astype(np.int32)
    large = np.minimum(large, 31)
    return np.where(n < 16, n, large)


def prep_shared(inp):
    f32 = np.float32
    g = lambda k: np.asarray(inp[k], dtype=f32)
    w_in = g("w_in")[0]
    cols = []
    for h in range(4):
        cols += list(range(h * 64, h * 64 + 64)) + list(range(256 + h * 64, 256 + h * 64 + 64))
    for h in range(4):
        cols += list(range(512 + h * 64, 512 + h * 64 + 64)) + list(range(768 + h * 64, 768 + h * 64 + 64))
    cols += list(range(1024, 3072))
    w_in_p = w_in[:, cols]
    chunked = lambda w: np.ascontiguousarray(w.reshape(8, 128, -1).transpose(1, 0, 2))
    sh = {}
    sh["w_in"] = chunked(w_in_p)
    sh["w_out"] = chunked(g("w_out")[0])
    sh["gains"] = np.stack([g("attn_norm")[0], g("moe_norm")[0], g("ple_norm")[0], g("final_norm")], 0)
    sh["lamv"] = np.stack([g("lambda_q1")[0], g("lambda_k1")[0], g("lambda_q2")[0], g("lambda_k2")[0]], 0)
    sh["subln"] = g("subln")
    rb = g("rel_bias")
    k = np.arange(128)[:, None]
    q = np.arange(128)[None, :]
    bn = np.zeros((128, 4, 2, 128), f32)
    for d in range(2):
        bk = _t5_bucket(q + 128 * d - k)
        bn[:, :, d, :] = rb[bk].transpose(0, 2, 1)
    sh["bnear"] = bn.reshape(128, -1)
    sh["c31"] = np.ascontiguousarray(np.broadcast_to(rb[31][None, :], (128, 4)))
    sh["rw"] = chunked(g("router_w")[0]).reshape(128, -1)
    sh["rb"] = g("router_b")
    wgu = g("w_gate_up")[0]
    glu = wgu[:, :, 0::2].reshape(NE, 8, 128, 8, 128)
    lin = wgu[:, :, 1::2].reshape(NE, 8, 128, 8, 128)
    gl = np.concatenate([glu, lin], axis=-1)
    sh["wgu"] = np.ascontiguousarray(gl.transpose(0, 3, 2, 1, 4))
    bgu = g("b_gate_up")[0]
    bg = bgu[:, 0::2].reshape(NE, 8, 128)
    bl = bgu[:, 1::2].reshape(NE, 8, 128)
    sh["bgl"] = np.ascontiguousarray(np.stack([bg, bl], -1).transpose(2, 0, 1, 3)).reshape(128, -1)
    sh["wd"] = np.ascontiguousarray(g("w_down")[0].reshape(NE, 8, 128, D).transpose(0, 2, 1, 3))
    sh["bd"] = g("b_down")[0]
    sh["pproj"] = np.ascontiguousarray(g("ple_proj")[0].reshape(2, 128, D).transpose(1, 0, 2))
    sh["pgate"] = chunked(g("ple_gate")[0])
    ident = np.eye(128, dtype=f32)
    jj = np.arange(128)[:, None]
    kk = np.arange(128)[None, :]
    negtri = -(jj >= kk).astype(f32)
    lt = (jj < kk).astype(f32)
    sh["cbf"] = np.concatenate([ident, negtri, -np.ones((128, 128), f32), lt, np.ones((128, 128), f32)], 1).astype(ml_dtypes.bfloat16)
    sh["cf32"] = np.concatenate([ident, (kk >= jj).astype(f32), (kk > jj).astype(f32)], 1)
    return sh


_CACHE = {}


def kernel(**inputs):
    sh = prep_shared(inputs)
    x = np.asarray(inputs["x"], dtype=np.float32)
    p = np.asarray(inputs["p"], dtype=np.float32)[0]
    if "nc" not in _CACHE:
        _CACHE["nc"] = build()
    nc = _CACHE["nc"]
    in_maps = []
    for c in range(8):
        m = dict(sh)
        m["x"] = np.ascontiguousarray(x[c])
        m["p"] = np.ascontiguousarray(p[c])
        in_maps.append(m)
    res = run_bass_kernel_spmd(nc, in_maps, core_ids=list(range(8)))
    return np.stack([np.asarray(r["out"], dtype=np.float32) for r in res.results], 0)
```
